# Optimizing a Trainium2 kernel written in Bass

```python
import jax, jax.numpy as jnp
from jax import lax
import numpy as np

D_MODEL = 1024
BATCH = 8
SEQ = 8192
DEPTH = 1

CHUNK = 64
Q_BLOCK = 128
D_MIX = D_MODEL
MLA_HEADS = 8
MLA_NOPE = 64
MLA_ROPE = 32
MLA_QK = MLA_NOPE + MLA_ROPE
MLA_V = 64
MLA_OUT = MLA_HEADS * MLA_V
Q_LORA = 256
KV_LORA = 128
ROPE_BASE = 10000.0
RNN_WIDTH = D_MIX - MLA_OUT
RNN_BLOCKS = 8
RNN_BLOCK_DIM = RNN_WIDTH // RNN_BLOCKS
CONV_WIDTH = 4
LRU_C = 8.0
MEM_TOKENS = 256
MEM_HEADS = 4
MEM_HEAD_DIM = D_MODEL // MEM_HEADS
N_GROUPS = 4
EXPERTS_PER_GROUP = 8
N_EXPERTS = N_GROUPS * EXPERTS_PER_GROUP
TOP_K = 2
D_EXPERT = 256
EPS = 1e-6
IN_SPLITS = (Q_LORA, Q_LORA + KV_LORA, Q_LORA + KV_LORA + MLA_ROPE,
             Q_LORA + KV_LORA + MLA_ROPE + RNN_WIDTH)
D_IN = Q_LORA + KV_LORA + MLA_ROPE + 2 * RNN_WIDTH

kernel_name = "hybrid_mla_rglru_hmoe_block"


def rms_norm(x, g):
    xf = x.astype(jnp.float32)
    y = xf * lax.rsqrt(jnp.mean(xf * xf, axis=-1, keepdims=True) + EPS)
    return (y * g.astype(jnp.float32)).astype(x.dtype)


def rope_tables(seq_len):
    pos = jnp.arange(seq_len, dtype=jnp.float32)
    inv_freq = ROPE_BASE ** (-jnp.arange(0, MLA_ROPE, 2, dtype=jnp.float32) / MLA_ROPE)
    ang = pos[:, None] * inv_freq[None, :]
    return jnp.cos(ang), jnp.sin(ang)


def apply_rope(x, cos, sin):
    half = x.shape[-1] // 2
    x1, x2 = x[..., :half], x[..., half:]
    c = cos[None, :, None, :].astype(x.dtype)
    s = sin[None, :, None, :].astype(x.dtype)
    return jnp.concatenate([x1 * c - x2 * s, x2 * c + x1 * s], axis=-1)


def block_causal_attention(q, k, v):
    B, S, H, Dq = q.shape
    nblk = S // Q_BLOCK
    scale = Dq ** -0.5
    key_chunk = jnp.arange(S) // CHUNK
    qb = q.reshape(B, nblk, Q_BLOCK, H, Dq).transpose(1, 0, 2, 3, 4)

    def one_block(args):
        qi, bi = args
        q_chunk = (bi * Q_BLOCK + jnp.arange(Q_BLOCK)) // CHUNK
        s = jnp.einsum('bqhd,bkhd->bhqk', qi, k, preferred_element_type=jnp.float32) * scale
        mask = key_chunk[None, :] <= q_chunk[:, None]
        s = jnp.where(mask[None, None], s, -jnp.inf)
        p = jax.nn.softmax(s, axis=-1).astype(v.dtype)
        return jnp.einsum('bhqk,bkhd->bqhd', p, v)

    o = lax.map(one_block, (qb, jnp.arange(nblk)))
    return o.transpose(1, 0, 2, 3, 4).reshape(B, S, H * v.shape[-1])


def rg_lru(u, conv_w, conv_b, w_rg, b_rg, w_ig, b_ig, lam):
    B, S, W = u.shape
    up = jnp.pad(u, ((0, 0), (CONV_WIDTH - 1, 0), (0, 0)))
    xc = sum(up[:, j:j + S] * conv_w[j] for j in range(CONV_WIDTH)) + conv_b
    xb = xc.reshape(B, S, RNN_BLOCKS, RNN_BLOCK_DIM)
    r = jax.nn.sigmoid(jnp.einsum('bsnc,ncd->bsnd', xb, w_rg).reshape(B, S, W) + b_rg)
    i = jax.nn.sigmoid(jnp.einsum('bsnc,ncd->bsnd', xb, w_ig).reshape(B, S, W) + b_ig)
    log_a = (-LRU_C * r.astype(jnp.float32)) * jax.nn.softplus(-lam.astype(jnp.float32))
    a = jnp.exp(log_a)
    bt = jnp.sqrt(-jnp.expm1(2.0 * log_a)) * (i * xc).astype(jnp.float32)

    def combine(left, right):
        a_l, b_l = left
        a_r, b_r = right
        return a_l * a_r, a_r * b_l + b_r

    _, h = lax.associative_scan(combine, (a, bt), axis=1)
    return h.astype(u.dtype)


def mem_cross_attention(h, m, w_mq, w_mk, w_mv, g_mqn, g_mkn, w_mo):
    B, S, _ = h.shape
    M = m.shape[1]
    q = rms_norm((h @ w_mq).reshape(B, S, MEM_HEADS, MEM_HEAD_DIM), g_mqn)
    k = rms_norm((m @ w_mk).reshape(B, M, MEM_HEADS, MEM_HEAD_DIM), g_mkn)
    v = (m @ w_mv).reshape(B, M, MEM_HEADS, MEM_HEAD_DIM)
    s = jnp.einsum('bshd,bmhd->bhsm', q, k, preferred_element_type=jnp.float32) * MEM_HEAD_DIM ** -0.5
    p = jax.nn.softmax(s, axis=-1).astype(v.dtype)
    o = jnp.einsum('bhsm,bmhd->bshd', p, v).reshape(B, S, MEM_HEADS * MEM_HEAD_DIM)
    return o @ w_mo


def hier_moe(h, w_group, b_group, w_expert, b_expert, w_e_gate, w_e_up, w_e_down):
    B, S, D = h.shape
    t = h.reshape(-1, D)
    p_group = jax.nn.softmax((t @ w_group).astype(jnp.float32) + b_group, axis=-1)
    g_idx = jnp.argmax(p_group, axis=-1)
    p_g = jnp.take_along_axis(p_group, g_idx[:, None], axis=-1)
    e_logits = ((t @ w_expert).astype(jnp.float32) + b_expert).reshape(-1, N_GROUPS, EXPERTS_PER_GROUP)
    e_sel = jnp.take_along_axis(e_logits, g_idx[:, None, None], axis=1)[:, 0]
    top_v, top_i = lax.top_k(jax.nn.softmax(e_sel, axis=-1), TOP_K)
    top_v = top_v / jnp.sum(top_v, axis=-1, keepdims=True)
    within = jnp.sum(jax.nn.one_hot(top_i, EXPERTS_PER_GROUP, dtype=jnp.float32) * top_v[..., None], axis=1)
    gates = (jax.nn.one_hot(g_idx, N_GROUPS, dtype=jnp.float32)[:, :, None]
             * (p_g * within)[:, None, :]).reshape(-1, N_EXPERTS).astype(t.dtype)
    y = jnp.zeros_like(t)
    for e in range(N_EXPERTS):
        he = jax.nn.silu(t @ w_e_gate[e]) * (t @ w_e_up[e])
        y = y + gates[:, e:e + 1] * (he @ w_e_down[e])
    return y.reshape(B, S, D)


def setup_inputs(seed: int = 0) -> dict:
    key = jax.random.key(seed)
    ks = iter(jax.random.split(key, 64))
    L = DEPTH
    f32 = jnp.float32

    def nrm(shape, fan_in):
        return jax.random.normal(next(ks), shape, f32) * fan_in ** -0.5

    def gain(shape):
        return 1.0 + 0.02 * jax.random.normal(next(ks), shape, f32)

    def bias(shape):
        return 0.01 * jax.random.normal(next(ks), shape, f32)

    x = jax.random.normal(next(ks), (BATCH, SEQ, D_MODEL), f32)
    mem = jax.random.normal(next(ks), (BATCH, MEM_TOKENS, D_MODEL), f32)
    a0 = jax.random.uniform(next(ks), (L, RNN_WIDTH), f32, minval=0.9, maxval=0.999)
    s0 = a0 ** (1.0 / LRU_C)
    lam = jnp.log(s0) - jnp.log1p(-s0)
    return {
        "x": x,
        "mem": mem,
        "g_mix": gain((L, D_MODEL)),
        "w_in": nrm((L, D_MODEL, D_IN), D_MODEL),
        "g_cq": gain((L, Q_LORA)),
        "w_uq": nrm((L, Q_LORA, MLA_HEADS * MLA_QK), Q_LORA),
        "g_ckv": gain((L, KV_LORA)),
        "w_ukv": nrm((L, KV_LORA, MLA_HEADS * (MLA_NOPE + MLA_V)), KV_LORA),
        "g_qn": gain((L, MLA_QK)),
        "g_kn": gain((L, MLA_QK)),
        "conv_w": nrm((L, CONV_WIDTH, RNN_WIDTH), CONV_WIDTH),
        "conv_b": bias((L, RNN_WIDTH)),
        "w_rg": nrm((L, RNN_BLOCKS, RNN_BLOCK_DIM, RNN_BLOCK_DIM), RNN_BLOCK_DIM),
        "b_rg": bias((L, RNN_WIDTH)),
        "w_ig": nrm((L, RNN_BLOCKS, RNN_BLOCK_DIM, RNN_BLOCK_DIM), RNN_BLOCK_DIM),
        "b_ig": bias((L, RNN_WIDTH)),
        "lam": lam,
        "g_attn_out": gain((L, MLA_OUT)),
        "g_rnn_out": gain((L, RNN_WIDTH)),
        "w_out": nrm((L, D_MIX, D_MODEL), D_MIX),
        "g_xq": gain((L, D_MODEL)),
        "g_mem": gain((L, D_MODEL)),
        "w_mq": nrm((L, D_MODEL, MEM_HEADS * MEM_HEAD_DIM), D_MODEL),
        "w_mk": nrm((L, D_MODEL, MEM_HEADS * MEM_HEAD_DIM), D_MODEL),
        "w_mv": nrm((L, D_MODEL, MEM_HEADS * MEM_HEAD_DIM), D_MODEL),
        "g_mqn": gain((L, MEM_HEAD_DIM)),
        "g_mkn": gain((L, MEM_HEAD_DIM)),
        "w_mo": nrm((L, MEM_HEADS * MEM_HEAD_DIM, D_MODEL), MEM_HEADS * MEM_HEAD_DIM),
        "g_ffn": gain((L, D_MODEL)),
        "w_group": nrm((L, D_MODEL, N_GROUPS), D_MODEL),
        "b_group": bias((L, N_GROUPS)),
        "w_expert": nrm((L, D_MODEL, N_EXPERTS), D_MODEL),
        "b_expert": bias((L, N_EXPERTS)),
        "w_e_gate": nrm((L, N_EXPERTS, D_MODEL, D_EXPERT), D_MODEL),
        "w_e_up": nrm((L, N_EXPERTS, D_MODEL, D_EXPERT), D_MODEL),
        "w_e_down": nrm((L, N_EXPERTS, D_EXPERT, D_MODEL), D_EXPERT),
    }


def reference(x, mem, g_mix, w_in, g_cq, w_uq, g_ckv, w_ukv, g_qn, g_kn,
              conv_w, conv_b, w_rg, b_rg, w_ig, b_ig, lam, g_attn_out, g_rnn_out, w_out,
              g_xq, g_mem, w_mq, w_mk, w_mv, g_mqn, g_mkn, w_mo,
              g_ffn, w_group, b_group, w_expert, b_expert, w_e_gate, w_e_up, w_e_down):
    B, S, _ = x.shape
    cos, sin = rope_tables(S)
    for l in range(DEPTH):
        h = rms_norm(x, g_mix[l])
        z = h @ w_in[l]
        cq, ckv, k_rope, u_gate, u_x = jnp.split(z, IN_SPLITS, axis=-1)
        q = (rms_norm(cq, g_cq[l]) @ w_uq[l]).reshape(B, S, MLA_HEADS, MLA_QK)
        kv = (rms_norm(ckv, g_ckv[l]) @ w_ukv[l]).reshape(B, S, MLA_HEADS, MLA_NOPE + MLA_V)
        k_nope, v = kv[..., :MLA_NOPE], kv[..., MLA_NOPE:]
        k = jnp.concatenate(
            [k_nope, jnp.broadcast_to(k_rope[:, :, None, :], (B, S, MLA_HEADS, MLA_ROPE))], axis=-1)
        q = rms_norm(q, g_qn[l])
        k = rms_norm(k, g_kn[l])
        q = jnp.concatenate([q[..., :MLA_NOPE], apply_rope(q[..., MLA_NOPE:], cos, sin)], axis=-1)
        k = jnp.concatenate([k[..., :MLA_NOPE], apply_rope(k[..., MLA_NOPE:], cos, sin)], axis=-1)
        o_attn = block_causal_attention(q, k, v)
        o_rnn = jax.nn.gelu(u_gate) * rg_lru(u_x, conv_w[l], conv_b[l], w_rg[l], b_rg[l],
                                             w_ig[l], b_ig[l], lam[l])
        mix = jnp.concatenate([rms_norm(o_attn, g_attn_out[l]), rms_norm(o_rnn, g_rnn_out[l])], axis=-1)
        x = x + mix @ w_out[l]
        x = x + mem_cross_attention(rms_norm(x, g_xq[l]), rms_norm(mem, g_mem[l]),
                                    w_mq[l], w_mk[l], w_mv[l], g_mqn[l], g_mkn[l], w_mo[l])
        x = x + hier_moe(rms_norm(x, g_ffn[l]), w_group[l], b_group[l], w_expert[l], b_expert[l],
                         w_e_gate[l], w_e_up[l], w_e_down[l])
    return x
```

```python
import os
import numpy as np
import ml_dtypes
import concourse.bass as bass
import concourse.mybir as mybir
from concourse.bass_utils import run_bass_kernel_spmd

F32 = mybir.dt.float32
BF16 = mybir.dt.bfloat16
AF = mybir.ActivationFunctionType
ALU = mybir.AluOpType
AX = mybir.AxisListType

ENGS = ("pe", "act", "dve", "pool", "sp")
EPS = 1e-6
D = 1024
NH = 8
DQK = 96
NE = 32
DE = 256
SB_LO = 16640
SB_HI = 228864


class Res:
    __slots__ = ("name", "w", "r")

    def __init__(self, name=""):
        self.name = name
        self.w = None
        self.r = []


class Op:
    __slots__ = ("eng", "fn", "deps", "isdma", "sem", "val", "needs_inc")

    def __init__(self, eng, fn, isdma):
        self.eng = eng
        self.fn = fn
        self.deps = []
        self.isdma = isdma
        self.sem = None
        self.val = None
        self.needs_inc = False


class Sched:
    def __init__(self, nc):
        self.nc = nc
        self.ops = {e: [] for e in ENGS}
        self.nd = {"sp": 24, "pool": 12, "act": 4}
        self.dma_rr = {e: 0 for e in self.nd}
        self.dma_last = {e: [None] * n for e, n in self.nd.items()}
        self.dma_cnt = {e: [0] * n for e, n in self.nd.items()}
        self.last = {e: None for e in ENGS}

    def op(self, eng, fn, reads=(), writes=(), dma=False, extra=()):
        o = Op(eng, fn, dma)
        deps = list(extra)
        for r in reads:
            if r.w is not None:
                deps.append(r.w)
        for w in writes:
            if w.w is not None:
                deps.append(w.w)
            deps.extend(w.r)
        if dma:
            slot = self.dma_rr[eng]
            self.dma_rr[eng] = (slot + 1) % self.nd[eng]
            prev = self.dma_last[eng][slot]
            if prev is not None:
                deps.append(prev)
            self.dma_last[eng][slot] = o
            self.dma_cnt[eng][slot] += 1
            o.sem = ("dma", eng, slot)
            o.val = 16 * self.dma_cnt[eng][slot]
        seen = set()
        for d in deps:
            if d is None or d is o or id(d) in seen:
                continue
            seen.add(id(d))
            if d.eng == "pe" and eng == "pe" and not d.isdma and not dma:
                continue
            o.deps.append(d)
            if not d.isdma:
                d.needs_inc = True
        for r in reads:
            if not dma:
                r.r = [x for x in r.r if x.isdma or x.eng != eng]
            r.r.append(o)
        for w in writes:
            w.w = o
            w.r = []
        self.ops[eng].append(o)
        if not dma:
            self.last[eng] = o
        return o

    def barrier(self):
        deps = [self.last[e] for e in ENGS if self.last[e] is not None]
        for e in self.nd:
            deps.extend(x for x in self.dma_last[e] if x is not None)
        for e in ENGS:
            self.op(e, lambda eng: eng.nop(), extra=deps)

    def emit(self, final_waits=()):
        nc = self.nc
        esem = {e: nc.alloc_semaphore(f"s_{e}") for e in ENGS}
        dsem = {e: [nc.alloc_semaphore(f"d_{e}{i}") for i in range(n)] for e, n in self.nd.items()}
        for e in ENGS:
            c = 0
            for o in self.ops[e]:
                if o.isdma:
                    o.sem = dsem[o.sem[1]][o.sem[2]]
                elif o.needs_inc:
                    c += 1
                    o.sem = esem[e]
                    o.val = c
        emap = {"pe": "tensor", "act": "scalar", "dve": "vector", "pool": "gpsimd", "sp": "sync"}

        def run(e, engobj):
            known = {}
            for o in self.ops[e]:
                need = {}
                for d in o.deps:
                    k = d.sem.num
                    if k not in need or need[k][1] < d.val:
                        need[k] = (d.sem, d.val)
                for k, (s, v) in need.items():
                    if known.get(k, 0) >= v:
                        continue
                    engobj.wait_ge(s, v)
                    known[k] = v
                ins = o.fn(engobj)
                if o.isdma:
                    ins.then_inc(o.sem, 16)
                elif o.needs_inc:
                    ins.then_inc(o.sem, 1)
            if e == "sp":
                for d in final_waits:
                    engobj.wait_ge(d.sem, d.val)

        with nc.Block() as block:
            for e in ENGS:
                getattr(block, emap[e])(lambda engobj, e=e: run(e, engobj))


class Sel:
    REG = []

    def __init__(self, bufs):
        self.bufs, self.i = bufs, 0
        Sel.REG.append(self)

    def __getitem__(self, k):
        return self.bufs[self.i][k]


class RSel:
    def __init__(self, n):
        self.rs, self.i = [Res() for _ in range(n)], 0
        Sel.REG.append(self)

    @property
    def w(self):
        return self.rs[self.i].w

    @w.setter
    def w(self, v):
        self.rs[self.i].w = v

    @property
    def r(self):
        return self.rs[self.i].r

    @r.setter
    def r(self, v):
        self.rs[self.i].r = v


def set_parity(p):
    if os.environ.get("NO_PAR"):
        p = 0
    for x in Sel.REG:
        x.i = p % len(x.bufs if isinstance(x, Sel) else x.rs)


def interleave(gen_iter, ways, admit_every=0):
    active = []
    gen_iter = iter(gen_iter)
    done = False
    rnd = 0
    last_admit = -10 ** 9
    while active or not done:
        while len(active) < ways and not done and (not active or rnd - last_admit >= admit_every):
            try:
                active.append(next(gen_iter))
                last_admit = rnd
            except StopIteration:
                done = True
        rnd += 1
        for g in list(active):
            try:
                next(g)
            except StopIteration:
                active.remove(g)


class Arena:
    def __init__(self, nc, lo, hi):
        self.nc, self.lo, self.hi, self.cur, self.n = nc, lo, hi, lo, 0

    def alloc(self, shape, dtype, name=None):
        nbytes = int(np.prod(shape[1:])) * (2 if dtype == BF16 else 4)
        off = (self.cur + 31) // 32 * 32
        assert off + nbytes <= self.hi, f"SBUF arena overflow {off + nbytes} > {self.hi} ({name})"
        self.cur = off + nbytes
        self.n += 1
        return self.nc.alloc_sbuf_tensor_at(f"{name or 't'}_{off}_{self.n}", list(shape), dtype, offset=off)


WEIGHT_NAMES = ["g_mix", "w_in", "g_cq", "w_uq", "g_ckv", "w_ukv", "g_qn", "g_kn", "conv_w", "conv_b",
                "w_rg", "b_rg", "w_ig", "b_ig", "lam", "g_attn_out", "g_rnn_out", "w_out", "g_xq", "g_mem",
                "w_mq", "w_mk", "w_mv", "g_mqn", "g_mkn", "w_mo", "g_ffn", "w_group", "b_group", "w_expert",
                "b_expert", "w_e_gate", "w_e_up", "w_e_down"]
WEIGHT_SHAPES = {
    "g_mix": [D], "w_in": [D, 1440], "g_cq": [256], "w_uq": [256, 768], "g_ckv": [128], "w_ukv": [128, 1024],
    "g_qn": [96], "g_kn": [96], "conv_w": [4, 512], "conv_b": [512], "w_rg": [8, 64, 64], "b_rg": [512],
    "w_ig": [8, 64, 64], "b_ig": [512], "lam": [512], "g_attn_out": [512], "g_rnn_out": [512], "w_out": [D, D],
    "g_xq": [D], "g_mem": [D], "w_mq": [D, D], "w_mk": [D, D], "w_mv": [D, D], "g_mqn": [256], "g_mkn": [256],
    "w_mo": [D, D], "g_ffn": [D], "w_group": [D, 4], "b_group": [4], "w_expert": [D, 32], "b_expert": [32],
    "w_e_gate": [NE, D, DE], "w_e_up": [NE, D, DE], "w_e_down": [NE, DE, D]}


def build(S_len, phases="ABCD", dbg=False, moe_stop="F"):
    NT = S_len // 128
    NG = S_len // 512
    nc = bass.Bass("TRN2", target_bir_lowering=False)
    x_d = nc.dram_tensor("x", [S_len, D], F32, kind="ExternalInput").ap()
    mem_d = nc.dram_tensor("mem", [256, D], F32, kind="ExternalInput").ap()
    W = {n: nc.dram_tensor(n, WEIGHT_SHAPES[n], F32, kind="ExternalInput").ap() for n in WEIGHT_NAMES}
    cs_d = nc.dram_tensor("cs_tab", [S_len, 64], F32, kind="ExternalInput").ap()
    idb_d = nc.dram_tensor("ident_bf", [128, 128], BF16, kind="ExternalInput").ap()
    idf_d = nc.dram_tensor("ident_f32", [128, 128], F32, kind="ExternalInput").ap()
    out_d = nc.dram_tensor("out", [S_len, D], F32, kind="ExternalOutput").ap()
    skind = "ExternalOutput" if dbg else "Internal"
    QT = nc.dram_tensor("QT", [NH, DQK, S_len], BF16, kind=skind).ap()
    KT = nc.dram_tensor("KT", [NH, DQK, S_len], BF16, kind=skind).ap()
    VA = nc.dram_tensor("VA", [NH, 128, NT, 65], BF16, kind=skind).ap()
    ORT = nc.dram_tensor("ORT", [4, 128, S_len], BF16, kind=skind).ap()
    OA = nc.dram_tensor("OA", [S_len, 512], BF16, kind=skind).ap()
    RR = nc.dram_tensor("RR", [128, NT], F32, kind=skind).ap()
    WGU2 = nc.dram_tensor("WGU2", [NE, 128, 8, 2 * DE], BF16).ap()
    WD2 = nc.dram_tensor("WD2", [NE, 128, 2, D], BF16).ap()
    NSLOT = ((2 * S_len) // 256 + NE) * 256
    H3 = nc.dram_tensor("H3", [S_len, D], BF16).ap()
    Hs = nc.dram_tensor("Hs", [NSLOT, D], BF16).ap()
    Ys = nc.dram_tensor("Ys", [NSLOT, D], BF16).ap()
    lst_d = nc.dram_tensor("lstrict", [128, 128], BF16, kind="ExternalInput").ap()
    thr_d = nc.dram_tensor("thr_tab", [32], F32, kind="ExternalInput").ap()
    iota_d = nc.dram_tensor("iota_tab", [NSLOT // 256], F32, kind="ExternalInput").ap()
    pidx_d = nc.dram_tensor("pidx_tab", [128], F32, kind="ExternalInput").ap()
    if dbg:
        DBG_widx = nc.dram_tensor("DBG_widx", [128, NSLOT // 256], mybir.dt.int32, kind="ExternalOutput").ap()
        DBG_pos = nc.dram_tensor("DBG_pos", [128, 2, NT], mybir.dt.int32, kind="ExternalOutput").ap()
        DBG_gab = nc.dram_tensor("DBG_gab", [128, NT, 2], F32, kind="ExternalOutput").ap()

    S = Sched(nc)
    P = Arena(nc, SB_LO, SB_LO + 6144)
    pairs = [nc.alloc_psum_tensor(f"pair{j}", [128, 1024], F32) for j in range(4)]
    banks = [pairs[i // 2][:, (i % 2) * 512:(i % 2 + 1) * 512] for i in range(8)]
    rb = [Res(f"bank{i}") for i in range(8)]

    def bview(i):
        return pairs[i // 2][:].bitcast(BF16)[:, (i % 2) * 1024:(i % 2 + 1) * 1024]

    def dma(q, out, in_, reads=(), writes=()):
        return S.op(q, lambda e: e.dma_start(out=out, in_=in_), reads=reads, writes=writes, dma=True)

    def dma_nc(q, out, in_, reads=(), writes=()):
        def f(e):
            with nc.allow_non_contiguous_dma(reason="small param vectors"):
                return e.dma_start(out=out, in_=in_)
        return S.op(q, f, reads=reads, writes=writes, dma=True)

    def mm(out, lhsT, rhs, start, stop, reads, writes, skip=False):
        return S.op("pe", lambda e: e.matmul(out, lhsT=lhsT, rhs=rhs, start=start, stop=stop,
                                             skip_group_check=skip), reads, writes)

    def act(out, in_, func, reads, writes, scale=1.0, bias=None, accum=None):
        kw = {}
        if bias is not None:
            kw["bias"] = bias
        if accum is not None:
            kw["accum_out"] = accum
        return S.op("act", lambda e: e.activation(out=out, in_=in_, func=func, scale=scale, **kw), reads, writes)

    def tt(eng, out, in0, in1, op, reads, writes):
        return S.op(eng, lambda e: e.tensor_tensor(out=out, in0=in0, in1=in1, op=op), reads, writes)

    def ts(eng, out, in0, s1, s2, op0, op1, reads, writes):
        if s2 is None:
            return S.op(eng, lambda e: e.tensor_scalar(out=out, in0=in0, scalar1=s1, scalar2=None, op0=op0), reads, writes)
        return S.op(eng, lambda e: e.tensor_scalar(out=out, in0=in0, scalar1=s1, scalar2=s2, op0=op0, op1=op1), reads, writes)

    def stt(eng, out, in0, scalar, in1, op0, op1, reads, writes):
        return S.op(eng, lambda e: e.scalar_tensor_tensor(out=out, in0=in0, scalar=scalar, in1=in1, op0=op0, op1=op1),
                    reads, writes)

    def cp(eng, out, in_, reads, writes):
        if eng == "act":
            return act(out, in_, AF.Copy, reads, writes)
        return S.op(eng, lambda e: e.tensor_copy(out=out, in_=in_), reads, writes)

    def red(out, in_, op, reads, writes):
        return S.op("dve", lambda e: e.tensor_reduce(out=out, in_=in_, axis=AX.X, op=op), reads, writes)

    def recip(out, in_, reads, writes):
        return S.op("dve", lambda e: e.reciprocal(out=out, in_=in_), reads, writes)

    def memset(eng, ap, val, writes):
        return S.op(eng, lambda e: e.memset(ap, val), (), writes)

    idb = P.alloc([128, 128], BF16, "idb"); r_idb = Res()
    idf = P.alloc([128, 128], F32, "idf"); r_idf = Res()
    ones = P.alloc([128, 2], BF16, "ones"); r_ones = Res()
    cneg = P.alloc([128, 1], F32, "cneg"); chalf = P.alloc([128, 1], F32, "chalf"); r_c = Res()
    rr_all = P.alloc([128, max(NT, 8)], F32, "rr_all"); r_rr = Res()
    dma("sp", idb[:], idb_d, writes=[r_idb])
    dma("sp", idf[:], idf_d, writes=[r_idf])
    memset("pool", ones[:], 1.0, [r_ones])
    memset("pool", cneg[:], -0.5, [r_c])
    memset("pool", chalf[:], 0.5, [r_c])
    cone = P.alloc([128, 1], F32, "cone")
    memset("pool", cone[:], 1.0, [r_c])

    def transpose(out, in_, reads, writes, f32=False):
        idt, rid = (idf, r_idf) if f32 else (idb, r_idb)
        return S.op("pe", lambda e: e.transpose(out=out, in_=in_, identity=idt[:]), list(reads) + [rid], writes)

    def rstd_chain(buf, n, inv_dim, reads_writes):
        ts("dve", buf, buf, inv_dim, EPS, ALU.mult, ALU.add, reads_writes, reads_writes)
        act(buf, buf, AF.Ln, reads_writes, reads_writes)
        act(buf, buf, AF.Exp, reads_writes, reads_writes, scale=-0.5)

    def colvec(arena, name, src, ncol):
        t = arena.alloc([128, ncol], F32, name)
        r = Res(name)
        dma_nc("sp", t[:], src.rearrange("(c p) -> p c", p=128), writes=[r])
        return t, r

    def bcast(arena, name, src, n):
        t = arena.alloc([128, n], F32, name)
        r = Res(name)
        dma("sp", t[:], src.partition_broadcast(128), writes=[r])
        return t, r

    r_wgu = [Res() for _ in range(NE)]
    r_wd = [Res() for _ in range(NE)]

    def convert_experts(e0, e1):
        for e in range(e0, min(e1, NE)):
            wg = WGU2[e].rearrange("p k n -> k p n")
            dma("pool", wg[:, :, 0:DE], W["w_e_gate"][e].rearrange("(k p) n -> k p n", p=128), writes=[r_wgu[e]])
            dma("pool", wg[:, :, DE:2 * DE], W["w_e_up"][e].rearrange("(k p) n -> k p n", p=128), writes=[r_wgu[e]])
            dma("pool", WD2[e].rearrange("p c n -> c p n"), W["w_e_down"][e].rearrange("(c p) n -> c p n", p=128),
                writes=[r_wd[e]])

    r_QT, r_KT, r_VA, r_ORT, r_OA = Res(), Res(), Res(), Res(), Res()

    def phaseA():
        A = Arena(nc, SB_LO + 6144, SB_HI)
        w_in = A.alloc([128, 8, 1440], BF16, "w_in"); r_win = Res()
        dma("pool", w_in[:], W["w_in"].rearrange("(k p) n -> p k n", p=128), writes=[r_win])
        w_uq = A.alloc([128, 2, 768], BF16, "w_uq"); r_wuq = Res()
        dma("pool", w_uq[:], W["w_uq"].rearrange("(k p) n -> p k n", p=128), writes=[r_wuq])
        w_ukv = A.alloc([128, 1024], BF16, "w_ukv"); r_wukv = Res()
        dma("pool", w_ukv[:], W["w_ukv"], writes=[r_wukv])
        wrg = A.alloc([128, 4, 128], BF16, "wrg"); wig = A.alloc([128, 4, 128], BF16, "wig"); r_wg = Res()
        memset("pool", wrg[:], 0.0, [r_wg])
        memset("pool", wig[:], 0.0, [r_wg])
        for c in range(4):
            for half in range(2):
                sl = slice(half * 64, half * 64 + 64)
                dma("pool", wrg[sl, c, sl], W["w_rg"][2 * c + half], writes=[r_wg])
                dma("pool", wig[sl, c, sl], W["w_ig"][2 * c + half], writes=[r_wg])
        gmix_b, r_gmix = bcast(A, "gmix_b", W["g_mix"], 1024)
        gq_b, r_gq = bcast(A, "gq_b", W["g_qn"], 96)
        gk_b, r_gk = bcast(A, "gk_b", W["g_kn"], 96)
        ts("dve", gq_b[:], gq_b[:], float(DQK) ** -0.5, None, ALU.mult, None, [r_gq], [r_gq])
        gcq, r_gcq = colvec(A, "gcq", W["g_cq"], 2)
        gckv, r_gckv = colvec(A, "gckv", W["g_ckv"], 1)
        cb, r_cb = colvec(A, "cb", W["conv_b"], 4)
        brg, r_brg = colvec(A, "brg", W["b_rg"], 4)
        big, r_big = colvec(A, "big", W["b_ig"], 4)
        lam, r_lam = colvec(A, "lam", W["lam"], 4)
        cw = A.alloc([128, 4, 4], F32, "cw"); r_cw = Res()
        for j in range(4):
            dma_nc("sp", cw[:, :, j], W["conv_w"][j].rearrange("(c p) -> p c", p=128), writes=[r_cw])
        c1 = A.alloc([128, 4], F32, "c1"); c2 = A.alloc([128, 4], F32, "c2")
        zt = A.alloc([128, 4], F32, "zt"); wv = A.alloc([128, 4], F32, "wv"); w2 = A.alloc([128, 4], F32, "w2")
        r_c12 = Res(); r_z = Res()
        act(zt[:], lam[:], AF.Exp, [r_lam], [r_z], scale=-1.0)
        ts("dve", wv[:], zt[:], 2.0, None, ALU.add, None, [r_z], [r_z])
        S.op("dve", lambda e: e.reciprocal(out=wv[:], in_=wv[:]), [r_z], [r_z])
        tt("dve", wv[:], wv[:], zt[:], ALU.mult, [r_z], [r_z])
        tt("dve", w2[:], wv[:], wv[:], ALU.mult, [r_z], [r_z])
        ts("dve", zt[:], w2[:], 1.0 / 9, 1.0 / 7, ALU.mult, ALU.add, [r_z], [r_z])
        tt("dve", zt[:], zt[:], w2[:], ALU.mult, [r_z], [r_z])
        ts("dve", zt[:], zt[:], 1.0 / 5, None, ALU.add, None, [r_z], [r_z])
        tt("dve", zt[:], zt[:], w2[:], ALU.mult, [r_z], [r_z])
        ts("dve", zt[:], zt[:], 1.0 / 3, None, ALU.add, None, [r_z], [r_z])
        tt("dve", zt[:], zt[:], w2[:], ALU.mult, [r_z], [r_z])
        ts("dve", zt[:], zt[:], 1.0, None, ALU.add, None, [r_z], [r_z])
        tt("dve", zt[:], zt[:], wv[:], ALU.mult, [r_z], [r_z])
        ts("dve", c1[:], zt[:], -16.0, None, ALU.mult, None, [r_z], [r_c12])
        ts("dve", c2[:], zt[:], -32.0, None, ALU.mult, None, [r_z], [r_c12])

        xb = [A.alloc([128, 1024], F32, "xb") for _ in range(4)]; r_xb = [Res() for _ in range(4)]
        junk = A.alloc([128, 1024], BF16, "junk"); r_junk = Res()
        ssx = A.alloc([128, 4], F32, "ssx"); r_ssx = Res()
        hb = [A.alloc([128, 1024], BF16, "hb") for _ in range(2)]; r_hb = [Res() for _ in range(2)]
        hT = [A.alloc([128, 8, 512], BF16, "hT") for _ in range(2)]; r_hT = [Res() for _ in range(2)]
        csb = [A.alloc([128, 4, 64], F32, "csb") for _ in range(2)]; r_cs = [Res() for _ in range(2)]
        cqT = A.alloc([128, 2, 512], BF16, "cqT"); r_cqT = Res()
        ckvT = A.alloc([128, 512], BF16, "ckvT"); r_ckvT = Res()
        sqc = A.alloc([128, 3, 512], BF16, "sqc"); r_sqc = Res()
        sqr = A.alloc([128, 4, 512], BF16, "sqr"); r_sqr = Res()
        ornb = A.alloc([128, 4, 512], BF16, "ornb"); r_ornb = Res()
        uxT = [A.alloc([128, 4, 515], F32, "uxT") for _ in range(2)]; r_ux = [Res() for _ in range(2)]
        memset("pool", uxT[0][:, :, 0:3], 0.0, [r_ux[0]])
        carry = A.alloc([128, 4], F32, "carry"); r_carry = Res()
        memset("pool", carry[:], 0.0, [r_carry])
        def two(name, dt=F32):
            return [A.alloc([128, 512], dt, name) for _ in range(2)], [Res() for _ in range(2)]
        ug, r_ug = two("ug"); gw, r_gw = two("gw"); gel, r_gel = two("gel")
        xc, r_xc = two("xc"); xcb, r_xcb = two("xcb", BF16)
        rg, r_rg = two("rg"); ig, r_ig = two("ig"); av, r_av = two("av"); hs, r_hs = two("hs"); orn, r_orn = two("orn")
        stc = A.alloc([128, 4, 4], F32, "stc"); r_stc = Res()
        qs = A.alloc([128, 8, 96], F32, "qs"); r_qs = Res()
        ks = A.alloc([128, 8, 96], F32, "ks"); r_ks = Res()
        tq = A.alloc([128, 8, 96], F32, "tq"); r_tq = Res()
        tk = A.alloc([128, 8, 96], F32, "tk"); r_tk = Res()
        ssq = A.alloc([128, 16], F32, "ssq"); r_ssq = Res()
        rt1 = A.alloc([128, 8, 32], F32, "rt1"); rt2 = A.alloc([128, 8, 32], F32, "rt2"); r_rt = Res()
        kt1 = A.alloc([128, 8, 32], F32, "kt1"); kt2 = A.alloc([128, 8, 32], F32, "kt2"); r_kt = Res()
        qb = A.alloc([128, 8, 96], BF16, "qb"); r_qb = Res()
        kb = A.alloc([128, 8, 96], BF16, "kb"); r_kb = Res()
        QTst = A.alloc([128, 8, 512], BF16, "QTst"); r_QTst = Res()
        KTst = A.alloc([128, 8, 512], BF16, "KTst"); r_KTst = Res()
        Vst = A.alloc([128, 8, 4, 65], BF16, "Vst"); r_Vst = Res()
        memset("pool", Vst[:], 1.0, [r_Vst])
        print("phase A SBUF used", A.cur)

        ZB = [0, 1]; TB = 2; SB = 3; QB = (4, 5); KVB = (6, 7)
        r_stat_c = Res(); r_stat_r = Res(); r_kr = Res()
        stat_c = banks[SB][:, 0:8].rearrange("p (t c) -> p t c", c=2)
        stat_r = banks[SB][:, 8:12]
        kr_ps = banks[SB][:, 64:192].rearrange("p (t c) -> p t c", c=32)
        zrot = [0]

        def zbank():
            b = ZB[zrot[0] % 2]
            zrot[0] += 1
            return b

        epg = -(-NE // NG)
        for G in range(NG):
            convert_experts(G * epg, (G + 1) * epg)
            hTg, r_hTg = hT[G % 2], r_hT[G % 2]
            cst, r_cst = csb[G % 2], r_cs[G % 2]
            dma("sp", cst[:], cs_d[G * 512:(G + 1) * 512, :].rearrange("(t p) c -> p t c", p=128), writes=[r_cst])
            for t in range(4):
                tok = G * 4 + t
                dma("sp", xb[t][:], x_d[tok * 128:(tok + 1) * 128, :], writes=[r_xb[t]])
                act(junk[:], xb[t][:], AF.Square, [r_xb[t]], [r_junk, r_ssx], accum=ssx[:, t:t + 1])
            rstd_chain(ssx[:], 4, 1.0 / D, [r_ssx])
            for t in range(4):
                h_, r_h = hb[t % 2], r_hb[t % 2]
                stt("dve", h_[:], xb[t][:], ssx[:, t:t + 1], gmix_b[:], ALU.mult, ALU.mult,
                    [r_xb[t], r_ssx, r_gmix], [r_h])
                tv = bview(TB).rearrange("p (k n) -> p k n", k=8)
                for k in range(8):
                    transpose(tv[:, k, :], h_[:, k * 128:(k + 1) * 128], [r_h], [rb[TB]])
                cp("act", hTg[:, :, t * 128:(t + 1) * 128], tv, [rb[TB]], [r_hTg])

            def zmm(col0, ncols, b):
                for k in range(8):
                    mm(banks[b][0:ncols, :], w_in[:, k, col0:col0 + ncols], hTg[:, k, :], k == 0, k == 7,
                       [r_win, r_hTg], [rb[b]])

            for j in range(2):
                b = zbank(); zmm(j * 128, 128, b)
                act(sqc[:, j, :], banks[b][:], AF.Square, [rb[b]], [r_sqc])
                act(cqT[:, j, :], banks[b][:], AF.Copy, [rb[b], r_gcq], [r_cqT], scale=gcq[:, j:j + 1])
            b = zbank(); zmm(256, 128, b)
            act(sqc[:, 2, :], banks[b][:], AF.Square, [rb[b]], [r_sqc])
            act(ckvT[:], banks[b][:], AF.Copy, [rb[b], r_gckv], [r_ckvT], scale=gckv[:, 0:1])
            for t in range(4):
                tsl = slice(t * 128, (t + 1) * 128)
                for j in range(2):
                    mm(stat_c[:, t, 0:1], sqc[:, j, tsl], ones[:, 0:1], j == 0, j == 1, [r_sqc, r_ones], [r_stat_c])
                mm(stat_c[:, t, 1:2], sqc[:, 2, tsl], ones[:, 0:1], True, True, [r_sqc, r_ones], [r_stat_c])
            ts("dve", stc[:, :, 0:1], stat_c[:, :, 0:1], 1.0 / 256, EPS, ALU.mult, ALU.add, [r_stat_c], [r_stc])
            ts("dve", stc[:, :, 1:2], stat_c[:, :, 1:2], 1.0 / 128, EPS, ALU.mult, ALU.add, [r_stat_c], [r_stc])
            stc2 = stc[:, :, 0:2]
            tt("pool", stc2, stc2, cneg[:, 0:1].unsqueeze(2).to_broadcast([128, 4, 2]), ALU.pow, [r_stc, r_c], [r_stc])

            def qk_gen():
                for t in range(4):
                    tsl = slice(t * 128, (t + 1) * 128)
                    qv = [banks[QB[0]][:, 0:384], banks[QB[1]][:, 0:384]]
                    for half in range(2):
                        for j in range(2):
                            mm(qv[half], cqT[:, j, tsl], w_uq[:, j, half * 384:(half + 1) * 384], j == 0, j == 1,
                               [r_cqT, r_wuq], [rb[QB[half]]])
                            yield
                    for half in range(2):
                        mm(banks[KVB[half]][:], ckvT[:, tsl], w_ukv[:, half * 512:(half + 1) * 512], True, True,
                           [r_ckvT, r_wukv], [rb[KVB[half]]])
                        yield
                    for k in range(8):
                        mm(kr_ps[:, t, :], hTg[:, k, tsl], w_in[:, k, 384:416], k == 0, k == 7, [r_hTg, r_win], [r_kr])
                        yield
                    rcq = stc[:, t, 0:1]; rckv = stc[:, t, 1:2]
                    for half in range(2):
                        hs4 = slice(half * 4, half * 4 + 4)
                        act(qs[:, hs4, :], qv[half].rearrange("p (h d) -> p h d", h=4), AF.Copy,
                            [rb[QB[half]], r_stc], [r_qs], scale=rcq)
                        yield
                        kvv = banks[KVB[half]][:].rearrange("p (h d) -> p h d", h=4)
                        act(ks[:, hs4, 0:64], kvv[:, :, 0:64], AF.Copy, [rb[KVB[half]], r_stc], [r_ks], scale=rckv)
                        yield
                        act(Vst[:, hs4, t, 0:64], kvv[:, :, 64:128], AF.Copy, [rb[KVB[half]], r_stc], [r_Vst], scale=rckv)
                        yield
                    cp("dve", ks[:, :, 64:96], kr_ps[:, t, :].unsqueeze(1).to_broadcast([128, 8, 32]), [r_kr], [r_ks])
                    yield
                    act(tq[:], qs[:], AF.Square, [r_qs], [r_tq])
                    yield
                    act(tk[:], ks[:], AF.Square, [r_ks], [r_tk])
                    yield
                    S.op("dve", lambda e: e.tensor_reduce(out=ssq[:, 0:8], in_=tq[:], axis=AX.X, op=ALU.add), [r_tq], [r_ssq])
                    yield
                    S.op("dve", lambda e: e.tensor_reduce(out=ssq[:, 8:16], in_=tk[:], axis=AX.X, op=ALU.add), [r_tk], [r_ssq])
                    yield
                    rstd_chain(ssq[:], 16, 1.0 / DQK, [r_ssq])
                    yield
                    for (src, r_src, tmp, r_tmp, g_b, r_g, o0, t1, t2, r_t, dst, r_dst, st, r_st) in (
                            (qs, r_qs, tq, r_tq, gq_b, r_gq, 0, rt1, rt2, r_rt, qb, r_qb, QTst, r_QTst),
                            (ks, r_ks, tk, r_tk, gk_b, r_gk, 8, kt1, kt2, r_kt, kb, r_kb, KTst, r_KTst)):
                        tt("dve", tmp[:], src[:], ssq[:, o0:o0 + 8].unsqueeze(2).to_broadcast([128, 8, 96]), ALU.mult,
                           [r_src, r_ssq], [r_tmp])
                        yield
                        tt("dve", tmp[:], tmp[:], g_b[:].unsqueeze(1).to_broadcast([128, 8, 96]), ALU.mult,
                           [r_tmp, r_g], [r_tmp])
                        yield
                        c2b = cst[:, t, 0:32].unsqueeze(1).to_broadcast([128, 8, 32])
                        tt("dve", t1[:], tmp[:, :, 64:96], c2b, ALU.mult, [r_tmp, r_cst], [r_t])
                        yield
                        tt("dve", t2[:, :, 0:16], tmp[:, :, 80:96],
                           cst[:, t, 32:48].unsqueeze(1).to_broadcast([128, 8, 16]), ALU.mult, [r_tmp, r_cst], [r_t])
                        yield
                        tt("dve", t2[:, :, 16:32], tmp[:, :, 64:80],
                           cst[:, t, 48:64].unsqueeze(1).to_broadcast([128, 8, 16]), ALU.mult, [r_tmp, r_cst], [r_t])
                        yield
                        tt("dve", dst[:, :, 64:96], t1[:], t2[:], ALU.add, [r_t], [r_dst])
                        yield
                        cp("act", dst[:, :, 0:64], tmp[:, :, 0:64], [r_tmp], [r_dst])
                        yield
                        tv = bview(TB).rearrange("p (h n) -> p h n", h=8)
                        for h in range(NH):
                            transpose(tv[0:96, h, :], dst[:, h, :], [r_dst], [rb[TB]])
                            yield
                        cp("dve", st[0:96, :, tsl], tv[0:96, :, :], [rb[TB]], [r_st])
                        yield


            def rnn_gen():
                U, r_U = uxT[G % 2], r_ux[G % 2]
                Un, r_Un = uxT[(G + 1) % 2], r_ux[(G + 1) % 2]
                for c in range(4):
                    pb = c % 2
                    b = zbank(); zmm(416 + c * 128, 128, b)
                    act(ug[pb][:], banks[b][:], AF.Copy, [rb[b]], [r_ug[pb]])
                    yield
                    act(gw[pb][:], banks[b][:], AF.Square, [rb[b]], [r_gw[pb]])
                    yield
                    ts("dve", gw[pb][:], gw[pb][:], 0.044715, 1.0, ALU.mult, ALU.add, [r_gw[pb]], [r_gw[pb]])
                    yield
                    tt("dve", gw[pb][:], gw[pb][:], ug[pb][:], ALU.mult, [r_gw[pb], r_ug[pb]], [r_gw[pb]])
                    yield
                    act(gw[pb][:], gw[pb][:], AF.Sigmoid, [r_gw[pb]], [r_gw[pb]], scale=1.5957691216057308)
                    yield
                    tt("dve", gel[pb][:], gw[pb][:], ug[pb][:], ALU.mult, [r_gw[pb], r_ug[pb]], [r_gel[pb]])
                    yield
                    b = zbank(); zmm(928 + c * 128, 128, b)
                    act(U[:, c, 3:515], banks[b][:], AF.Copy, [rb[b]], [r_U])
                    yield
                    cp("pool", Un[:, c, 0:3], U[:, c, 512:515], [r_U], [r_Un])
                    yield
                    ts("dve", xc[pb][:], U[:, c, 3:515], cw[:, c, 3:4], cb[:, c:c + 1], ALU.mult, ALU.add,
                       [r_U, r_cw, r_cb], [r_xc[pb]])
                    yield
                    for j in (2, 1, 0):
                        stt("dve", xc[pb][:], U[:, c, j:j + 512], cw[:, c, j:j + 1], xc[pb][:], ALU.mult, ALU.add,
                            [r_U, r_cw, r_xc[pb]], [r_xc[pb]])
                        yield
                    cp("act", xcb[pb][:], xc[pb][:], [r_xc[pb]], [r_xcb[pb]])
                    yield
                    b1 = zbank()
                    mm(banks[b1][:], wrg[:, c, :], xcb[pb][:], True, True, [r_wg, r_xcb[pb]], [rb[b1]])
                    yield
                    act(rg[pb][:], banks[b1][:], AF.Sigmoid, [rb[b1], r_brg], [r_rg[pb]], bias=brg[:, c:c + 1])
                    yield
                    b2 = zbank()
                    mm(banks[b2][:], wig[:, c, :], xcb[pb][:], True, True, [r_wg, r_xcb[pb]], [rb[b2]])
                    yield
                    act(ig[pb][:], banks[b2][:], AF.Sigmoid, [rb[b2], r_big], [r_ig[pb]], bias=big[:, c:c + 1])
                    yield
                    act(av[pb][:], rg[pb][:], AF.Exp, [r_rg[pb], r_c12], [r_av[pb]], scale=c1[:, c:c + 1])
                    yield
                    act(rg[pb][:], rg[pb][:], AF.Exp, [r_rg[pb], r_c12], [r_rg[pb]], scale=c2[:, c:c + 1])
                    yield
                    act(rg[pb][:], rg[pb][:], AF.Relu, [r_rg[pb], r_c], [r_rg[pb]], scale=-1.0, bias=cone[:, 0:1])
                    yield
                    act(rg[pb][:], rg[pb][:], AF.Sqrt, [r_rg[pb]], [r_rg[pb]])
                    yield
                    tt("dve", ig[pb][:], ig[pb][:], xc[pb][:], ALU.mult, [r_ig[pb], r_xc[pb]], [r_ig[pb]])
                    yield
                    tt("dve", rg[pb][:], rg[pb][:], ig[pb][:], ALU.mult, [r_rg[pb], r_ig[pb]], [r_rg[pb]])
                    yield
                    S.op("dve", lambda e, pb=pb, c=c: e.tensor_tensor_scan(
                        out=hs[pb][:], data0=av[pb][:], data1=rg[pb][:], initial=carry[:, c:c + 1],
                        op0=ALU.mult, op1=ALU.add), [r_av[pb], r_rg[pb], r_carry], [r_hs[pb]])
                    yield
                    cp("pool", carry[:, c:c + 1], hs[pb][:, 511:512], [r_hs[pb]], [r_carry])
                    yield
                    tt("dve", orn[pb][:], gel[pb][:], hs[pb][:], ALU.mult, [r_gel[pb], r_hs[pb]], [r_orn[pb]])
                    yield
                    act(sqr[:, c, :], orn[pb][:], AF.Square, [r_orn[pb]], [r_sqr])
                    yield
                    cp("act", ornb[:, c, :], orn[pb][:], [r_orn[pb]], [r_ornb])
                    yield

            gens = [qk_gen(), rnn_gen()]
            while gens:
                for g in list(gens):
                    try:
                        next(g)
                    except StopIteration:
                        gens.remove(g)
            dma("sp", ORT[:, :, G * 512:(G + 1) * 512].rearrange("c p s -> p c s"), ornb[:], [r_ornb], [r_ORT])
            for t in range(4):
                for c in range(4):
                    mm(stat_r[:, t:t + 1], sqr[:, c, t * 128:(t + 1) * 128], ones[:, 0:1], c == 0, c == 3,
                       [r_sqr, r_ones], [r_stat_r])
            rsl = rr_all[:, G * 4:(G + 1) * 4]
            ts("dve", rsl, stat_r, 1.0 / 512, EPS, ALU.mult, ALU.add, [r_stat_r], [r_rr])
            tt("pool", rsl, rsl, cneg[:, 0:1].to_broadcast([128, 4]), ALU.pow, [r_rr, r_c], [r_rr])
            gsl = slice(G * 512, (G + 1) * 512)
            dma("sp", QT[:, :, gsl].rearrange("h d s -> d h s"), QTst[0:96, :, :], [r_QTst], [r_QT])
            dma("sp", KT[:, :, gsl].rearrange("h d s -> d h s"), KTst[0:96, :, :], [r_KTst], [r_KT])
            dma("sp", VA[:, :, G * 4:(G + 1) * 4, :].rearrange("h p t c -> p h t c"), Vst[:], [r_Vst], [r_VA])
        if dbg:
            dma("sp", RR, rr_all[:, 0:NT], [r_rr], [])

    def phaseB():
        Bn = Arena(nc, SB_LO + 6144, SB_HI)
        QTh = [Bn.alloc([128, S_len], BF16, "QTh") for _ in range(2)]; r_QTh = [Res() for _ in range(2)]
        KTh = [Bn.alloc([128, S_len], BF16, "KTh") for _ in range(2)]; r_KTh = [Res() for _ in range(2)]
        Vh = [Bn.alloc([128, NT, 65], BF16, "Vh") for _ in range(2)]; r_Vh = [Res() for _ in range(2)]
        NPT = 4
        pT = [Bn.alloc([128, 1024], BF16, "pT") for _ in range(NPT)]; r_pT = [Res() for _ in range(NPT)]
        ost = [Bn.alloc([128, 4, 64], BF16, "ost") for _ in range(2)]; r_ost = [Res() for _ in range(2)]
        rec = [Bn.alloc([128, 4], F32, "rec") for _ in range(2)]; r_rec = [Res() for _ in range(2)]
        units = []
        for h in range(NH):
            for G in range(NG):
                for kt in range(0, 4 * G, 2):
                    units.append((h, G, [kt, kt + 1]))
                for j in range(4):
                    units.append((h, G, [4 * G + j]))
        LOOK = 1
        state = {"s_emitted": 0}

        def load_head(h):
            p = h % 2
            dma("sp", QTh[p][0:96, :], QT[h], [r_QT], [r_QTh[p]])
            dma("sp", KTh[p][0:96, :], KT[h], [r_KT], [r_KTh[p]])
            dma("sp", Vh[p][:], VA[h], [r_VA], [r_Vh[p]])

        def emit_score(u):
            h, G, kts = units[u]
            if G == 0 and kts[0] == 0:
                load_head(h)
            p = h % 2
            sp_ = u % 2
            for i, kt in enumerate(kts):
                q0 = max(kt - 4 * G, 0) * 128
                mm(pairs[sp_][:, i * 512 + q0:(i + 1) * 512], KTh[p][0:96, kt * 128:(kt + 1) * 128],
                   QTh[p][0:96, G * 512 + q0:(G + 1) * 512], True, True, [r_KTh[p], r_QTh[p]], [rb[2 * sp_]])

        for u, (h, G, kts) in enumerate(units):
            while state["s_emitted"] < min(len(units), u + 1 + LOOK):
                emit_score(state["s_emitted"])
                state["s_emitted"] += 1
            p = h % 2
            sp_ = u % 2
            pi = u % NPT
            gi = (h * NG + G) % 2
            ob = 4 + gi
            o_ps = banks[ob][:, 0:260].rearrange("p (t c) -> p t c", c=65)
            j0 = kts[0] - 4 * G
            q0 = max(j0, 0) * 128
            w = 512 * len(kts)
            act(pT[pi][:, q0:w], pairs[sp_][:, q0:w], AF.Exp, [rb[2 * sp_]], [r_pT[pi]])
            if j0 >= 0:
                memset("pool", pT[pi][64:128, q0:q0 + 64], 0.0, [r_pT[pi]])
            for i, kt in enumerate(kts):
                j = kt - 4 * G
                for qt in range(max(j, 0), 4):
                    mm(o_ps[:, qt, :], pT[pi][:, i * 512 + qt * 128:i * 512 + (qt + 1) * 128], Vh[p][:, kt, :],
                       kt == 0 and qt == 0, kt == 4 * G + qt, [r_pT[pi], r_Vh[p]], [rb[ob]], skip=True)
            if kts[-1] == 4 * G + 3:
                S.op("dve", lambda e, gi=gi, o_ps=o_ps: e.reciprocal(out=rec[gi][:], in_=o_ps[:, :, 64]),
                     [rb[ob]], [r_rec[gi]])
                tt("dve", ost[gi][:], o_ps[:, :, 0:64], rec[gi][:].unsqueeze(2).to_broadcast([128, 4, 64]), ALU.mult,
                   [rb[ob], r_rec[gi]], [r_ost[gi]])
                dma_nc("sp", OA[G * 512:(G + 1) * 512, h * 64:(h + 1) * 64].rearrange("(t p) d -> p t d", p=128),
                       ost[gi][:], [r_ost[gi]], [r_OA])

    def phaseCD():
        NSETS = int(os.environ.get('NSETS', '3'))
        Cn = Arena(nc, SB_LO + 6144, SB_HI)
        TB = 7
        w_out = Cn.alloc([128, 8, 1024], BF16, "w_out"); r_wout = Res()
        dma("pool", w_out[:], W["w_out"].rearrange("(k p) n -> p k n", p=128), writes=[r_wout])
        grnn, r_grnn = colvec(Cn, "grnn", W["g_rnn_out"], 4)
        for c in range(4):
            ts("dve", w_out[:, 4 + c, :], w_out[:, 4 + c, :], grnn[:, c:c + 1], None, ALU.mult, None,
               [r_wout, r_grnn], [r_wout])
        w_mq = Cn.alloc([128, 8, 1024], BF16, "w_mq"); r_wmq = Res()
        dma("pool", w_mq[:], W["w_mq"].rearrange("(k p) n -> p k n", p=128), writes=[r_wmq])
        w_mo = Cn.alloc([128, 8, 1024], BF16, "w_mo"); r_wmo = Res()
        dma("pool", w_mo[:], W["w_mo"].rearrange("(k p) n -> p k n", p=128), writes=[r_wmo])
        ga_b, r_ga = bcast(Cn, "ga_b", W["g_attn_out"], 512)
        gxq_b, r_gxq = bcast(Cn, "gxq_b", W["g_xq"], 1024)
        gffn_b, r_gffn = bcast(Cn, "gffn_b", W["g_ffn"], 1024)
        gmq_b, r_gmq = bcast(Cn, "gmq_b", W["g_mqn"], 256)
        ts("dve", gmq_b[:], gmq_b[:], 256.0 ** -0.5, None, ALU.mult, None, [r_gmq], [r_gmq])
        wr = Cn.alloc([128, 8, 36], F32, "wr"); r_wr = Res()
        dma_nc("sp", wr[:, :, 0:4], W["w_group"].rearrange("(k p) n -> p k n", p=128), writes=[r_wr])
        dma_nc("sp", wr[:, :, 4:36], W["w_expert"].rearrange("(k p) n -> p k n", p=128), writes=[r_wr])
        br_b = Cn.alloc([128, 36], F32, "br_b"); r_br = Res()
        dma("sp", br_b[:, 0:4], W["b_group"].partition_broadcast(128), writes=[r_br])
        dma("sp", br_b[:, 4:36], W["b_expert"].partition_broadcast(128), writes=[r_br])
        KmT = Cn.alloc([128, 8, 256], BF16, "KmT"); r_KmT = Res()
        Vm = Cn.alloc([128, 2, 4, 256], BF16, "Vm"); r_Vm = Res()
        I32 = mybir.dt.int32
        T_SL = 256
        NTILE = (2 * S_len) // T_SL + NE
        gAB = Cn.alloc([128, NT, 2], F32, "gAB"); r_gAB = Res()
        widx = Cn.alloc([128, NTILE], I32, "widx"); r_widx = Res()
        pos_i = Cn.alloc([128, 2, NT], I32, "pos_i"); r_pos = Res()
        mark2 = Cn.cur
        xt = Sel([Cn.alloc([128, 1024], F32, "xt") for _ in range(NSETS)]); r_xt = RSel(NSETS)
        junk = Sel([Cn.alloc([128, 1024], BF16, "junkc") for _ in range(NSETS)]); r_junk = RSel(NSETS)
        ssv = Sel([Cn.alloc([128, 4], F32, "ssv") for _ in range(NSETS)]); r_ssv = RSel(NSETS)
        h2 = Sel([Cn.alloc([128, 1024], BF16, "h2") for _ in range(NSETS)]); r_h2 = RSel(NSETS)
        h2T = Sel([Cn.alloc([128, 8, 128], BF16, "h2T") for _ in range(NSETS)]); r_h2T = RSel(NSETS)
        tmpf = Sel([Cn.alloc([128, 1024], F32, "tmpf") for _ in range(NSETS)]); r_tmpf = RSel(NSETS)
        ssm = Sel([Cn.alloc([128, 4], F32, "ssm") for _ in range(NSETS)]); r_ssm = RSel(NSETS)
        qmb = Sel([Cn.alloc([128, 1024], BF16, "qmb") for _ in range(NSETS)]); r_qmb = RSel(NSETS)
        mark = Cn.cur
        tvb = bview(TB).rearrange("p (k n) -> p k n", k=8)

        def norm_to_bf16(src, r_src, g_b, r_g, dst, r_dst, sscol, f32dst=None, r_f32=None):
            act(junk[:], src, AF.Square, [r_src], [r_junk, r_ssv], accum=ssv[:, sscol:sscol + 1])
            rstd_chain(ssv[:, sscol:sscol + 1], 1, 1.0 / D, [r_ssv])
            if f32dst is None:
                stt("dve", dst, src, ssv[:, sscol:sscol + 1], g_b[:], ALU.mult, ALU.mult, [r_src, r_ssv, r_g], [r_dst])
            else:
                stt("dve", f32dst, src, ssv[:, sscol:sscol + 1], g_b[:], ALU.mult, ALU.mult, [r_src, r_ssv, r_g], [r_f32])
                cp("act", dst, f32dst, [r_f32], [r_dst])

        def transpose8(src, r_src, dstT, r_dstT, n=8, dst_sl=None):
            for k in range(n):
                transpose(tvb[:, k, :], src[:, k * 128:(k + 1) * 128], [r_src], [rb[TB]])
            cp("act", dstT if dst_sl is None else dst_sl, tvb[:, 0:n, :], [rb[TB]], [r_dstT])

        def head_norm(pb0, g_b, r_g, dst, r_dst):
            for half in range(2):
                act(tmpf[:, half * 512:(half + 1) * 512], banks[pb0 + half][:], AF.Square, [rb[pb0 + half]], [r_tmpf])
            red(ssm[:], tmpf[:].rearrange("p (h d) -> p h d", h=4), ALU.add, [r_tmpf], [r_ssm])
            rstd_chain(ssm[:], 4, 1.0 / 256, [r_ssm])
            for half in range(2):
                tt("dve", tmpf[:, half * 512:(half + 1) * 512].rearrange("p (h d) -> p h d", h=2),
                   banks[pb0 + half][:].rearrange("p (h d) -> p h d", h=2),
                   ssm[:, half * 2:half * 2 + 2].unsqueeze(2).to_broadcast([128, 2, 256]), ALU.mult,
                   [rb[pb0 + half], r_ssm], [r_tmpf])
            tt("dve", dst.rearrange("p (h d) -> p h d", h=4), tmpf[:].rearrange("p (h d) -> p h d", h=4),
               g_b[:].unsqueeze(1).to_broadcast([128, 4, 256]), ALU.mult, [r_tmpf, r_g], [r_dst])

        w_mk = Cn.alloc([128, 8, 1024], BF16, "w_mk"); r_wmk = Res()
        dma("pool", w_mk[:], W["w_mk"].rearrange("(k p) n -> p k n", p=128), writes=[r_wmk])
        w_mv = Cn.alloc([128, 8, 1024], BF16, "w_mv"); r_wmv = Res()
        dma("pool", w_mv[:], W["w_mv"].rearrange("(k p) n -> p k n", p=128), writes=[r_wmv])
        gmem_b, r_gmem = bcast(Cn, "gmem_b", W["g_mem"], 1024)
        gmk_b, r_gmk = bcast(Cn, "gmk_b", W["g_mkn"], 256)
        mnT = Cn.alloc([128, 8, 256], BF16, "mnT"); r_mnT = Res()
        for mt in range(2):
            dma("sp", xt[:], mem_d[mt * 128:(mt + 1) * 128, :], writes=[r_xt])
            norm_to_bf16(xt[:], r_xt, gmem_b, r_gmem, h2[:], r_h2, 0)
            transpose8(h2, r_h2, None, r_mnT, dst_sl=mnT[:, :, mt * 128:(mt + 1) * 128])
        for mt in range(2):
            msl = slice(mt * 128, (mt + 1) * 128)
            for half in range(2):
                for k in range(8):
                    mm(banks[half][:], mnT[:, k, msl], w_mk[:, k, half * 512:(half + 1) * 512], k == 0, k == 7,
                       [r_mnT, r_wmk], [rb[half]])
                for k in range(8):
                    mm(banks[2 + half][:], mnT[:, k, msl], w_mv[:, k, half * 512:(half + 1) * 512], k == 0, k == 7,
                       [r_mnT, r_wmv], [rb[2 + half]])
                act(Vm[:, mt, half * 2:half * 2 + 2, :], banks[2 + half][:].rearrange("p (h d) -> p h d", h=2),
                    AF.Copy, [rb[2 + half]], [r_Vm])
            head_norm(0, gmk_b, r_gmk, qmb[:], r_qmb)
            transpose8(qmb, r_qmb, None, r_KmT, dst_sl=KmT[:, :, msl])
        S.barrier()
        Cn.cur = mark

        oa = Sel([Cn.alloc([128, 512], BF16, "oa") for _ in range(NSETS)]); r_oa = RSel(NSETS)
        ornT = Sel([Cn.alloc([128, 4, 128], BF16, "ornT") for _ in range(NSETS)]); r_ornT = RSel(NSETS)
        mixA = Sel([Cn.alloc([128, 512], BF16, "mixA") for _ in range(NSETS)]); r_mixA = RSel(NSETS)
        mixAT = Sel([Cn.alloc([128, 4, 128], BF16, "mixAT") for _ in range(NSETS)]); r_mixAT = RSel(NSETS)
        x1 = Sel([Cn.alloc([128, 1024], F32, "x1") for _ in range(NSETS)]); r_x1 = RSel(NSETS)
        qmT = Sel([Cn.alloc([128, 8, 128], BF16, "qmT") for _ in range(NSETS)]); r_qmT = RSel(NSETS)
        pm = Sel([Cn.alloc([128, 8, 128], BF16, "pm") for _ in range(NSETS)]); r_pm = RSel(NSETS)
        recm = Sel([Cn.alloc([128, 4], F32, "recm") for _ in range(NSETS)]); r_recm = RSel(NSETS)
        omb = h2; r_omb = r_h2
        omT = h2T; r_omT = r_h2T
        h3f = tmpf; r_h3f = r_tmpf
        h3 = qmb; r_h3 = r_qmb
        h3fT = Sel([Cn.alloc([128, 8, 128], F32, "h3fT") for _ in range(NSETS)]); r_h3fT = RSel(NSETS)
        lg = Sel([Cn.alloc([128, 36], F32, "lg") for _ in range(NSETS)]); r_lg = RSel(NSETS)
        rt = Sel([Cn.alloc([128, 64], F32, "rt") for _ in range(NSETS)]); r_rt = RSel(NSETS)
        I32 = mybir.dt.int32
        T_SL = 256
        NTILE = (2 * S_len) // T_SL + NE
        WGUv = WGU2.rearrange("e p k n -> (e p) (k n)")
        WDv = WD2.rearrange("e p c n -> (e p) (c n)")
        x2 = xt; r_x2 = r_xt
        rank_all = Cn.alloc([128, NT, 32], F32, "rank_all"); r_rank = Res()
        selA = Cn.alloc([128, NT, 32], F32, "selA"); selB = Cn.alloc([128, NT, 32], F32, "selB"); r_sel = Res()
        carryc = Cn.alloc([128, 32], F32, "carryc"); r_carryc = Res()
        memset("pool", carryc[:], 0.0, [r_carryc])
        Mf = Sel([Cn.alloc([128, 32], F32, "Mf") for _ in range(2)])
        Mb = Sel([Cn.alloc([128, 32], BF16, "Mb") for _ in range(NSETS)]); r_M = RSel(NSETS)
        Lst = Cn.alloc([128, 128], BF16, "Lst"); r_Lst = Res()
        dma("sp", Lst[:], lst_d, writes=[r_Lst])
        ones128 = Cn.alloc([128, 128], BF16, "ones128")
        memset("pool", ones128[:], 1.0, [r_Lst])
        r_H3 = [Res() for _ in range(NT)]
        r_out = [Res() for _ in range(NT)]
        print("phase C SBUF used", Cn.cur, "of", SB_HI)
        N_SKEW = int(os.environ.get('N_SKEW', '12'))

        def tile_body(gt):
            if True:
                t8 = 0
                rows = slice(gt * 128, (gt + 1) * 128)
                dma("sp", xt[:], x_d[rows, :], writes=[r_xt])
                yield
                dma("sp", oa[:], OA[rows, :], [r_OA], [r_oa])
                yield
                dma("sp", ornT[:], ORT[:, :, rows].rearrange("c p s -> p c s"), [r_ORT], [r_ornT])
                yield
                act(junk[:, 0:512], oa[:], AF.Square, [r_oa], [r_junk, r_ssv], accum=ssv[:, 0:1])
                yield
                rstd_chain(ssv[:, 0:1], 1, 1.0 / 512, [r_ssv])
                yield
                tt("dve", mixA[:], oa[:], ga_b[:], ALU.mult, [r_oa, r_ga], [r_mixA])
                yield
                transpose8(mixA, r_mixA, mixAT[:], r_mixAT, n=4)
                yield
                for half in range(2):
                    hsl = slice(half * 512, (half + 1) * 512)
                    for k in range(4):
                        mm(banks[half][:], mixAT[:, k, :], w_out[:, k, hsl], k == 0, k == 3, [r_mixAT, r_wout], [rb[half]])
                    for k in range(4):
                        mm(banks[2 + half][:], ornT[:, k, :], w_out[:, 4 + k, hsl], k == 0, k == 3,
                           [r_ornT, r_wout], [rb[2 + half]])
                    stt("dve", x1[:, hsl], banks[half][:], ssv[:, 0:1], xt[:, hsl], ALU.mult, ALU.add,
                        [rb[half], r_ssv, r_xt], [r_x1])
                    stt("dve", x1[:, hsl], banks[2 + half][:], rr_all[:, gt:gt + 1], x1[:, hsl], ALU.mult, ALU.add,
                        [rb[2 + half], r_rr, r_x1], [r_x1])
                yield
                norm_to_bf16(x1[:], r_x1, gxq_b, r_gxq, h2[:], r_h2, 1)
                yield
                transpose8(h2, r_h2, h2T[:], r_h2T)
                yield
                for half in range(2):
                    for k in range(8):
                        mm(banks[4 + half][:], h2T[:, k, :], w_mq[:, k, half * 512:(half + 1) * 512], k == 0, k == 7,
                           [r_h2T, r_wmq], [rb[4 + half]])
                head_norm(4, gmq_b, r_gmq, qmb[:], r_qmb)
                yield
                transpose8(qmb, r_qmb, qmT[:], r_qmT)
                yield
                for hh in range(4):
                    for mt in range(2):
                        slot = hh * 2 + mt
                        for kk in range(2):
                            mm(banks[slot // 4][:, (slot % 4) * 128:(slot % 4 + 1) * 128],
                               KmT[:, hh * 2 + kk, mt * 128:(mt + 1) * 128], qmT[:, hh * 2 + kk, :], kk == 0, kk == 1,
                               [r_KmT, r_qmT], [rb[slot // 4]])
                for bk in range(2):
                    act(pm[:, bk * 4:(bk + 1) * 4, :], banks[bk][:].rearrange("p (s n) -> p s n", s=4), AF.Exp,
                        [rb[bk]], [r_pm])
                yield
                for hh in range(4):
                    ob = 2 + hh // 2
                    for mt in range(2):
                        mm(banks[ob][:, (hh % 2) * 256:(hh % 2 + 1) * 256], pm[:, hh * 2 + mt, :], Vm[:, mt, hh, :],
                           mt == 0, mt == 1, [r_pm, r_Vm], [rb[ob]])
                    for mt in range(2):
                        mm(banks[6][:, hh:hh + 1], pm[:, hh * 2 + mt, :], ones[:, 0:1], mt == 0, mt == 1,
                           [r_pm, r_ones], [rb[6]])
                recip(recm[:], banks[6][:, 0:4], [rb[6]], [r_recm])
                for bk in range(2):
                    tt("dve", omb[:, bk * 512:(bk + 1) * 512].rearrange("p (h d) -> p h d", h=2),
                       banks[2 + bk][:].rearrange("p (h d) -> p h d", h=2),
                       recm[:, bk * 2:bk * 2 + 2].unsqueeze(2).to_broadcast([128, 2, 256]), ALU.mult,
                       [rb[2 + bk], r_recm], [r_omb])
                yield
                transpose8(omb, r_omb, omT[:], r_omT)
                yield
                for half in range(2):
                    hsl = slice(half * 512, (half + 1) * 512)
                    for k in range(8):
                        mm(banks[4 + half][:], omT[:, k, :], w_mo[:, k, hsl], k == 0, k == 7, [r_omT, r_wmo], [rb[4 + half]])
                    tt("dve", x2[:, hsl], banks[4 + half][:], x1[:, hsl], ALU.add, [rb[4 + half], r_x1], [r_x2])
                yield
                dma("sp", out_d[rows, :], x2[:], [r_x2], [r_out[gt]])
                yield
                act(junk[:], x2[:], AF.Square, [r_x2], [r_junk, r_ssv], accum=ssv[:, 2:3])
                yield
                rstd_chain(ssv[:, 2:3], 1, 1.0 / D, [r_ssv])
                yield
                stt("dve", h3f[:], x2[:], ssv[:, 2:3], gffn_b[:], ALU.mult, ALU.mult, [r_x2, r_ssv, r_gffn], [r_h3f])
                yield
                cp("act", h3[:], h3f[:], [r_h3f], [r_h3])
                yield
                dma("sp", H3[rows, :], h3[:], [r_h3], [r_H3[gt]])
                yield
                for k in range(8):
                    transpose(banks[k // 4][:, (k % 4) * 128:(k % 4 + 1) * 128], h3f[:, k * 128:(k + 1) * 128],
                              [r_h3f], [rb[k // 4]], f32=True)
                for bk in range(2):
                    cp("act", h3fT[:, bk * 4:(bk + 1) * 4, :], banks[bk][:].rearrange("p (s n) -> p s n", s=4),
                       [rb[bk]], [r_h3fT])
                yield
                for k in range(8):
                    mm(banks[6][:, 64:100], h3fT[:, k, :], wr[:, k, :], k == 0, k == 7, [r_h3fT, r_wr], [rb[6]])
                tt("dve", lg[:], banks[6][:, 64:100], br_b[:], ALU.add, [rb[6], r_br], [r_lg])
                R = [r_lg, r_rt]
                gmax, ngmax, sumg, oh = rt[:, 0:1], rt[:, 1:2], rt[:, 2:3], rt[:, 4:8]
                eg, es, emax, nemax = rt[:, 8:12], rt[:, 16:24], rt[:, 12:13], rt[:, 13:14]
                ex, top8, den, msk = rt[:, 24:32], rt[:, 32:40], rt[:, 14:15], rt[:, 40:48]
                sel32 = tmpf[:, 0:32]
                yield
                red(gmax, lg[:, 0:4], ALU.max, R, [r_rt])
                yield
                ts("dve", oh, lg[:, 0:4], gmax, None, ALU.is_ge, None, R, [r_rt])
                yield
                ts("dve", ngmax, gmax, -1.0, None, ALU.mult, None, R, [r_rt])
                yield
                act(eg, lg[:, 0:4], AF.Exp, R, [r_rt], bias=ngmax, accum=sumg)
                yield
                recip(sumg, sumg, R, [r_rt])
                yield
                tt("dve", sel32.rearrange("p (g e) -> p g e", g=4), lg[:, 4:36].rearrange("p (g e) -> p g e", g=4),
                   oh.unsqueeze(2).to_broadcast([128, 4, 8]), ALU.mult, R, [r_tmpf])
                yield
                red(es, sel32.rearrange("p (g e) -> p e g", g=4), ALU.add, [r_tmpf], [r_rt])
                yield
                red(emax, es, ALU.max, R, [r_rt])
                yield
                ts("dve", nemax, emax, -1.0, None, ALU.mult, None, R, [r_rt])
                yield
                act(ex, es, AF.Exp, R, [r_rt], bias=nemax)
                yield
                S.op("dve", lambda e, top8=top8, ex=ex: e.max(out=top8, in_=ex), R, [r_rt])
                yield
                tt("dve", den, top8[:, 0:1], top8[:, 1:2], ALU.add, R, [r_rt])
                yield
                recip(den, den, R, [r_rt])
                yield
                tt("dve", den, den, sumg, ALU.mult, R, [r_rt])
                mskA, msk2 = rt[:, 48:56], rt[:, 40:48]
                yield
                ts("dve", mskA, ex, top8[:, 0:1], None, ALU.is_ge, None, R, [r_rt])
                yield
                ts("dve", msk2, ex, top8[:, 1:2], None, ALU.is_ge, None, R, [r_rt])
                ohb = oh.unsqueeze(2).to_broadcast([128, 4, 8])
                yield
                tt("dve", selA[:, gt, :].rearrange("p (g e) -> p g e", g=4), ohb,
                   mskA.unsqueeze(1).to_broadcast([128, 4, 8]), ALU.mult, R, [r_sel])
                yield
                tt("dve", Mf[:].rearrange("p (g e) -> p g e", g=4), ohb,
                   msk2.unsqueeze(1).to_broadcast([128, 4, 8]), ALU.mult, R, [r_M])
                yield
                tt("dve", selB[:, gt, :], Mf[:], selA[:, gt, :], ALU.subtract, [r_M, r_sel], [r_sel])
                yield
                ts("dve", gAB[:, gt, :], top8[:, 0:2], den, None, ALU.mult, None, R, [r_gAB])
                yield
                cp("dve", Mb[:], Mf[:], [r_M], [r_M])
                yield
                mm(banks[6][:, 128:160], Lst[:], Mb[:], True, True, [r_Lst, r_M], [rb[6]])
                mm(banks[6][:, 160:192], ones128[:], Mb[:], True, True, [r_Lst, r_M], [rb[6]])
                tt("dve", rank_all[:, gt, :], banks[6][:, 128:160], carryc[:], ALU.add, [rb[6], r_carryc], [r_rank])
                tt("dve", carryc[:], banks[6][:, 160:192], carryc[:], ALU.add, [rb[6], r_carryc], [r_carryc])

        FILL = int(os.environ.get("FILL", "0"))
        fcount = [0]

        def wrap(gt):
            g = tile_body(gt)
            while True:
                set_parity(gt)
                try:
                    next(g)
                except StopIteration:
                    return
                fcount[0] += 1
                if FILL and fcount[0] % FILL == 0:
                    S.op("pe", lambda e: e.matmul(banks[6][:, 192:512], lhsT=idb[:], rhs=w_out[:, 0, 0:320],
                                                  start=True, stop=True, skip_group_check=True), [], [])
                yield

        interleave((wrap(gt) for gt in range(NT)), NSETS, admit_every=N_SKEW)
        set_parity(0)

        S.barrier()
        top_c = Cn.cur
        Cn.cur = mark2
        thr_b, r_thr = bcast(Cn, "thr_b", thr_d, 32)
        iota_b, r_iota = bcast(Cn, "iota_b", iota_d, NTILE)
        pidx, r_pidx = colvec(Cn, "pidx", pidx_d, 1)
        onesf = Cn.alloc([128, 32], F32, "onesf"); r_onesf = Res()
        memset("pool", onesf[:], 1.0, [r_onesf])
        ntile = Cn.alloc([128, 32], F32, "ntile"); endc = Cn.alloc([128, 32], F32, "endc")
        startT = Cn.alloc([128, 32], F32, "startT"); r_bk = Res()
        cmpi = Cn.alloc([128, NTILE, 32], F32, "cmpi"); r_cmpi = Res()
        tef = Cn.alloc([128, NTILE], F32, "tef"); r_tef = Res()
        posf = Cn.alloc([128, 2, NT], F32, "posf"); r_posf = Res()
        cmp3t = Cn.alloc([128, 1024], F32, "cmp3t"); r_tmpf = Res()
        cmp3 = cmp3t[:].rearrange("p (e m) -> p e m", e=32)
        assert Cn.cur <= top_c
        tt("dve", cmp3, carryc[:].unsqueeze(2).to_broadcast([128, 32, 32]),
           thr_b[:].unsqueeze(1).to_broadcast([128, 32, 32]), ALU.is_gt, [r_carryc, r_thr], [r_tmpf])
        S.op("dve", lambda e: e.tensor_reduce(out=ntile[:], in_=cmp3, axis=AX.X, op=ALU.add), [r_tmpf], [r_bk])
        S.op("dve", lambda e: e.tensor_tensor_scan(out=endc[:], data0=onesf[:], data1=ntile[:], initial=0.0,
                                                   op0=ALU.mult, op1=ALU.add), [r_bk, r_onesf], [r_bk])
        tt("dve", startT[:], endc[:], ntile[:], ALU.subtract, [r_bk], [r_bk])
        ts("dve", startT[:], startT[:], float(T_SL), None, ALU.mult, None, [r_bk], [r_bk])
        tt("dve", cmpi[:], iota_b[:].unsqueeze(2).to_broadcast([128, NTILE, 32]),
           endc[:].unsqueeze(1).to_broadcast([128, NTILE, 32]), ALU.is_ge, [r_iota, r_bk], [r_cmpi])
        S.op("dve", lambda e: e.tensor_reduce(out=tef[:], in_=cmpi[:], axis=AX.X, op=ALU.add), [r_cmpi], [r_tef])
        ts("dve", tef[:], tef[:], float(NE - 1), None, ALU.min, None, [r_tef], [r_tef])
        ts("dve", tef[:], tef[:], 128.0, pidx[:, 0:1], ALU.mult, ALU.add, [r_tef, r_pidx], [r_tef])
        cp("dve", widx[:], tef[:], [r_tef], [r_widx])
        tt("dve", rank_all[:], rank_all[:], startT[:].unsqueeze(1).to_broadcast([128, NT, 32]), ALU.add,
           [r_rank, r_bk], [r_rank])
        tt("dve", selA[:], selA[:], rank_all[:], ALU.mult, [r_sel, r_rank], [r_sel])
        tt("dve", selB[:], selB[:], rank_all[:], ALU.mult, [r_sel, r_rank], [r_sel])
        S.op("dve", lambda e: e.tensor_reduce(out=posf[:, 0, :], in_=selA[:], axis=AX.X, op=ALU.add), [r_sel], [r_posf])
        S.op("dve", lambda e: e.tensor_reduce(out=posf[:, 1, :], in_=selB[:], axis=AX.X, op=ALU.add), [r_sel], [r_posf])
        cp("dve", pos_i[:], posf[:], [r_posf], [r_pos])
        if dbg:
            dma("sp", DBG_widx, widx[:], [r_widx], [])
            dma("sp", DBG_pos, pos_i[:], [r_pos], [])
            dma("sp", DBG_gab, gAB[:], [r_gAB], [])
        S.barrier()
        Cn.cur = mark2
        if moe_stop == "C":
            return [dma("sp", out_d[0:128, :], x_d[0:128, :])]

        hsb = [Cn.alloc([128, 1024], BF16, "hsb") for _ in range(2)]; r_hsb = [Res() for _ in range(2)]
        for gt in range(NT):
            q = gt % 2
            dma("sp", hsb[q][:], H3[gt * 128:(gt + 1) * 128, :], [r_H3[gt]], [r_hsb[q]])
            for j in range(2):
                S.op("pool", lambda e, q=q, gt=gt, j=j: e.indirect_dma_start(
                    out=Hs[:, :], out_offset=bass.IndirectOffsetOnAxis(ap=pos_i[:, j, gt:gt + 1], axis=0),
                    in_=hsb[q][:, :], in_offset=None), [r_hsb[q], r_pos], [Res()], dma=True)
        S.barrier()
        if moe_stop == "S":
            return [dma("sp", out_d[0:128, :], x_d[0:128, :])]

        Wgu2 = [Cn.alloc([128, 4096], BF16, "Wgu2") for _ in range(2)]; r_Wgu2 = [Res() for _ in range(2)]
        Wd2 = [Cn.alloc([128, 2048], BF16, "Wd2") for _ in range(2)]; r_Wd2 = [Res() for _ in range(2)]
        hst = [Cn.alloc([128, 1024], BF16, "hst") for _ in range(2)]; r_hst = [Res() for _ in range(2)]
        hTs = [Cn.alloc([128, 8, 128], BF16, "hTs") for _ in range(2)]; r_hTs = [Res() for _ in range(2)]
        sgt = [Cn.alloc([128, 256], F32, "sgt") for _ in range(2)]; r_sgt = [Res() for _ in range(2)]
        het = [Cn.alloc([128, 256], BF16, "het") for _ in range(2)]; r_het = [Res() for _ in range(2)]
        heT = [Cn.alloc([128, 2, 128], BF16, "heT") for _ in range(2)]; r_heT = [Res() for _ in range(2)]
        yst = [Cn.alloc([128, 1024], BF16, "yst") for _ in range(2)]; r_yst = [Res() for _ in range(2)]
        NWB = 3
        NSET = 4
        for lst_, shape, dt_, nm in ((Wgu2, [128, 4096], BF16, "Wgu2"), (Wd2, [128, 2048], BF16, "Wd2")):
            while len(lst_) < NWB:
                lst_.append(Cn.alloc(shape, dt_, nm))
        r_Wgu2 = [Res() for _ in range(NWB)]; r_Wd2 = [Res() for _ in range(NWB)]
        for lst_, shape, dt_, nm in ((hst, [128, 1024], BF16, "hst"), (hTs, [128, 8, 128], BF16, "hTs"),
                                     (sgt, [128, 256], F32, "sgt"), (het, [128, 256], BF16, "het"),
                                     (heT, [128, 2, 128], BF16, "heT"), (yst, [128, 1024], BF16, "yst")):
            while len(lst_) < NSET:
                lst_.append(Cn.alloc(shape, dt_, nm))
        r_hst = [Res() for _ in range(NSET)]; r_hTs = [Res() for _ in range(NSET)]; r_sgt = [Res() for _ in range(NSET)]
        r_het = [Res() for _ in range(NSET)]; r_heT = [Res() for _ in range(NSET)]; r_yst = [Res() for _ in range(NSET)]

        def sub_gen(i, sub, q):
            p = i % NWB
            if sub == 0:
                S.op("pool", lambda e, p=p, i=i: e.indirect_dma_start(
                    out=Wgu2[p][:, :], out_offset=None, in_=WGUv,
                    in_offset=bass.IndirectOffsetOnAxis(ap=widx[:, i:i + 1], axis=0)), [r_widx], [r_Wgu2[p]], dma=True)
                S.op("pool", lambda e, p=p, i=i: e.indirect_dma_start(
                    out=Wd2[p][:, :], out_offset=None, in_=WDv,
                    in_offset=bass.IndirectOffsetOnAxis(ap=widx[:, i:i + 1], axis=0)), [r_widx], [r_Wd2[p]], dma=True)
            r0 = i * T_SL + sub * 128
            dma("sp", hst[q][:], Hs[r0:r0 + 128, :], [], [r_hst[q]])
            yield
            transpose8(hst[q], r_hst[q], hTs[q][:], r_hTs[q])
            yield
            gb = q
            for k in range(8):
                mm(banks[gb][:], hTs[q][:, k, :], Wgu2[p][:, k * 512:(k + 1) * 512], k == 0, k == 7,
                   [r_hTs[q], r_Wgu2[p]], [rb[gb]])
            yield
            act(sgt[q][:], banks[gb][:, 0:256], AF.Silu, [rb[gb]], [r_sgt[q]])
            yield
            tt("dve", het[q][:], sgt[q][:], banks[gb][:, 256:512], ALU.mult, [r_sgt[q], rb[gb]], [r_het[q]])
            yield
            transpose8(het[q], r_het[q], heT[q][:], r_heT[q], n=2)
            yield
            yb = (4, 5)
            for half in range(2):
                for c in range(2):
                    mm(banks[yb[half]][:], heT[q][:, c, :], Wd2[p][:, c * 1024 + half * 512:c * 1024 + (half + 1) * 512],
                       c == 0, c == 1, [r_heT[q], r_Wd2[p]], [rb[yb[half]]])
            cp("act", yst[q][:, 0:512], banks[yb[0]][:], [rb[yb[0]]], [r_yst[q]])
            cp("dve", yst[q][:, 512:1024], banks[yb[1]][:], [rb[yb[1]]], [r_yst[q]])
            yield
            dma("sp", Ys[r0:r0 + 128, :], yst[q][:], [r_yst[q]], [Res()])

        def all_subs():
            cnt = 0
            for i in range(NTILE):
                for sub in range(T_SL // 128):
                    yield sub_gen(i, sub, cnt % NSET)
                    cnt += 1

        interleave(all_subs(), NSET)
        S.barrier()
        if moe_stop == "E":
            return [dma("sp", out_d[0:128, :], x_d[0:128, :])]

        xo = [Cn.alloc([128, 1024], F32, "xo") for _ in range(2)]; r_xo = [Res() for _ in range(2)]
        yA = [Cn.alloc([128, 1024], BF16, "yA") for _ in range(2)]; r_yA = [Res() for _ in range(2)]
        yB = [Cn.alloc([128, 1024], BF16, "yB") for _ in range(2)]; r_yB = [Res() for _ in range(2)]
        print("phase E/F SBUF used", Cn.cur, "of", SB_HI)
        outs = []
        for gt in range(NT):
            q = gt % 2
            rows = slice(gt * 128, (gt + 1) * 128)
            dma("sp", xo[q][:], out_d[rows, :], [r_out[gt]], [r_xo[q]])
            for j, (yy, r_yy) in enumerate(((yA, r_yA), (yB, r_yB))):
                S.op("pool", lambda e, q=q, gt=gt, j=j, yy=yy: e.indirect_dma_start(
                    out=yy[q][:, :], out_offset=None, in_=Ys[:, :],
                    in_offset=bass.IndirectOffsetOnAxis(ap=pos_i[:, j, gt:gt + 1], axis=0)), [r_pos], [r_yy[q]], dma=True)
            stt("dve", xo[q][:], yA[q][:], gAB[:, gt, 0:1], xo[q][:], ALU.mult, ALU.add, [r_yA[q], r_gAB, r_xo[q]], [r_xo[q]])
            stt("dve", xo[q][:], yB[q][:], gAB[:, gt, 1:2], xo[q][:], ALU.mult, ALU.add, [r_yB[q], r_gAB, r_xo[q]], [r_xo[q]])
            outs.append(dma("sp", out_d[rows, :], xo[q][:], [r_xo[q]], [r_out[gt]]))
        return outs

    if "A" in phases:
        phaseA()
    S.barrier()
    if "B" in phases:
        phaseB()
    S.barrier()
    S.barrier()
    outs = []
    if "D" in phases:
        outs = phaseCD()
    else:
        outs = [dma("sp", out_d[0:128, :], x_d[0:128, :])]
    return nc, S, outs


def host_consts(S_len):
    pos = np.arange(S_len, dtype=np.float32)
    inv_freq = (np.float32(10000.0) ** (-np.arange(0, 32, 2, dtype=np.float32) / np.float32(32))).astype(np.float32)
    ang = (pos[:, None] * inv_freq[None, :]).astype(np.float32)
    c, s = np.cos(ang).astype(np.float32), np.sin(ang).astype(np.float32)
    cs = np.concatenate([c, c, -s, s], axis=1).astype(np.float32)
    ntile = (2 * S_len) // 256 + NE
    lst = np.triu(np.ones((128, 128), np.float32), 1).astype(ml_dtypes.bfloat16)
    return {"cs_tab": cs, "ident_bf": np.eye(128).astype(ml_dtypes.bfloat16), "ident_f32": np.eye(128, dtype=np.float32),
            "lstrict": lst, "thr_tab": (np.arange(32) * 256).astype(np.float32),
            "iota_tab": np.arange(ntile).astype(np.float32), "pidx_tab": np.arange(128).astype(np.float32)}


_CACHE = {}


def kernel(**inputs):
    x = np.asarray(inputs["x"], dtype=np.float32)
    B, S_len, _ = x.shape
    if S_len not in _CACHE:
        nc, S, outs = build(S_len)
        S.emit(final_waits=outs)
        _CACHE[S_len] = nc
    nc = _CACHE[S_len]
    consts = host_consts(S_len)
    wts = {n: np.ascontiguousarray(np.asarray(inputs[n], dtype=np.float32)[0]) for n in WEIGHT_NAMES}
    mem = np.asarray(inputs["mem"], dtype=np.float32)
    in_maps = []
    for b in range(B):
        m = {"x": np.ascontiguousarray(x[b]), "mem": np.ascontiguousarray(mem[b])}
        m.update(wts)
        m.update(consts)
        in_maps.append(m)
    res = run_bass_kernel_spmd(nc, in_maps, core_ids=list(range(B)))
    return np.stack([np.asarray(r["out"], dtype=np.float32) for r in res.results], axis=0)
```

```python
import os
import numpy as np
import ml_dtypes
import concourse.bass as bass
import concourse.mybir as mybir
from concourse.bass_utils import run_bass_kernel_spmd

F32 = mybir.dt.float32
BF16 = mybir.dt.bfloat16
AF = mybir.ActivationFunctionType
ALU = mybir.AluOpType
AX = mybir.AxisListType

ENGS = ("pe", "act", "dve", "pool", "sp")
EPS = 1e-6
D = 1024
NH = 8
DQK = 96
NE = 32
DE = 256
SB_LO = 16640
SB_HI = 228864


class Res:
    __slots__ = ("name", "w", "r")

    def __init__(self, name=""):
        self.name = name
        self.w = None
        self.r = []


class Op:
    __slots__ = ("eng", "fn", "deps", "isdma", "sem", "val", "needs_inc")

    def __init__(self, eng, fn, isdma):
        self.eng = eng
        self.fn = fn
        self.deps = []
        self.isdma = isdma
        self.sem = None
        self.val = None
        self.needs_inc = False


class Sched:
    def __init__(self, nc):
        self.nc = nc
        self.ops = {e: [] for e in ENGS}
        self.nd = {"sp": 24, "pool": 12, "act": 4}
        self.dma_rr = {e: 0 for e in self.nd}
        self.dma_last = {e: [None] * n for e, n in self.nd.items()}
        self.dma_cnt = {e: [0] * n for e, n in self.nd.items()}
        self.last = {e: None for e in ENGS}

    def op(self, eng, fn, reads=(), writes=(), dma=False, extra=()):
        o = Op(eng, fn, dma)
        deps = list(extra)
        for r in reads:
            if r.w is not None:
                deps.append(r.w)
        for w in writes:
            if w.w is not None:
                deps.append(w.w)
            deps.extend(w.r)
        if dma:
            slot = self.dma_rr[eng]
            self.dma_rr[eng] = (slot + 1) % self.nd[eng]
            prev = self.dma_last[eng][slot]
            if prev is not None:
                deps.append(prev)
            self.dma_last[eng][slot] = o
            self.dma_cnt[eng][slot] += 1
            o.sem = ("dma", eng, slot)
            o.val = 16 * self.dma_cnt[eng][slot]
        seen = set()
        for d in deps:
            if d is None or d is o or id(d) in seen:
                continue
            seen.add(id(d))
            if d.eng == "pe" and eng == "pe" and not d.isdma and not dma:
                continue
            o.deps.append(d)
            if not d.isdma:
                d.needs_inc = True
        for r in reads:
            if not dma:
                r.r = [x for x in r.r if x.isdma or x.eng != eng]
            r.r.append(o)
        for w in writes:
            w.w = o
            w.r = []
        self.ops[eng].append(o)
        if not dma:
            self.last[eng] = o
        return o

    def barrier(self):
        deps = [self.last[e] for e in ENGS if self.last[e] is not None]
        for e in self.nd:
            deps.extend(x for x in self.dma_last[e] if x is not None)
        for e in ENGS:
            self.op(e, lambda eng: eng.nop(), extra=deps)

    def emit(self, final_waits=()):
        nc = self.nc
        esem = {e: nc.alloc_semaphore(f"s_{e}") for e in ENGS}
        dsem = {e: [nc.alloc_semaphore(f"d_{e}{i}") for i in range(n)] for e, n in self.nd.items()}
        for e in ENGS:
            c = 0
            for o in self.ops[e]:
                if o.isdma:
                    o.sem = dsem[o.sem[1]][o.sem[2]]
                elif o.needs_inc:
                    c += 1
                    o.sem = esem[e]
                    o.val = c
        emap = {"pe": "tensor", "act": "scalar", "dve": "vector", "pool": "gpsimd", "sp": "sync"}

        def run(e, engobj):
            known = {}
            for o in self.ops[e]:
                need = {}
                for d in o.deps:
                    k = d.sem.num
                    if k not in need or need[k][1] < d.val:
                        need[k] = (d.sem, d.val)
                for k, (s, v) in need.items():
                    if known.get(k, 0) >= v:
                        continue
                    engobj.wait_ge(s, v)
                    known[k] = v
                ins = o.fn(engobj)
                if o.isdma:
                    ins.then_inc(o.sem, 16)
                elif o.needs_inc:
                    ins.then_inc(o.sem, 1)
            if e == "sp":
                for d in final_waits:
                    engobj.wait_ge(d.sem, d.val)

        with nc.Block() as block:
            for e in ENGS:
                getattr(block, emap[e])(lambda engobj, e=e: run(e, engobj))


class Sel:
    REG = []

    def __init__(self, bufs):
        self.bufs, self.i = bufs, 0
        Sel.REG.append(self)

    def __getitem__(self, k):
        return self.bufs[self.i][k]


class RSel:
    def __init__(self, n):
        self.rs, self.i = [Res() for _ in range(n)], 0
        Sel.REG.append(self)

    @property
    def w(self):
        return self.rs[self.i].w

    @w.setter
    def w(self, v):
        self.rs[self.i].w = v

    @property
    def r(self):
        return self.rs[self.i].r

    @r.setter
    def r(self, v):
        self.rs[self.i].r = v


def set_parity(p):
    if os.environ.get("NO_PAR"):
        p = 0
    for x in Sel.REG:
        x.i = p % len(x.bufs if isinstance(x, Sel) else x.rs)


def interleave(gen_iter, ways, admit_every=0):
    active = []
    gen_iter = iter(gen_iter)
    done = False
    rnd = 0
    last_admit = -10 ** 9
    while active or not done:
        while len(active) < ways and not done and (not active or rnd - last_admit >= admit_every):
            try:
                active.append(next(gen_iter))
                last_admit = rnd
            except StopIteration:
                done = True
        rnd += 1
        for g in list(active):
            try:
                next(g)
            except StopIteration:
                active.remove(g)


class Arena:
    def __init__(self, nc, lo, hi):
        self.nc, self.lo, self.hi, self.cur, self.n = nc, lo, hi, lo, 0

    def alloc(self, shape, dtype, name=None):
        nbytes = int(np.prod(shape[1:])) * (2 if dtype == BF16 else 4)
        off = (self.cur + 31) // 32 * 32
        assert off + nbytes <= self.hi, f"SBUF arena overflow {off + nbytes} > {self.hi} ({name})"
        self.cur = off + nbytes
        self.n += 1
        return self.nc.alloc_sbuf_tensor_at(f"{name or 't'}_{off}_{self.n}", list(shape), dtype, offset=off)


WEIGHT_NAMES = ["g_mix", "w_in", "g_cq", "w_uq", "g_ckv", "w_ukv", "g_qn", "g_kn", "conv_w", "conv_b",
                "w_rg", "b_rg", "w_ig", "b_ig", "lam", "g_attn_out", "g_rnn_out", "w_out", "g_xq", "g_mem",
                "w_mq", "w_mk", "w_mv", "g_mqn", "g_mkn", "w_mo", "g_ffn", "w_group", "b_group", "w_expert",
                "b_expert", "w_e_gate", "w_e_up", "w_e_down"]
WEIGHT_SHAPES = {
    "g_mix": [D], "w_in": [D, 1440], "g_cq": [256], "w_uq": [256, 768], "g_ckv": [128], "w_ukv": [128, 1024],
    "g_qn": [96], "g_kn": [96], "conv_w": [4, 512], "conv_b": [512], "w_rg": [8, 64, 64], "b_rg": [512],
    "w_ig": [8, 64, 64], "b_ig": [512], "lam": [512], "g_attn_out": [512], "g_rnn_out": [512], "w_out": [D, D],
    "g_xq": [D], "g_mem": [D], "w_mq": [D, D], "w_mk": [D, D], "w_mv": [D, D], "g_mqn": [256], "g_mkn": [256],
    "w_mo": [D, D], "g_ffn": [D], "w_group": [D, 4], "b_group": [4], "w_expert": [D, 32], "b_expert": [32],
    "w_e_gate": [NE, D, DE], "w_e_up": [NE, D, DE], "w_e_down": [NE, DE, D]}


def build(S_len, phases="ABCD", dbg=False, moe_stop="F"):
    NT = S_len // 128
    NG = S_len // 512
    nc = bass.Bass("TRN2", target_bir_lowering=False)
    x_d = nc.dram_tensor("x", [S_len, D], F32, kind="ExternalInput").ap()
    mem_d = nc.dram_tensor("mem", [256, D], F32, kind="ExternalInput").ap()
    W = {n: nc.dram_tensor(n, WEIGHT_SHAPES[n], F32, kind="ExternalInput").ap() for n in WEIGHT_NAMES}
    cs_d = nc.dram_tensor("cs_tab", [S_len, 64], F32, kind="ExternalInput").ap()
    idb_d = nc.dram_tensor("ident_bf", [128, 128], BF16, kind="ExternalInput").ap()
    idf_d = nc.dram_tensor("ident_f32", [128, 128], F32, kind="ExternalInput").ap()
    out_d = nc.dram_tensor("out", [S_len, D], F32, kind="ExternalOutput").ap()
    skind = "ExternalOutput" if dbg else "Internal"
    QT = nc.dram_tensor("QT", [NH, DQK, S_len], BF16, kind=skind).ap()
    KT = nc.dram_tensor("KT", [NH, DQK, S_len], BF16, kind=skind).ap()
    VA = nc.dram_tensor("VA", [NH, 128, NT, 65], BF16, kind=skind).ap()
    ORT = nc.dram_tensor("ORT", [4, 128, S_len], BF16, kind=skind).ap()
    OA = nc.dram_tensor("OA", [S_len, 512], BF16, kind=skind).ap()
    RR = nc.dram_tensor("RR", [128, NT], F32, kind=skind).ap()
    WGU2 = nc.dram_tensor("WGU2", [NE, 128, 8, 2 * DE], BF16).ap()
    WD2 = nc.dram_tensor("WD2", [NE, 128, 2, D], BF16).ap()
    NSLOT = ((2 * S_len) // 256 + NE) * 256
    H3 = nc.dram_tensor("H3", [S_len, D], BF16).ap()
    Hs = nc.dram_tensor("Hs", [NSLOT, D], BF16).ap()
    Ys = nc.dram_tensor("Ys", [NSLOT, D], BF16).ap()
    lst_d = nc.dram_tensor("lstrict", [128, 128], BF16, kind="ExternalInput").ap()
    thr_d = nc.dram_tensor("thr_tab", [32], F32, kind="ExternalInput").ap()
    iota_d = nc.dram_tensor("iota_tab", [NSLOT // 256], F32, kind="ExternalInput").ap()
    pidx_d = nc.dram_tensor("pidx_tab", [128], F32, kind="ExternalInput").ap()
    if dbg:
        DBG_widx = nc.dram_tensor("DBG_widx", [128, NSLOT // 256], mybir.dt.int32, kind="ExternalOutput").ap()
        DBG_pos = nc.dram_tensor("DBG_pos", [128, 2, NT], mybir.dt.int32, kind="ExternalOutput").ap()
        DBG_gab = nc.dram_tensor("DBG_gab", [128, NT, 2], F32, kind="ExternalOutput").ap()

    S = Sched(nc)
    P = Arena(nc, SB_LO, SB_LO + 6144)
    pairs = [nc.alloc_psum_tensor(f"pair{j}", [128, 1024], F32) for j in range(4)]
    banks = [pairs[i // 2][:, (i % 2) * 512:(i % 2 + 1) * 512] for i in range(8)]
    rb = [Res(f"bank{i}") for i in range(8)]

    def bview(i):
        return pairs[i // 2][:].bitcast(BF16)[:, (i % 2) * 1024:(i % 2 + 1) * 1024]

    def dma(q, out, in_, reads=(), writes=()):
        return S.op(q, lambda e: e.dma_start(out=out, in_=in_), reads=reads, writes=writes, dma=True)

    def dma_nc(q, out, in_, reads=(), writes=()):
        def f(e):
            with nc.allow_non_contiguous_dma(reason="small param vectors"):
                return e.dma_start(out=out, in_=in_)
        return S.op(q, f, reads=reads, writes=writes, dma=True)

    def mm(out, lhsT, rhs, start, stop, reads, writes, skip=False):
        return S.op("pe", lambda e: e.matmul(out, lhsT=lhsT, rhs=rhs, start=start, stop=stop,
                                             skip_group_check=skip), reads, writes)

    def act(out, in_, func, reads, writes, scale=1.0, bias=None, accum=None):
        kw = {}
        if bias is not None:
            kw["bias"] = bias
        if accum is not None:
            kw["accum_out"] = accum
        return S.op("act", lambda e: e.activation(out=out, in_=in_, func=func, scale=scale, **kw), reads, writes)

    def tt(eng, out, in0, in1, op, reads, writes):
        return S.op(eng, lambda e: e.tensor_tensor(out=out, in0=in0, in1=in1, op=op), reads, writes)

    def ts(eng, out, in0, s1, s2, op0, op1, reads, writes):
        if s2 is None:
            return S.op(eng, lambda e: e.tensor_scalar(out=out, in0=in0, scalar1=s1, scalar2=None, op0=op0), reads, writes)
        return S.op(eng, lambda e: e.tensor_scalar(out=out, in0=in0, scalar1=s1, scalar2=s2, op0=op0, op1=op1), reads, writes)

    def stt(eng, out, in0, scalar, in1, op0, op1, reads, writes):
        return S.op(eng, lambda e: e.scalar_tensor_tensor(out=out, in0=in0, scalar=scalar, in1=in1, op0=op0, op1=op1),
                    reads, writes)

    def cp(eng, out, in_, reads, writes):
        if eng == "act":
            return act(out, in_, AF.Copy, reads, writes)
        return S.op(eng, lambda e: e.tensor_copy(out=out, in_=in_), reads, writes)

    def red(out, in_, op, reads, writes):
        return S.op("dve", lambda e: e.tensor_reduce(out=out, in_=in_, axis=AX.X, op=op), reads, writes)

    def recip(out, in_, reads, writes):
        return S.op("dve", lambda e: e.reciprocal(out=out, in_=in_), reads, writes)

    def memset(eng, ap, val, writes):
        return S.op(eng, lambda e: e.memset(ap, val), (), writes)

    idb = P.alloc([128, 128], BF16, "idb"); r_idb = Res()
    idf = P.alloc([128, 128], F32, "idf"); r_idf = Res()
    ones = P.alloc([128, 2], BF16, "ones"); r_ones = Res()
    cneg = P.alloc([128, 1], F32, "cneg"); chalf = P.alloc([128, 1], F32, "chalf"); r_c = Res()
    rr_all = P.alloc([128, max(NT, 8)], F32, "rr_all"); r_rr = Res()
    dma("sp", idb[:], idb_d, writes=[r_idb])
    dma("sp", idf[:], idf_d, writes=[r_idf])
    memset("pool", ones[:], 1.0, [r_ones])
    memset("pool", cneg[:], -0.5, [r_c])
    memset("pool", chalf[:], 0.5, [r_c])
    cone = P.alloc([128, 1], F32, "cone")
    memset("pool", cone[:], 1.0, [r_c])

    def transpose(out, in_, reads, writes, f32=False):
        idt, rid = (idf, r_idf) if f32 else (idb, r_idb)
        return S.op("pe", lambda e: e.transpose(out=out, in_=in_, identity=idt[:]), list(reads) + [rid], writes)

    def rstd_chain(buf, n, inv_dim, reads_writes):
        ts("dve", buf, buf, inv_dim, EPS, ALU.mult, ALU.add, reads_writes, reads_writes)
        act(buf, buf, AF.Ln, reads_writes, reads_writes)
        act(buf, buf, AF.Exp, reads_writes, reads_writes, scale=-0.5)

    def colvec(arena, name, src, ncol):
        t = arena.alloc([128, ncol], F32, name)
        r = Res(name)
        dma_nc("sp", t[:], src.rearrange("(c p) -> p c", p=128), writes=[r])
        return t, r

    def bcast(arena, name, src, n):
        t = arena.alloc([128, n], F32, name)
        r = Res(name)
        dma("sp", t[:], src.partition_broadcast(128), writes=[r])
        return t, r

    r_wgu = [Res() for _ in range(NE)]
    r_wd = [Res() for _ in range(NE)]

    def convert_experts(e0, e1):
        for e in range(e0, min(e1, NE)):
            wg = WGU2[e].rearrange("p k n -> k p n")
            dma("pool", wg[:, :, 0:DE], W["w_e_gate"][e].rearrange("(k p) n -> k p n", p=128), writes=[r_wgu[e]])
            dma("pool", wg[:, :, DE:2 * DE], W["w_e_up"][e].rearrange("(k p) n -> k p n", p=128), writes=[r_wgu[e]])
            dma("pool", WD2[e].rearrange("p c n -> c p n"), W["w_e_down"][e].rearrange("(c p) n -> c p n", p=128),
                writes=[r_wd[e]])

    r_QT, r_KT, r_VA, r_ORT, r_OA = Res(), Res(), Res(), Res(), Res()

    def phaseA():
        A = Arena(nc, SB_LO + 6144, SB_HI)
        w_in = A.alloc([128, 8, 1440], BF16, "w_in"); r_win = Res()
        dma("pool", w_in[:], W["w_in"].rearrange("(k p) n -> p k n", p=128), writes=[r_win])
        w_uq = A.alloc([128, 2, 768], BF16, "w_uq"); r_wuq = Res()
        dma("pool", w_uq[:], W["w_uq"].rearrange("(k p) n -> p k n", p=128), writes=[r_wuq])
        w_ukv = A.alloc([128, 1024], BF16, "w_ukv"); r_wukv = Res()
        dma("pool", w_ukv[:], W["w_ukv"], writes=[r_wukv])
        wrg = A.alloc([128, 4, 128], BF16, "wrg"); wig = A.alloc([128, 4, 128], BF16, "wig"); r_wg = Res()
        memset("pool", wrg[:], 0.0, [r_wg])
        memset("pool", wig[:], 0.0, [r_wg])
        for c in range(4):
            for half in range(2):
                sl = slice(half * 64, half * 64 + 64)
                dma("pool", wrg[sl, c, sl], W["w_rg"][2 * c + half], writes=[r_wg])
                dma("pool", wig[sl, c, sl], W["w_ig"][2 * c + half], writes=[r_wg])
        gmix_b, r_gmix = bcast(A, "gmix_b", W["g_mix"], 1024)
        gq_b, r_gq = bcast(A, "gq_b", W["g_qn"], 96)
        gk_b, r_gk = bcast(A, "gk_b", W["g_kn"], 96)
        ts("dve", gq_b[:], gq_b[:], float(DQK) ** -0.5, None, ALU.mult, None, [r_gq], [r_gq])
        gcq, r_gcq = colvec(A, "gcq", W["g_cq"], 2)
        gckv, r_gckv = colvec(A, "gckv", W["g_ckv"], 1)
        cb, r_cb = colvec(A, "cb", W["conv_b"], 4)
        brg, r_brg = colvec(A, "brg", W["b_rg"], 4)
        big, r_big = colvec(A, "big", W["b_ig"], 4)
        lam, r_lam = colvec(A, "lam", W["lam"], 4)
        cw = A.alloc([128, 4, 4], F32, "cw"); r_cw = Res()
        for j in range(4):
            dma_nc("sp", cw[:, :, j], W["conv_w"][j].rearrange("(c p) -> p c", p=128), writes=[r_cw])
        c1 = A.alloc([128, 4], F32, "c1"); c2 = A.alloc([128, 4], F32, "c2")
        zt = A.alloc([128, 4], F32, "zt"); wv = A.alloc([128, 4], F32, "wv"); w2 = A.alloc([128, 4], F32, "w2")
        r_c12 = Res(); r_z = Res()
        act(zt[:], lam[:], AF.Exp, [r_lam], [r_z], scale=-1.0)
        ts("dve", wv[:], zt[:], 2.0, None, ALU.add, None, [r_z], [r_z])
        S.op("dve", lambda e: e.reciprocal(out=wv[:], in_=wv[:]), [r_z], [r_z])
        tt("dve", wv[:], wv[:], zt[:], ALU.mult, [r_z], [r_z])
        tt("dve", w2[:], wv[:], wv[:], ALU.mult, [r_z], [r_z])
        ts("dve", zt[:], w2[:], 1.0 / 9, 1.0 / 7, ALU.mult, ALU.add, [r_z], [r_z])
        tt("dve", zt[:], zt[:], w2[:], ALU.mult, [r_z], [r_z])
        ts("dve", zt[:], zt[:], 1.0 / 5, None, ALU.add, None, [r_z], [r_z])
        tt("dve", zt[:], zt[:], w2[:], ALU.mult, [r_z], [r_z])
        ts("dve", zt[:], zt[:], 1.0 / 3, None, ALU.add, None, [r_z], [r_z])
        tt("dve", zt[:], zt[:], w2[:], ALU.mult, [r_z], [r_z])
        ts("dve", zt[:], zt[:], 1.0, None, ALU.add, None, [r_z], [r_z])
        tt("dve", zt[:], zt[:], wv[:], ALU.mult, [r_z], [r_z])
        ts("dve", c1[:], zt[:], -16.0, None, ALU.mult, None, [r_z], [r_c12])
        ts("dve", c2[:], zt[:], -32.0, None, ALU.mult, None, [r_z], [r_c12])

        xb = [A.alloc([128, 1024], F32, "xb") for _ in range(4)]; r_xb = [Res() for _ in range(4)]
        junk = A.alloc([128, 1024], BF16, "junk"); r_junk = Res()
        ssx = A.alloc([128, 4], F32, "ssx"); r_ssx = Res()
        hb = [A.alloc([128, 1024], BF16, "hb") for _ in range(2)]; r_hb = [Res() for _ in range(2)]
        hT = [A.alloc([128, 8, 512], BF16, "hT") for _ in range(2)]; r_hT = [Res() for _ in range(2)]
        csb = [A.alloc([128, 4, 64], F32, "csb") for _ in range(2)]; r_cs = [Res() for _ in range(2)]
        cqT = A.alloc([128, 2, 512], BF16, "cqT"); r_cqT = Res()
        ckvT = A.alloc([128, 512], BF16, "ckvT"); r_ckvT = Res()
        sqc = A.alloc([128, 3, 512], BF16, "sqc"); r_sqc = Res()
        sqr = A.alloc([128, 4, 512], BF16, "sqr"); r_sqr = Res()
        ornb = A.alloc([128, 4, 512], BF16, "ornb"); r_ornb = Res()
        uxT = [A.alloc([128, 4, 515], F32, "uxT") for _ in range(2)]; r_ux = [Res() for _ in range(2)]
        memset("pool", uxT[0][:, :, 0:3], 0.0, [r_ux[0]])
        carry = A.alloc([128, 4], F32, "carry"); r_carry = Res()
        memset("pool", carry[:], 0.0, [r_carry])
        def two(name, dt=F32):
            return [A.alloc([128, 512], dt, name) for _ in range(2)], [Res() for _ in range(2)]
        ug, r_ug = two("ug"); gw, r_gw = two("gw"); gel, r_gel = two("gel")
        xc, r_xc = two("xc"); xcb, r_xcb = two("xcb", BF16)
        rg, r_rg = two("rg"); ig, r_ig = two("ig"); av, r_av = two("av"); hs, r_hs = two("hs"); orn, r_orn = two("orn")
        stc = A.alloc([128, 4, 4], F32, "stc"); r_stc = Res()
        qs = A.alloc([128, 8, 96], F32, "qs"); r_qs = Res()
        ks = A.alloc([128, 8, 96], F32, "ks"); r_ks = Res()
        tq = A.alloc([128, 8, 96], F32, "tq"); r_tq = Res()
        tk = A.alloc([128, 8, 96], F32, "tk"); r_tk = Res()
        ssq = A.alloc([128, 16], F32, "ssq"); r_ssq = Res()
        rt1 = A.alloc([128, 8, 32], F32, "rt1"); rt2 = A.alloc([128, 8, 32], F32, "rt2"); r_rt = Res()
        kt1 = A.alloc([128, 8, 32], F32, "kt1"); kt2 = A.alloc([128, 8, 32], F32, "kt2"); r_kt = Res()
        qb = A.alloc([128, 8, 96], BF16, "qb"); r_qb = Res()
        kb = A.alloc([128, 8, 96], BF16, "kb"); r_kb = Res()
        QTst = A.alloc([128, 8, 512], BF16, "QTst"); r_QTst = Res()
        KTst = A.alloc([128, 8, 512], BF16, "KTst"); r_KTst = Res()
        Vst = A.alloc([128, 8, 4, 65], BF16, "Vst"); r_Vst = Res()
        memset("pool", Vst[:], 1.0, [r_Vst])
        print("phase A SBUF used", A.cur)

        ZB = [0, 1]; TB = 2; SB = 3; QB = (4, 5); KVB = (6, 7)
        r_stat_c = Res(); r_stat_r = Res(); r_kr = Res()
        stat_c = banks[SB][:, 0:8].rearrange("p (t c) -> p t c", c=2)
        stat_r = banks[SB][:, 8:12]
        kr_ps = banks[SB][:, 64:192].rearrange("p (t c) -> p t c", c=32)
        zrot = [0]

        def zbank():
            b = ZB[zrot[0] % 2]
            zrot[0] += 1
            return b

        epg = -(-NE // NG)
        for G in range(NG):
            convert_experts(G * epg, (G + 1) * epg)
            hTg, r_hTg = hT[G % 2], r_hT[G % 2]
            cst, r_cst = csb[G % 2], r_cs[G % 2]
            dma("sp", cst[:], cs_d[G * 512:(G + 1) * 512, :].rearrange("(t p) c -> p t c", p=128), writes=[r_cst])
            for t in range(4):
                tok = G * 4 + t
                dma("sp", xb[t][:], x_d[tok * 128:(tok + 1) * 128, :], writes=[r_xb[t]])
                act(junk[:], xb[t][:], AF.Square, [r_xb[t]], [r_junk, r_ssx], accum=ssx[:, t:t + 1])
            rstd_chain(ssx[:], 4, 1.0 / D, [r_ssx])
            for t in range(4):
                h_, r_h = hb[t % 2], r_hb[t % 2]
                stt("dve", h_[:], xb[t][:], ssx[:, t:t + 1], gmix_b[:], ALU.mult, ALU.mult,
                    [r_xb[t], r_ssx, r_gmix], [r_h])
                tv = bview(TB).rearrange("p (k n) -> p k n", k=8)
                for k in range(8):
                    transpose(tv[:, k, :], h_[:, k * 128:(k + 1) * 128], [r_h], [rb[TB]])
                cp("act", hTg[:, :, t * 128:(t + 1) * 128], tv, [rb[TB]], [r_hTg])

            def zmm(col0, ncols, b):
                for k in range(8):
                    mm(banks[b][0:ncols, :], w_in[:, k, col0:col0 + ncols], hTg[:, k, :], k == 0, k == 7,
                       [r_win, r_hTg], [rb[b]])

            for j in range(2):
                b = zbank(); zmm(j * 128, 128, b)
                act(sqc[:, j, :], banks[b][:], AF.Square, [rb[b]], [r_sqc])
                act(cqT[:, j, :], banks[b][:], AF.Copy, [rb[b], r_gcq], [r_cqT], scale=gcq[:, j:j + 1])
            b = zbank(); zmm(256, 128, b)
            act(sqc[:, 2, :], banks[b][:], AF.Square, [rb[b]], [r_sqc])
            act(ckvT[:], banks[b][:], AF.Copy, [rb[b], r_gckv], [r_ckvT], scale=gckv[:, 0:1])
            for t in range(4):
                tsl = slice(t * 128, (t + 1) * 128)
                for j in range(2):
                    mm(stat_c[:, t, 0:1], sqc[:, j, tsl], ones[:, 0:1], j == 0, j == 1, [r_sqc, r_ones], [r_stat_c])
                mm(stat_c[:, t, 1:2], sqc[:, 2, tsl], ones[:, 0:1], True, True, [r_sqc, r_ones], [r_stat_c])
            ts("dve", stc[:, :, 0:1], stat_c[:, :, 0:1], 1.0 / 256, EPS, ALU.mult, ALU.add, [r_stat_c], [r_stc])
            ts("dve", stc[:, :, 1:2], stat_c[:, :, 1:2], 1.0 / 128, EPS, ALU.mult, ALU.add, [r_stat_c], [r_stc])
            stc2 = stc[:, :, 0:2]
            tt("pool", stc2, stc2, cneg[:, 0:1].unsqueeze(2).to_broadcast([128, 4, 2]), ALU.pow, [r_stc, r_c], [r_stc])

            def qk_gen():
                for t in range(4):
                    tsl = slice(t * 128, (t + 1) * 128)
                    qv = [banks[QB[0]][:, 0:384], banks[QB[1]][:, 0:384]]
                    for half in range(2):
                        for j in range(2):
                            mm(qv[half], cqT[:, j, tsl], w_uq[:, j, half * 384:(half + 1) * 384], j == 0, j == 1,
                               [r_cqT, r_wuq], [rb[QB[half]]])
                            yield
                    for half in range(2):
                        mm(banks[KVB[half]][:], ckvT[:, tsl], w_ukv[:, half * 512:(half + 1) * 512], True, True,
                           [r_ckvT, r_wukv], [rb[KVB[half]]])
                        yield
                    for k in range(8):
                        mm(kr_ps[:, t, :], hTg[:, k, tsl], w_in[:, k, 384:416], k == 0, k == 7, [r_hTg, r_win], [r_kr])
                        yield
                    rcq = stc[:, t, 0:1]; rckv = stc[:, t, 1:2]
                    for half in range(2):
                        hs4 = slice(half * 4, half * 4 + 4)
                        act(qs[:, hs4, :], qv[half].rearrange("p (h d) -> p h d", h=4), AF.Copy,
                            [rb[QB[half]], r_stc], [r_qs], scale=rcq)
                        yield
                        kvv = banks[KVB[half]][:].rearrange("p (h d) -> p h d", h=4)
                        act(ks[:, hs4, 0:64], kvv[:, :, 0:64], AF.Copy, [rb[KVB[half]], r_stc], [r_ks], scale=rckv)
                        yield
                        act(Vst[:, hs4, t, 0:64], kvv[:, :, 64:128], AF.Copy, [rb[KVB[half]], r_stc], [r_Vst], scale=rckv)
                        yield
                    cp("dve", ks[:, :, 64:96], kr_ps[:, t, :].unsqueeze(1).to_broadcast([128, 8, 32]), [r_kr], [r_ks])
                    yield
                    act(tq[:], qs[:], AF.Square, [r_qs], [r_tq])
                    yield
                    act(tk[:], ks[:], AF.Square, [r_ks], [r_tk])
                    yield
                    S.op("dve", lambda e: e.tensor_reduce(out=ssq[:, 0:8], in_=tq[:], axis=AX.X, op=ALU.add), [r_tq], [r_ssq])
                    yield
                    S.op("dve", lambda e: e.tensor_reduce(out=ssq[:, 8:16], in_=tk[:], axis=AX.X, op=ALU.add), [r_tk], [r_ssq])
                    yield
                    rstd_chain(ssq[:], 16, 1.0 / DQK, [r_ssq])
                    yield
                    for (src, r_src, tmp, r_tmp, g_b, r_g, o0, t1, t2, r_t, dst, r_dst, st, r_st) in (
                            (qs, r_qs, tq, r_tq, gq_b, r_gq, 0, rt1, rt2, r_rt, qb, r_qb, QTst, r_QTst),
                            (ks, r_ks, tk, r_tk, gk_b, r_gk, 8, kt1, kt2, r_kt, kb, r_kb, KTst, r_KTst)):
                        tt("dve", tmp[:], src[:], ssq[:, o0:o0 + 8].unsqueeze(2).to_broadcast([128, 8, 96]), ALU.mult,
                           [r_src, r_ssq], [r_tmp])
                        yield
                        tt("dve", tmp[:], tmp[:], g_b[:].unsqueeze(1).to_broadcast([128, 8, 96]), ALU.mult,
                           [r_tmp, r_g], [r_tmp])
                        yield
                        c2b = cst[:, t, 0:32].unsqueeze(1).to_broadcast([128, 8, 32])
                        tt("dve", t1[:], tmp[:, :, 64:96], c2b, ALU.mult, [r_tmp, r_cst], [r_t])
                        yield
                        tt("dve", t2[:, :, 0:16], tmp[:, :, 80:96],
                           cst[:, t, 32:48].unsqueeze(1).to_broadcast([128, 8, 16]), ALU.mult, [r_tmp, r_cst], [r_t])
                        yield
                        tt("dve", t2[:, :, 16:32], tmp[:, :, 64:80],
                           cst[:, t, 48:64].unsqueeze(1).to_broadcast([128, 8, 16]), ALU.mult, [r_tmp, r_cst], [r_t])
                        yield
                        tt("dve", dst[:, :, 64:96], t1[:], t2[:], ALU.add, [r_t], [r_dst])
                        yield
                        cp("act", dst[:, :, 0:64], tmp[:, :, 0:64], [r_tmp], [r_dst])
                        yield
                        tv = bview(TB).rearrange("p (h n) -> p h n", h=8)
                        for h in range(NH):
                            transpose(tv[0:96, h, :], dst[:, h, :], [r_dst], [rb[TB]])
                            yield
                        cp("dve", st[0:96, :, tsl], tv[0:96, :, :], [rb[TB]], [r_st])
                        yield


            def rnn_gen():
                U, r_U = uxT[G % 2], r_ux[G % 2]
                Un, r_Un = uxT[(G + 1) % 2], r_ux[(G + 1) % 2]
                for c in range(4):
                    pb = c % 2
                    b = zbank(); zmm(416 + c * 128, 128, b)
                    act(ug[pb][:], banks[b][:], AF.Copy, [rb[b]], [r_ug[pb]])
                    yield
                    act(gw[pb][:], banks[b][:], AF.Square, [rb[b]], [r_gw[pb]])
                    yield
                    ts("dve", gw[pb][:], gw[pb][:], 0.044715, 1.0, ALU.mult, ALU.add, [r_gw[pb]], [r_gw[pb]])
                    yield
                    tt("dve", gw[pb][:], gw[pb][:], ug[pb][:], ALU.mult, [r_gw[pb], r_ug[pb]], [r_gw[pb]])
                    yield
                    act(gw[pb][:], gw[pb][:], AF.Sigmoid, [r_gw[pb]], [r_gw[pb]], scale=1.5957691216057308)
                    yield
                    tt("dve", gel[pb][:], gw[pb][:], ug[pb][:], ALU.mult, [r_gw[pb], r_ug[pb]], [r_gel[pb]])
                    yield
                    b = zbank(); zmm(928 + c * 128, 128, b)
                    act(U[:, c, 3:515], banks[b][:], AF.Copy, [rb[b]], [r_U])
                    yield
                    cp("pool", Un[:, c, 0:3], U[:, c, 512:515], [r_U], [r_Un])
                    yield
                    ts("dve", xc[pb][:], U[:, c, 3:515], cw[:, c, 3:4], cb[:, c:c + 1], ALU.mult, ALU.add,
                       [r_U, r_cw, r_cb], [r_xc[pb]])
                    yield
                    for j in (2, 1, 0):
                        stt("dve", xc[pb][:], U[:, c, j:j + 512], cw[:, c, j:j + 1], xc[pb][:], ALU.mult, ALU.add,
                            [r_U, r_cw, r_xc[pb]], [r_xc[pb]])
                        yield
                    cp("act", xcb[pb][:], xc[pb][:], [r_xc[pb]], [r_xcb[pb]])
                    yield
                    b1 = zbank()
                    mm(banks[b1][:], wrg[:, c, :], xcb[pb][:], True, True, [r_wg, r_xcb[pb]], [rb[b1]])
                    yield
                    act(rg[pb][:], banks[b1][:], AF.Sigmoid, [rb[b1], r_brg], [r_rg[pb]], bias=brg[:, c:c + 1])
                    yield
                    b2 = zbank()
                    mm(banks[b2][:], wig[:, c, :], xcb[pb][:], True, True, [r_wg, r_xcb[pb]], [rb[b2]])
                    yield
                    act(ig[pb][:], banks[b2][:], AF.Sigmoid, [rb[b2], r_big], [r_ig[pb]], bias=big[:, c:c + 1])
                    yield
                    act(av[pb][:], rg[pb][:], AF.Exp, [r_rg[pb], r_c12], [r_av[pb]], scale=c1[:, c:c + 1])
                    yield
                    act(rg[pb][:], rg[pb][:], AF.Exp, [r_rg[pb], r_c12], [r_rg[pb]], scale=c2[:, c:c + 1])
                    yield
                    act(rg[pb][:], rg[pb][:], AF.Relu, [r_rg[pb], r_c], [r_rg[pb]], scale=-1.0, bias=cone[:, 0:1])
                    yield
                    act(rg[pb][:], rg[pb][:], AF.Sqrt, [r_rg[pb]], [r_rg[pb]])
                    yield
                    tt("dve", ig[pb][:], ig[pb][:], xc[pb][:], ALU.mult, [r_ig[pb], r_xc[pb]], [r_ig[pb]])
                    yield
                    tt("dve", rg[pb][:], rg[pb][:], ig[pb][:], ALU.mult, [r_rg[pb], r_ig[pb]], [r_rg[pb]])
                    yield
                    S.op("dve", lambda e, pb=pb, c=c: e.tensor_tensor_scan(
                        out=hs[pb][:], data0=av[pb][:], data1=rg[pb][:], initial=carry[:, c:c + 1],
                        op0=ALU.mult, op1=ALU.add), [r_av[pb], r_rg[pb], r_carry], [r_hs[pb]])
                    yield
                    cp("pool", carry[:, c:c + 1], hs[pb][:, 511:512], [r_hs[pb]], [r_carry])
                    yield
                    tt("dve", orn[pb][:], gel[pb][:], hs[pb][:], ALU.mult, [r_gel[pb], r_hs[pb]], [r_orn[pb]])
                    yield
                    act(sqr[:, c, :], orn[pb][:], AF.Square, [r_orn[pb]], [r_sqr])
                    yield
                    cp("act", ornb[:, c, :], orn[pb][:], [r_orn[pb]], [r_ornb])
                    yield

            gens = [qk_gen(), rnn_gen()]
            while gens:
                for g in list(gens):
                    try:
                        next(g)
                    except StopIteration:
                        gens.remove(g)
            dma("sp", ORT[:, :, G * 512:(G + 1) * 512].rearrange("c p s -> p c s"), ornb[:], [r_ornb], [r_ORT])
            for t in range(4):
                for c in range(4):
                    mm(stat_r[:, t:t + 1], sqr[:, c, t * 128:(t + 1) * 128], ones[:, 0:1], c == 0, c == 3,
                       [r_sqr, r_ones], [r_stat_r])
            rsl = rr_all[:, G * 4:(G + 1) * 4]
            ts("dve", rsl, stat_r, 1.0 / 512, EPS, ALU.mult, ALU.add, [r_stat_r], [r_rr])
            tt("pool", rsl, rsl, cneg[:, 0:1].to_broadcast([128, 4]), ALU.pow, [r_rr, r_c], [r_rr])
            gsl = slice(G * 512, (G + 1) * 512)
            dma("sp", QT[:, :, gsl].rearrange("h d s -> d h s"), QTst[0:96, :, :], [r_QTst], [r_QT])
            dma("sp", KT[:, :, gsl].rearrange("h d s -> d h s"), KTst[0:96, :, :], [r_KTst], [r_KT])
            dma("sp", VA[:, :, G * 4:(G + 1) * 4, :].rearrange("h p t c -> p h t c"), Vst[:], [r_Vst], [r_VA])
        if dbg:
            dma("sp", RR, rr_all[:, 0:NT], [r_rr], [])

    def phaseB():
        Bn = Arena(nc, SB_LO + 6144, SB_HI)
        QTh = [Bn.alloc([128, S_len], BF16, "QTh") for _ in range(2)]; r_QTh = [Res() for _ in range(2)]
        KTh = [Bn.alloc([128, S_len], BF16, "KTh") for _ in range(2)]; r_KTh = [Res() for _ in range(2)]
        Vh = [Bn.alloc([128, NT, 65], BF16, "Vh") for _ in range(2)]; r_Vh = [Res() for _ in range(2)]
        NPT = 4
        pT = [Bn.alloc([128, 1024], BF16, "pT") for _ in range(NPT)]; r_pT = [Res() for _ in range(NPT)]
        ost = [Bn.alloc([128, 4, 64], BF16, "ost") for _ in range(2)]; r_ost = [Res() for _ in range(2)]
        rec = [Bn.alloc([128, 4], F32, "rec") for _ in range(2)]; r_rec = [Res() for _ in range(2)]
        units = []
        for h in range(NH):
            for G in range(NG):
                for kt in range(0, 4 * G, 2):
                    units.append((h, G, [kt, kt + 1]))
                for j in range(4):
                    units.append((h, G, [4 * G + j]))
        LOOK = 2
        NSP = 3
        state = {"s_emitted": 0}

        def load_head(h):
            p = h % 2
            dma("sp", QTh[p][0:96, :], QT[h], [r_QT], [r_QTh[p]])
            dma("sp", KTh[p][0:96, :], KT[h], [r_KT], [r_KTh[p]])
            dma("sp", Vh[p][:], VA[h], [r_VA], [r_Vh[p]])

        def emit_score(u):
            h, G, kts = units[u]
            if G == 0 and kts[0] == 0:
                load_head(h)
            p = h % 2
            sp_ = u % NSP
            for i, kt in enumerate(kts):
                q0 = max(kt - 4 * G, 0) * 128
                mm(pairs[sp_][:, i * 512 + q0:(i + 1) * 512], KTh[p][0:96, kt * 128:(kt + 1) * 128],
                   QTh[p][0:96, G * 512 + q0:(G + 1) * 512], True, True, [r_KTh[p], r_QTh[p]], [rb[2 * sp_]])

        for u, (h, G, kts) in enumerate(units):
            while state["s_emitted"] < min(len(units), u + 1 + LOOK):
                emit_score(state["s_emitted"])
                state["s_emitted"] += 1
            p = h % 2
            sp_ = u % NSP
            pi = u % NPT
            gi = (h * NG + G) % 2
            ob = 6 + gi
            o_ps = banks[ob][:, 0:260].rearrange("p (t c) -> p t c", c=65)
            j0 = kts[0] - 4 * G
            q0 = max(j0, 0) * 128
            w = 512 * len(kts)
            act(pT[pi][:, q0:w], pairs[sp_][:, q0:w], AF.Exp, [rb[2 * sp_]], [r_pT[pi]])
            if j0 >= 0:
                memset("pool", pT[pi][64:128, q0:q0 + 64], 0.0, [r_pT[pi]])
            for i, kt in enumerate(kts):
                j = kt - 4 * G
                for qt in range(max(j, 0), 4):
                    mm(o_ps[:, qt, :], pT[pi][:, i * 512 + qt * 128:i * 512 + (qt + 1) * 128], Vh[p][:, kt, :],
                       kt == 0 and qt == 0, kt == 4 * G + qt, [r_pT[pi], r_Vh[p]], [rb[ob]], skip=True)
            if kts[-1] == 4 * G + 3:
                S.op("dve", lambda e, gi=gi, o_ps=o_ps: e.reciprocal(out=rec[gi][:], in_=o_ps[:, :, 64]),
                     [rb[ob]], [r_rec[gi]])
                tt("dve", ost[gi][:], o_ps[:, :, 0:64], rec[gi][:].unsqueeze(2).to_broadcast([128, 4, 64]), ALU.mult,
                   [rb[ob], r_rec[gi]], [r_ost[gi]])
                dma_nc("sp", OA[G * 512:(G + 1) * 512, h * 64:(h + 1) * 64].rearrange("(t p) d -> p t d", p=128),
                       ost[gi][:], [r_ost[gi]], [r_OA])

    def phaseCD():
        NSETS = int(os.environ.get('NSETS', '3'))
        Cn = Arena(nc, SB_LO + 6144, SB_HI)
        TB = 7
        w_out = Cn.alloc([128, 8, 1024], BF16, "w_out"); r_wout = Res()
        dma("pool", w_out[:], W["w_out"].rearrange("(k p) n -> p k n", p=128), writes=[r_wout])
        grnn, r_grnn = colvec(Cn, "grnn", W["g_rnn_out"], 4)
        for c in range(4):
            ts("dve", w_out[:, 4 + c, :], w_out[:, 4 + c, :], grnn[:, c:c + 1], None, ALU.mult, None,
               [r_wout, r_grnn], [r_wout])
        w_mq = Cn.alloc([128, 8, 1024], BF16, "w_mq"); r_wmq = Res()
        dma("pool", w_mq[:], W["w_mq"].rearrange("(k p) n -> p k n", p=128), writes=[r_wmq])
        w_mo = Cn.alloc([128, 8, 1024], BF16, "w_mo"); r_wmo = Res()
        dma("pool", w_mo[:], W["w_mo"].rearrange("(k p) n -> p k n", p=128), writes=[r_wmo])
        ga_b, r_ga = bcast(Cn, "ga_b", W["g_attn_out"], 512)
        gxq_b, r_gxq = bcast(Cn, "gxq_b", W["g_xq"], 1024)
        gffn_b, r_gffn = bcast(Cn, "gffn_b", W["g_ffn"], 1024)
        gmq_b, r_gmq = bcast(Cn, "gmq_b", W["g_mqn"], 256)
        ts("dve", gmq_b[:], gmq_b[:], 256.0 ** -0.5, None, ALU.mult, None, [r_gmq], [r_gmq])
        wr = Cn.alloc([128, 8, 36], F32, "wr"); r_wr = Res()
        dma_nc("sp", wr[:, :, 0:4], W["w_group"].rearrange("(k p) n -> p k n", p=128), writes=[r_wr])
        dma_nc("sp", wr[:, :, 4:36], W["w_expert"].rearrange("(k p) n -> p k n", p=128), writes=[r_wr])
        br_b = Cn.alloc([128, 36], F32, "br_b"); r_br = Res()
        dma("sp", br_b[:, 0:4], W["b_group"].partition_broadcast(128), writes=[r_br])
        dma("sp", br_b[:, 4:36], W["b_expert"].partition_broadcast(128), writes=[r_br])
        KmT = Cn.alloc([128, 8, 256], BF16, "KmT"); r_KmT = Res()
        Vm = Cn.alloc([128, 2, 4, 256], BF16, "Vm"); r_Vm = Res()
        I32 = mybir.dt.int32
        T_SL = 256
        NTILE = (2 * S_len) // T_SL + NE
        gAB = Cn.alloc([128, NT, 2], F32, "gAB"); r_gAB = Res()
        widx = Cn.alloc([128, NTILE], I32, "widx"); r_widx = Res()
        pos_i = Cn.alloc([128, 2, NT], I32, "pos_i"); r_pos = Res()
        mark2 = Cn.cur
        xt = Sel([Cn.alloc([128, 1024], F32, "xt") for _ in range(NSETS)]); r_xt = RSel(NSETS)
        junk = Sel([Cn.alloc([128, 1024], BF16, "junkc") for _ in range(NSETS)]); r_junk = RSel(NSETS)
        ssv = Sel([Cn.alloc([128, 4], F32, "ssv") for _ in range(NSETS)]); r_ssv = RSel(NSETS)
        h2 = Sel([Cn.alloc([128, 1024], BF16, "h2") for _ in range(NSETS)]); r_h2 = RSel(NSETS)
        h2T = Sel([Cn.alloc([128, 8, 128], BF16, "h2T") for _ in range(NSETS)]); r_h2T = RSel(NSETS)
        tmpf = Sel([Cn.alloc([128, 1024], F32, "tmpf") for _ in range(NSETS)]); r_tmpf = RSel(NSETS)
        ssm = Sel([Cn.alloc([128, 4], F32, "ssm") for _ in range(NSETS)]); r_ssm = RSel(NSETS)
        qmb = Sel([Cn.alloc([128, 1024], BF16, "qmb") for _ in range(NSETS)]); r_qmb = RSel(NSETS)
        mark = Cn.cur
        tvb = bview(TB).rearrange("p (k n) -> p k n", k=8)

        def norm_to_bf16(src, r_src, g_b, r_g, dst, r_dst, sscol, f32dst=None, r_f32=None):
            act(junk[:], src, AF.Square, [r_src], [r_junk, r_ssv], accum=ssv[:, sscol:sscol + 1])
            rstd_chain(ssv[:, sscol:sscol + 1], 1, 1.0 / D, [r_ssv])
            if f32dst is None:
                stt("dve", dst, src, ssv[:, sscol:sscol + 1], g_b[:], ALU.mult, ALU.mult, [r_src, r_ssv, r_g], [r_dst])
            else:
                stt("dve", f32dst, src, ssv[:, sscol:sscol + 1], g_b[:], ALU.mult, ALU.mult, [r_src, r_ssv, r_g], [r_f32])
                cp("act", dst, f32dst, [r_f32], [r_dst])

        def transpose8(src, r_src, dstT, r_dstT, n=8, dst_sl=None):
            for k in range(n):
                transpose(tvb[:, k, :], src[:, k * 128:(k + 1) * 128], [r_src], [rb[TB]])
            cp("act", dstT if dst_sl is None else dst_sl, tvb[:, 0:n, :], [rb[TB]], [r_dstT])

        def head_norm(pb0, g_b, r_g, dst, r_dst):
            for half in range(2):
                act(tmpf[:, half * 512:(half + 1) * 512], banks[pb0 + half][:], AF.Square, [rb[pb0 + half]], [r_tmpf])
            red(ssm[:], tmpf[:].rearrange("p (h d) -> p h d", h=4), ALU.add, [r_tmpf], [r_ssm])
            rstd_chain(ssm[:], 4, 1.0 / 256, [r_ssm])
            for half in range(2):
                tt("dve", tmpf[:, half * 512:(half + 1) * 512].rearrange("p (h d) -> p h d", h=2),
                   banks[pb0 + half][:].rearrange("p (h d) -> p h d", h=2),
                   ssm[:, half * 2:half * 2 + 2].unsqueeze(2).to_broadcast([128, 2, 256]), ALU.mult,
                   [rb[pb0 + half], r_ssm], [r_tmpf])
            tt("dve", dst.rearrange("p (h d) -> p h d", h=4), tmpf[:].rearrange("p (h d) -> p h d", h=4),
               g_b[:].unsqueeze(1).to_broadcast([128, 4, 256]), ALU.mult, [r_tmpf, r_g], [r_dst])

        w_mk = Cn.alloc([128, 8, 1024], BF16, "w_mk"); r_wmk = Res()
        dma("pool", w_mk[:], W["w_mk"].rearrange("(k p) n -> p k n", p=128), writes=[r_wmk])
        w_mv = Cn.alloc([128, 8, 1024], BF16, "w_mv"); r_wmv = Res()
        dma("pool", w_mv[:], W["w_mv"].rearrange("(k p) n -> p k n", p=128), writes=[r_wmv])
        gmem_b, r_gmem = bcast(Cn, "gmem_b", W["g_mem"], 1024)
        gmk_b, r_gmk = bcast(Cn, "gmk_b", W["g_mkn"], 256)
        mnT = Cn.alloc([128, 8, 256], BF16, "mnT"); r_mnT = Res()
        for mt in range(2):
            dma("sp", xt[:], mem_d[mt * 128:(mt + 1) * 128, :], writes=[r_xt])
            norm_to_bf16(xt[:], r_xt, gmem_b, r_gmem, h2[:], r_h2, 0)
            transpose8(h2, r_h2, None, r_mnT, dst_sl=mnT[:, :, mt * 128:(mt + 1) * 128])
        for mt in range(2):
            msl = slice(mt * 128, (mt + 1) * 128)
            for half in range(2):
                for k in range(8):
                    mm(banks[half][:], mnT[:, k, msl], w_mk[:, k, half * 512:(half + 1) * 512], k == 0, k == 7,
                       [r_mnT, r_wmk], [rb[half]])
                for k in range(8):
                    mm(banks[2 + half][:], mnT[:, k, msl], w_mv[:, k, half * 512:(half + 1) * 512], k == 0, k == 7,
                       [r_mnT, r_wmv], [rb[2 + half]])
                act(Vm[:, mt, half * 2:half * 2 + 2, :], banks[2 + half][:].rearrange("p (h d) -> p h d", h=2),
                    AF.Copy, [rb[2 + half]], [r_Vm])
            head_norm(0, gmk_b, r_gmk, qmb[:], r_qmb)
            transpose8(qmb, r_qmb, None, r_KmT, dst_sl=KmT[:, :, msl])
        S.barrier()
        Cn.cur = mark

        oa = Sel([Cn.alloc([128, 512], BF16, "oa") for _ in range(NSETS)]); r_oa = RSel(NSETS)
        ornT = Sel([Cn.alloc([128, 4, 128], BF16, "ornT") for _ in range(NSETS)]); r_ornT = RSel(NSETS)
        mixA = Sel([Cn.alloc([128, 512], BF16, "mixA") for _ in range(NSETS)]); r_mixA = RSel(NSETS)
        mixAT = Sel([Cn.alloc([128, 4, 128], BF16, "mixAT") for _ in range(NSETS)]); r_mixAT = RSel(NSETS)
        x1 = Sel([Cn.alloc([128, 1024], F32, "x1") for _ in range(NSETS)]); r_x1 = RSel(NSETS)
        qmT = Sel([Cn.alloc([128, 8, 128], BF16, "qmT") for _ in range(NSETS)]); r_qmT = RSel(NSETS)
        pm = Sel([Cn.alloc([128, 8, 128], BF16, "pm") for _ in range(NSETS)]); r_pm = RSel(NSETS)
        recm = Sel([Cn.alloc([128, 4], F32, "recm") for _ in range(NSETS)]); r_recm = RSel(NSETS)
        omb = h2; r_omb = r_h2
        omT = h2T; r_omT = r_h2T
        h3f = tmpf; r_h3f = r_tmpf
        h3 = qmb; r_h3 = r_qmb
        h3fT = Sel([Cn.alloc([128, 8, 128], F32, "h3fT") for _ in range(NSETS)]); r_h3fT = RSel(NSETS)
        lg = Sel([Cn.alloc([128, 36], F32, "lg") for _ in range(NSETS)]); r_lg = RSel(NSETS)
        rt = Sel([Cn.alloc([128, 64], F32, "rt") for _ in range(NSETS)]); r_rt = RSel(NSETS)
        I32 = mybir.dt.int32
        T_SL = 256
        NTILE = (2 * S_len) // T_SL + NE
        WGUv = WGU2.rearrange("e p k n -> (e p) (k n)")
        WDv = WD2.rearrange("e p c n -> (e p) (c n)")
        x2 = xt; r_x2 = r_xt
        rank_all = Cn.alloc([128, NT, 32], F32, "rank_all"); r_rank = Res()
        selA = Cn.alloc([128, NT, 32], F32, "selA"); selB = Cn.alloc([128, NT, 32], F32, "selB"); r_sel = Res()
        carryc = Cn.alloc([128, 32], F32, "carryc"); r_carryc = Res()
        memset("pool", carryc[:], 0.0, [r_carryc])
        Mf = Sel([Cn.alloc([128, 32], F32, "Mf") for _ in range(2)])
        Mb = Sel([Cn.alloc([128, 32], BF16, "Mb") for _ in range(NSETS)]); r_M = RSel(NSETS)
        Lst = Cn.alloc([128, 128], BF16, "Lst"); r_Lst = Res()
        dma("sp", Lst[:], lst_d, writes=[r_Lst])
        ones128 = Cn.alloc([128, 128], BF16, "ones128")
        memset("pool", ones128[:], 1.0, [r_Lst])
        r_H3 = [Res() for _ in range(NT)]
        r_out = [Res() for _ in range(NT)]
        print("phase C SBUF used", Cn.cur, "of", SB_HI)
        N_SKEW = int(os.environ.get('N_SKEW', '12'))

        def tile_body(gt):
            if True:
                t8 = 0
                rows = slice(gt * 128, (gt + 1) * 128)
                dma("sp", xt[:], x_d[rows, :], writes=[r_xt])
                yield
                dma("sp", oa[:], OA[rows, :], [r_OA], [r_oa])
                yield
                dma("sp", ornT[:], ORT[:, :, rows].rearrange("c p s -> p c s"), [r_ORT], [r_ornT])
                yield
                act(junk[:, 0:512], oa[:], AF.Square, [r_oa], [r_junk, r_ssv], accum=ssv[:, 0:1])
                yield
                rstd_chain(ssv[:, 0:1], 1, 1.0 / 512, [r_ssv])
                yield
                tt("dve", mixA[:], oa[:], ga_b[:], ALU.mult, [r_oa, r_ga], [r_mixA])
                yield
                transpose8(mixA, r_mixA, mixAT[:], r_mixAT, n=4)
                yield
                for half in range(2):
                    hsl = slice(half * 512, (half + 1) * 512)
                    for k in range(4):
                        mm(banks[half][:], mixAT[:, k, :], w_out[:, k, hsl], k == 0, k == 3, [r_mixAT, r_wout], [rb[half]])
                    for k in range(4):
                        mm(banks[2 + half][:], ornT[:, k, :], w_out[:, 4 + k, hsl], k == 0, k == 3,
                           [r_ornT, r_wout], [rb[2 + half]])
                    stt("dve", x1[:, hsl], banks[half][:], ssv[:, 0:1], xt[:, hsl], ALU.mult, ALU.add,
                        [rb[half], r_ssv, r_xt], [r_x1])
                    stt("dve", x1[:, hsl], banks[2 + half][:], rr_all[:, gt:gt + 1], x1[:, hsl], ALU.mult, ALU.add,
                        [rb[2 + half], r_rr, r_x1], [r_x1])
                yield
                norm_to_bf16(x1[:], r_x1, gxq_b, r_gxq, h2[:], r_h2, 1)
                yield
                transpose8(h2, r_h2, h2T[:], r_h2T)
                yield
                for half in range(2):
                    for k in range(8):
                        mm(banks[4 + half][:], h2T[:, k, :], w_mq[:, k, half * 512:(half + 1) * 512], k == 0, k == 7,
                           [r_h2T, r_wmq], [rb[4 + half]])
                head_norm(4, gmq_b, r_gmq, qmb[:], r_qmb)
                yield
                transpose8(qmb, r_qmb, qmT[:], r_qmT)
                yield
                for hh in range(4):
                    for mt in range(2):
                        slot = hh * 2 + mt
                        for kk in range(2):
                            mm(banks[slot // 4][:, (slot % 4) * 128:(slot % 4 + 1) * 128],
                               KmT[:, hh * 2 + kk, mt * 128:(mt + 1) * 128], qmT[:, hh * 2 + kk, :], kk == 0, kk == 1,
                               [r_KmT, r_qmT], [rb[slot // 4]])
                for bk in range(2):
                    act(pm[:, bk * 4:(bk + 1) * 4, :], banks[bk][:].rearrange("p (s n) -> p s n", s=4), AF.Exp,
                        [rb[bk]], [r_pm])
                yield
                for hh in range(4):
                    ob = 2 + hh // 2
                    for mt in range(2):
                        mm(banks[ob][:, (hh % 2) * 256:(hh % 2 + 1) * 256], pm[:, hh * 2 + mt, :], Vm[:, mt, hh, :],
                           mt == 0, mt == 1, [r_pm, r_Vm], [rb[ob]])
                    for mt in range(2):
                        mm(banks[6][:, hh:hh + 1], pm[:, hh * 2 + mt, :], ones[:, 0:1], mt == 0, mt == 1,
                           [r_pm, r_ones], [rb[6]])
                recip(recm[:], banks[6][:, 0:4], [rb[6]], [r_recm])
                for bk in range(2):
                    tt("dve", omb[:, bk * 512:(bk + 1) * 512].rearrange("p (h d) -> p h d", h=2),
                       banks[2 + bk][:].rearrange("p (h d) -> p h d", h=2),
                       recm[:, bk * 2:bk * 2 + 2].unsqueeze(2).to_broadcast([128, 2, 256]), ALU.mult,
                       [rb[2 + bk], r_recm], [r_omb])
                yield
                transpose8(omb, r_omb, omT[:], r_omT)
                yield
                for half in range(2):
                    hsl = slice(half * 512, (half + 1) * 512)
                    for k in range(8):
                        mm(banks[4 + half][:], omT[:, k, :], w_mo[:, k, hsl], k == 0, k == 7, [r_omT, r_wmo], [rb[4 + half]])
                    tt("dve", x2[:, hsl], banks[4 + half][:], x1[:, hsl], ALU.add, [rb[4 + half], r_x1], [r_x2])
                yield
                dma("sp", out_d[rows, :], x2[:], [r_x2], [r_out[gt]])
                yield
                act(junk[:], x2[:], AF.Square, [r_x2], [r_junk, r_ssv], accum=ssv[:, 2:3])
                yield
                rstd_chain(ssv[:, 2:3], 1, 1.0 / D, [r_ssv])
                yield
                stt("dve", h3f[:], x2[:], ssv[:, 2:3], gffn_b[:], ALU.mult, ALU.mult, [r_x2, r_ssv, r_gffn], [r_h3f])
                yield
                cp("act", h3[:], h3f[:], [r_h3f], [r_h3])
                yield
                dma("sp", H3[rows, :], h3[:], [r_h3], [r_H3[gt]])
                yield
                for k in range(8):
                    transpose(banks[k // 4][:, (k % 4) * 128:(k % 4 + 1) * 128], h3f[:, k * 128:(k + 1) * 128],
                              [r_h3f], [rb[k // 4]], f32=True)
                for bk in range(2):
                    cp("act", h3fT[:, bk * 4:(bk + 1) * 4, :], banks[bk][:].rearrange("p (s n) -> p s n", s=4),
                       [rb[bk]], [r_h3fT])
                yield
                for k in range(8):
                    mm(banks[6][:, 64:100], h3fT[:, k, :], wr[:, k, :], k == 0, k == 7, [r_h3fT, r_wr], [rb[6]])
                tt("dve", lg[:], banks[6][:, 64:100], br_b[:], ALU.add, [rb[6], r_br], [r_lg])
                R = [r_lg, r_rt]
                gmax, ngmax, sumg, oh = rt[:, 0:1], rt[:, 1:2], rt[:, 2:3], rt[:, 4:8]
                eg, es, emax, nemax = rt[:, 8:12], rt[:, 16:24], rt[:, 12:13], rt[:, 13:14]
                ex, top8, den, msk = rt[:, 24:32], rt[:, 32:40], rt[:, 14:15], rt[:, 40:48]
                sel32 = tmpf[:, 0:32]
                yield
                red(gmax, lg[:, 0:4], ALU.max, R, [r_rt])
                yield
                ts("dve", oh, lg[:, 0:4], gmax, None, ALU.is_ge, None, R, [r_rt])
                yield
                ts("dve", ngmax, gmax, -1.0, None, ALU.mult, None, R, [r_rt])
                yield
                act(eg, lg[:, 0:4], AF.Exp, R, [r_rt], bias=ngmax, accum=sumg)
                yield
                recip(sumg, sumg, R, [r_rt])
                yield
                tt("dve", sel32.rearrange("p (g e) -> p g e", g=4), lg[:, 4:36].rearrange("p (g e) -> p g e", g=4),
                   oh.unsqueeze(2).to_broadcast([128, 4, 8]), ALU.mult, R, [r_tmpf])
                yield
                red(es, sel32.rearrange("p (g e) -> p e g", g=4), ALU.add, [r_tmpf], [r_rt])
                yield
                red(emax, es, ALU.max, R, [r_rt])
                yield
                ts("dve", nemax, emax, -1.0, None, ALU.mult, None, R, [r_rt])
                yield
                act(ex, es, AF.Exp, R, [r_rt], bias=nemax)
                yield
                S.op("dve", lambda e, top8=top8, ex=ex: e.max(out=top8, in_=ex), R, [r_rt])
                yield
                tt("dve", den, top8[:, 0:1], top8[:, 1:2], ALU.add, R, [r_rt])
                yield
                recip(den, den, R, [r_rt])
                yield
                tt("dve", den, den, sumg, ALU.mult, R, [r_rt])
                mskA, msk2 = rt[:, 48:56], rt[:, 40:48]
                yield
                ts("dve", mskA, ex, top8[:, 0:1], None, ALU.is_ge, None, R, [r_rt])
                yield
                ts("dve", msk2, ex, top8[:, 1:2], None, ALU.is_ge, None, R, [r_rt])
                ohb = oh.unsqueeze(2).to_broadcast([128, 4, 8])
                yield
                tt("dve", selA[:, gt, :].rearrange("p (g e) -> p g e", g=4), ohb,
                   mskA.unsqueeze(1).to_broadcast([128, 4, 8]), ALU.mult, R, [r_sel])
                yield
                tt("dve", Mf[:].rearrange("p (g e) -> p g e", g=4), ohb,
                   msk2.unsqueeze(1).to_broadcast([128, 4, 8]), ALU.mult, R, [r_M])
                yield
                tt("dve", selB[:, gt, :], Mf[:], selA[:, gt, :], ALU.subtract, [r_M, r_sel], [r_sel])
                yield
                ts("dve", gAB[:, gt, :], top8[:, 0:2], den, None, ALU.mult, None, R, [r_gAB])
                yield
                cp("dve", Mb[:], Mf[:], [r_M], [r_M])
                yield
                mm(banks[6][:, 128:160], Lst[:], Mb[:], True, True, [r_Lst, r_M], [rb[6]])
                mm(banks[6][:, 160:192], ones128[:], Mb[:], True, True, [r_Lst, r_M], [rb[6]])
                tt("dve", rank_all[:, gt, :], banks[6][:, 128:160], carryc[:], ALU.add, [rb[6], r_carryc], [r_rank])
                tt("dve", carryc[:], banks[6][:, 160:192], carryc[:], ALU.add, [rb[6], r_carryc], [r_carryc])

        FILL = int(os.environ.get("FILL", "0"))
        fcount = [0]

        def wrap(gt):
            g = tile_body(gt)
            while True:
                set_parity(gt)
                try:
                    next(g)
                except StopIteration:
                    return
                fcount[0] += 1
                if FILL and fcount[0] % FILL == 0:
                    S.op("pe", lambda e: e.matmul(banks[6][:, 192:512], lhsT=idb[:], rhs=w_out[:, 0, 0:320],
                                                  start=True, stop=True, skip_group_check=True), [], [])
                yield

        interleave((wrap(gt) for gt in range(NT)), NSETS, admit_every=N_SKEW)
        set_parity(0)

        S.barrier()
        top_c = Cn.cur
        Cn.cur = mark2
        thr_b, r_thr = bcast(Cn, "thr_b", thr_d, 32)
        iota_b, r_iota = bcast(Cn, "iota_b", iota_d, NTILE)
        pidx, r_pidx = colvec(Cn, "pidx", pidx_d, 1)
        onesf = Cn.alloc([128, 32], F32, "onesf"); r_onesf = Res()
        memset("pool", onesf[:], 1.0, [r_onesf])
        ntile = Cn.alloc([128, 32], F32, "ntile"); endc = Cn.alloc([128, 32], F32, "endc")
        startT = Cn.alloc([128, 32], F32, "startT"); r_bk = Res()
        cmpi = Cn.alloc([128, NTILE, 32], F32, "cmpi"); r_cmpi = Res()
        tef = Cn.alloc([128, NTILE], F32, "tef"); r_tef = Res()
        posf = Cn.alloc([128, 2, NT], F32, "posf"); r_posf = Res()
        cmp3t = Cn.alloc([128, 1024], F32, "cmp3t"); r_tmpf = Res()
        cmp3 = cmp3t[:].rearrange("p (e m) -> p e m", e=32)
        assert Cn.cur <= top_c
        tt("dve", cmp3, carryc[:].unsqueeze(2).to_broadcast([128, 32, 32]),
           thr_b[:].unsqueeze(1).to_broadcast([128, 32, 32]), ALU.is_gt, [r_carryc, r_thr], [r_tmpf])
        S.op("dve", lambda e: e.tensor_reduce(out=ntile[:], in_=cmp3, axis=AX.X, op=ALU.add), [r_tmpf], [r_bk])
        S.op("dve", lambda e: e.tensor_tensor_scan(out=endc[:], data0=onesf[:], data1=ntile[:], initial=0.0,
                                                   op0=ALU.mult, op1=ALU.add), [r_bk, r_onesf], [r_bk])
        tt("dve", startT[:], endc[:], ntile[:], ALU.subtract, [r_bk], [r_bk])
        ts("dve", startT[:], startT[:], float(T_SL), None, ALU.mult, None, [r_bk], [r_bk])
        tt("dve", cmpi[:], iota_b[:].unsqueeze(2).to_broadcast([128, NTILE, 32]),
           endc[:].unsqueeze(1).to_broadcast([128, NTILE, 32]), ALU.is_ge, [r_iota, r_bk], [r_cmpi])
        S.op("dve", lambda e: e.tensor_reduce(out=tef[:], in_=cmpi[:], axis=AX.X, op=ALU.add), [r_cmpi], [r_tef])
        ts("dve", tef[:], tef[:], float(NE - 1), None, ALU.min, None, [r_tef], [r_tef])
        ts("dve", tef[:], tef[:], 128.0, pidx[:, 0:1], ALU.mult, ALU.add, [r_tef, r_pidx], [r_tef])
        cp("dve", widx[:], tef[:], [r_tef], [r_widx])
        tt("dve", rank_all[:], rank_all[:], startT[:].unsqueeze(1).to_broadcast([128, NT, 32]), ALU.add,
           [r_rank, r_bk], [r_rank])
        tt("dve", selA[:], selA[:], rank_all[:], ALU.mult, [r_sel, r_rank], [r_sel])
        tt("dve", selB[:], selB[:], rank_all[:], ALU.mult, [r_sel, r_rank], [r_sel])
        S.op("dve", lambda e: e.tensor_reduce(out=posf[:, 0, :], in_=selA[:], axis=AX.X, op=ALU.add), [r_sel], [r_posf])
        S.op("dve", lambda e: e.tensor_reduce(out=posf[:, 1, :], in_=selB[:], axis=AX.X, op=ALU.add), [r_sel], [r_posf])
        cp("dve", pos_i[:], posf[:], [r_posf], [r_pos])
        if dbg:
            dma("sp", DBG_widx, widx[:], [r_widx], [])
            dma("sp", DBG_pos, pos_i[:], [r_pos], [])
            dma("sp", DBG_gab, gAB[:], [r_gAB], [])
        S.barrier()
        Cn.cur = mark2
        if moe_stop == "C":
            return [dma("sp", out_d[0:128, :], x_d[0:128, :])]

        hsb = [Cn.alloc([128, 1024], BF16, "hsb") for _ in range(2)]; r_hsb = [Res() for _ in range(2)]
        for gt in range(NT):
            q = gt % 2
            dma("sp", hsb[q][:], H3[gt * 128:(gt + 1) * 128, :], [r_H3[gt]], [r_hsb[q]])
            for j in range(2):
                S.op("pool", lambda e, q=q, gt=gt, j=j: e.indirect_dma_start(
                    out=Hs[:, :], out_offset=bass.IndirectOffsetOnAxis(ap=pos_i[:, j, gt:gt + 1], axis=0),
                    in_=hsb[q][:, :], in_offset=None), [r_hsb[q], r_pos], [Res()], dma=True)
        S.barrier()
        if moe_stop == "S":
            return [dma("sp", out_d[0:128, :], x_d[0:128, :])]

        Wgu2 = [Cn.alloc([128, 4096], BF16, "Wgu2") for _ in range(2)]; r_Wgu2 = [Res() for _ in range(2)]
        Wd2 = [Cn.alloc([128, 2048], BF16, "Wd2") for _ in range(2)]; r_Wd2 = [Res() for _ in range(2)]
        hst = [Cn.alloc([128, 1024], BF16, "hst") for _ in range(2)]; r_hst = [Res() for _ in range(2)]
        hTs = [Cn.alloc([128, 8, 128], BF16, "hTs") for _ in range(2)]; r_hTs = [Res() for _ in range(2)]
        sgt = [Cn.alloc([128, 256], F32, "sgt") for _ in range(2)]; r_sgt = [Res() for _ in range(2)]
        het = [Cn.alloc([128, 256], BF16, "het") for _ in range(2)]; r_het = [Res() for _ in range(2)]
        heT = [Cn.alloc([128, 2, 128], BF16, "heT") for _ in range(2)]; r_heT = [Res() for _ in range(2)]
        yst = [Cn.alloc([128, 1024], BF16, "yst") for _ in range(2)]; r_yst = [Res() for _ in range(2)]
        NWB = 3
        NSET = 4
        for lst_, shape, dt_, nm in ((Wgu2, [128, 4096], BF16, "Wgu2"), (Wd2, [128, 2048], BF16, "Wd2")):
            while len(lst_) < NWB:
                lst_.append(Cn.alloc(shape, dt_, nm))
        r_Wgu2 = [Res() for _ in range(NWB)]; r_Wd2 = [Res() for _ in range(NWB)]
        for lst_, shape, dt_, nm in ((hst, [128, 1024], BF16, "hst"), (hTs, [128, 8, 128], BF16, "hTs"),
                                     (sgt, [128, 256], F32, "sgt"), (het, [128, 256], BF16, "het"),
                                     (heT, [128, 2, 128], BF16, "heT"), (yst, [128, 1024], BF16, "yst")):
            while len(lst_) < NSET:
                lst_.append(Cn.alloc(shape, dt_, nm))
        r_hst = [Res() for _ in range(NSET)]; r_hTs = [Res() for _ in range(NSET)]; r_sgt = [Res() for _ in range(NSET)]
        r_het = [Res() for _ in range(NSET)]; r_heT = [Res() for _ in range(NSET)]; r_yst = [Res() for _ in range(NSET)]

        def sub_gen(i, sub, q):
            p = i % NWB
            if sub == 0:
                S.op("pool", lambda e, p=p, i=i: e.indirect_dma_start(
                    out=Wgu2[p][:, :], out_offset=None, in_=WGUv,
                    in_offset=bass.IndirectOffsetOnAxis(ap=widx[:, i:i + 1], axis=0)), [r_widx], [r_Wgu2[p]], dma=True)
                S.op("pool", lambda e, p=p, i=i: e.indirect_dma_start(
                    out=Wd2[p][:, :], out_offset=None, in_=WDv,
                    in_offset=bass.IndirectOffsetOnAxis(ap=widx[:, i:i + 1], axis=0)), [r_widx], [r_Wd2[p]], dma=True)
            r0 = i * T_SL + sub * 128
            dma("sp", hst[q][:], Hs[r0:r0 + 128, :], [], [r_hst[q]])
            yield
            transpose8(hst[q], r_hst[q], hTs[q][:], r_hTs[q])
            yield
            gb = q
            for k in range(8):
                mm(banks[gb][:], hTs[q][:, k, :], Wgu2[p][:, k * 512:(k + 1) * 512], k == 0, k == 7,
                   [r_hTs[q], r_Wgu2[p]], [rb[gb]])
            yield
            act(sgt[q][:], banks[gb][:, 0:256], AF.Silu, [rb[gb]], [r_sgt[q]])
            yield
            tt("dve", het[q][:], sgt[q][:], banks[gb][:, 256:512], ALU.mult, [r_sgt[q], rb[gb]], [r_het[q]])
            yield
            transpose8(het[q], r_het[q], heT[q][:], r_heT[q], n=2)
            yield
            yb = (4, 5)
            for half in range(2):
                for c in range(2):
                    mm(banks[yb[half]][:], heT[q][:, c, :], Wd2[p][:, c * 1024 + half * 512:c * 1024 + (half + 1) * 512],
                       c == 0, c == 1, [r_heT[q], r_Wd2[p]], [rb[yb[half]]])
            cp("act", yst[q][:, 0:512], banks[yb[0]][:], [rb[yb[0]]], [r_yst[q]])
            cp("dve", yst[q][:, 512:1024], banks[yb[1]][:], [rb[yb[1]]], [r_yst[q]])
            yield
            dma("sp", Ys[r0:r0 + 128, :], yst[q][:], [r_yst[q]], [Res()])

        def all_subs():
            cnt = 0
            for i in range(NTILE):
                for sub in range(T_SL // 128):
                    yield sub_gen(i, sub, cnt % NSET)
                    cnt += 1

        interleave(all_subs(), NSET)
        S.barrier()
        if moe_stop == "E":
            return [dma("sp", out_d[0:128, :], x_d[0:128, :])]

        xo = [Cn.alloc([128, 1024], F32, "xo") for _ in range(2)]; r_xo = [Res() for _ in range(2)]
        yA = [Cn.alloc([128, 1024], BF16, "yA") for _ in range(2)]; r_yA = [Res() for _ in range(2)]
        yB = [Cn.alloc([128, 1024], BF16, "yB") for _ in range(2)]; r_yB = [Res() for _ in range(2)]
        print("phase E/F SBUF used", Cn.cur, "of", SB_HI)
        outs = []
        for gt in range(NT):
            q = gt % 2
            rows = slice(gt * 128, (gt + 1) * 128)
            dma("sp", xo[q][:], out_d[rows, :], [r_out[gt]], [r_xo[q]])
            for j, (yy, r_yy) in enumerate(((yA, r_yA), (yB, r_yB))):
                S.op("pool", lambda e, q=q, gt=gt, j=j, yy=yy: e.indirect_dma_start(
                    out=yy[q][:, :], out_offset=None, in_=Ys[:, :],
                    in_offset=bass.IndirectOffsetOnAxis(ap=pos_i[:, j, gt:gt + 1], axis=0)), [r_pos], [r_yy[q]], dma=True)
            stt("dve", xo[q][:], yA[q][:], gAB[:, gt, 0:1], xo[q][:], ALU.mult, ALU.add, [r_yA[q], r_gAB, r_xo[q]], [r_xo[q]])
            stt("dve", xo[q][:], yB[q][:], gAB[:, gt, 1:2], xo[q][:], ALU.mult, ALU.add, [r_yB[q], r_gAB, r_xo[q]], [r_xo[q]])
            outs.append(dma("sp", out_d[rows, :], xo[q][:], [r_xo[q]], [r_out[gt]]))
        return outs

    if "A" in phases:
        phaseA()
    S.barrier()
    if "B" in phases:
        phaseB()
    S.barrier()
    S.barrier()
    outs = []
    if "D" in phases:
        outs = phaseCD()
    else:
        outs = [dma("sp", out_d[0:128, :], x_d[0:128, :])]
    return nc, S, outs


def host_consts(S_len):
    pos = np.arange(S_len, dtype=np.float32)
    inv_freq = (np.float32(10000.0) ** (-np.arange(0, 32, 2, dtype=np.float32) / np.float32(32))).astype(np.float32)
    ang = (pos[:, None] * inv_freq[None, :]).astype(np.float32)
    c, s = np.cos(ang).astype(np.float32), np.sin(ang).astype(np.float32)
    cs = np.concatenate([c, c, -s, s], axis=1).astype(np.float32)
    ntile = (2 * S_len) // 256 + NE
    lst = np.triu(np.ones((128, 128), np.float32), 1).astype(ml_dtypes.bfloat16)
    return {"cs_tab": cs, "ident_bf": np.eye(128).astype(ml_dtypes.bfloat16), "ident_f32": np.eye(128, dtype=np.float32),
            "lstrict": lst, "thr_tab": (np.arange(32) * 256).astype(np.float32),
            "iota_tab": np.arange(ntile).astype(np.float32), "pidx_tab": np.arange(128).astype(np.float32)}


_CACHE = {}


def kernel(**inputs):
    x = np.asarray(inputs["x"], dtype=np.float32)
    B, S_len, _ = x.shape
    if S_len not in _CACHE:
        nc, S, outs = build(S_len)
        S.emit(final_waits=outs)
        _CACHE[S_len] = nc
    nc = _CACHE[S_len]
    consts = host_consts(S_len)
    wts = {n: np.ascontiguousarray(np.asarray(inputs[n], dtype=np.float32)[0]) for n in WEIGHT_NAMES}
    mem = np.asarray(inputs["mem"], dtype=np.float32)
    in_maps = []
    for b in range(B):
        m = {"x": np.ascontiguousarray(x[b]), "mem": np.ascontiguousarray(mem[b])}
        m.update(wts)
        m.update(consts)
        in_maps.append(m)
    res = run_bass_kernel_spmd(nc, in_maps, core_ids=list(range(B)))
    return np.stack([np.asarray(r["out"], dtype=np.float32) for r in res.results], axis=0)
```

```python
import os
import numpy as np
import ml_dtypes
import concourse.bass as bass
import concourse.mybir as mybir
from concourse.bass_utils import run_bass_kernel_spmd

F32 = mybir.dt.float32
BF16 = mybir.dt.bfloat16
AF = mybir.ActivationFunctionType
ALU = mybir.AluOpType
AX = mybir.AxisListType

ENGS = ("pe", "act", "dve", "pool", "sp")
EPS = 1e-6
D = 1024
NH = 8
DQK = 96
NE = 32
DE = 256
SB_LO = 16640
SB_HI = 228864


class Res:
    __slots__ = ("name", "w", "r")

    def __init__(self, name=""):
        self.name = name
        self.w = None
        self.r = []


class Op:
    __slots__ = ("eng", "fn", "deps", "isdma", "sem", "val", "needs_inc")

    def __init__(self, eng, fn, isdma):
        self.eng = eng
        self.fn = fn
        self.deps = []
        self.isdma = isdma
        self.sem = None
        self.val = None
        self.needs_inc = False


class Sched:
    def __init__(self, nc):
        self.nc = nc
        self.ops = {e: [] for e in ENGS}
        self.nd = {"sp": 24, "pool": 12, "act": 4}
        self.dma_rr = {e: 0 for e in self.nd}
        self.dma_last = {e: [None] * n for e, n in self.nd.items()}
        self.dma_cnt = {e: [0] * n for e, n in self.nd.items()}
        self.last = {e: None for e in ENGS}

    def op(self, eng, fn, reads=(), writes=(), dma=False, extra=()):
        o = Op(eng, fn, dma)
        deps = list(extra)
        for r in reads:
            if r.w is not None:
                deps.append(r.w)
        for w in writes:
            if w.w is not None:
                deps.append(w.w)
            deps.extend(w.r)
        if dma:
            slot = self.dma_rr[eng]
            self.dma_rr[eng] = (slot + 1) % self.nd[eng]
            prev = self.dma_last[eng][slot]
            if prev is not None:
                deps.append(prev)
            self.dma_last[eng][slot] = o
            self.dma_cnt[eng][slot] += 1
            o.sem = ("dma", eng, slot)
            o.val = 16 * self.dma_cnt[eng][slot]
        seen = set()
        for d in deps:
            if d is None or d is o or id(d) in seen:
                continue
            seen.add(id(d))
            if d.eng == "pe" and eng == "pe" and not d.isdma and not dma:
                continue
            o.deps.append(d)
            if not d.isdma:
                d.needs_inc = True
        for r in reads:
            if not dma:
                r.r = [x for x in r.r if x.isdma or x.eng != eng]
            r.r.append(o)
        for w in writes:
            w.w = o
            w.r = []
        self.ops[eng].append(o)
        if not dma:
            self.last[eng] = o
        return o

    def barrier(self):
        deps = [self.last[e] for e in ENGS if self.last[e] is not None]
        for e in self.nd:
            deps.extend(x for x in self.dma_last[e] if x is not None)
        for e in ENGS:
            self.op(e, lambda eng: eng.nop(), extra=deps)

    def emit(self, final_waits=()):
        nc = self.nc
        esem = {e: nc.alloc_semaphore(f"s_{e}") for e in ENGS}
        dsem = {e: [nc.alloc_semaphore(f"d_{e}{i}") for i in range(n)] for e, n in self.nd.items()}
        for e in ENGS:
            c = 0
            for o in self.ops[e]:
                if o.isdma:
                    o.sem = dsem[o.sem[1]][o.sem[2]]
                elif o.needs_inc:
                    c += 1
                    o.sem = esem[e]
                    o.val = c
        emap = {"pe": "tensor", "act": "scalar", "dve": "vector", "pool": "gpsimd", "sp": "sync"}

        def run(e, engobj):
            known = {}
            for o in self.ops[e]:
                need = {}
                for d in o.deps:
                    k = d.sem.num
                    if k not in need or need[k][1] < d.val:
                        need[k] = (d.sem, d.val)
                for k, (s, v) in need.items():
                    if known.get(k, 0) >= v:
                        continue
                    engobj.wait_ge(s, v)
                    known[k] = v
                ins = o.fn(engobj)
                if o.isdma:
                    ins.then_inc(o.sem, 16)
                elif o.needs_inc:
                    ins.then_inc(o.sem, 1)
            if e == "sp":
                for d in final_waits:
                    engobj.wait_ge(d.sem, d.val)

        with nc.Block() as block:
            for e in ENGS:
                getattr(block, emap[e])(lambda engobj, e=e: run(e, engobj))


class Sel:
    REG = []

    def __init__(self, bufs):
        self.bufs, self.i = bufs, 0
        Sel.REG.append(self)

    def __getitem__(self, k):
        return self.bufs[self.i][k]


class RSel:
    def __init__(self, n):
        self.rs, self.i = [Res() for _ in range(n)], 0
        Sel.REG.append(self)

    @property
    def w(self):
        return self.rs[self.i].w

    @w.setter
    def w(self, v):
        self.rs[self.i].w = v

    @property
    def r(self):
        return self.rs[self.i].r

    @r.setter
    def r(self, v):
        self.rs[self.i].r = v


def set_parity(p):
    if os.environ.get("NO_PAR"):
        p = 0
    for x in Sel.REG:
        x.i = p % len(x.bufs if isinstance(x, Sel) else x.rs)


def interleave(gen_iter, ways, admit_every=0):
    active = []
    gen_iter = iter(gen_iter)
    done = False
    rnd = 0
    last_admit = -10 ** 9
    while active or not done:
        while len(active) < ways and not done and (not active or rnd - last_admit >= admit_every):
            try:
                active.append(next(gen_iter))
                last_admit = rnd
            except StopIteration:
                done = True
        rnd += 1
        for g in list(active):
            try:
                next(g)
            except StopIteration:
                active.remove(g)


class Arena:
    def __init__(self, nc, lo, hi):
        self.nc, self.lo, self.hi, self.cur, self.n = nc, lo, hi, lo, 0

    def alloc(self, shape, dtype, name=None):
        nbytes = int(np.prod(shape[1:])) * (2 if dtype == BF16 else 4)
        off = (self.cur + 31) // 32 * 32
        assert off + nbytes <= self.hi, f"SBUF arena overflow {off + nbytes} > {self.hi} ({name})"
        self.cur = off + nbytes
        self.n += 1
        return self.nc.alloc_sbuf_tensor_at(f"{name or 't'}_{off}_{self.n}", list(shape), dtype, offset=off)


WEIGHT_NAMES = ["g_mix", "w_in", "g_cq", "w_uq", "g_ckv", "w_ukv", "g_qn", "g_kn", "conv_w", "conv_b",
                "w_rg", "b_rg", "w_ig", "b_ig", "lam", "g_attn_out", "g_rnn_out", "w_out", "g_xq", "g_mem",
                "w_mq", "w_mk", "w_mv", "g_mqn", "g_mkn", "w_mo", "g_ffn", "w_group", "b_group", "w_expert",
                "b_expert", "w_e_gate", "w_e_up", "w_e_down"]
WEIGHT_SHAPES = {
    "g_mix": [D], "w_in": [D, 1440], "g_cq": [256], "w_uq": [256, 768], "g_ckv": [128], "w_ukv": [128, 1024],
    "g_qn": [96], "g_kn": [96], "conv_w": [4, 512], "conv_b": [512], "w_rg": [8, 64, 64], "b_rg": [512],
    "w_ig": [8, 64, 64], "b_ig": [512], "lam": [512], "g_attn_out": [512], "g_rnn_out": [512], "w_out": [D, D],
    "g_xq": [D], "g_mem": [D], "w_mq": [D, D], "w_mk": [D, D], "w_mv": [D, D], "g_mqn": [256], "g_mkn": [256],
    "w_mo": [D, D], "g_ffn": [D], "w_group": [D, 4], "b_group": [4], "w_expert": [D, 32], "b_expert": [32],
    "w_e_gate": [NE, D, DE], "w_e_up": [NE, D, DE], "w_e_down": [NE, DE, D]}


def build(S_len, phases="ABCD", dbg=False, moe_stop="F"):
    NT = S_len // 128
    NG = S_len // 512
    nc = bass.Bass("TRN2", target_bir_lowering=False)
    x_d = nc.dram_tensor("x", [S_len, D], F32, kind="ExternalInput").ap()
    mem_d = nc.dram_tensor("mem", [256, D], F32, kind="ExternalInput").ap()
    W = {n: nc.dram_tensor(n, WEIGHT_SHAPES[n], F32, kind="ExternalInput").ap() for n in WEIGHT_NAMES}
    cs_d = nc.dram_tensor("cs_tab", [S_len, 64], F32, kind="ExternalInput").ap()
    idb_d = nc.dram_tensor("ident_bf", [128, 128], BF16, kind="ExternalInput").ap()
    idf_d = nc.dram_tensor("ident_f32", [128, 128], F32, kind="ExternalInput").ap()
    out_d = nc.dram_tensor("out", [S_len, D], F32, kind="ExternalOutput").ap()
    skind = "ExternalOutput" if dbg else "Internal"
    QT = nc.dram_tensor("QT", [NH, DQK, S_len], BF16, kind=skind).ap()
    KT = nc.dram_tensor("KT", [NH, DQK, S_len], BF16, kind=skind).ap()
    VA = nc.dram_tensor("VA", [NH, 128, NT, 65], BF16, kind=skind).ap()
    ORT = nc.dram_tensor("ORT", [4, 128, S_len], BF16, kind=skind).ap()
    OA = nc.dram_tensor("OA", [S_len, 512], BF16, kind=skind).ap()
    RR = nc.dram_tensor("RR", [128, NT], F32, kind=skind).ap()
    WGU2 = nc.dram_tensor("WGU2", [NE, 128, 8, 2 * DE], BF16).ap()
    WD2 = nc.dram_tensor("WD2", [NE, 128, 2, D], BF16).ap()
    NSLOT = ((2 * S_len) // 256 + NE) * 256
    H3 = nc.dram_tensor("H3", [S_len, D], BF16).ap()
    Hs = nc.dram_tensor("Hs", [NSLOT, D], BF16).ap()
    Ys = nc.dram_tensor("Ys", [NSLOT, D], BF16).ap()
    lst_d = nc.dram_tensor("lstrict", [128, 128], BF16, kind="ExternalInput").ap()
    thr_d = nc.dram_tensor("thr_tab", [32], F32, kind="ExternalInput").ap()
    iota_d = nc.dram_tensor("iota_tab", [NSLOT // 256], F32, kind="ExternalInput").ap()
    pidx_d = nc.dram_tensor("pidx_tab", [128], F32, kind="ExternalInput").ap()
    if dbg:
        DBG_widx = nc.dram_tensor("DBG_widx", [128, NSLOT // 256], mybir.dt.int32, kind="ExternalOutput").ap()
        DBG_pos = nc.dram_tensor("DBG_pos", [128, 2, NT], mybir.dt.int32, kind="ExternalOutput").ap()
        DBG_gab = nc.dram_tensor("DBG_gab", [128, NT, 2], F32, kind="ExternalOutput").ap()

    S = Sched(nc)
    P = Arena(nc, SB_LO, SB_LO + 6144)
    pairs = [nc.alloc_psum_tensor(f"pair{j}", [128, 1024], F32) for j in range(4)]
    banks = [pairs[i // 2][:, (i % 2) * 512:(i % 2 + 1) * 512] for i in range(8)]
    rb = [Res(f"bank{i}") for i in range(8)]

    def bview(i):
        return pairs[i // 2][:].bitcast(BF16)[:, (i % 2) * 1024:(i % 2 + 1) * 1024]

    def dma(q, out, in_, reads=(), writes=()):
        return S.op(q, lambda e: e.dma_start(out=out, in_=in_), reads=reads, writes=writes, dma=True)

    def dma_nc(q, out, in_, reads=(), writes=()):
        def f(e):
            with nc.allow_non_contiguous_dma(reason="small param vectors"):
                return e.dma_start(out=out, in_=in_)
        return S.op(q, f, reads=reads, writes=writes, dma=True)

    def mm(out, lhsT, rhs, start, stop, reads, writes, skip=False):
        return S.op("pe", lambda e: e.matmul(out, lhsT=lhsT, rhs=rhs, start=start, stop=stop,
                                             skip_group_check=skip), reads, writes)

    def act(out, in_, func, reads, writes, scale=1.0, bias=None, accum=None):
        kw = {}
        if bias is not None:
            kw["bias"] = bias
        if accum is not None:
            kw["accum_out"] = accum
        return S.op("act", lambda e: e.activation(out=out, in_=in_, func=func, scale=scale, **kw), reads, writes)

    def tt(eng, out, in0, in1, op, reads, writes):
        return S.op(eng, lambda e: e.tensor_tensor(out=out, in0=in0, in1=in1, op=op), reads, writes)

    def ts(eng, out, in0, s1, s2, op0, op1, reads, writes):
        if s2 is None:
            return S.op(eng, lambda e: e.tensor_scalar(out=out, in0=in0, scalar1=s1, scalar2=None, op0=op0), reads, writes)
        return S.op(eng, lambda e: e.tensor_scalar(out=out, in0=in0, scalar1=s1, scalar2=s2, op0=op0, op1=op1), reads, writes)

    def stt(eng, out, in0, scalar, in1, op0, op1, reads, writes):
        return S.op(eng, lambda e: e.scalar_tensor_tensor(out=out, in0=in0, scalar=scalar, in1=in1, op0=op0, op1=op1),
                    reads, writes)

    def cp(eng, out, in_, reads, writes):
        if eng == "act":
            return act(out, in_, AF.Copy, reads, writes)
        return S.op(eng, lambda e: e.tensor_copy(out=out, in_=in_), reads, writes)

    def red(out, in_, op, reads, writes):
        return S.op("dve", lambda e: e.tensor_reduce(out=out, in_=in_, axis=AX.X, op=op), reads, writes)

    def recip(out, in_, reads, writes):
        return S.op("dve", lambda e: e.reciprocal(out=out, in_=in_), reads, writes)

    def memset(eng, ap, val, writes):
        return S.op(eng, lambda e: e.memset(ap, val), (), writes)

    idb = P.alloc([128, 128], BF16, "idb"); r_idb = Res()
    idf = P.alloc([128, 128], F32, "idf"); r_idf = Res()
    ones = P.alloc([128, 2], BF16, "ones"); r_ones = Res()
    cneg = P.alloc([128, 1], F32, "cneg"); chalf = P.alloc([128, 1], F32, "chalf"); r_c = Res()
    rr_all = P.alloc([128, max(NT, 8)], F32, "rr_all"); r_rr = Res()
    dma("sp", idb[:], idb_d, writes=[r_idb])
    dma("sp", idf[:], idf_d, writes=[r_idf])
    memset("pool", ones[:], 1.0, [r_ones])
    memset("pool", cneg[:], -0.5, [r_c])
    memset("pool", chalf[:], 0.5, [r_c])
    cone = P.alloc([128, 1], F32, "cone")
    memset("pool", cone[:], 1.0, [r_c])

    def transpose(out, in_, reads, writes, f32=False):
        idt, rid = (idf, r_idf) if f32 else (idb, r_idb)
        return S.op("pe", lambda e: e.transpose(out=out, in_=in_, identity=idt[:]), list(reads) + [rid], writes)

    def rstd_chain(buf, n, inv_dim, reads_writes):
        ts("dve", buf, buf, inv_dim, EPS, ALU.mult, ALU.add, reads_writes, reads_writes)
        act(buf, buf, AF.Ln, reads_writes, reads_writes)
        act(buf, buf, AF.Exp, reads_writes, reads_writes, scale=-0.5)

    def colvec(arena, name, src, ncol):
        t = arena.alloc([128, ncol], F32, name)
        r = Res(name)
        dma_nc("sp", t[:], src.rearrange("(c p) -> p c", p=128), writes=[r])
        return t, r

    def bcast(arena, name, src, n):
        t = arena.alloc([128, n], F32, name)
        r = Res(name)
        dma("sp", t[:], src.partition_broadcast(128), writes=[r])
        return t, r

    r_wgu = [Res() for _ in range(NE)]
    r_wd = [Res() for _ in range(NE)]

    def convert_experts(e0, e1):
        for e in range(e0, min(e1, NE)):
            wg = WGU2[e].rearrange("p k n -> k p n")
            dma("pool", wg[:, :, 0:DE], W["w_e_gate"][e].rearrange("(k p) n -> k p n", p=128), writes=[r_wgu[e]])
            dma("pool", wg[:, :, DE:2 * DE], W["w_e_up"][e].rearrange("(k p) n -> k p n", p=128), writes=[r_wgu[e]])
            dma("pool", WD2[e].rearrange("p c n -> c p n"), W["w_e_down"][e].rearrange("(c p) n -> c p n", p=128),
                writes=[r_wd[e]])

    r_QT, r_KT, r_VA, r_ORT, r_OA = Res(), Res(), Res(), Res(), Res()

    def phaseA():
        A = Arena(nc, SB_LO + 6144, SB_HI)
        w_in = A.alloc([128, 8, 1440], BF16, "w_in"); r_win = Res()
        dma("pool", w_in[:], W["w_in"].rearrange("(k p) n -> p k n", p=128), writes=[r_win])
        w_uq = A.alloc([128, 2, 768], BF16, "w_uq"); r_wuq = Res()
        dma("pool", w_uq[:], W["w_uq"].rearrange("(k p) n -> p k n", p=128), writes=[r_wuq])
        w_ukv = A.alloc([128, 1024], BF16, "w_ukv"); r_wukv = Res()
        dma("pool", w_ukv[:], W["w_ukv"], writes=[r_wukv])
        wrg = A.alloc([128, 4, 128], BF16, "wrg"); wig = A.alloc([128, 4, 128], BF16, "wig"); r_wg = Res()
        memset("pool", wrg[:], 0.0, [r_wg])
        memset("pool", wig[:], 0.0, [r_wg])
        for c in range(4):
            for half in range(2):
                sl = slice(half * 64, half * 64 + 64)
                dma("pool", wrg[sl, c, sl], W["w_rg"][2 * c + half], writes=[r_wg])
                dma("pool", wig[sl, c, sl], W["w_ig"][2 * c + half], writes=[r_wg])
        gmix_b, r_gmix = bcast(A, "gmix_b", W["g_mix"], 1024)
        gq_b, r_gq = bcast(A, "gq_b", W["g_qn"], 96)
        gk_b, r_gk = bcast(A, "gk_b", W["g_kn"], 96)
        ts("dve", gq_b[:], gq_b[:], float(DQK) ** -0.5, None, ALU.mult, None, [r_gq], [r_gq])
        gcq, r_gcq = colvec(A, "gcq", W["g_cq"], 2)
        gckv, r_gckv = colvec(A, "gckv", W["g_ckv"], 1)
        cb, r_cb = colvec(A, "cb", W["conv_b"], 4)
        brg, r_brg = colvec(A, "brg", W["b_rg"], 4)
        big, r_big = colvec(A, "big", W["b_ig"], 4)
        lam, r_lam = colvec(A, "lam", W["lam"], 4)
        cw = A.alloc([128, 4, 4], F32, "cw"); r_cw = Res()
        for j in range(4):
            dma_nc("sp", cw[:, :, j], W["conv_w"][j].rearrange("(c p) -> p c", p=128), writes=[r_cw])
        c1 = A.alloc([128, 4], F32, "c1"); c2 = A.alloc([128, 4], F32, "c2")
        zt = A.alloc([128, 4], F32, "zt"); wv = A.alloc([128, 4], F32, "wv"); w2 = A.alloc([128, 4], F32, "w2")
        r_c12 = Res(); r_z = Res()
        act(zt[:], lam[:], AF.Exp, [r_lam], [r_z], scale=-1.0)
        ts("dve", wv[:], zt[:], 2.0, None, ALU.add, None, [r_z], [r_z])
        S.op("dve", lambda e: e.reciprocal(out=wv[:], in_=wv[:]), [r_z], [r_z])
        tt("dve", wv[:], wv[:], zt[:], ALU.mult, [r_z], [r_z])
        tt("dve", w2[:], wv[:], wv[:], ALU.mult, [r_z], [r_z])
        ts("dve", zt[:], w2[:], 1.0 / 9, 1.0 / 7, ALU.mult, ALU.add, [r_z], [r_z])
        tt("dve", zt[:], zt[:], w2[:], ALU.mult, [r_z], [r_z])
        ts("dve", zt[:], zt[:], 1.0 / 5, None, ALU.add, None, [r_z], [r_z])
        tt("dve", zt[:], zt[:], w2[:], ALU.mult, [r_z], [r_z])
        ts("dve", zt[:], zt[:], 1.0 / 3, None, ALU.add, None, [r_z], [r_z])
        tt("dve", zt[:], zt[:], w2[:], ALU.mult, [r_z], [r_z])
        ts("dve", zt[:], zt[:], 1.0, None, ALU.add, None, [r_z], [r_z])
        tt("dve", zt[:], zt[:], wv[:], ALU.mult, [r_z], [r_z])
        ts("dve", c1[:], zt[:], -16.0, None, ALU.mult, None, [r_z], [r_c12])
        ts("dve", c2[:], zt[:], -32.0, None, ALU.mult, None, [r_z], [r_c12])

        xb = [A.alloc([128, 1024], F32, "xb") for _ in range(4)]; r_xb = [Res() for _ in range(4)]
        junk = A.alloc([128, 1024], BF16, "junk"); r_junk = Res()
        ssx = A.alloc([128, 4], F32, "ssx"); r_ssx = Res()
        hb = [A.alloc([128, 1024], BF16, "hb") for _ in range(2)]; r_hb = [Res() for _ in range(2)]
        hT = [A.alloc([128, 8, 512], BF16, "hT") for _ in range(2)]; r_hT = [Res() for _ in range(2)]
        csb = [A.alloc([128, 4, 64], F32, "csb") for _ in range(2)]; r_cs = [Res() for _ in range(2)]
        cqT = A.alloc([128, 2, 512], BF16, "cqT"); r_cqT = Res()
        ckvT = A.alloc([128, 512], BF16, "ckvT"); r_ckvT = Res()
        sqc = A.alloc([128, 3, 512], BF16, "sqc"); r_sqc = Res()
        sqr = A.alloc([128, 4, 512], BF16, "sqr"); r_sqr = Res()
        ornb = A.alloc([128, 4, 512], BF16, "ornb"); r_ornb = Res()
        uxT = [A.alloc([128, 4, 515], F32, "uxT") for _ in range(2)]; r_ux = [Res() for _ in range(2)]
        memset("pool", uxT[0][:, :, 0:3], 0.0, [r_ux[0]])
        carry = A.alloc([128, 4], F32, "carry"); r_carry = Res()
        memset("pool", carry[:], 0.0, [r_carry])
        def two(name, dt=F32):
            return [A.alloc([128, 512], dt, name) for _ in range(2)], [Res() for _ in range(2)]
        ug, r_ug = two("ug"); gw, r_gw = two("gw"); gel, r_gel = two("gel")
        xc, r_xc = two("xc"); xcb, r_xcb = two("xcb", BF16)
        rg, r_rg = two("rg"); ig, r_ig = two("ig"); av, r_av = two("av"); hs, r_hs = two("hs"); orn, r_orn = two("orn")
        stc = A.alloc([128, 4, 4], F32, "stc"); r_stc = Res()
        qs = A.alloc([128, 8, 96], F32, "qs"); r_qs = Res()
        ks = A.alloc([128, 8, 96], F32, "ks"); r_ks = Res()
        tq = A.alloc([128, 8, 96], F32, "tq"); r_tq = Res()
        tk = A.alloc([128, 8, 96], F32, "tk"); r_tk = Res()
        ssq = A.alloc([128, 16], F32, "ssq"); r_ssq = Res()
        rt1 = A.alloc([128, 8, 32], F32, "rt1"); rt2 = A.alloc([128, 8, 32], F32, "rt2"); r_rt = Res()
        kt1 = A.alloc([128, 8, 32], F32, "kt1"); kt2 = A.alloc([128, 8, 32], F32, "kt2"); r_kt = Res()
        qb = A.alloc([128, 8, 96], BF16, "qb"); r_qb = Res()
        kb = A.alloc([128, 8, 96], BF16, "kb"); r_kb = Res()
        QTst = A.alloc([128, 8, 512], BF16, "QTst"); r_QTst = Res()
        KTst = A.alloc([128, 8, 512], BF16, "KTst"); r_KTst = Res()
        Vst = A.alloc([128, 8, 4, 65], BF16, "Vst"); r_Vst = Res()
        memset("pool", Vst[:], 1.0, [r_Vst])
        print("phase A SBUF used", A.cur)

        ZB = [0, 1]; TB = 2; SB = 3; QB = (4, 5); KVB = (6, 7)
        r_stat_c = Res(); r_stat_r = Res(); r_kr = Res()
        stat_c = banks[SB][:, 0:8].rearrange("p (t c) -> p t c", c=2)
        stat_r = banks[SB][:, 8:12]
        kr_ps = banks[SB][:, 64:192].rearrange("p (t c) -> p t c", c=32)
        zrot = [0]

        def zbank():
            b = ZB[zrot[0] % 2]
            zrot[0] += 1
            return b

        epg = -(-NE // NG)
        for G in range(NG):
            convert_experts(G * epg, (G + 1) * epg)
            hTg, r_hTg = hT[G % 2], r_hT[G % 2]
            cst, r_cst = csb[G % 2], r_cs[G % 2]
            dma("sp", cst[:], cs_d[G * 512:(G + 1) * 512, :].rearrange("(t p) c -> p t c", p=128), writes=[r_cst])
            for t in range(4):
                tok = G * 4 + t
                dma("sp", xb[t][:], x_d[tok * 128:(tok + 1) * 128, :], writes=[r_xb[t]])
                act(junk[:], xb[t][:], AF.Square, [r_xb[t]], [r_junk, r_ssx], accum=ssx[:, t:t + 1])
            rstd_chain(ssx[:], 4, 1.0 / D, [r_ssx])
            for t in range(4):
                h_, r_h = hb[t % 2], r_hb[t % 2]
                stt("dve", h_[:], xb[t][:], ssx[:, t:t + 1], gmix_b[:], ALU.mult, ALU.mult,
                    [r_xb[t], r_ssx, r_gmix], [r_h])
                tv = bview(TB).rearrange("p (k n) -> p k n", k=8)
                for k in range(8):
                    transpose(tv[:, k, :], h_[:, k * 128:(k + 1) * 128], [r_h], [rb[TB]])
                cp("act", hTg[:, :, t * 128:(t + 1) * 128], tv, [rb[TB]], [r_hTg])

            def zmm(col0, ncols, b):
                for k in range(8):
                    mm(banks[b][0:ncols, :], w_in[:, k, col0:col0 + ncols], hTg[:, k, :], k == 0, k == 7,
                       [r_win, r_hTg], [rb[b]])

            for j in range(2):
                b = zbank(); zmm(j * 128, 128, b)
                act(sqc[:, j, :], banks[b][:], AF.Square, [rb[b]], [r_sqc])
                act(cqT[:, j, :], banks[b][:], AF.Copy, [rb[b], r_gcq], [r_cqT], scale=gcq[:, j:j + 1])
            b = zbank(); zmm(256, 128, b)
            act(sqc[:, 2, :], banks[b][:], AF.Square, [rb[b]], [r_sqc])
            act(ckvT[:], banks[b][:], AF.Copy, [rb[b], r_gckv], [r_ckvT], scale=gckv[:, 0:1])
            for t in range(4):
                tsl = slice(t * 128, (t + 1) * 128)
                for j in range(2):
                    mm(stat_c[:, t, 0:1], sqc[:, j, tsl], ones[:, 0:1], j == 0, j == 1, [r_sqc, r_ones], [r_stat_c])
                mm(stat_c[:, t, 1:2], sqc[:, 2, tsl], ones[:, 0:1], True, True, [r_sqc, r_ones], [r_stat_c])
            ts("dve", stc[:, :, 0:1], stat_c[:, :, 0:1], 1.0 / 256, EPS, ALU.mult, ALU.add, [r_stat_c], [r_stc])
            ts("dve", stc[:, :, 1:2], stat_c[:, :, 1:2], 1.0 / 128, EPS, ALU.mult, ALU.add, [r_stat_c], [r_stc])
            stc2 = stc[:, :, 0:2]
            tt("pool", stc2, stc2, cneg[:, 0:1].unsqueeze(2).to_broadcast([128, 4, 2]), ALU.pow, [r_stc, r_c], [r_stc])

            def qk_gen():
                for t in range(4):
                    tsl = slice(t * 128, (t + 1) * 128)
                    qv = [banks[QB[0]][:, 0:384], banks[QB[1]][:, 0:384]]
                    for half in range(2):
                        for j in range(2):
                            mm(qv[half], cqT[:, j, tsl], w_uq[:, j, half * 384:(half + 1) * 384], j == 0, j == 1,
                               [r_cqT, r_wuq], [rb[QB[half]]])
                            yield
                    for half in range(2):
                        mm(banks[KVB[half]][:], ckvT[:, tsl], w_ukv[:, half * 512:(half + 1) * 512], True, True,
                           [r_ckvT, r_wukv], [rb[KVB[half]]])
                        yield
                    for k in range(8):
                        mm(kr_ps[:, t, :], hTg[:, k, tsl], w_in[:, k, 384:416], k == 0, k == 7, [r_hTg, r_win], [r_kr])
                        yield
                    rcq = stc[:, t, 0:1]; rckv = stc[:, t, 1:2]
                    for half in range(2):
                        hs4 = slice(half * 4, half * 4 + 4)
                        act(qs[:, hs4, :], qv[half].rearrange("p (h d) -> p h d", h=4), AF.Copy,
                            [rb[QB[half]], r_stc], [r_qs], scale=rcq)
                        yield
                        kvv = banks[KVB[half]][:].rearrange("p (h d) -> p h d", h=4)
                        act(ks[:, hs4, 0:64], kvv[:, :, 0:64], AF.Copy, [rb[KVB[half]], r_stc], [r_ks], scale=rckv)
                        yield
                        act(Vst[:, hs4, t, 0:64], kvv[:, :, 64:128], AF.Copy, [rb[KVB[half]], r_stc], [r_Vst], scale=rckv)
                        yield
                    cp("dve", ks[:, :, 64:96], kr_ps[:, t, :].unsqueeze(1).to_broadcast([128, 8, 32]), [r_kr], [r_ks])
                    yield
                    act(tq[:], qs[:], AF.Square, [r_qs], [r_tq])
                    yield
                    act(tk[:], ks[:], AF.Square, [r_ks], [r_tk])
                    yield
                    S.op("dve", lambda e: e.tensor_reduce(out=ssq[:, 0:8], in_=tq[:], axis=AX.X, op=ALU.add), [r_tq], [r_ssq])
                    yield
                    S.op("dve", lambda e: e.tensor_reduce(out=ssq[:, 8:16], in_=tk[:], axis=AX.X, op=ALU.add), [r_tk], [r_ssq])
                    yield
                    rstd_chain(ssq[:], 16, 1.0 / DQK, [r_ssq])
                    yield
                    for (src, r_src, tmp, r_tmp, g_b, r_g, o0, t1, t2, r_t, dst, r_dst, st, r_st) in (
                            (qs, r_qs, tq, r_tq, gq_b, r_gq, 0, rt1, rt2, r_rt, qb, r_qb, QTst, r_QTst),
                            (ks, r_ks, tk, r_tk, gk_b, r_gk, 8, kt1, kt2, r_kt, kb, r_kb, KTst, r_KTst)):
                        tt("dve", tmp[:], src[:], ssq[:, o0:o0 + 8].unsqueeze(2).to_broadcast([128, 8, 96]), ALU.mult,
                           [r_src, r_ssq], [r_tmp])
                        yield
                        tt("dve", tmp[:], tmp[:], g_b[:].unsqueeze(1).to_broadcast([128, 8, 96]), ALU.mult,
                           [r_tmp, r_g], [r_tmp])
                        yield
                        c2b = cst[:, t, 0:32].unsqueeze(1).to_broadcast([128, 8, 32])
                        tt("dve", t1[:], tmp[:, :, 64:96], c2b, ALU.mult, [r_tmp, r_cst], [r_t])
                        yield
                        tt("dve", t2[:, :, 0:16], tmp[:, :, 80:96],
                           cst[:, t, 32:48].unsqueeze(1).to_broadcast([128, 8, 16]), ALU.mult, [r_tmp, r_cst], [r_t])
                        yield
                        tt("dve", t2[:, :, 16:32], tmp[:, :, 64:80],
                           cst[:, t, 48:64].unsqueeze(1).to_broadcast([128, 8, 16]), ALU.mult, [r_tmp, r_cst], [r_t])
                        yield
                        tt("dve", dst[:, :, 64:96], t1[:], t2[:], ALU.add, [r_t], [r_dst])
                        yield
                        cp("act", dst[:, :, 0:64], tmp[:, :, 0:64], [r_tmp], [r_dst])
                        yield
                        tv = bview(TB).rearrange("p (h n) -> p h n", h=8)
                        for h in range(NH):
                            transpose(tv[0:96, h, :], dst[:, h, :], [r_dst], [rb[TB]])
                            yield
                        cp("dve", st[0:96, :, tsl], tv[0:96, :, :], [rb[TB]], [r_st])
                        yield


            def rnn_gen():
                U, r_U = uxT[G % 2], r_ux[G % 2]
                Un, r_Un = uxT[(G + 1) % 2], r_ux[(G + 1) % 2]
                for c in range(4):
                    pb = c % 2
                    b = zbank(); zmm(416 + c * 128, 128, b)
                    act(ug[pb][:], banks[b][:], AF.Copy, [rb[b]], [r_ug[pb]])
                    yield
                    act(gw[pb][:], banks[b][:], AF.Square, [rb[b]], [r_gw[pb]])
                    yield
                    ts("dve", gw[pb][:], gw[pb][:], 0.044715, 1.0, ALU.mult, ALU.add, [r_gw[pb]], [r_gw[pb]])
                    yield
                    tt("dve", gw[pb][:], gw[pb][:], ug[pb][:], ALU.mult, [r_gw[pb], r_ug[pb]], [r_gw[pb]])
                    yield
                    act(gw[pb][:], gw[pb][:], AF.Sigmoid, [r_gw[pb]], [r_gw[pb]], scale=1.5957691216057308)
                    yield
                    tt("dve", gel[pb][:], gw[pb][:], ug[pb][:], ALU.mult, [r_gw[pb], r_ug[pb]], [r_gel[pb]])
                    yield
                    b = zbank(); zmm(928 + c * 128, 128, b)
                    act(U[:, c, 3:515], banks[b][:], AF.Copy, [rb[b]], [r_U])
                    yield
                    cp("pool", Un[:, c, 0:3], U[:, c, 512:515], [r_U], [r_Un])
                    yield
                    ts("dve", xc[pb][:], U[:, c, 3:515], cw[:, c, 3:4], cb[:, c:c + 1], ALU.mult, ALU.add,
                       [r_U, r_cw, r_cb], [r_xc[pb]])
                    yield
                    for j in (2, 1, 0):
                        stt("dve", xc[pb][:], U[:, c, j:j + 512], cw[:, c, j:j + 1], xc[pb][:], ALU.mult, ALU.add,
                            [r_U, r_cw, r_xc[pb]], [r_xc[pb]])
                        yield
                    cp("act", xcb[pb][:], xc[pb][:], [r_xc[pb]], [r_xcb[pb]])
                    yield
                    b1 = zbank()
                    mm(banks[b1][:], wrg[:, c, :], xcb[pb][:], True, True, [r_wg, r_xcb[pb]], [rb[b1]])
                    yield
                    act(rg[pb][:], banks[b1][:], AF.Sigmoid, [rb[b1], r_brg], [r_rg[pb]], bias=brg[:, c:c + 1])
                    yield
                    b2 = zbank()
                    mm(banks[b2][:], wig[:, c, :], xcb[pb][:], True, True, [r_wg, r_xcb[pb]], [rb[b2]])
                    yield
                    act(ig[pb][:], banks[b2][:], AF.Sigmoid, [rb[b2], r_big], [r_ig[pb]], bias=big[:, c:c + 1])
                    yield
                    act(av[pb][:], rg[pb][:], AF.Exp, [r_rg[pb], r_c12], [r_av[pb]], scale=c1[:, c:c + 1])
                    yield
                    act(rg[pb][:], rg[pb][:], AF.Exp, [r_rg[pb], r_c12], [r_rg[pb]], scale=c2[:, c:c + 1])
                    yield
                    act(rg[pb][:], rg[pb][:], AF.Relu, [r_rg[pb], r_c], [r_rg[pb]], scale=-1.0, bias=cone[:, 0:1])
                    yield
                    act(rg[pb][:], rg[pb][:], AF.Sqrt, [r_rg[pb]], [r_rg[pb]])
                    yield
                    tt("dve", ig[pb][:], ig[pb][:], xc[pb][:], ALU.mult, [r_ig[pb], r_xc[pb]], [r_ig[pb]])
                    yield
                    tt("dve", rg[pb][:], rg[pb][:], ig[pb][:], ALU.mult, [r_rg[pb], r_ig[pb]], [r_rg[pb]])
                    yield
                    S.op("dve", lambda e, pb=pb, c=c: e.tensor_tensor_scan(
                        out=hs[pb][:], data0=av[pb][:], data1=rg[pb][:], initial=carry[:, c:c + 1],
                        op0=ALU.mult, op1=ALU.add), [r_av[pb], r_rg[pb], r_carry], [r_hs[pb]])
                    yield
                    cp("pool", carry[:, c:c + 1], hs[pb][:, 511:512], [r_hs[pb]], [r_carry])
                    yield
                    tt("dve", orn[pb][:], gel[pb][:], hs[pb][:], ALU.mult, [r_gel[pb], r_hs[pb]], [r_orn[pb]])
                    yield
                    act(sqr[:, c, :], orn[pb][:], AF.Square, [r_orn[pb]], [r_sqr])
                    yield
                    cp("act", ornb[:, c, :], orn[pb][:], [r_orn[pb]], [r_ornb])
                    yield

            gens = [qk_gen(), rnn_gen()]
            while gens:
                for g in list(gens):
                    try:
                        next(g)
                    except StopIteration:
                        gens.remove(g)
            dma("sp", ORT[:, :, G * 512:(G + 1) * 512].rearrange("c p s -> p c s"), ornb[:], [r_ornb], [r_ORT])
            for t in range(4):
                for c in range(4):
                    mm(stat_r[:, t:t + 1], sqr[:, c, t * 128:(t + 1) * 128], ones[:, 0:1], c == 0, c == 3,
                       [r_sqr, r_ones], [r_stat_r])
            rsl = rr_all[:, G * 4:(G + 1) * 4]
            ts("dve", rsl, stat_r, 1.0 / 512, EPS, ALU.mult, ALU.add, [r_stat_r], [r_rr])
            tt("pool", rsl, rsl, cneg[:, 0:1].to_broadcast([128, 4]), ALU.pow, [r_rr, r_c], [r_rr])
            gsl = slice(G * 512, (G + 1) * 512)
            dma("sp", QT[:, :, gsl].rearrange("h d s -> d h s"), QTst[0:96, :, :], [r_QTst], [r_QT])
            dma("sp", KT[:, :, gsl].rearrange("h d s -> d h s"), KTst[0:96, :, :], [r_KTst], [r_KT])
            dma("sp", VA[:, :, G * 4:(G + 1) * 4, :].rearrange("h p t c -> p h t c"), Vst[:], [r_Vst], [r_VA])
        if dbg:
            dma("sp", RR, rr_all[:, 0:NT], [r_rr], [])

    def phaseB():
        Bn = Arena(nc, SB_LO + 6144, SB_HI)
        QTh = [Bn.alloc([128, S_len], BF16, "QTh") for _ in range(2)]; r_QTh = [Res() for _ in range(2)]
        KTh = [Bn.alloc([128, S_len], BF16, "KTh") for _ in range(2)]; r_KTh = [Res() for _ in range(2)]
        Vh = [Bn.alloc([128, NT, 65], BF16, "Vh") for _ in range(2)]; r_Vh = [Res() for _ in range(2)]
        NPT = 4
        pT = [Bn.alloc([128, 1024], BF16, "pT") for _ in range(NPT)]; r_pT = [Res() for _ in range(NPT)]
        ost = [Bn.alloc([128, 4, 64], BF16, "ost") for _ in range(2)]; r_ost = [Res() for _ in range(2)]
        rec = [Bn.alloc([128, 4], F32, "rec") for _ in range(2)]; r_rec = [Res() for _ in range(2)]
        units = []
        for h in range(NH):
            for G in range(NG):
                for kt in range(0, 4 * G, 2):
                    units.append((h, G, [kt, kt + 1]))
                for j in range(4):
                    units.append((h, G, [4 * G + j]))
        LOOK = 2
        NSP = 3
        state = {"s_emitted": 0}

        def load_head(h):
            p = h % 2
            dma("sp", QTh[p][0:96, :], QT[h], [r_QT], [r_QTh[p]])
            dma("sp", KTh[p][0:96, :], KT[h], [r_KT], [r_KTh[p]])
            dma("sp", Vh[p][:], VA[h], [r_VA], [r_Vh[p]])

        def emit_score(u):
            h, G, kts = units[u]
            if G == 0 and kts[0] == 0:
                load_head(h)
            p = h % 2
            sp_ = u % NSP
            for i, kt in enumerate(kts):
                q0 = max(kt - 4 * G, 0) * 128
                mm(pairs[sp_][:, i * 512 + q0:(i + 1) * 512], KTh[p][0:96, kt * 128:(kt + 1) * 128],
                   QTh[p][0:96, G * 512 + q0:(G + 1) * 512], True, True, [r_KTh[p], r_QTh[p]], [rb[2 * sp_]])

        for u, (h, G, kts) in enumerate(units):
            while state["s_emitted"] < min(len(units), u + 1 + LOOK):
                emit_score(state["s_emitted"])
                state["s_emitted"] += 1
            p = h % 2
            sp_ = u % NSP
            pi = u % NPT
            gi = (h * NG + G) % 2
            ob = 6 + gi
            o_ps = banks[ob][:, 0:260].rearrange("p (t c) -> p t c", c=65)
            j0 = kts[0] - 4 * G
            q0 = max(j0, 0) * 128
            w = 512 * len(kts)
            act(pT[pi][:, q0:w], pairs[sp_][:, q0:w], AF.Exp, [rb[2 * sp_]], [r_pT[pi]])
            if j0 >= 0:
                memset("pool", pT[pi][64:128, q0:q0 + 64], 0.0, [r_pT[pi]])
            for i, kt in enumerate(kts):
                j = kt - 4 * G
                for qt in range(max(j, 0), 4):
                    mm(o_ps[:, qt, :], pT[pi][:, i * 512 + qt * 128:i * 512 + (qt + 1) * 128], Vh[p][:, kt, :],
                       kt == 0 and qt == 0, kt == 4 * G + qt, [r_pT[pi], r_Vh[p]], [rb[ob]], skip=True)
            if kts[-1] == 4 * G + 3:
                S.op("dve", lambda e, gi=gi, o_ps=o_ps: e.reciprocal(out=rec[gi][:], in_=o_ps[:, :, 64]),
                     [rb[ob]], [r_rec[gi]])
                tt("dve", ost[gi][:], o_ps[:, :, 0:64], rec[gi][:].unsqueeze(2).to_broadcast([128, 4, 64]), ALU.mult,
                   [rb[ob], r_rec[gi]], [r_ost[gi]])
                dma_nc("sp", OA[G * 512:(G + 1) * 512, h * 64:(h + 1) * 64].rearrange("(t p) d -> p t d", p=128),
                       ost[gi][:], [r_ost[gi]], [r_OA])

    def phaseCD():
        NSETS = int(os.environ.get('NSETS', '3'))
        Cn = Arena(nc, SB_LO + 6144, SB_HI)
        TB = 7
        w_out = Cn.alloc([128, 8, 1024], BF16, "w_out"); r_wout = Res()
        dma("pool", w_out[:], W["w_out"].rearrange("(k p) n -> p k n", p=128), writes=[r_wout])
        grnn, r_grnn = colvec(Cn, "grnn", W["g_rnn_out"], 4)
        for c in range(4):
            ts("dve", w_out[:, 4 + c, :], w_out[:, 4 + c, :], grnn[:, c:c + 1], None, ALU.mult, None,
               [r_wout, r_grnn], [r_wout])
        w_mq = Cn.alloc([128, 8, 1024], BF16, "w_mq"); r_wmq = Res()
        dma("pool", w_mq[:], W["w_mq"].rearrange("(k p) n -> p k n", p=128), writes=[r_wmq])
        w_mo = Cn.alloc([128, 8, 1024], BF16, "w_mo"); r_wmo = Res()
        dma("pool", w_mo[:], W["w_mo"].rearrange("(k p) n -> p k n", p=128), writes=[r_wmo])
        ga_b, r_ga = bcast(Cn, "ga_b", W["g_attn_out"], 512)
        gxq_b, r_gxq = bcast(Cn, "gxq_b", W["g_xq"], 1024)
        gffn_b, r_gffn = bcast(Cn, "gffn_b", W["g_ffn"], 1024)
        gmq_b, r_gmq = bcast(Cn, "gmq_b", W["g_mqn"], 256)
        ts("dve", gmq_b[:], gmq_b[:], 256.0 ** -0.5, None, ALU.mult, None, [r_gmq], [r_gmq])
        wr = Cn.alloc([128, 8, 36], F32, "wr"); r_wr = Res()
        dma_nc("sp", wr[:, :, 0:4], W["w_group"].rearrange("(k p) n -> p k n", p=128), writes=[r_wr])
        dma_nc("sp", wr[:, :, 4:36], W["w_expert"].rearrange("(k p) n -> p k n", p=128), writes=[r_wr])
        br_b = Cn.alloc([128, 36], F32, "br_b"); r_br = Res()
        dma("sp", br_b[:, 0:4], W["b_group"].partition_broadcast(128), writes=[r_br])
        dma("sp", br_b[:, 4:36], W["b_expert"].partition_broadcast(128), writes=[r_br])
        KmT = Cn.alloc([128, 8, 256], BF16, "KmT"); r_KmT = Res()
        Vm = Cn.alloc([128, 2, 4, 256], BF16, "Vm"); r_Vm = Res()
        I32 = mybir.dt.int32
        T_SL = 256
        NTILE = (2 * S_len) // T_SL + NE
        gAB = Cn.alloc([128, NT, 2], F32, "gAB"); r_gAB = Res()
        widx = Cn.alloc([128, NTILE], I32, "widx"); r_widx = Res()
        pos_i = Cn.alloc([128, 2, NT], I32, "pos_i"); r_pos = Res()
        mark2 = Cn.cur
        xt = Sel([Cn.alloc([128, 1024], F32, "xt") for _ in range(NSETS)]); r_xt = RSel(NSETS)
        junk = Sel([Cn.alloc([128, 1024], BF16, "junkc") for _ in range(NSETS)]); r_junk = RSel(NSETS)
        ssv = Sel([Cn.alloc([128, 4], F32, "ssv") for _ in range(NSETS)]); r_ssv = RSel(NSETS)
        h2 = Sel([Cn.alloc([128, 1024], BF16, "h2") for _ in range(NSETS)]); r_h2 = RSel(NSETS)
        h2T = Sel([Cn.alloc([128, 8, 128], BF16, "h2T") for _ in range(NSETS)]); r_h2T = RSel(NSETS)
        tmpf = Sel([Cn.alloc([128, 1024], F32, "tmpf") for _ in range(NSETS)]); r_tmpf = RSel(NSETS)
        ssm = Sel([Cn.alloc([128, 4], F32, "ssm") for _ in range(NSETS)]); r_ssm = RSel(NSETS)
        qmb = Sel([Cn.alloc([128, 1024], BF16, "qmb") for _ in range(NSETS)]); r_qmb = RSel(NSETS)
        mark = Cn.cur
        tvb = bview(TB).rearrange("p (k n) -> p k n", k=8)

        def norm_to_bf16(src, r_src, g_b, r_g, dst, r_dst, sscol, f32dst=None, r_f32=None):
            act(junk[:], src, AF.Square, [r_src], [r_junk, r_ssv], accum=ssv[:, sscol:sscol + 1])
            rstd_chain(ssv[:, sscol:sscol + 1], 1, 1.0 / D, [r_ssv])
            if f32dst is None:
                stt("dve", dst, src, ssv[:, sscol:sscol + 1], g_b[:], ALU.mult, ALU.mult, [r_src, r_ssv, r_g], [r_dst])
            else:
                stt("dve", f32dst, src, ssv[:, sscol:sscol + 1], g_b[:], ALU.mult, ALU.mult, [r_src, r_ssv, r_g], [r_f32])
                cp("act", dst, f32dst, [r_f32], [r_dst])

        def transpose8(src, r_src, dstT, r_dstT, n=8, dst_sl=None):
            for k in range(n):
                transpose(tvb[:, k, :], src[:, k * 128:(k + 1) * 128], [r_src], [rb[TB]])
            cp("act", dstT if dst_sl is None else dst_sl, tvb[:, 0:n, :], [rb[TB]], [r_dstT])

        def head_norm(pb0, g_b, r_g, dst, r_dst):
            for half in range(2):
                act(tmpf[:, half * 512:(half + 1) * 512], banks[pb0 + half][:], AF.Square, [rb[pb0 + half]], [r_tmpf])
            red(ssm[:], tmpf[:].rearrange("p (h d) -> p h d", h=4), ALU.add, [r_tmpf], [r_ssm])
            rstd_chain(ssm[:], 4, 1.0 / 256, [r_ssm])
            for half in range(2):
                tt("dve", tmpf[:, half * 512:(half + 1) * 512].rearrange("p (h d) -> p h d", h=2),
                   banks[pb0 + half][:].rearrange("p (h d) -> p h d", h=2),
                   ssm[:, half * 2:half * 2 + 2].unsqueeze(2).to_broadcast([128, 2, 256]), ALU.mult,
                   [rb[pb0 + half], r_ssm], [r_tmpf])
            tt("dve", dst.rearrange("p (h d) -> p h d", h=4), tmpf[:].rearrange("p (h d) -> p h d", h=4),
               g_b[:].unsqueeze(1).to_broadcast([128, 4, 256]), ALU.mult, [r_tmpf, r_g], [r_dst])

        w_mk = Cn.alloc([128, 8, 1024], BF16, "w_mk"); r_wmk = Res()
        dma("pool", w_mk[:], W["w_mk"].rearrange("(k p) n -> p k n", p=128), writes=[r_wmk])
        w_mv = Cn.alloc([128, 8, 1024], BF16, "w_mv"); r_wmv = Res()
        dma("pool", w_mv[:], W["w_mv"].rearrange("(k p) n -> p k n", p=128), writes=[r_wmv])
        gmem_b, r_gmem = bcast(Cn, "gmem_b", W["g_mem"], 1024)
        gmk_b, r_gmk = bcast(Cn, "gmk_b", W["g_mkn"], 256)
        mnT = Cn.alloc([128, 8, 256], BF16, "mnT"); r_mnT = Res()
        for mt in range(2):
            dma("sp", xt[:], mem_d[mt * 128:(mt + 1) * 128, :], writes=[r_xt])
            norm_to_bf16(xt[:], r_xt, gmem_b, r_gmem, h2[:], r_h2, 0)
            transpose8(h2, r_h2, None, r_mnT, dst_sl=mnT[:, :, mt * 128:(mt + 1) * 128])
        for mt in range(2):
            msl = slice(mt * 128, (mt + 1) * 128)
            for half in range(2):
                for k in range(8):
                    mm(banks[half][:], mnT[:, k, msl], w_mk[:, k, half * 512:(half + 1) * 512], k == 0, k == 7,
                       [r_mnT, r_wmk], [rb[half]])
                for k in range(8):
                    mm(banks[2 + half][:], mnT[:, k, msl], w_mv[:, k, half * 512:(half + 1) * 512], k == 0, k == 7,
                       [r_mnT, r_wmv], [rb[2 + half]])
                act(Vm[:, mt, half * 2:half * 2 + 2, :], banks[2 + half][:].rearrange("p (h d) -> p h d", h=2),
                    AF.Copy, [rb[2 + half]], [r_Vm])
            head_norm(0, gmk_b, r_gmk, qmb[:], r_qmb)
            transpose8(qmb, r_qmb, None, r_KmT, dst_sl=KmT[:, :, msl])
        S.barrier()
        Cn.cur = mark

        oa = Sel([Cn.alloc([128, 512], BF16, "oa") for _ in range(NSETS)]); r_oa = RSel(NSETS)
        ornT = Sel([Cn.alloc([128, 4, 128], BF16, "ornT") for _ in range(NSETS)]); r_ornT = RSel(NSETS)
        mixA = Sel([Cn.alloc([128, 512], BF16, "mixA") for _ in range(NSETS)]); r_mixA = RSel(NSETS)
        mixAT = Sel([Cn.alloc([128, 4, 128], BF16, "mixAT") for _ in range(NSETS)]); r_mixAT = RSel(NSETS)
        x1 = Sel([Cn.alloc([128, 1024], F32, "x1") for _ in range(NSETS)]); r_x1 = RSel(NSETS)
        qmT = Sel([Cn.alloc([128, 8, 128], BF16, "qmT") for _ in range(NSETS)]); r_qmT = RSel(NSETS)
        pm = Sel([Cn.alloc([128, 8, 128], BF16, "pm") for _ in range(NSETS)]); r_pm = RSel(NSETS)
        recm = Sel([Cn.alloc([128, 4], F32, "recm") for _ in range(NSETS)]); r_recm = RSel(NSETS)
        omb = h2; r_omb = r_h2
        omT = h2T; r_omT = r_h2T
        h3f = tmpf; r_h3f = r_tmpf
        h3 = qmb; r_h3 = r_qmb
        h3fT = Sel([Cn.alloc([128, 8, 128], F32, "h3fT") for _ in range(NSETS)]); r_h3fT = RSel(NSETS)
        lg = Sel([Cn.alloc([128, 36], F32, "lg") for _ in range(NSETS)]); r_lg = RSel(NSETS)
        rt = Sel([Cn.alloc([128, 64], F32, "rt") for _ in range(NSETS)]); r_rt = RSel(NSETS)
        I32 = mybir.dt.int32
        T_SL = 256
        NTILE = (2 * S_len) // T_SL + NE
        WGUv = WGU2.rearrange("e p k n -> (e p) (k n)")
        WDv = WD2.rearrange("e p c n -> (e p) (c n)")
        x2 = xt; r_x2 = r_xt
        rank_all = Cn.alloc([128, NT, 32], F32, "rank_all"); r_rank = Res()
        selA = Cn.alloc([128, NT, 32], F32, "selA"); selB = Cn.alloc([128, NT, 32], F32, "selB"); r_sel = Res()
        carryc = Cn.alloc([128, 32], F32, "carryc"); r_carryc = Res()
        memset("pool", carryc[:], 0.0, [r_carryc])
        Mf = Sel([Cn.alloc([128, 32], F32, "Mf") for _ in range(2)])
        Mb = Sel([Cn.alloc([128, 32], BF16, "Mb") for _ in range(NSETS)]); r_M = RSel(NSETS)
        Lst = Cn.alloc([128, 128], BF16, "Lst"); r_Lst = Res()
        dma("sp", Lst[:], lst_d, writes=[r_Lst])
        ones128 = Cn.alloc([128, 128], BF16, "ones128")
        memset("pool", ones128[:], 1.0, [r_Lst])
        r_H3 = [Res() for _ in range(NT)]
        r_out = [Res() for _ in range(NT)]
        print("phase C SBUF used", Cn.cur, "of", SB_HI)
        N_SKEW = int(os.environ.get('N_SKEW', '2'))

        def tile_body(gt):
            if True:
                t8 = 0
                rows = slice(gt * 128, (gt + 1) * 128)
                dma("sp", xt[:], x_d[rows, :], writes=[r_xt])
                yield
                dma("sp", oa[:], OA[rows, :], [r_OA], [r_oa])
                yield
                dma("sp", ornT[:], ORT[:, :, rows].rearrange("c p s -> p c s"), [r_ORT], [r_ornT])
                yield
                act(junk[:, 0:512], oa[:], AF.Square, [r_oa], [r_junk, r_ssv], accum=ssv[:, 0:1])
                yield
                rstd_chain(ssv[:, 0:1], 1, 1.0 / 512, [r_ssv])
                yield
                tt("dve", mixA[:], oa[:], ga_b[:], ALU.mult, [r_oa, r_ga], [r_mixA])
                yield
                transpose8(mixA, r_mixA, mixAT[:], r_mixAT, n=4)
                yield
                for half in range(2):
                    hsl = slice(half * 512, (half + 1) * 512)
                    for k in range(4):
                        mm(banks[half][:], mixAT[:, k, :], w_out[:, k, hsl], k == 0, k == 3, [r_mixAT, r_wout], [rb[half]])
                    for k in range(4):
                        mm(banks[2 + half][:], ornT[:, k, :], w_out[:, 4 + k, hsl], k == 0, k == 3,
                           [r_ornT, r_wout], [rb[2 + half]])
                    stt("dve", x1[:, hsl], banks[half][:], ssv[:, 0:1], xt[:, hsl], ALU.mult, ALU.add,
                        [rb[half], r_ssv, r_xt], [r_x1])
                    stt("dve", x1[:, hsl], banks[2 + half][:], rr_all[:, gt:gt + 1], x1[:, hsl], ALU.mult, ALU.add,
                        [rb[2 + half], r_rr, r_x1], [r_x1])
                yield
                norm_to_bf16(x1[:], r_x1, gxq_b, r_gxq, h2[:], r_h2, 1)
                yield
                transpose8(h2, r_h2, h2T[:], r_h2T)
                yield
                for half in range(2):
                    for k in range(8):
                        mm(banks[4 + half][:], h2T[:, k, :], w_mq[:, k, half * 512:(half + 1) * 512], k == 0, k == 7,
                           [r_h2T, r_wmq], [rb[4 + half]])
                head_norm(4, gmq_b, r_gmq, qmb[:], r_qmb)
                yield
                transpose8(qmb, r_qmb, qmT[:], r_qmT)
                yield
                for hh in range(4):
                    for mt in range(2):
                        slot = hh * 2 + mt
                        for kk in range(2):
                            mm(banks[slot // 4][:, (slot % 4) * 128:(slot % 4 + 1) * 128],
                               KmT[:, hh * 2 + kk, mt * 128:(mt + 1) * 128], qmT[:, hh * 2 + kk, :], kk == 0, kk == 1,
                               [r_KmT, r_qmT], [rb[slot // 4]])
                for bk in range(2):
                    act(pm[:, bk * 4:(bk + 1) * 4, :], banks[bk][:].rearrange("p (s n) -> p s n", s=4), AF.Exp,
                        [rb[bk]], [r_pm])
                yield
                for hh in range(4):
                    ob = 2 + hh // 2
                    for mt in range(2):
                        mm(banks[ob][:, (hh % 2) * 256:(hh % 2 + 1) * 256], pm[:, hh * 2 + mt, :], Vm[:, mt, hh, :],
                           mt == 0, mt == 1, [r_pm, r_Vm], [rb[ob]])
                    for mt in range(2):
                        mm(banks[6][:, hh:hh + 1], pm[:, hh * 2 + mt, :], ones[:, 0:1], mt == 0, mt == 1,
                           [r_pm, r_ones], [rb[6]])
                recip(recm[:], banks[6][:, 0:4], [rb[6]], [r_recm])
                for bk in range(2):
                    tt("dve", omb[:, bk * 512:(bk + 1) * 512].rearrange("p (h d) -> p h d", h=2),
                       banks[2 + bk][:].rearrange("p (h d) -> p h d", h=2),
                       recm[:, bk * 2:bk * 2 + 2].unsqueeze(2).to_broadcast([128, 2, 256]), ALU.mult,
                       [rb[2 + bk], r_recm], [r_omb])
                yield
                transpose8(omb, r_omb, omT[:], r_omT)
                yield
                for half in range(2):
                    hsl = slice(half * 512, (half + 1) * 512)
                    for k in range(8):
                        mm(banks[4 + half][:], omT[:, k, :], w_mo[:, k, hsl], k == 0, k == 7, [r_omT, r_wmo], [rb[4 + half]])
                    tt("dve", x2[:, hsl], banks[4 + half][:], x1[:, hsl], ALU.add, [rb[4 + half], r_x1], [r_x2])
                yield
                dma("sp", out_d[rows, :], x2[:], [r_x2], [r_out[gt]])
                yield
                act(junk[:], x2[:], AF.Square, [r_x2], [r_junk, r_ssv], accum=ssv[:, 2:3])
                yield
                rstd_chain(ssv[:, 2:3], 1, 1.0 / D, [r_ssv])
                yield
                stt("dve", h3f[:], x2[:], ssv[:, 2:3], gffn_b[:], ALU.mult, ALU.mult, [r_x2, r_ssv, r_gffn], [r_h3f])
                yield
                cp("act", h3[:], h3f[:], [r_h3f], [r_h3])
                yield
                dma("sp", H3[rows, :], h3[:], [r_h3], [r_H3[gt]])
                yield
                for k in range(8):
                    transpose(banks[k // 4][:, (k % 4) * 128:(k % 4 + 1) * 128], h3f[:, k * 128:(k + 1) * 128],
                              [r_h3f], [rb[k // 4]], f32=True)
                for bk in range(2):
                    cp("act", h3fT[:, bk * 4:(bk + 1) * 4, :], banks[bk][:].rearrange("p (s n) -> p s n", s=4),
                       [rb[bk]], [r_h3fT])
                yield
                for k in range(8):
                    mm(banks[6][:, 64:100], h3fT[:, k, :], wr[:, k, :], k == 0, k == 7, [r_h3fT, r_wr], [rb[6]])
                tt("dve", lg[:], banks[6][:, 64:100], br_b[:], ALU.add, [rb[6], r_br], [r_lg])
                R = [r_lg, r_rt]
                gmax, ngmax, sumg, oh = rt[:, 0:1], rt[:, 1:2], rt[:, 2:3], rt[:, 4:8]
                eg, es, emax, nemax = rt[:, 8:12], rt[:, 16:24], rt[:, 12:13], rt[:, 13:14]
                ex, top8, den, msk = rt[:, 24:32], rt[:, 32:40], rt[:, 14:15], rt[:, 40:48]
                sel32 = tmpf[:, 0:32]
                yield
                red(gmax, lg[:, 0:4], ALU.max, R, [r_rt])
                yield
                ts("dve", oh, lg[:, 0:4], gmax, None, ALU.is_ge, None, R, [r_rt])
                yield
                ts("dve", ngmax, gmax, -1.0, None, ALU.mult, None, R, [r_rt])
                yield
                act(eg, lg[:, 0:4], AF.Exp, R, [r_rt], bias=ngmax, accum=sumg)
                yield
                recip(sumg, sumg, R, [r_rt])
                yield
                tt("dve", sel32.rearrange("p (g e) -> p g e", g=4), lg[:, 4:36].rearrange("p (g e) -> p g e", g=4),
                   oh.unsqueeze(2).to_broadcast([128, 4, 8]), ALU.mult, R, [r_tmpf])
                yield
                red(es, sel32.rearrange("p (g e) -> p e g", g=4), ALU.add, [r_tmpf], [r_rt])
                yield
                red(emax, es, ALU.max, R, [r_rt])
                yield
                ts("dve", nemax, emax, -1.0, None, ALU.mult, None, R, [r_rt])
                yield
                act(ex, es, AF.Exp, R, [r_rt], bias=nemax)
                yield
                S.op("dve", lambda e, top8=top8, ex=ex: e.max(out=top8, in_=ex), R, [r_rt])
                yield
                tt("dve", den, top8[:, 0:1], top8[:, 1:2], ALU.add, R, [r_rt])
                yield
                recip(den, den, R, [r_rt])
                yield
                tt("dve", den, den, sumg, ALU.mult, R, [r_rt])
                mskA, msk2 = rt[:, 48:56], rt[:, 40:48]
                yield
                ts("dve", mskA, ex, top8[:, 0:1], None, ALU.is_ge, None, R, [r_rt])
                yield
                ts("dve", msk2, ex, top8[:, 1:2], None, ALU.is_ge, None, R, [r_rt])
                ohb = oh.unsqueeze(2).to_broadcast([128, 4, 8])
                yield
                tt("dve", selA[:, gt, :].rearrange("p (g e) -> p g e", g=4), ohb,
                   mskA.unsqueeze(1).to_broadcast([128, 4, 8]), ALU.mult, R, [r_sel])
                yield
                tt("dve", Mf[:].rearrange("p (g e) -> p g e", g=4), ohb,
                   msk2.unsqueeze(1).to_broadcast([128, 4, 8]), ALU.mult, R, [r_M])
                yield
                tt("dve", selB[:, gt, :], Mf[:], selA[:, gt, :], ALU.subtract, [r_M, r_sel], [r_sel])
                yield
                ts("dve", gAB[:, gt, :], top8[:, 0:2], den, None, ALU.mult, None, R, [r_gAB])
                yield
                cp("dve", Mb[:], Mf[:], [r_M], [r_M])
                yield
                mm(banks[6][:, 128:160], Lst[:], Mb[:], True, True, [r_Lst, r_M], [rb[6]])
                mm(banks[6][:, 160:192], ones128[:], Mb[:], True, True, [r_Lst, r_M], [rb[6]])
                tt("dve", rank_all[:, gt, :], banks[6][:, 128:160], carryc[:], ALU.add, [rb[6], r_carryc], [r_rank])
                tt("dve", carryc[:], banks[6][:, 160:192], carryc[:], ALU.add, [rb[6], r_carryc], [r_carryc])

        FILL = int(os.environ.get("FILL", "0"))
        fcount = [0]

        def wrap(gt):
            g = tile_body(gt)
            while True:
                set_parity(gt)
                try:
                    next(g)
                except StopIteration:
                    return
                fcount[0] += 1
                if FILL and fcount[0] % FILL == 0:
                    S.op("pe", lambda e: e.matmul(banks[6][:, 192:512], lhsT=idb[:], rhs=w_out[:, 0, 0:320],
                                                  start=True, stop=True, skip_group_check=True), [], [])
                yield

        interleave((wrap(gt) for gt in range(NT)), NSETS, admit_every=N_SKEW)
        set_parity(0)

        S.barrier()
        top_c = Cn.cur
        Cn.cur = mark2
        thr_b, r_thr = bcast(Cn, "thr_b", thr_d, 32)
        iota_b, r_iota = bcast(Cn, "iota_b", iota_d, NTILE)
        pidx, r_pidx = colvec(Cn, "pidx", pidx_d, 1)
        onesf = Cn.alloc([128, 32], F32, "onesf"); r_onesf = Res()
        memset("pool", onesf[:], 1.0, [r_onesf])
        ntile = Cn.alloc([128, 32], F32, "ntile"); endc = Cn.alloc([128, 32], F32, "endc")
        startT = Cn.alloc([128, 32], F32, "startT"); r_bk = Res()
        cmpi = Cn.alloc([128, NTILE, 32], F32, "cmpi"); r_cmpi = Res()
        tef = Cn.alloc([128, NTILE], F32, "tef"); r_tef = Res()
        posf = Cn.alloc([128, 2, NT], F32, "posf"); r_posf = Res()
        cmp3t = Cn.alloc([128, 1024], F32, "cmp3t"); r_tmpf = Res()
        cmp3 = cmp3t[:].rearrange("p (e m) -> p e m", e=32)
        assert Cn.cur <= top_c
        tt("dve", cmp3, carryc[:].unsqueeze(2).to_broadcast([128, 32, 32]),
           thr_b[:].unsqueeze(1).to_broadcast([128, 32, 32]), ALU.is_gt, [r_carryc, r_thr], [r_tmpf])
        S.op("dve", lambda e: e.tensor_reduce(out=ntile[:], in_=cmp3, axis=AX.X, op=ALU.add), [r_tmpf], [r_bk])
        S.op("dve", lambda e: e.tensor_tensor_scan(out=endc[:], data0=onesf[:], data1=ntile[:], initial=0.0,
                                                   op0=ALU.mult, op1=ALU.add), [r_bk, r_onesf], [r_bk])
        tt("dve", startT[:], endc[:], ntile[:], ALU.subtract, [r_bk], [r_bk])
        ts("dve", startT[:], startT[:], float(T_SL), None, ALU.mult, None, [r_bk], [r_bk])
        tt("dve", cmpi[:], iota_b[:].unsqueeze(2).to_broadcast([128, NTILE, 32]),
           endc[:].unsqueeze(1).to_broadcast([128, NTILE, 32]), ALU.is_ge, [r_iota, r_bk], [r_cmpi])
        S.op("dve", lambda e: e.tensor_reduce(out=tef[:], in_=cmpi[:], axis=AX.X, op=ALU.add), [r_cmpi], [r_tef])
        ts("dve", tef[:], tef[:], float(NE - 1), None, ALU.min, None, [r_tef], [r_tef])
        ts("dve", tef[:], tef[:], 128.0, pidx[:, 0:1], ALU.mult, ALU.add, [r_tef, r_pidx], [r_tef])
        cp("dve", widx[:], tef[:], [r_tef], [r_widx])
        tt("dve", rank_all[:], rank_all[:], startT[:].unsqueeze(1).to_broadcast([128, NT, 32]), ALU.add,
           [r_rank, r_bk], [r_rank])
        tt("dve", selA[:], selA[:], rank_all[:], ALU.mult, [r_sel, r_rank], [r_sel])
        tt("dve", selB[:], selB[:], rank_all[:], ALU.mult, [r_sel, r_rank], [r_sel])
        S.op("dve", lambda e: e.tensor_reduce(out=posf[:, 0, :], in_=selA[:], axis=AX.X, op=ALU.add), [r_sel], [r_posf])
        S.op("dve", lambda e: e.tensor_reduce(out=posf[:, 1, :], in_=selB[:], axis=AX.X, op=ALU.add), [r_sel], [r_posf])
        cp("dve", pos_i[:], posf[:], [r_posf], [r_pos])
        if dbg:
            dma("sp", DBG_widx, widx[:], [r_widx], [])
            dma("sp", DBG_pos, pos_i[:], [r_pos], [])
            dma("sp", DBG_gab, gAB[:], [r_gAB], [])
        S.barrier()
        Cn.cur = mark2
        if moe_stop == "C":
            return [dma("sp", out_d[0:128, :], x_d[0:128, :])]

        hsb = [Cn.alloc([128, 1024], BF16, "hsb") for _ in range(2)]; r_hsb = [Res() for _ in range(2)]
        for gt in range(NT):
            q = gt % 2
            dma("sp", hsb[q][:], H3[gt * 128:(gt + 1) * 128, :], [r_H3[gt]], [r_hsb[q]])
            for j in range(2):
                S.op("pool", lambda e, q=q, gt=gt, j=j: e.indirect_dma_start(
                    out=Hs[:, :], out_offset=bass.IndirectOffsetOnAxis(ap=pos_i[:, j, gt:gt + 1], axis=0),
                    in_=hsb[q][:, :], in_offset=None), [r_hsb[q], r_pos], [Res()], dma=True)
        S.barrier()
        if moe_stop == "S":
            return [dma("sp", out_d[0:128, :], x_d[0:128, :])]

        Wgu2 = [Cn.alloc([128, 4096], BF16, "Wgu2") for _ in range(2)]; r_Wgu2 = [Res() for _ in range(2)]
        Wd2 = [Cn.alloc([128, 2048], BF16, "Wd2") for _ in range(2)]; r_Wd2 = [Res() for _ in range(2)]
        hst = [Cn.alloc([128, 1024], BF16, "hst") for _ in range(2)]; r_hst = [Res() for _ in range(2)]
        hTs = [Cn.alloc([128, 8, 128], BF16, "hTs") for _ in range(2)]; r_hTs = [Res() for _ in range(2)]
        sgt = [Cn.alloc([128, 256], F32, "sgt") for _ in range(2)]; r_sgt = [Res() for _ in range(2)]
        het = [Cn.alloc([128, 256], BF16, "het") for _ in range(2)]; r_het = [Res() for _ in range(2)]
        heT = [Cn.alloc([128, 2, 128], BF16, "heT") for _ in range(2)]; r_heT = [Res() for _ in range(2)]
        yst = [Cn.alloc([128, 1024], BF16, "yst") for _ in range(2)]; r_yst = [Res() for _ in range(2)]
        NWB = 3
        NSET = 4
        for lst_, shape, dt_, nm in ((Wgu2, [128, 4096], BF16, "Wgu2"), (Wd2, [128, 2048], BF16, "Wd2")):
            while len(lst_) < NWB:
                lst_.append(Cn.alloc(shape, dt_, nm))
        r_Wgu2 = [Res() for _ in range(NWB)]; r_Wd2 = [Res() for _ in range(NWB)]
        for lst_, shape, dt_, nm in ((hst, [128, 1024], BF16, "hst"), (hTs, [128, 8, 128], BF16, "hTs"),
                                     (sgt, [128, 256], F32, "sgt"), (het, [128, 256], BF16, "het"),
                                     (heT, [128, 2, 128], BF16, "heT"), (yst, [128, 1024], BF16, "yst")):
            while len(lst_) < NSET:
                lst_.append(Cn.alloc(shape, dt_, nm))
        r_hst = [Res() for _ in range(NSET)]; r_hTs = [Res() for _ in range(NSET)]; r_sgt = [Res() for _ in range(NSET)]
        r_het = [Res() for _ in range(NSET)]; r_heT = [Res() for _ in range(NSET)]; r_yst = [Res() for _ in range(NSET)]

        def sub_gen(i, sub, q):
            p = i % NWB
            if sub == 0:
                S.op("pool", lambda e, p=p, i=i: e.indirect_dma_start(
                    out=Wgu2[p][:, :], out_offset=None, in_=WGUv,
                    in_offset=bass.IndirectOffsetOnAxis(ap=widx[:, i:i + 1], axis=0)), [r_widx], [r_Wgu2[p]], dma=True)
                S.op("pool", lambda e, p=p, i=i: e.indirect_dma_start(
                    out=Wd2[p][:, :], out_offset=None, in_=WDv,
                    in_offset=bass.IndirectOffsetOnAxis(ap=widx[:, i:i + 1], axis=0)), [r_widx], [r_Wd2[p]], dma=True)
            r0 = i * T_SL + sub * 128
            dma("sp", hst[q][:], Hs[r0:r0 + 128, :], [], [r_hst[q]])
            yield
            transpose8(hst[q], r_hst[q], hTs[q][:], r_hTs[q])
            yield
            gb = q
            for k in range(8):
                mm(banks[gb][:], hTs[q][:, k, :], Wgu2[p][:, k * 512:(k + 1) * 512], k == 0, k == 7,
                   [r_hTs[q], r_Wgu2[p]], [rb[gb]])
            yield
            act(sgt[q][:], banks[gb][:, 0:256], AF.Silu, [rb[gb]], [r_sgt[q]])
            yield
            tt("dve", het[q][:], sgt[q][:], banks[gb][:, 256:512], ALU.mult, [r_sgt[q], rb[gb]], [r_het[q]])
            yield
            transpose8(het[q], r_het[q], heT[q][:], r_heT[q], n=2)
            yield
            yb = (4, 5)
            for half in range(2):
                for c in range(2):
                    mm(banks[yb[half]][:], heT[q][:, c, :], Wd2[p][:, c * 1024 + half * 512:c * 1024 + (half + 1) * 512],
                       c == 0, c == 1, [r_heT[q], r_Wd2[p]], [rb[yb[half]]])
            cp("act", yst[q][:, 0:512], banks[yb[0]][:], [rb[yb[0]]], [r_yst[q]])
            cp("dve", yst[q][:, 512:1024], banks[yb[1]][:], [rb[yb[1]]], [r_yst[q]])
            yield
            dma("sp", Ys[r0:r0 + 128, :], yst[q][:], [r_yst[q]], [Res()])

        def all_subs():
            cnt = 0
            for i in range(NTILE):
                for sub in range(T_SL // 128):
                    yield sub_gen(i, sub, cnt % NSET)
                    cnt += 1

        interleave(all_subs(), NSET)
        S.barrier()
        if moe_stop == "E":
            return [dma("sp", out_d[0:128, :], x_d[0:128, :])]

        xo = [Cn.alloc([128, 1024], F32, "xo") for _ in range(2)]; r_xo = [Res() for _ in range(2)]
        yA = [Cn.alloc([128, 1024], BF16, "yA") for _ in range(2)]; r_yA = [Res() for _ in range(2)]
        yB = [Cn.alloc([128, 1024], BF16, "yB") for _ in range(2)]; r_yB = [Res() for _ in range(2)]
        print("phase E/F SBUF used", Cn.cur, "of", SB_HI)
        outs = []
        for gt in range(NT):
            q = gt % 2
            rows = slice(gt * 128, (gt + 1) * 128)
            dma("sp", xo[q][:], out_d[rows, :], [r_out[gt]], [r_xo[q]])
            for j, (yy, r_yy) in enumerate(((yA, r_yA), (yB, r_yB))):
                S.op("pool", lambda e, q=q, gt=gt, j=j, yy=yy: e.indirect_dma_start(
                    out=yy[q][:, :], out_offset=None, in_=Ys[:, :],
                    in_offset=bass.IndirectOffsetOnAxis(ap=pos_i[:, j, gt:gt + 1], axis=0)), [r_pos], [r_yy[q]], dma=True)
            stt("dve", xo[q][:], yA[q][:], gAB[:, gt, 0:1], xo[q][:], ALU.mult, ALU.add, [r_yA[q], r_gAB, r_xo[q]], [r_xo[q]])
            stt("dve", xo[q][:], yB[q][:], gAB[:, gt, 1:2], xo[q][:], ALU.mult, ALU.add, [r_yB[q], r_gAB, r_xo[q]], [r_xo[q]])
            outs.append(dma("sp", out_d[rows, :], xo[q][:], [r_xo[q]], [r_out[gt]]))
        return outs

    if "A" in phases:
        phaseA()
    S.barrier()
    if "B" in phases:
        phaseB()
    S.barrier()
    S.barrier()
    outs = []
    if "D" in phases:
        outs = phaseCD()
    else:
        outs = [dma("sp", out_d[0:128, :], x_d[0:128, :])]
    return nc, S, outs


def host_consts(S_len):
    pos = np.arange(S_len, dtype=np.float32)
    inv_freq = (np.float32(10000.0) ** (-np.arange(0, 32, 2, dtype=np.float32) / np.float32(32))).astype(np.float32)
    ang = (pos[:, None] * inv_freq[None, :]).astype(np.float32)
    c, s = np.cos(ang).astype(np.float32), np.sin(ang).astype(np.float32)
    cs = np.concatenate([c, c, -s, s], axis=1).astype(np.float32)
    ntile = (2 * S_len) // 256 + NE
    lst = np.triu(np.ones((128, 128), np.float32), 1).astype(ml_dtypes.bfloat16)
    return {"cs_tab": cs, "ident_bf": np.eye(128).astype(ml_dtypes.bfloat16), "ident_f32": np.eye(128, dtype=np.float32),
            "lstrict": lst, "thr_tab": (np.arange(32) * 256).astype(np.float32),
            "iota_tab": np.arange(ntile).astype(np.float32), "pidx_tab": np.arange(128).astype(np.float32)}


_CACHE = {}


def kernel(**inputs):
    x = np.asarray(inputs["x"], dtype=np.float32)
    B, S_len, _ = x.shape
    if S_len not in _CACHE:
        nc, S, outs = build(S_len)
        S.emit(final_waits=outs)
        _CACHE[S_len] = nc
    nc = _CACHE[S_len]
    consts = host_consts(S_len)
    wts = {n: np.ascontiguousarray(np.asarray(inputs[n], dtype=np.float32)[0]) for n in WEIGHT_NAMES}
    mem = np.asarray(inputs["mem"], dtype=np.float32)
    in_maps = []
    for b in range(B):
        m = {"x": np.ascontiguousarray(x[b]), "mem": np.ascontiguousarray(mem[b])}
        m.update(wts)
        m.update(consts)
        in_maps.append(m)
    res = run_bass_kernel_spmd(nc, in_maps, core_ids=list(range(B)))
    return np.stack([np.asarray(r["out"], dtype=np.float32) for r in res.results], axis=0)
```

```python
import os
import numpy as np
import ml_dtypes
import concourse.bass as bass
import concourse.mybir as mybir
from concourse.bass_utils import run_bass_kernel_spmd

F32 = mybir.dt.float32
BF16 = mybir.dt.bfloat16
AF = mybir.ActivationFunctionType
ALU = mybir.AluOpType
AX = mybir.AxisListType

ENGS = ("pe", "act", "dve", "pool", "sp")
EPS = 1e-6
D = 1024
NH = 8
DQK = 96
NE = 32
DE = 256
SB_LO = 16640
SB_HI = 228864


class Res:
    __slots__ = ("name", "w", "r")

    def __init__(self, name=""):
        self.name = name
        self.w = None
        self.r = []


class Op:
    __slots__ = ("eng", "fn", "deps", "isdma", "sem", "val", "needs_inc")

    def __init__(self, eng, fn, isdma):
        self.eng = eng
        self.fn = fn
        self.deps = []
        self.isdma = isdma
        self.sem = None
        self.val = None
        self.needs_inc = False


class Sched:
    def __init__(self, nc):
        self.nc = nc
        self.ops = {e: [] for e in ENGS}
        self.nd = {"sp": 24, "pool": 12, "act": 4}
        self.dma_rr = {e: 0 for e in self.nd}
        self.dma_last = {e: [None] * n for e, n in self.nd.items()}
        self.dma_cnt = {e: [0] * n for e, n in self.nd.items()}
        self.last = {e: None for e in ENGS}

    def op(self, eng, fn, reads=(), writes=(), dma=False, extra=()):
        o = Op(eng, fn, dma)
        deps = list(extra)
        for r in reads:
            if r.w is not None:
                deps.append(r.w)
        for w in writes:
            if w.w is not None:
                deps.append(w.w)
            deps.extend(w.r)
        if dma:
            slot = self.dma_rr[eng]
            self.dma_rr[eng] = (slot + 1) % self.nd[eng]
            prev = self.dma_last[eng][slot]
            if prev is not None:
                deps.append(prev)
            self.dma_last[eng][slot] = o
            self.dma_cnt[eng][slot] += 1
            o.sem = ("dma", eng, slot)
            o.val = 16 * self.dma_cnt[eng][slot]
        seen = set()
        for d in deps:
            if d is None or d is o or id(d) in seen:
                continue
            seen.add(id(d))
            if d.eng == "pe" and eng == "pe" and not d.isdma and not dma:
                continue
            o.deps.append(d)
            if not d.isdma:
                d.needs_inc = True
        for r in reads:
            if not dma:
                r.r = [x for x in r.r if x.isdma or x.eng != eng]
            r.r.append(o)
        for w in writes:
            w.w = o
            w.r = []
        self.ops[eng].append(o)
        if not dma:
            self.last[eng] = o
        return o

    def barrier(self):
        deps = [self.last[e] for e in ENGS if self.last[e] is not None]
        for e in self.nd:
            deps.extend(x for x in self.dma_last[e] if x is not None)
        for e in ENGS:
            self.op(e, lambda eng: eng.nop(), extra=deps)

    def emit(self, final_waits=()):
        nc = self.nc
        esem = {e: nc.alloc_semaphore(f"s_{e}") for e in ENGS}
        dsem = {e: [nc.alloc_semaphore(f"d_{e}{i}") for i in range(n)] for e, n in self.nd.items()}
        for e in ENGS:
            c = 0
            for o in self.ops[e]:
                if o.isdma:
                    o.sem = dsem[o.sem[1]][o.sem[2]]
                elif o.needs_inc:
                    c += 1
                    o.sem = esem[e]
                    o.val = c
        emap = {"pe": "tensor", "act": "scalar", "dve": "vector", "pool": "gpsimd", "sp": "sync"}

        def run(e, engobj):
            known = {}
            for o in self.ops[e]:
                need = {}
                for d in o.deps:
                    k = d.sem.num
                    if k not in need or need[k][1] < d.val:
                        need[k] = (d.sem, d.val)
                for k, (s, v) in need.items():
                    if known.get(k, 0) >= v:
                        continue
                    engobj.wait_ge(s, v)
                    known[k] = v
                ins = o.fn(engobj)
                if o.isdma:
                    ins.then_inc(o.sem, 16)
                elif o.needs_inc:
                    ins.then_inc(o.sem, 1)
            if e == "sp":
                for d in final_waits:
                    engobj.wait_ge(d.sem, d.val)

        with nc.Block() as block:
            for e in ENGS:
                getattr(block, emap[e])(lambda engobj, e=e: run(e, engobj))


class Sel:
    REG = []

    def __init__(self, bufs):
        self.bufs, self.i = bufs, 0
        Sel.REG.append(self)

    def __getitem__(self, k):
        return self.bufs[self.i][k]


class RSel:
    def __init__(self, n):
        self.rs, self.i = [Res() for _ in range(n)], 0
        Sel.REG.append(self)

    @property
    def w(self):
        return self.rs[self.i].w

    @w.setter
    def w(self, v):
        self.rs[self.i].w = v

    @property
    def r(self):
        return self.rs[self.i].r

    @r.setter
    def r(self, v):
        self.rs[self.i].r = v


def set_parity(p):
    if os.environ.get("NO_PAR"):
        p = 0
    for x in Sel.REG:
        x.i = p % len(x.bufs if isinstance(x, Sel) else x.rs)


def interleave(gen_iter, ways, admit_every=0):
    active = []
    gen_iter = iter(gen_iter)
    done = False
    rnd = 0
    last_admit = -10 ** 9
    while active or not done:
        while len(active) < ways and not done and (not active or rnd - last_admit >= admit_every):
            try:
                active.append(next(gen_iter))
                last_admit = rnd
            except StopIteration:
                done = True
        rnd += 1
        for g in list(active):
            try:
                next(g)
            except StopIteration:
                active.remove(g)


class Arena:
    def __init__(self, nc, lo, hi):
        self.nc, self.lo, self.hi, self.cur, self.n = nc, lo, hi, lo, 0

    def alloc(self, shape, dtype, name=None):
        nbytes = int(np.prod(shape[1:])) * (2 if dtype == BF16 else 4)
        off = (self.cur + 31) // 32 * 32
        assert off + nbytes <= self.hi, f"SBUF arena overflow {off + nbytes} > {self.hi} ({name})"
        self.cur = off + nbytes
        self.n += 1
        return self.nc.alloc_sbuf_tensor_at(f"{name or 't'}_{off}_{self.n}", list(shape), dtype, offset=off)


WEIGHT_NAMES = ["g_mix", "w_in", "g_cq", "w_uq", "g_ckv", "w_ukv", "g_qn", "g_kn", "conv_w", "conv_b",
                "w_rg", "b_rg", "w_ig", "b_ig", "lam", "g_attn_out", "g_rnn_out", "w_out", "g_xq", "g_mem",
                "w_mq", "w_mk", "w_mv", "g_mqn", "g_mkn", "w_mo", "g_ffn", "w_group", "b_group", "w_expert",
                "b_expert", "w_e_gate", "w_e_up", "w_e_down"]
WEIGHT_SHAPES = {
    "g_mix": [D], "w_in": [D, 1440], "g_cq": [256], "w_uq": [256, 768], "g_ckv": [128], "w_ukv": [128, 1024],
    "g_qn": [96], "g_kn": [96], "conv_w": [4, 512], "conv_b": [512], "w_rg": [8, 64, 64], "b_rg": [512],
    "w_ig": [8, 64, 64], "b_ig": [512], "lam": [512], "g_attn_out": [512], "g_rnn_out": [512], "w_out": [D, D],
    "g_xq": [D], "g_mem": [D], "w_mq": [D, D], "w_mk": [D, D], "w_mv": [D, D], "g_mqn": [256], "g_mkn": [256],
    "w_mo": [D, D], "g_ffn": [D], "w_group": [D, 4], "b_group": [4], "w_expert": [D, 32], "b_expert": [32],
    "w_e_gate": [NE, D, DE], "w_e_up": [NE, D, DE], "w_e_down": [NE, DE, D]}


def build(S_len, phases="ABCD", dbg=False, moe_stop="F"):
    NT = S_len // 128
    NG = S_len // 512
    nc = bass.Bass("TRN2", target_bir_lowering=False)
    x_d = nc.dram_tensor("x", [S_len, D], F32, kind="ExternalInput").ap()
    mem_d = nc.dram_tensor("mem", [256, D], F32, kind="ExternalInput").ap()
    W = {n: nc.dram_tensor(n, WEIGHT_SHAPES[n], F32, kind="ExternalInput").ap() for n in WEIGHT_NAMES}
    cs_d = nc.dram_tensor("cs_tab", [S_len, 64], F32, kind="ExternalInput").ap()
    idb_d = nc.dram_tensor("ident_bf", [128, 128], BF16, kind="ExternalInput").ap()
    idf_d = nc.dram_tensor("ident_f32", [128, 128], F32, kind="ExternalInput").ap()
    out_d = nc.dram_tensor("out", [S_len, D], F32, kind="ExternalOutput").ap()
    skind = "ExternalOutput" if dbg else "Internal"
    QT = nc.dram_tensor("QT", [NH, DQK, S_len], BF16, kind=skind).ap()
    KT = nc.dram_tensor("KT", [NH, DQK, S_len], BF16, kind=skind).ap()
    VA = nc.dram_tensor("VA", [NH, 128, NT, 65], BF16, kind=skind).ap()
    ORT = nc.dram_tensor("ORT", [4, 128, S_len], BF16, kind=skind).ap()
    OA = nc.dram_tensor("OA", [S_len, 512], BF16, kind=skind).ap()
    RR = nc.dram_tensor("RR", [128, NT], F32, kind=skind).ap()
    WGU2 = nc.dram_tensor("WGU2", [NE, 128, 8, 2 * DE], BF16).ap()
    WD2 = nc.dram_tensor("WD2", [NE, 128, 2, D], BF16).ap()
    NSLOT = ((2 * S_len) // 256 + NE) * 256
    H3 = nc.dram_tensor("H3", [S_len, D], BF16).ap()
    Hs = nc.dram_tensor("Hs", [NSLOT, D], BF16).ap()
    Ys = nc.dram_tensor("Ys", [NSLOT, D], BF16).ap()
    lst_d = nc.dram_tensor("lstrict", [128, 128], BF16, kind="ExternalInput").ap()
    thr_d = nc.dram_tensor("thr_tab", [32], F32, kind="ExternalInput").ap()
    iota_d = nc.dram_tensor("iota_tab", [NSLOT // 256], F32, kind="ExternalInput").ap()
    pidx_d = nc.dram_tensor("pidx_tab", [128], F32, kind="ExternalInput").ap()
    if dbg:
        DBG_widx = nc.dram_tensor("DBG_widx", [128, NSLOT // 256], mybir.dt.int32, kind="ExternalOutput").ap()
        DBG_pos = nc.dram_tensor("DBG_pos", [128, 2, NT], mybir.dt.int32, kind="ExternalOutput").ap()
        DBG_gab = nc.dram_tensor("DBG_gab", [128, NT, 2], F32, kind="ExternalOutput").ap()

    S = Sched(nc)
    P = Arena(nc, SB_LO, SB_LO + 6144)
    pairs = [nc.alloc_psum_tensor(f"pair{j}", [128, 1024], F32) for j in range(4)]
    banks = [pairs[i // 2][:, (i % 2) * 512:(i % 2 + 1) * 512] for i in range(8)]
    rb = [Res(f"bank{i}") for i in range(8)]

    def bview(i):
        return pairs[i // 2][:].bitcast(BF16)[:, (i % 2) * 1024:(i % 2 + 1) * 1024]

    def dma(q, out, in_, reads=(), writes=()):
        return S.op(q, lambda e: e.dma_start(out=out, in_=in_), reads=reads, writes=writes, dma=True)

    def dma_nc(q, out, in_, reads=(), writes=()):
        def f(e):
            with nc.allow_non_contiguous_dma(reason="small param vectors"):
                return e.dma_start(out=out, in_=in_)
        return S.op(q, f, reads=reads, writes=writes, dma=True)

    def mm(out, lhsT, rhs, start, stop, reads, writes, skip=False):
        return S.op("pe", lambda e: e.matmul(out, lhsT=lhsT, rhs=rhs, start=start, stop=stop,
                                             skip_group_check=skip), reads, writes)

    def act(out, in_, func, reads, writes, scale=1.0, bias=None, accum=None):
        kw = {}
        if bias is not None:
            kw["bias"] = bias
        if accum is not None:
            kw["accum_out"] = accum
        return S.op("act", lambda e: e.activation(out=out, in_=in_, func=func, scale=scale, **kw), reads, writes)

    def tt(eng, out, in0, in1, op, reads, writes):
        return S.op(eng, lambda e: e.tensor_tensor(out=out, in0=in0, in1=in1, op=op), reads, writes)

    def ts(eng, out, in0, s1, s2, op0, op1, reads, writes):
        if s2 is None:
            return S.op(eng, lambda e: e.tensor_scalar(out=out, in0=in0, scalar1=s1, scalar2=None, op0=op0), reads, writes)
        return S.op(eng, lambda e: e.tensor_scalar(out=out, in0=in0, scalar1=s1, scalar2=s2, op0=op0, op1=op1), reads, writes)

    def stt(eng, out, in0, scalar, in1, op0, op1, reads, writes):
        return S.op(eng, lambda e: e.scalar_tensor_tensor(out=out, in0=in0, scalar=scalar, in1=in1, op0=op0, op1=op1),
                    reads, writes)

    def cp(eng, out, in_, reads, writes):
        if eng == "act":
            return act(out, in_, AF.Copy, reads, writes)
        return S.op(eng, lambda e: e.tensor_copy(out=out, in_=in_), reads, writes)

    def red(out, in_, op, reads, writes):
        return S.op("dve", lambda e: e.tensor_reduce(out=out, in_=in_, axis=AX.X, op=op), reads, writes)

    def recip(out, in_, reads, writes):
        return S.op("dve", lambda e: e.reciprocal(out=out, in_=in_), reads, writes)

    def memset(eng, ap, val, writes):
        return S.op(eng, lambda e: e.memset(ap, val), (), writes)

    idb = P.alloc([128, 128], BF16, "idb"); r_idb = Res()
    idf = P.alloc([128, 128], F32, "idf"); r_idf = Res()
    ones = P.alloc([128, 2], BF16, "ones"); r_ones = Res()
    cneg = P.alloc([128, 1], F32, "cneg"); chalf = P.alloc([128, 1], F32, "chalf"); r_c = Res()
    rr_all = P.alloc([128, max(NT, 8)], F32, "rr_all"); r_rr = Res()
    dma("sp", idb[:], idb_d, writes=[r_idb])
    dma("sp", idf[:], idf_d, writes=[r_idf])
    memset("pool", ones[:], 1.0, [r_ones])
    memset("pool", cneg[:], -0.5, [r_c])
    memset("pool", chalf[:], 0.5, [r_c])
    cone = P.alloc([128, 1], F32, "cone")
    memset("pool", cone[:], 1.0, [r_c])

    def transpose(out, in_, reads, writes, f32=False):
        idt, rid = (idf, r_idf) if f32 else (idb, r_idb)
        return S.op("pe", lambda e: e.transpose(out=out, in_=in_, identity=idt[:]), list(reads) + [rid], writes)

    def rstd_chain(buf, n, inv_dim, reads_writes):
        ts("dve", buf, buf, inv_dim, EPS, ALU.mult, ALU.add, reads_writes, reads_writes)
        act(buf, buf, AF.Ln, reads_writes, reads_writes)
        act(buf, buf, AF.Exp, reads_writes, reads_writes, scale=-0.5)

    def colvec(arena, name, src, ncol):
        t = arena.alloc([128, ncol], F32, name)
        r = Res(name)
        dma_nc("sp", t[:], src.rearrange("(c p) -> p c", p=128), writes=[r])
        return t, r

    def bcast(arena, name, src, n):
        t = arena.alloc([128, n], F32, name)
        r = Res(name)
        dma("sp", t[:], src.partition_broadcast(128), writes=[r])
        return t, r

    r_wgu = [Res() for _ in range(NE)]
    r_wd = [Res() for _ in range(NE)]

    def convert_experts(e0, e1):
        for e in range(e0, min(e1, NE)):
            wg = WGU2[e].rearrange("p k n -> k p n")
            dma("pool", wg[:, :, 0:DE], W["w_e_gate"][e].rearrange("(k p) n -> k p n", p=128), writes=[r_wgu[e]])
            dma("pool", wg[:, :, DE:2 * DE], W["w_e_up"][e].rearrange("(k p) n -> k p n", p=128), writes=[r_wgu[e]])
            dma("pool", WD2[e].rearrange("p c n -> c p n"), W["w_e_down"][e].rearrange("(c p) n -> c p n", p=128),
                writes=[r_wd[e]])

    r_QT, r_KT, r_VA, r_ORT, r_OA = Res(), Res(), Res(), Res(), Res()

    def phaseA():
        A = Arena(nc, SB_LO + 6144, SB_HI)
        w_in = A.alloc([128, 8, 1440], BF16, "w_in"); r_win = Res()
        dma("pool", w_in[:], W["w_in"].rearrange("(k p) n -> p k n", p=128), writes=[r_win])
        w_uq = A.alloc([128, 2, 768], BF16, "w_uq"); r_wuq = Res()
        dma("pool", w_uq[:], W["w_uq"].rearrange("(k p) n -> p k n", p=128), writes=[r_wuq])
        w_ukv = A.alloc([128, 1024], BF16, "w_ukv"); r_wukv = Res()
        dma("pool", w_ukv[:], W["w_ukv"], writes=[r_wukv])
        wrg = A.alloc([128, 4, 128], BF16, "wrg"); wig = A.alloc([128, 4, 128], BF16, "wig"); r_wg = Res()
        memset("pool", wrg[:], 0.0, [r_wg])
        memset("pool", wig[:], 0.0, [r_wg])
        for c in range(4):
            for half in range(2):
                sl = slice(half * 64, half * 64 + 64)
                dma("pool", wrg[sl, c, sl], W["w_rg"][2 * c + half], writes=[r_wg])
                dma("pool", wig[sl, c, sl], W["w_ig"][2 * c + half], writes=[r_wg])
        gmix_b, r_gmix = bcast(A, "gmix_b", W["g_mix"], 1024)
        gq_b, r_gq = bcast(A, "gq_b", W["g_qn"], 96)
        gk_b, r_gk = bcast(A, "gk_b", W["g_kn"], 96)
        ts("dve", gq_b[:], gq_b[:], float(DQK) ** -0.5, None, ALU.mult, None, [r_gq], [r_gq])
        gcq, r_gcq = colvec(A, "gcq", W["g_cq"], 2)
        gckv, r_gckv = colvec(A, "gckv", W["g_ckv"], 1)
        cb, r_cb = colvec(A, "cb", W["conv_b"], 4)
        brg, r_brg = colvec(A, "brg", W["b_rg"], 4)
        big, r_big = colvec(A, "big", W["b_ig"], 4)
        lam, r_lam = colvec(A, "lam", W["lam"], 4)
        cw = A.alloc([128, 4, 4], F32, "cw"); r_cw = Res()
        for j in range(4):
            dma_nc("sp", cw[:, :, j], W["conv_w"][j].rearrange("(c p) -> p c", p=128), writes=[r_cw])
        c1 = A.alloc([128, 4], F32, "c1"); c2 = A.alloc([128, 4], F32, "c2")
        zt = A.alloc([128, 4], F32, "zt"); wv = A.alloc([128, 4], F32, "wv"); w2 = A.alloc([128, 4], F32, "w2")
        r_c12 = Res(); r_z = Res()
        act(zt[:], lam[:], AF.Exp, [r_lam], [r_z], scale=-1.0)
        ts("dve", wv[:], zt[:], 2.0, None, ALU.add, None, [r_z], [r_z])
        S.op("dve", lambda e: e.reciprocal(out=wv[:], in_=wv[:]), [r_z], [r_z])
        tt("dve", wv[:], wv[:], zt[:], ALU.mult, [r_z], [r_z])
        tt("dve", w2[:], wv[:], wv[:], ALU.mult, [r_z], [r_z])
        ts("dve", zt[:], w2[:], 1.0 / 9, 1.0 / 7, ALU.mult, ALU.add, [r_z], [r_z])
        tt("dve", zt[:], zt[:], w2[:], ALU.mult, [r_z], [r_z])
        ts("dve", zt[:], zt[:], 1.0 / 5, None, ALU.add, None, [r_z], [r_z])
        tt("dve", zt[:], zt[:], w2[:], ALU.mult, [r_z], [r_z])
        ts("dve", zt[:], zt[:], 1.0 / 3, None, ALU.add, None, [r_z], [r_z])
        tt("dve", zt[:], zt[:], w2[:], ALU.mult, [r_z], [r_z])
        ts("dve", zt[:], zt[:], 1.0, None, ALU.add, None, [r_z], [r_z])
        tt("dve", zt[:], zt[:], wv[:], ALU.mult, [r_z], [r_z])
        ts("dve", c1[:], zt[:], -16.0, None, ALU.mult, None, [r_z], [r_c12])
        ts("dve", c2[:], zt[:], -32.0, None, ALU.mult, None, [r_z], [r_c12])

        xb = [A.alloc([128, 1024], F32, "xb") for _ in range(4)]; r_xb = [Res() for _ in range(4)]
        junk = A.alloc([128, 1024], BF16, "junk"); r_junk = Res()
        ssx = A.alloc([128, 4], F32, "ssx"); r_ssx = Res()
        hb = [A.alloc([128, 1024], BF16, "hb") for _ in range(2)]; r_hb = [Res() for _ in range(2)]
        hT = [A.alloc([128, 8, 512], BF16, "hT") for _ in range(2)]; r_hT = [Res() for _ in range(2)]
        csb = [A.alloc([128, 4, 64], F32, "csb") for _ in range(2)]; r_cs = [Res() for _ in range(2)]
        cqT = A.alloc([128, 2, 512], BF16, "cqT"); r_cqT = Res()
        ckvT = A.alloc([128, 512], BF16, "ckvT"); r_ckvT = Res()
        sqc = A.alloc([128, 3, 512], BF16, "sqc"); r_sqc = Res()
        sqr = A.alloc([128, 4, 512], BF16, "sqr"); r_sqr = Res()
        ornb = A.alloc([128, 4, 512], BF16, "ornb"); r_ornb = Res()
        uxT = [A.alloc([128, 4, 515], F32, "uxT") for _ in range(2)]; r_ux = [Res() for _ in range(2)]
        memset("pool", uxT[0][:, :, 0:3], 0.0, [r_ux[0]])
        carry = A.alloc([128, 4], F32, "carry"); r_carry = Res()
        memset("pool", carry[:], 0.0, [r_carry])
        def two(name, dt=F32):
            return [A.alloc([128, 512], dt, name) for _ in range(2)], [Res() for _ in range(2)]
        ug, r_ug = two("ug"); gw, r_gw = two("gw"); gel, r_gel = two("gel")
        xc, r_xc = two("xc"); xcb, r_xcb = two("xcb", BF16)
        rg, r_rg = two("rg"); ig, r_ig = two("ig"); av, r_av = two("av"); hs, r_hs = two("hs"); orn, r_orn = two("orn")
        stc = A.alloc([128, 4, 4], F32, "stc"); r_stc = Res()
        qs = A.alloc([128, 8, 96], F32, "qs"); r_qs = Res()
        ks = A.alloc([128, 8, 96], F32, "ks"); r_ks = Res()
        tq = A.alloc([128, 8, 96], F32, "tq"); r_tq = Res()
        tk = A.alloc([128, 8, 96], F32, "tk"); r_tk = Res()
        ssq = A.alloc([128, 16], F32, "ssq"); r_ssq = Res()
        rt1 = A.alloc([128, 8, 32], F32, "rt1"); rt2 = A.alloc([128, 8, 32], F32, "rt2"); r_rt = Res()
        kt1 = A.alloc([128, 8, 32], F32, "kt1"); kt2 = A.alloc([128, 8, 32], F32, "kt2"); r_kt = Res()
        qb = A.alloc([128, 8, 96], BF16, "qb"); r_qb = Res()
        kb = A.alloc([128, 8, 96], BF16, "kb"); r_kb = Res()
        QTst = A.alloc([128, 8, 512], BF16, "QTst"); r_QTst = Res()
        KTst = A.alloc([128, 8, 512], BF16, "KTst"); r_KTst = Res()
        Vst = A.alloc([128, 8, 4, 65], BF16, "Vst"); r_Vst = Res()
        memset("pool", Vst[:], 1.0, [r_Vst])
        print("phase A SBUF used", A.cur)

        ZB = [0, 1]; TB = 2; SB = 3; QB = (4, 5); KVB = (6, 7)
        r_stat_c = Res(); r_stat_r = Res(); r_kr = Res()
        stat_c = banks[SB][:, 0:8].rearrange("p (t c) -> p t c", c=2)
        stat_r = banks[SB][:, 8:12]
        kr_ps = banks[SB][:, 64:192].rearrange("p (t c) -> p t c", c=32)
        zrot = [0]

        def zbank():
            b = ZB[zrot[0] % 2]
            zrot[0] += 1
            return b

        epg = -(-NE // NG)
        for G in range(NG):
            convert_experts(G * epg, (G + 1) * epg)
            hTg, r_hTg = hT[G % 2], r_hT[G % 2]
            cst, r_cst = csb[G % 2], r_cs[G % 2]
            dma("sp", cst[:], cs_d[G * 512:(G + 1) * 512, :].rearrange("(t p) c -> p t c", p=128), writes=[r_cst])
            for t in range(4):
                tok = G * 4 + t
                dma("sp", xb[t][:], x_d[tok * 128:(tok + 1) * 128, :], writes=[r_xb[t]])
                act(junk[:], xb[t][:], AF.Square, [r_xb[t]], [r_junk, r_ssx], accum=ssx[:, t:t + 1])
            rstd_chain(ssx[:], 4, 1.0 / D, [r_ssx])
            for t in range(4):
                h_, r_h = hb[t % 2], r_hb[t % 2]
                stt("dve", h_[:], xb[t][:], ssx[:, t:t + 1], gmix_b[:], ALU.mult, ALU.mult,
                    [r_xb[t], r_ssx, r_gmix], [r_h])
                tv = bview(TB).rearrange("p (k n) -> p k n", k=8)
                for k in range(8):
                    transpose(tv[:, k, :], h_[:, k * 128:(k + 1) * 128], [r_h], [rb[TB]])
                cp("act", hTg[:, :, t * 128:(t + 1) * 128], tv, [rb[TB]], [r_hTg])

            def zmm(col0, ncols, b):
                for k in range(8):
                    mm(banks[b][0:ncols, :], w_in[:, k, col0:col0 + ncols], hTg[:, k, :], k == 0, k == 7,
                       [r_win, r_hTg], [rb[b]])

            for j in range(2):
                b = zbank(); zmm(j * 128, 128, b)
                act(sqc[:, j, :], banks[b][:], AF.Square, [rb[b]], [r_sqc])
                act(cqT[:, j, :], banks[b][:], AF.Copy, [rb[b], r_gcq], [r_cqT], scale=gcq[:, j:j + 1])
            b = zbank(); zmm(256, 128, b)
            act(sqc[:, 2, :], banks[b][:], AF.Square, [rb[b]], [r_sqc])
            act(ckvT[:], banks[b][:], AF.Copy, [rb[b], r_gckv], [r_ckvT], scale=gckv[:, 0:1])
            for t in range(4):
                tsl = slice(t * 128, (t + 1) * 128)
                for j in range(2):
                    mm(stat_c[:, t, 0:1], sqc[:, j, tsl], ones[:, 0:1], j == 0, j == 1, [r_sqc, r_ones], [r_stat_c])
                mm(stat_c[:, t, 1:2], sqc[:, 2, tsl], ones[:, 0:1], True, True, [r_sqc, r_ones], [r_stat_c])
            ts("dve", stc[:, :, 0:1], stat_c[:, :, 0:1], 1.0 / 256, EPS, ALU.mult, ALU.add, [r_stat_c], [r_stc])
            ts("dve", stc[:, :, 1:2], stat_c[:, :, 1:2], 1.0 / 128, EPS, ALU.mult, ALU.add, [r_stat_c], [r_stc])
            stc2 = stc[:, :, 0:2]
            tt("pool", stc2, stc2, cneg[:, 0:1].unsqueeze(2).to_broadcast([128, 4, 2]), ALU.pow, [r_stc, r_c], [r_stc])

            def qk_gen():
                for t in range(4):
                    tsl = slice(t * 128, (t + 1) * 128)
                    qv = [banks[QB[0]][:, 0:384], banks[QB[1]][:, 0:384]]
                    for half in range(2):
                        for j in range(2):
                            mm(qv[half], cqT[:, j, tsl], w_uq[:, j, half * 384:(half + 1) * 384], j == 0, j == 1,
                               [r_cqT, r_wuq], [rb[QB[half]]])
                            yield
                    for half in range(2):
                        mm(banks[KVB[half]][:], ckvT[:, tsl], w_ukv[:, half * 512:(half + 1) * 512], True, True,
                           [r_ckvT, r_wukv], [rb[KVB[half]]])
                        yield
                    for k in range(8):
                        mm(kr_ps[:, t, :], hTg[:, k, tsl], w_in[:, k, 384:416], k == 0, k == 7, [r_hTg, r_win], [r_kr])
                        yield
                    rcq = stc[:, t, 0:1]; rckv = stc[:, t, 1:2]
                    for half in range(2):
                        hs4 = slice(half * 4, half * 4 + 4)
                        act(qs[:, hs4, :], qv[half].rearrange("p (h d) -> p h d", h=4), AF.Copy,
                            [rb[QB[half]], r_stc], [r_qs], scale=rcq)
                        yield
                        kvv = banks[KVB[half]][:].rearrange("p (h d) -> p h d", h=4)
                        act(ks[:, hs4, 0:64], kvv[:, :, 0:64], AF.Copy, [rb[KVB[half]], r_stc], [r_ks], scale=rckv)
                        yield
                        act(Vst[:, hs4, t, 0:64], kvv[:, :, 64:128], AF.Copy, [rb[KVB[half]], r_stc], [r_Vst], scale=rckv)
                        yield
                    cp("dve", ks[:, :, 64:96], kr_ps[:, t, :].unsqueeze(1).to_broadcast([128, 8, 32]), [r_kr], [r_ks])
                    yield
                    act(tq[:], qs[:], AF.Square, [r_qs], [r_tq])
                    yield
                    act(tk[:], ks[:], AF.Square, [r_ks], [r_tk])
                    yield
                    S.op("dve", lambda e: e.tensor_reduce(out=ssq[:, 0:8], in_=tq[:], axis=AX.X, op=ALU.add), [r_tq], [r_ssq])
                    yield
                    S.op("dve", lambda e: e.tensor_reduce(out=ssq[:, 8:16], in_=tk[:], axis=AX.X, op=ALU.add), [r_tk], [r_ssq])
                    yield
                    rstd_chain(ssq[:], 16, 1.0 / DQK, [r_ssq])
                    yield
                    for (src, r_src, tmp, r_tmp, g_b, r_g, o0, t1, t2, r_t, dst, r_dst, st, r_st) in (
                            (qs, r_qs, tq, r_tq, gq_b, r_gq, 0, rt1, rt2, r_rt, qb, r_qb, QTst, r_QTst),
                            (ks, r_ks, tk, r_tk, gk_b, r_gk, 8, kt1, kt2, r_kt, kb, r_kb, KTst, r_KTst)):
                        tt("dve", tmp[:], src[:], ssq[:, o0:o0 + 8].unsqueeze(2).to_broadcast([128, 8, 96]), ALU.mult,
                           [r_src, r_ssq], [r_tmp])
                        yield
                        tt("dve", tmp[:], tmp[:], g_b[:].unsqueeze(1).to_broadcast([128, 8, 96]), ALU.mult,
                           [r_tmp, r_g], [r_tmp])
                        yield
                        c2b = cst[:, t, 0:32].unsqueeze(1).to_broadcast([128, 8, 32])
                        tt("dve", t1[:], tmp[:, :, 64:96], c2b, ALU.mult, [r_tmp, r_cst], [r_t])
                        yield
                        tt("dve", t2[:, :, 0:16], tmp[:, :, 80:96],
                           cst[:, t, 32:48].unsqueeze(1).to_broadcast([128, 8, 16]), ALU.mult, [r_tmp, r_cst], [r_t])
                        yield
                        tt("dve", t2[:, :, 16:32], tmp[:, :, 64:80],
                           cst[:, t, 48:64].unsqueeze(1).to_broadcast([128, 8, 16]), ALU.mult, [r_tmp, r_cst], [r_t])
                        yield
                        tt("dve", dst[:, :, 64:96], t1[:], t2[:], ALU.add, [r_t], [r_dst])
                        yield
                        cp("act", dst[:, :, 0:64], tmp[:, :, 0:64], [r_tmp], [r_dst])
                        yield
                        tv = bview(TB).rearrange("p (h n) -> p h n", h=8)
                        for h in range(NH):
                            transpose(tv[0:96, h, :], dst[:, h, :], [r_dst], [rb[TB]])
                            yield
                        cp("dve", st[0:96, :, tsl], tv[0:96, :, :], [rb[TB]], [r_st])
                        yield


            def rnn_gen():
                U, r_U = uxT[G % 2], r_ux[G % 2]
                Un, r_Un = uxT[(G + 1) % 2], r_ux[(G + 1) % 2]
                for c in range(4):
                    pb = c % 2
                    b = zbank(); zmm(416 + c * 128, 128, b)
                    act(ug[pb][:], banks[b][:], AF.Copy, [rb[b]], [r_ug[pb]])
                    yield
                    act(gw[pb][:], banks[b][:], AF.Square, [rb[b]], [r_gw[pb]])
                    yield
                    ts("dve", gw[pb][:], gw[pb][:], 0.044715, 1.0, ALU.mult, ALU.add, [r_gw[pb]], [r_gw[pb]])
                    yield
                    tt("dve", gw[pb][:], gw[pb][:], ug[pb][:], ALU.mult, [r_gw[pb], r_ug[pb]], [r_gw[pb]])
                    yield
                    act(gw[pb][:], gw[pb][:], AF.Sigmoid, [r_gw[pb]], [r_gw[pb]], scale=1.5957691216057308)
                    yield
                    tt("dve", gel[pb][:], gw[pb][:], ug[pb][:], ALU.mult, [r_gw[pb], r_ug[pb]], [r_gel[pb]])
                    yield
                    b = zbank(); zmm(928 + c * 128, 128, b)
                    act(U[:, c, 3:515], banks[b][:], AF.Copy, [rb[b]], [r_U])
                    yield
                    cp("pool", Un[:, c, 0:3], U[:, c, 512:515], [r_U], [r_Un])
                    yield
                    ts("dve", xc[pb][:], U[:, c, 3:515], cw[:, c, 3:4], cb[:, c:c + 1], ALU.mult, ALU.add,
                       [r_U, r_cw, r_cb], [r_xc[pb]])
                    yield
                    for j in (2, 1, 0):
                        stt("dve", xc[pb][:], U[:, c, j:j + 512], cw[:, c, j:j + 1], xc[pb][:], ALU.mult, ALU.add,
                            [r_U, r_cw, r_xc[pb]], [r_xc[pb]])
                        yield
                    cp("act", xcb[pb][:], xc[pb][:], [r_xc[pb]], [r_xcb[pb]])
                    yield
                    b1 = zbank()
                    mm(banks[b1][:], wrg[:, c, :], xcb[pb][:], True, True, [r_wg, r_xcb[pb]], [rb[b1]])
                    yield
                    act(rg[pb][:], banks[b1][:], AF.Sigmoid, [rb[b1], r_brg], [r_rg[pb]], bias=brg[:, c:c + 1])
                    yield
                    b2 = zbank()
                    mm(banks[b2][:], wig[:, c, :], xcb[pb][:], True, True, [r_wg, r_xcb[pb]], [rb[b2]])
                    yield
                    act(ig[pb][:], banks[b2][:], AF.Sigmoid, [rb[b2], r_big], [r_ig[pb]], bias=big[:, c:c + 1])
                    yield
                    act(av[pb][:], rg[pb][:], AF.Exp, [r_rg[pb], r_c12], [r_av[pb]], scale=c1[:, c:c + 1])
                    yield
                    act(rg[pb][:], rg[pb][:], AF.Exp, [r_rg[pb], r_c12], [r_rg[pb]], scale=c2[:, c:c + 1])
                    yield
                    act(rg[pb][:], rg[pb][:], AF.Relu, [r_rg[pb], r_c], [r_rg[pb]], scale=-1.0, bias=cone[:, 0:1])
                    yield
                    act(rg[pb][:], rg[pb][:], AF.Sqrt, [r_rg[pb]], [r_rg[pb]])
                    yield
                    tt("dve", ig[pb][:], ig[pb][:], xc[pb][:], ALU.mult, [r_ig[pb], r_xc[pb]], [r_ig[pb]])
                    yield
                    tt("dve", rg[pb][:], rg[pb][:], ig[pb][:], ALU.mult, [r_rg[pb], r_ig[pb]], [r_rg[pb]])
                    yield
                    S.op("dve", lambda e, pb=pb, c=c: e.tensor_tensor_scan(
                        out=hs[pb][:], data0=av[pb][:], data1=rg[pb][:], initial=carry[:, c:c + 1],
                        op0=ALU.mult, op1=ALU.add), [r_av[pb], r_rg[pb], r_carry], [r_hs[pb]])
                    yield
                    cp("pool", carry[:, c:c + 1], hs[pb][:, 511:512], [r_hs[pb]], [r_carry])
                    yield
                    tt("dve", orn[pb][:], gel[pb][:], hs[pb][:], ALU.mult, [r_gel[pb], r_hs[pb]], [r_orn[pb]])
                    yield
                    act(sqr[:, c, :], orn[pb][:], AF.Square, [r_orn[pb]], [r_sqr])
                    yield
                    cp("act", ornb[:, c, :], orn[pb][:], [r_orn[pb]], [r_ornb])
                    yield

            gens = [qk_gen(), rnn_gen()]
            while gens:
                for g in list(gens):
                    try:
                        next(g)
                    except StopIteration:
                        gens.remove(g)
            dma("sp", ORT[:, :, G * 512:(G + 1) * 512].rearrange("c p s -> p c s"), ornb[:], [r_ornb], [r_ORT])
            for t in range(4):
                for c in range(4):
                    mm(stat_r[:, t:t + 1], sqr[:, c, t * 128:(t + 1) * 128], ones[:, 0:1], c == 0, c == 3,
                       [r_sqr, r_ones], [r_stat_r])
            rsl = rr_all[:, G * 4:(G + 1) * 4]
            ts("dve", rsl, stat_r, 1.0 / 512, EPS, ALU.mult, ALU.add, [r_stat_r], [r_rr])
            tt("pool", rsl, rsl, cneg[:, 0:1].to_broadcast([128, 4]), ALU.pow, [r_rr, r_c], [r_rr])
            gsl = slice(G * 512, (G + 1) * 512)
            dma("sp", QT[:, :, gsl].rearrange("h d s -> d h s"), QTst[0:96, :, :], [r_QTst], [r_QT])
            dma("sp", KT[:, :, gsl].rearrange("h d s -> d h s"), KTst[0:96, :, :], [r_KTst], [r_KT])
            dma("sp", VA[:, :, G * 4:(G + 1) * 4, :].rearrange("h p t c -> p h t c"), Vst[:], [r_Vst], [r_VA])
        if dbg:
            dma("sp", RR, rr_all[:, 0:NT], [r_rr], [])

    def phaseB():
        Bn = Arena(nc, SB_LO + 6144, SB_HI)
        QTh = [Bn.alloc([128, S_len], BF16, "QTh") for _ in range(2)]; r_QTh = [Res() for _ in range(2)]
        KTh = [Bn.alloc([128, S_len], BF16, "KTh") for _ in range(2)]; r_KTh = [Res() for _ in range(2)]
        Vh = [Bn.alloc([128, NT, 65], BF16, "Vh") for _ in range(2)]; r_Vh = [Res() for _ in range(2)]
        NPT = 4
        pT = [Bn.alloc([128, 1024], BF16, "pT") for _ in range(NPT)]; r_pT = [Res() for _ in range(NPT)]
        ost = [Bn.alloc([128, 4, 64], BF16, "ost") for _ in range(2)]; r_ost = [Res() for _ in range(2)]
        rec = [Bn.alloc([128, 4], F32, "rec") for _ in range(2)]; r_rec = [Res() for _ in range(2)]
        units = []
        for h in range(NH):
            for G in range(NG):
                for kt in range(0, 4 * G, 2):
                    units.append((h, G, [kt, kt + 1]))
                for j in range(4):
                    units.append((h, G, [4 * G + j]))
        LOOK = 2
        NSP = 3
        state = {"s_emitted": 0}

        def load_head(h):
            p = h % 2
            dma("sp", QTh[p][0:96, :], QT[h], [r_QT], [r_QTh[p]])
            dma("sp", KTh[p][0:96, :], KT[h], [r_KT], [r_KTh[p]])
            dma("sp", Vh[p][:], VA[h], [r_VA], [r_Vh[p]])

        def emit_score(u):
            h, G, kts = units[u]
            if G == 0 and kts[0] == 0:
                load_head(h)
            p = h % 2
            sp_ = u % NSP
            for i, kt in enumerate(kts):
                q0 = max(kt - 4 * G, 0) * 128
                mm(pairs[sp_][:, i * 512 + q0:(i + 1) * 512], KTh[p][0:96, kt * 128:(kt + 1) * 128],
                   QTh[p][0:96, G * 512 + q0:(G + 1) * 512], True, True, [r_KTh[p], r_QTh[p]], [rb[2 * sp_]])

        for u, (h, G, kts) in enumerate(units):
            while state["s_emitted"] < min(len(units), u + 1 + LOOK):
                emit_score(state["s_emitted"])
                state["s_emitted"] += 1
            p = h % 2
            sp_ = u % NSP
            pi = u % NPT
            gi = (h * NG + G) % 2
            ob = 6 + gi
            o_ps = banks[ob][:, 0:260].rearrange("p (t c) -> p t c", c=65)
            j0 = kts[0] - 4 * G
            q0 = max(j0, 0) * 128
            w = 512 * len(kts)
            act(pT[pi][:, q0:w], pairs[sp_][:, q0:w], AF.Exp, [rb[2 * sp_]], [r_pT[pi]])
            if j0 >= 0:
                memset("pool", pT[pi][64:128, q0:q0 + 64], 0.0, [r_pT[pi]])
            for i, kt in enumerate(kts):
                j = kt - 4 * G
                for qt in range(max(j, 0), 4):
                    mm(o_ps[:, qt, :], pT[pi][:, i * 512 + qt * 128:i * 512 + (qt + 1) * 128], Vh[p][:, kt, :],
                       kt == 0 and qt == 0, kt == 4 * G + qt, [r_pT[pi], r_Vh[p]], [rb[ob]], skip=True)
            if kts[-1] == 4 * G + 3:
                S.op("dve", lambda e, gi=gi, o_ps=o_ps: e.reciprocal(out=rec[gi][:], in_=o_ps[:, :, 64]),
                     [rb[ob]], [r_rec[gi]])
                tt("dve", ost[gi][:], o_ps[:, :, 0:64], rec[gi][:].unsqueeze(2).to_broadcast([128, 4, 64]), ALU.mult,
                   [rb[ob], r_rec[gi]], [r_ost[gi]])
                dma_nc("sp", OA[G * 512:(G + 1) * 512, h * 64:(h + 1) * 64].rearrange("(t p) d -> p t d", p=128),
                       ost[gi][:], [r_ost[gi]], [r_OA])

    def phaseCD():
        NSETS = int(os.environ.get('NSETS', '4'))
        Cn = Arena(nc, SB_LO + 6144, SB_HI)
        TB = 7
        w_out = Cn.alloc([128, 8, 1024], BF16, "w_out"); r_wout = Res()
        dma("pool", w_out[:], W["w_out"].rearrange("(k p) n -> p k n", p=128), writes=[r_wout])
        grnn, r_grnn = colvec(Cn, "grnn", W["g_rnn_out"], 4)
        for c in range(4):
            ts("dve", w_out[:, 4 + c, :], w_out[:, 4 + c, :], grnn[:, c:c + 1], None, ALU.mult, None,
               [r_wout, r_grnn], [r_wout])
        w_mq = Cn.alloc([128, 8, 1024], BF16, "w_mq"); r_wmq = Res()
        dma("pool", w_mq[:], W["w_mq"].rearrange("(k p) n -> p k n", p=128), writes=[r_wmq])
        w_mo = Cn.alloc([128, 8, 1024], BF16, "w_mo"); r_wmo = Res()
        dma("pool", w_mo[:], W["w_mo"].rearrange("(k p) n -> p k n", p=128), writes=[r_wmo])
        ga_b, r_ga = bcast(Cn, "ga_b", W["g_attn_out"], 512)
        gxq_b, r_gxq = bcast(Cn, "gxq_b", W["g_xq"], 1024)
        gffn_b, r_gffn = bcast(Cn, "gffn_b", W["g_ffn"], 1024)
        gmq_b, r_gmq = bcast(Cn, "gmq_b", W["g_mqn"], 256)
        ts("dve", gmq_b[:], gmq_b[:], 256.0 ** -0.5, None, ALU.mult, None, [r_gmq], [r_gmq])
        wr = Cn.alloc([128, 8, 36], F32, "wr"); r_wr = Res()
        dma_nc("sp", wr[:, :, 0:4], W["w_group"].rearrange("(k p) n -> p k n", p=128), writes=[r_wr])
        dma_nc("sp", wr[:, :, 4:36], W["w_expert"].rearrange("(k p) n -> p k n", p=128), writes=[r_wr])
        br_b = Cn.alloc([128, 36], F32, "br_b"); r_br = Res()
        dma("sp", br_b[:, 0:4], W["b_group"].partition_broadcast(128), writes=[r_br])
        dma("sp", br_b[:, 4:36], W["b_expert"].partition_broadcast(128), writes=[r_br])
        KmT = Cn.alloc([128, 8, 256], BF16, "KmT"); r_KmT = Res()
        Vm = Cn.alloc([128, 2, 4, 256], BF16, "Vm"); r_Vm = Res()
        I32 = mybir.dt.int32
        T_SL = 256
        NTILE = (2 * S_len) // T_SL + NE
        gAB = Cn.alloc([128, NT, 2], F32, "gAB"); r_gAB = Res()
        widx = Cn.alloc([128, NTILE], I32, "widx"); r_widx = Res()
        pos_i = Cn.alloc([128, 2, NT], I32, "pos_i"); r_pos = Res()
        mark2 = Cn.cur
        xt = Sel([Cn.alloc([128, 1024], F32, "xt") for _ in range(NSETS)]); r_xt = RSel(NSETS)
        ssv = Sel([Cn.alloc([128, 4], F32, "ssv") for _ in range(NSETS)]); r_ssv = RSel(NSETS)
        h2 = Sel([Cn.alloc([128, 1024], BF16, "h2") for _ in range(NSETS)]); r_h2 = RSel(NSETS)
        h2T = Sel([Cn.alloc([128, 8, 128], BF16, "h2T") for _ in range(NSETS)]); r_h2T = RSel(NSETS)
        tmpf = Sel([Cn.alloc([128, 1024], F32, "tmpf") for _ in range(NSETS)]); r_tmpf = RSel(NSETS)
        ssm = Sel([Cn.alloc([128, 4], F32, "ssm") for _ in range(NSETS)]); r_ssm = RSel(NSETS)
        qmb = Sel([Cn.alloc([128, 1024], BF16, "qmb") for _ in range(NSETS)]); r_qmb = RSel(NSETS)
        junk = qmb; r_junk = r_qmb
        mark = Cn.cur
        tvb = bview(TB).rearrange("p (k n) -> p k n", k=8)

        def norm_to_bf16(src, r_src, g_b, r_g, dst, r_dst, sscol, f32dst=None, r_f32=None):
            act(junk[:], src, AF.Square, [r_src], [r_junk, r_ssv], accum=ssv[:, sscol:sscol + 1])
            rstd_chain(ssv[:, sscol:sscol + 1], 1, 1.0 / D, [r_ssv])
            if f32dst is None:
                stt("dve", dst, src, ssv[:, sscol:sscol + 1], g_b[:], ALU.mult, ALU.mult, [r_src, r_ssv, r_g], [r_dst])
            else:
                stt("dve", f32dst, src, ssv[:, sscol:sscol + 1], g_b[:], ALU.mult, ALU.mult, [r_src, r_ssv, r_g], [r_f32])
                cp("act", dst, f32dst, [r_f32], [r_dst])

        def transpose8(src, r_src, dstT, r_dstT, n=8, dst_sl=None):
            for k in range(n):
                transpose(tvb[:, k, :], src[:, k * 128:(k + 1) * 128], [r_src], [rb[TB]])
            cp("act", dstT if dst_sl is None else dst_sl, tvb[:, 0:n, :], [rb[TB]], [r_dstT])

        def head_norm(pb0, g_b, r_g, dst, r_dst):
            for half in range(2):
                act(tmpf[:, half * 512:(half + 1) * 512], banks[pb0 + half][:], AF.Square, [rb[pb0 + half]], [r_tmpf])
            red(ssm[:], tmpf[:].rearrange("p (h d) -> p h d", h=4), ALU.add, [r_tmpf], [r_ssm])
            rstd_chain(ssm[:], 4, 1.0 / 256, [r_ssm])
            for half in range(2):
                tt("dve", tmpf[:, half * 512:(half + 1) * 512].rearrange("p (h d) -> p h d", h=2),
                   banks[pb0 + half][:].rearrange("p (h d) -> p h d", h=2),
                   ssm[:, half * 2:half * 2 + 2].unsqueeze(2).to_broadcast([128, 2, 256]), ALU.mult,
                   [rb[pb0 + half], r_ssm], [r_tmpf])
            tt("dve", dst.rearrange("p (h d) -> p h d", h=4), tmpf[:].rearrange("p (h d) -> p h d", h=4),
               g_b[:].unsqueeze(1).to_broadcast([128, 4, 256]), ALU.mult, [r_tmpf, r_g], [r_dst])

        w_mk = Cn.alloc([128, 8, 1024], BF16, "w_mk"); r_wmk = Res()
        dma("pool", w_mk[:], W["w_mk"].rearrange("(k p) n -> p k n", p=128), writes=[r_wmk])
        w_mv = Cn.alloc([128, 8, 1024], BF16, "w_mv"); r_wmv = Res()
        dma("pool", w_mv[:], W["w_mv"].rearrange("(k p) n -> p k n", p=128), writes=[r_wmv])
        gmem_b, r_gmem = bcast(Cn, "gmem_b", W["g_mem"], 1024)
        gmk_b, r_gmk = bcast(Cn, "gmk_b", W["g_mkn"], 256)
        mnT = Cn.alloc([128, 8, 256], BF16, "mnT"); r_mnT = Res()
        for mt in range(2):
            dma("sp", xt[:], mem_d[mt * 128:(mt + 1) * 128, :], writes=[r_xt])
            norm_to_bf16(xt[:], r_xt, gmem_b, r_gmem, h2[:], r_h2, 0)
            transpose8(h2, r_h2, None, r_mnT, dst_sl=mnT[:, :, mt * 128:(mt + 1) * 128])
        for mt in range(2):
            msl = slice(mt * 128, (mt + 1) * 128)
            for half in range(2):
                for k in range(8):
                    mm(banks[half][:], mnT[:, k, msl], w_mk[:, k, half * 512:(half + 1) * 512], k == 0, k == 7,
                       [r_mnT, r_wmk], [rb[half]])
                for k in range(8):
                    mm(banks[2 + half][:], mnT[:, k, msl], w_mv[:, k, half * 512:(half + 1) * 512], k == 0, k == 7,
                       [r_mnT, r_wmv], [rb[2 + half]])
                act(Vm[:, mt, half * 2:half * 2 + 2, :], banks[2 + half][:].rearrange("p (h d) -> p h d", h=2),
                    AF.Copy, [rb[2 + half]], [r_Vm])
            head_norm(0, gmk_b, r_gmk, qmb[:], r_qmb)
            transpose8(qmb, r_qmb, None, r_KmT, dst_sl=KmT[:, :, msl])
        S.barrier()
        Cn.cur = mark

        oa = Sel([Cn.alloc([128, 512], BF16, "oa") for _ in range(NSETS)]); r_oa = RSel(NSETS)
        ornT = Sel([Cn.alloc([128, 4, 128], BF16, "ornT") for _ in range(NSETS)]); r_ornT = RSel(NSETS)
        mixA = Sel([Cn.alloc([128, 512], BF16, "mixA") for _ in range(NSETS)]); r_mixA = RSel(NSETS)
        mixAT = Sel([Cn.alloc([128, 4, 128], BF16, "mixAT") for _ in range(NSETS)]); r_mixAT = RSel(NSETS)
        x1 = Sel([Cn.alloc([128, 1024], F32, "x1") for _ in range(NSETS)]); r_x1 = RSel(NSETS)
        qmT = h2T; r_qmT = r_h2T
        pm_ = h2; r_pm = r_h2
        recm = Sel([Cn.alloc([128, 4], F32, "recm") for _ in range(NSETS)]); r_recm = RSel(NSETS)
        omb = h2; r_omb = r_h2
        omT = h2T; r_omT = r_h2T
        h3f = tmpf; r_h3f = r_tmpf
        h3 = qmb; r_h3 = r_qmb
        h3fT = Sel([Cn.alloc([128, 8, 128], F32, "h3fT") for _ in range(NSETS)]); r_h3fT = RSel(NSETS)
        lg = Sel([Cn.alloc([128, 36], F32, "lg") for _ in range(NSETS)]); r_lg = RSel(NSETS)
        rt = Sel([Cn.alloc([128, 64], F32, "rt") for _ in range(NSETS)]); r_rt = RSel(NSETS)
        I32 = mybir.dt.int32
        T_SL = 256
        NTILE = (2 * S_len) // T_SL + NE
        WGUv = WGU2.rearrange("e p k n -> (e p) (k n)")
        WDv = WD2.rearrange("e p c n -> (e p) (c n)")
        x2 = xt; r_x2 = r_xt
        rank_all = Cn.alloc([128, NT, 32], F32, "rank_all"); r_rank = Res()
        selA = Cn.alloc([128, NT, 32], BF16, "selA"); selB = Cn.alloc([128, NT, 32], BF16, "selB"); r_sel = Res()
        carryc = Cn.alloc([128, 32], F32, "carryc"); r_carryc = Res()
        memset("pool", carryc[:], 0.0, [r_carryc])
        Mf = Sel([Cn.alloc([128, 32], F32, "Mf") for _ in range(2)])
        Mb = Sel([Cn.alloc([128, 32], BF16, "Mb") for _ in range(NSETS)]); r_M = RSel(NSETS)
        Lst = Cn.alloc([128, 128], BF16, "Lst"); r_Lst = Res()
        dma("sp", Lst[:], lst_d, writes=[r_Lst])
        ones128 = Cn.alloc([128, 128], BF16, "ones128")
        memset("pool", ones128[:], 1.0, [r_Lst])
        r_H3 = [Res() for _ in range(NT)]
        r_out = [Res() for _ in range(NT)]
        print("phase C SBUF used", Cn.cur, "of", SB_HI)
        N_SKEW = int(os.environ.get('N_SKEW', '2'))

        def tile_body(gt):
            if True:
                t8 = 0
                rows = slice(gt * 128, (gt + 1) * 128)
                dma("sp", xt[:], x_d[rows, :], writes=[r_xt])
                yield
                dma("sp", oa[:], OA[rows, :], [r_OA], [r_oa])
                yield
                dma("sp", ornT[:], ORT[:, :, rows].rearrange("c p s -> p c s"), [r_ORT], [r_ornT])
                yield
                act(junk[:, 0:512], oa[:], AF.Square, [r_oa], [r_junk, r_ssv], accum=ssv[:, 0:1])
                yield
                rstd_chain(ssv[:, 0:1], 1, 1.0 / 512, [r_ssv])
                yield
                tt("dve", mixA[:], oa[:], ga_b[:], ALU.mult, [r_oa, r_ga], [r_mixA])
                yield
                transpose8(mixA, r_mixA, mixAT[:], r_mixAT, n=4)
                yield
                for half in range(2):
                    hsl = slice(half * 512, (half + 1) * 512)
                    for k in range(4):
                        mm(banks[half][:], mixAT[:, k, :], w_out[:, k, hsl], k == 0, k == 3, [r_mixAT, r_wout], [rb[half]])
                    for k in range(4):
                        mm(banks[2 + half][:], ornT[:, k, :], w_out[:, 4 + k, hsl], k == 0, k == 3,
                           [r_ornT, r_wout], [rb[2 + half]])
                    stt("dve", x1[:, hsl], banks[half][:], ssv[:, 0:1], xt[:, hsl], ALU.mult, ALU.add,
                        [rb[half], r_ssv, r_xt], [r_x1])
                    stt("dve", x1[:, hsl], banks[2 + half][:], rr_all[:, gt:gt + 1], x1[:, hsl], ALU.mult, ALU.add,
                        [rb[2 + half], r_rr, r_x1], [r_x1])
                yield
                norm_to_bf16(x1[:], r_x1, gxq_b, r_gxq, h2[:], r_h2, 1)
                yield
                transpose8(h2, r_h2, h2T[:], r_h2T)
                yield
                for half in range(2):
                    for k in range(8):
                        mm(banks[4 + half][:], h2T[:, k, :], w_mq[:, k, half * 512:(half + 1) * 512], k == 0, k == 7,
                           [r_h2T, r_wmq], [rb[4 + half]])
                head_norm(4, gmq_b, r_gmq, qmb[:], r_qmb)
                yield
                transpose8(qmb, r_qmb, qmT[:], r_qmT)
                yield
                for hh in range(4):
                    for mt in range(2):
                        slot = hh * 2 + mt
                        for kk in range(2):
                            mm(banks[slot // 4][:, (slot % 4) * 128:(slot % 4 + 1) * 128],
                               KmT[:, hh * 2 + kk, mt * 128:(mt + 1) * 128], qmT[:, hh * 2 + kk, :], kk == 0, kk == 1,
                               [r_KmT, r_qmT], [rb[slot // 4]])
                for bk in range(2):
                    act(pm_[:].rearrange("p (s n) -> p s n", s=8)[:, bk * 4:(bk + 1) * 4, :], banks[bk][:].rearrange("p (s n) -> p s n", s=4), AF.Exp,
                        [rb[bk]], [r_pm])
                yield
                for hh in range(4):
                    ob = 2 + hh // 2
                    for mt in range(2):
                        mm(banks[ob][:, (hh % 2) * 256:(hh % 2 + 1) * 256], pm_[:, (hh * 2 + mt) * 128:(hh * 2 + mt + 1) * 128], Vm[:, mt, hh, :],
                           mt == 0, mt == 1, [r_pm, r_Vm], [rb[ob]])
                    for mt in range(2):
                        mm(banks[6][:, hh:hh + 1], pm_[:, (hh * 2 + mt) * 128:(hh * 2 + mt + 1) * 128], ones[:, 0:1], mt == 0, mt == 1,
                           [r_pm, r_ones], [rb[6]])
                recip(recm[:], banks[6][:, 0:4], [rb[6]], [r_recm])
                for bk in range(2):
                    tt("dve", omb[:, bk * 512:(bk + 1) * 512].rearrange("p (h d) -> p h d", h=2),
                       banks[2 + bk][:].rearrange("p (h d) -> p h d", h=2),
                       recm[:, bk * 2:bk * 2 + 2].unsqueeze(2).to_broadcast([128, 2, 256]), ALU.mult,
                       [rb[2 + bk], r_recm], [r_omb])
                yield
                transpose8(omb, r_omb, omT[:], r_omT)
                yield
                for half in range(2):
                    hsl = slice(half * 512, (half + 1) * 512)
                    for k in range(8):
                        mm(banks[4 + half][:], omT[:, k, :], w_mo[:, k, hsl], k == 0, k == 7, [r_omT, r_wmo], [rb[4 + half]])
                    tt("dve", x2[:, hsl], banks[4 + half][:], x1[:, hsl], ALU.add, [rb[4 + half], r_x1], [r_x2])
                yield
                dma("sp", out_d[rows, :], x2[:], [r_x2], [r_out[gt]])
                yield
                act(junk[:], x2[:], AF.Square, [r_x2], [r_junk, r_ssv], accum=ssv[:, 2:3])
                yield
                rstd_chain(ssv[:, 2:3], 1, 1.0 / D, [r_ssv])
                yield
                stt("dve", h3f[:], x2[:], ssv[:, 2:3], gffn_b[:], ALU.mult, ALU.mult, [r_x2, r_ssv, r_gffn], [r_h3f])
                yield
                cp("act", h3[:], h3f[:], [r_h3f], [r_h3])
                yield
                dma("sp", H3[rows, :], h3[:], [r_h3], [r_H3[gt]])
                yield
                for k in range(8):
                    transpose(banks[k // 4][:, (k % 4) * 128:(k % 4 + 1) * 128], h3f[:, k * 128:(k + 1) * 128],
                              [r_h3f], [rb[k // 4]], f32=True)
                for bk in range(2):
                    cp("act", h3fT[:, bk * 4:(bk + 1) * 4, :], banks[bk][:].rearrange("p (s n) -> p s n", s=4),
                       [rb[bk]], [r_h3fT])
                yield
                for k in range(8):
                    mm(banks[6][:, 64:100], h3fT[:, k, :], wr[:, k, :], k == 0, k == 7, [r_h3fT, r_wr], [rb[6]])
                tt("dve", lg[:], banks[6][:, 64:100], br_b[:], ALU.add, [rb[6], r_br], [r_lg])
                R = [r_lg, r_rt]
                gmax, ngmax, sumg, oh = rt[:, 0:1], rt[:, 1:2], rt[:, 2:3], rt[:, 4:8]
                eg, es, emax, nemax = rt[:, 8:12], rt[:, 16:24], rt[:, 12:13], rt[:, 13:14]
                ex, top8, den, msk = rt[:, 24:32], rt[:, 32:40], rt[:, 14:15], rt[:, 40:48]
                sel32 = tmpf[:, 0:32]
                yield
                red(gmax, lg[:, 0:4], ALU.max, R, [r_rt])
                yield
                ts("dve", oh, lg[:, 0:4], gmax, None, ALU.is_ge, None, R, [r_rt])
                yield
                ts("dve", ngmax, gmax, -1.0, None, ALU.mult, None, R, [r_rt])
                yield
                act(eg, lg[:, 0:4], AF.Exp, R, [r_rt], bias=ngmax, accum=sumg)
                yield
                recip(sumg, sumg, R, [r_rt])
                yield
                tt("dve", sel32.rearrange("p (g e) -> p g e", g=4), lg[:, 4:36].rearrange("p (g e) -> p g e", g=4),
                   oh.unsqueeze(2).to_broadcast([128, 4, 8]), ALU.mult, R, [r_tmpf])
                yield
                red(es, sel32.rearrange("p (g e) -> p e g", g=4), ALU.add, [r_tmpf], [r_rt])
                yield
                red(emax, es, ALU.max, R, [r_rt])
                yield
                ts("dve", nemax, emax, -1.0, None, ALU.mult, None, R, [r_rt])
                yield
                act(ex, es, AF.Exp, R, [r_rt], bias=nemax)
                yield
                S.op("dve", lambda e, top8=top8, ex=ex: e.max(out=top8, in_=ex), R, [r_rt])
                yield
                tt("dve", den, top8[:, 0:1], top8[:, 1:2], ALU.add, R, [r_rt])
                yield
                recip(den, den, R, [r_rt])
                yield
                tt("dve", den, den, sumg, ALU.mult, R, [r_rt])
                mskA, msk2 = rt[:, 48:56], rt[:, 40:48]
                yield
                ts("dve", mskA, ex, top8[:, 0:1], None, ALU.is_ge, None, R, [r_rt])
                yield
                ts("dve", msk2, ex, top8[:, 1:2], None, ALU.is_ge, None, R, [r_rt])
                ohb = oh.unsqueeze(2).to_broadcast([128, 4, 8])
                yield
                tt("dve", selA[:, gt, :].rearrange("p (g e) -> p g e", g=4), ohb,
                   mskA.unsqueeze(1).to_broadcast([128, 4, 8]), ALU.mult, R, [r_sel])
                yield
                tt("dve", Mf[:].rearrange("p (g e) -> p g e", g=4), ohb,
                   msk2.unsqueeze(1).to_broadcast([128, 4, 8]), ALU.mult, R, [r_M])
                yield
                tt("dve", selB[:, gt, :], Mf[:], selA[:, gt, :], ALU.subtract, [r_M, r_sel], [r_sel])
                yield
                ts("dve", gAB[:, gt, :], top8[:, 0:2], den, None, ALU.mult, None, R, [r_gAB])
                yield
                cp("dve", Mb[:], Mf[:], [r_M], [r_M])
                yield
                mm(banks[6][:, 128:160], Lst[:], Mb[:], True, True, [r_Lst, r_M], [rb[6]])
                mm(banks[6][:, 160:192], ones128[:], Mb[:], True, True, [r_Lst, r_M], [rb[6]])
                tt("dve", rank_all[:, gt, :], banks[6][:, 128:160], carryc[:], ALU.add, [rb[6], r_carryc], [r_rank])
                tt("dve", carryc[:], banks[6][:, 160:192], carryc[:], ALU.add, [rb[6], r_carryc], [r_carryc])

        FILL = int(os.environ.get("FILL", "0"))
        fcount = [0]

        def wrap(gt):
            g = tile_body(gt)
            while True:
                set_parity(gt)
                try:
                    next(g)
                except StopIteration:
                    return
                fcount[0] += 1
                if FILL and fcount[0] % FILL == 0:
                    S.op("pe", lambda e: e.matmul(banks[6][:, 192:512], lhsT=idb[:], rhs=w_out[:, 0, 0:320],
                                                  start=True, stop=True, skip_group_check=True), [], [])
                yield

        interleave((wrap(gt) for gt in range(NT)), NSETS, admit_every=N_SKEW)
        set_parity(0)

        S.barrier()
        top_c = Cn.cur
        Cn.cur = mark2
        thr_b, r_thr = bcast(Cn, "thr_b", thr_d, 32)
        iota_b, r_iota = bcast(Cn, "iota_b", iota_d, NTILE)
        pidx, r_pidx = colvec(Cn, "pidx", pidx_d, 1)
        onesf = Cn.alloc([128, 32], F32, "onesf"); r_onesf = Res()
        memset("pool", onesf[:], 1.0, [r_onesf])
        ntile = Cn.alloc([128, 32], F32, "ntile"); endc = Cn.alloc([128, 32], F32, "endc")
        startT = Cn.alloc([128, 32], F32, "startT"); r_bk = Res()
        cmpi = Cn.alloc([128, NTILE, 32], F32, "cmpi"); r_cmpi = Res()
        tef = Cn.alloc([128, NTILE], F32, "tef"); r_tef = Res()
        posf = Cn.alloc([128, 2, NT], F32, "posf"); r_posf = Res()
        cmp3t = Cn.alloc([128, 1024], F32, "cmp3t"); r_tmpf = Res()
        cmp3 = cmp3t[:].rearrange("p (e m) -> p e m", e=32)
        assert Cn.cur <= top_c
        tt("dve", cmp3, carryc[:].unsqueeze(2).to_broadcast([128, 32, 32]),
           thr_b[:].unsqueeze(1).to_broadcast([128, 32, 32]), ALU.is_gt, [r_carryc, r_thr], [r_tmpf])
        S.op("dve", lambda e: e.tensor_reduce(out=ntile[:], in_=cmp3, axis=AX.X, op=ALU.add), [r_tmpf], [r_bk])
        S.op("dve", lambda e: e.tensor_tensor_scan(out=endc[:], data0=onesf[:], data1=ntile[:], initial=0.0,
                                                   op0=ALU.mult, op1=ALU.add), [r_bk, r_onesf], [r_bk])
        tt("dve", startT[:], endc[:], ntile[:], ALU.subtract, [r_bk], [r_bk])
        ts("dve", startT[:], startT[:], float(T_SL), None, ALU.mult, None, [r_bk], [r_bk])
        tt("dve", cmpi[:], iota_b[:].unsqueeze(2).to_broadcast([128, NTILE, 32]),
           endc[:].unsqueeze(1).to_broadcast([128, NTILE, 32]), ALU.is_ge, [r_iota, r_bk], [r_cmpi])
        S.op("dve", lambda e: e.tensor_reduce(out=tef[:], in_=cmpi[:], axis=AX.X, op=ALU.add), [r_cmpi], [r_tef])
        ts("dve", tef[:], tef[:], float(NE - 1), None, ALU.min, None, [r_tef], [r_tef])
        ts("dve", tef[:], tef[:], 128.0, pidx[:, 0:1], ALU.mult, ALU.add, [r_tef, r_pidx], [r_tef])
        cp("dve", widx[:], tef[:], [r_tef], [r_widx])
        tt("dve", rank_all[:], rank_all[:], startT[:].unsqueeze(1).to_broadcast([128, NT, 32]), ALU.add,
           [r_rank, r_bk], [r_rank])
        prod = cmpi[:, 0:NT, :]
        tt("dve", prod, selA[:], rank_all[:], ALU.mult, [r_sel, r_rank, r_tef], [r_cmpi])
        red(posf[:, 0, :], prod, ALU.add, [r_cmpi], [r_posf])
        tt("dve", prod, selB[:], rank_all[:], ALU.mult, [r_sel, r_rank, r_posf], [r_cmpi])
        red(posf[:, 1, :], prod, ALU.add, [r_cmpi], [r_posf])
        cp("dve", pos_i[:], posf[:], [r_posf], [r_pos])
        if dbg:
            dma("sp", DBG_widx, widx[:], [r_widx], [])
            dma("sp", DBG_pos, pos_i[:], [r_pos], [])
            dma("sp", DBG_gab, gAB[:], [r_gAB], [])
        S.barrier()
        Cn.cur = mark2
        if moe_stop == "C":
            return [dma("sp", out_d[0:128, :], x_d[0:128, :])]

        hsb = [Cn.alloc([128, 1024], BF16, "hsb") for _ in range(2)]; r_hsb = [Res() for _ in range(2)]
        for gt in range(NT):
            q = gt % 2
            dma("sp", hsb[q][:], H3[gt * 128:(gt + 1) * 128, :], [r_H3[gt]], [r_hsb[q]])
            for j in range(2):
                S.op("pool", lambda e, q=q, gt=gt, j=j: e.indirect_dma_start(
                    out=Hs[:, :], out_offset=bass.IndirectOffsetOnAxis(ap=pos_i[:, j, gt:gt + 1], axis=0),
                    in_=hsb[q][:, :], in_offset=None), [r_hsb[q], r_pos], [Res()], dma=True)
        S.barrier()
        if moe_stop == "S":
            return [dma("sp", out_d[0:128, :], x_d[0:128, :])]

        Wgu2 = [Cn.alloc([128, 4096], BF16, "Wgu2") for _ in range(2)]; r_Wgu2 = [Res() for _ in range(2)]
        Wd2 = [Cn.alloc([128, 2048], BF16, "Wd2") for _ in range(2)]; r_Wd2 = [Res() for _ in range(2)]
        hst = [Cn.alloc([128, 1024], BF16, "hst") for _ in range(2)]; r_hst = [Res() for _ in range(2)]
        hTs = [Cn.alloc([128, 8, 128], BF16, "hTs") for _ in range(2)]; r_hTs = [Res() for _ in range(2)]
        sgt = [Cn.alloc([128, 256], F32, "sgt") for _ in range(2)]; r_sgt = [Res() for _ in range(2)]
        het = [Cn.alloc([128, 256], BF16, "het") for _ in range(2)]; r_het = [Res() for _ in range(2)]
        heT = [Cn.alloc([128, 2, 128], BF16, "heT") for _ in range(2)]; r_heT = [Res() for _ in range(2)]
        yst = [Cn.alloc([128, 1024], BF16, "yst") for _ in range(2)]; r_yst = [Res() for _ in range(2)]
        NWB = 3
        NSET = 4
        for lst_, shape, dt_, nm in ((Wgu2, [128, 4096], BF16, "Wgu2"), (Wd2, [128, 2048], BF16, "Wd2")):
            while len(lst_) < NWB:
                lst_.append(Cn.alloc(shape, dt_, nm))
        r_Wgu2 = [Res() for _ in range(NWB)]; r_Wd2 = [Res() for _ in range(NWB)]
        for lst_, shape, dt_, nm in ((hst, [128, 1024], BF16, "hst"), (hTs, [128, 8, 128], BF16, "hTs"),
                                     (sgt, [128, 256], F32, "sgt"), (het, [128, 256], BF16, "het"),
                                     (heT, [128, 2, 128], BF16, "heT"), (yst, [128, 1024], BF16, "yst")):
            while len(lst_) < NSET:
                lst_.append(Cn.alloc(shape, dt_, nm))
        r_hst = [Res() for _ in range(NSET)]; r_hTs = [Res() for _ in range(NSET)]; r_sgt = [Res() for _ in range(NSET)]
        r_het = [Res() for _ in range(NSET)]; r_heT = [Res() for _ in range(NSET)]; r_yst = [Res() for _ in range(NSET)]

        def sub_gen(i, sub, q):
            p = i % NWB
            if sub == 0:
                S.op("pool", lambda e, p=p, i=i: e.indirect_dma_start(
                    out=Wgu2[p][:, :], out_offset=None, in_=WGUv,
                    in_offset=bass.IndirectOffsetOnAxis(ap=widx[:, i:i + 1], axis=0)), [r_widx], [r_Wgu2[p]], dma=True)
                S.op("pool", lambda e, p=p, i=i: e.indirect_dma_start(
                    out=Wd2[p][:, :], out_offset=None, in_=WDv,
                    in_offset=bass.IndirectOffsetOnAxis(ap=widx[:, i:i + 1], axis=0)), [r_widx], [r_Wd2[p]], dma=True)
            r0 = i * T_SL + sub * 128
            dma("sp", hst[q][:], Hs[r0:r0 + 128, :], [], [r_hst[q]])
            yield
            transpose8(hst[q], r_hst[q], hTs[q][:], r_hTs[q])
            yield
            gb = q
            for k in range(8):
                mm(banks[gb][:], hTs[q][:, k, :], Wgu2[p][:, k * 512:(k + 1) * 512], k == 0, k == 7,
                   [r_hTs[q], r_Wgu2[p]], [rb[gb]])
            yield
            act(sgt[q][:], banks[gb][:, 0:256], AF.Silu, [rb[gb]], [r_sgt[q]])
            yield
            tt("dve", het[q][:], sgt[q][:], banks[gb][:, 256:512], ALU.mult, [r_sgt[q], rb[gb]], [r_het[q]])
            yield
            transpose8(het[q], r_het[q], heT[q][:], r_heT[q], n=2)
            yield
            yb = (4, 5)
            for half in range(2):
                for c in range(2):
                    mm(banks[yb[half]][:], heT[q][:, c, :], Wd2[p][:, c * 1024 + half * 512:c * 1024 + (half + 1) * 512],
                       c == 0, c == 1, [r_heT[q], r_Wd2[p]], [rb[yb[half]]])
            cp("act", yst[q][:, 0:512], banks[yb[0]][:], [rb[yb[0]]], [r_yst[q]])
            cp("dve", yst[q][:, 512:1024], banks[yb[1]][:], [rb[yb[1]]], [r_yst[q]])
            yield
            dma("sp", Ys[r0:r0 + 128, :], yst[q][:], [r_yst[q]], [Res()])

        def all_subs():
            cnt = 0
            for i in range(NTILE):
                for sub in range(T_SL // 128):
                    yield sub_gen(i, sub, cnt % NSET)
                    cnt += 1

        interleave(all_subs(), NSET)
        S.barrier()
        if moe_stop == "E":
            return [dma("sp", out_d[0:128, :], x_d[0:128, :])]

        xo = [Cn.alloc([128, 1024], F32, "xo") for _ in range(2)]; r_xo = [Res() for _ in range(2)]
        yA = [Cn.alloc([128, 1024], BF16, "yA") for _ in range(2)]; r_yA = [Res() for _ in range(2)]
        yB = [Cn.alloc([128, 1024], BF16, "yB") for _ in range(2)]; r_yB = [Res() for _ in range(2)]
        print("phase E/F SBUF used", Cn.cur, "of", SB_HI)
        outs = []
        for gt in range(NT):
            q = gt % 2
            rows = slice(gt * 128, (gt + 1) * 128)
            dma("sp", xo[q][:], out_d[rows, :], [r_out[gt]], [r_xo[q]])
            for j, (yy, r_yy) in enumerate(((yA, r_yA), (yB, r_yB))):
                S.op("pool", lambda e, q=q, gt=gt, j=j, yy=yy: e.indirect_dma_start(
                    out=yy[q][:, :], out_offset=None, in_=Ys[:, :],
                    in_offset=bass.IndirectOffsetOnAxis(ap=pos_i[:, j, gt:gt + 1], axis=0)), [r_pos], [r_yy[q]], dma=True)
            stt("dve", xo[q][:], yA[q][:], gAB[:, gt, 0:1], xo[q][:], ALU.mult, ALU.add, [r_yA[q], r_gAB, r_xo[q]], [r_xo[q]])
            stt("dve", xo[q][:], yB[q][:], gAB[:, gt, 1:2], xo[q][:], ALU.mult, ALU.add, [r_yB[q], r_gAB, r_xo[q]], [r_xo[q]])
            outs.append(dma("sp", out_d[rows, :], xo[q][:], [r_xo[q]], [r_out[gt]]))
        return outs

    if "A" in phases:
        phaseA()
    S.barrier()
    if "B" in phases:
        phaseB()
    S.barrier()
    S.barrier()
    outs = []
    if "D" in phases:
        outs = phaseCD()
    else:
        outs = [dma("sp", out_d[0:128, :], x_d[0:128, :])]
    return nc, S, outs


def host_consts(S_len):
    pos = np.arange(S_len, dtype=np.float32)
    inv_freq = (np.float32(10000.0) ** (-np.arange(0, 32, 2, dtype=np.float32) / np.float32(32))).astype(np.float32)
    ang = (pos[:, None] * inv_freq[None, :]).astype(np.float32)
    c, s = np.cos(ang).astype(np.float32), np.sin(ang).astype(np.float32)
    cs = np.concatenate([c, c, -s, s], axis=1).astype(np.float32)
    ntile = (2 * S_len) // 256 + NE
    lst = np.triu(np.ones((128, 128), np.float32), 1).astype(ml_dtypes.bfloat16)
    return {"cs_tab": cs, "ident_bf": np.eye(128).astype(ml_dtypes.bfloat16), "ident_f32": np.eye(128, dtype=np.float32),
            "lstrict": lst, "thr_tab": (np.arange(32) * 256).astype(np.float32),
            "iota_tab": np.arange(ntile).astype(np.float32), "pidx_tab": np.arange(128).astype(np.float32)}


_CACHE = {}


def kernel(**inputs):
    x = np.asarray(inputs["x"], dtype=np.float32)
    B, S_len, _ = x.shape
    if S_len not in _CACHE:
        nc, S, outs = build(S_len)
        S.emit(final_waits=outs)
        _CACHE[S_len] = nc
    nc = _CACHE[S_len]
    consts = host_consts(S_len)
    wts = {n: np.ascontiguousarray(np.asarray(inputs[n], dtype=np.float32)[0]) for n in WEIGHT_NAMES}
    mem = np.asarray(inputs["mem"], dtype=np.float32)
    in_maps = []
    for b in range(B):
        m = {"x": np.ascontiguousarray(x[b]), "mem": np.ascontiguousarray(mem[b])}
        m.update(wts)
        m.update(consts)
        in_maps.append(m)
    res = run_bass_kernel_spmd(nc, in_maps, core_ids=list(range(B)))
    return np.stack([np.asarray(r["out"], dtype=np.float32) for r in res.results], axis=0)
```

```python
import os
import numpy as np
import ml_dtypes
import concourse.bass as bass
import concourse.mybir as mybir
from concourse.bass_utils import run_bass_kernel_spmd

F32 = mybir.dt.float32
BF16 = mybir.dt.bfloat16
AF = mybir.ActivationFunctionType
ALU = mybir.AluOpType
AX = mybir.AxisListType

ENGS = ("pe", "act", "dve", "pool", "sp")
EPS = 1e-6
D = 1024
NH = 8
DQK = 96
NE = 32
DE = 256
SB_LO = 16640
SB_HI = 228864


class Res:
    __slots__ = ("name", "w", "r")

    def __init__(self, name=""):
        self.name = name
        self.w = None
        self.r = []


class Op:
    __slots__ = ("eng", "fn", "deps", "isdma", "sem", "val", "needs_inc")

    def __init__(self, eng, fn, isdma):
        self.eng = eng
        self.fn = fn
        self.deps = []
        self.isdma = isdma
        self.sem = None
        self.val = None
        self.needs_inc = False


class Sched:
    def __init__(self, nc):
        self.nc = nc
        self.ops = {e: [] for e in ENGS}
        self.nd = {"sp": 24, "pool": 12, "act": 4}
        self.dma_rr = {e: 0 for e in self.nd}
        self.dma_last = {e: [None] * n for e, n in self.nd.items()}
        self.dma_cnt = {e: [0] * n for e, n in self.nd.items()}
        self.last = {e: None for e in ENGS}

    def op(self, eng, fn, reads=(), writes=(), dma=False, extra=()):
        o = Op(eng, fn, dma)
        deps = list(extra)
        for r in reads:
            if r.w is not None:
                deps.append(r.w)
        for w in writes:
            if w.w is not None:
                deps.append(w.w)
            deps.extend(w.r)
        if dma:
            slot = self.dma_rr[eng]
            self.dma_rr[eng] = (slot + 1) % self.nd[eng]
            prev = self.dma_last[eng][slot]
            if prev is not None:
                deps.append(prev)
            self.dma_last[eng][slot] = o
            self.dma_cnt[eng][slot] += 1
            o.sem = ("dma", eng, slot)
            o.val = 16 * self.dma_cnt[eng][slot]
        seen = set()
        for d in deps:
            if d is None or d is o or id(d) in seen:
                continue
            seen.add(id(d))
            if d.eng == "pe" and eng == "pe" and not d.isdma and not dma:
                continue
            o.deps.append(d)
            if not d.isdma:
                d.needs_inc = True
        for r in reads:
            if not dma:
                r.r = [x for x in r.r if x.isdma or x.eng != eng]
            r.r.append(o)
        for w in writes:
            w.w = o
            w.r = []
        self.ops[eng].append(o)
        if not dma:
            self.last[eng] = o
        return o

    def barrier(self):
        deps = [self.last[e] for e in ENGS if self.last[e] is not None]
        for e in self.nd:
            deps.extend(x for x in self.dma_last[e] if x is not None)
        for e in ENGS:
            self.op(e, lambda eng: eng.nop(), extra=deps)

    def emit(self, final_waits=()):
        nc = self.nc
        esem = {e: nc.alloc_semaphore(f"s_{e}") for e in ENGS}
        dsem = {e: [nc.alloc_semaphore(f"d_{e}{i}") for i in range(n)] for e, n in self.nd.items()}
        for e in ENGS:
            c = 0
            for o in self.ops[e]:
                if o.isdma:
                    o.sem = dsem[o.sem[1]][o.sem[2]]
                elif o.needs_inc:
                    c += 1
                    o.sem = esem[e]
                    o.val = c
        emap = {"pe": "tensor", "act": "scalar", "dve": "vector", "pool": "gpsimd", "sp": "sync"}

        def run(e, engobj):
            known = {}
            for o in self.ops[e]:
                need = {}
                for d in o.deps:
                    k = d.sem.num
                    if k not in need or need[k][1] < d.val:
                        need[k] = (d.sem, d.val)
                for k, (s, v) in need.items():
                    if known.get(k, 0) >= v:
                        continue
                    engobj.wait_ge(s, v)
                    known[k] = v
                ins = o.fn(engobj)
                if o.isdma:
                    ins.then_inc(o.sem, 16)
                elif o.needs_inc:
                    ins.then_inc(o.sem, 1)
            if e == "sp":
                for d in final_waits:
                    engobj.wait_ge(d.sem, d.val)

        with nc.Block() as block:
            for e in ENGS:
                getattr(block, emap[e])(lambda engobj, e=e: run(e, engobj))


class Sel:
    REG = []

    def __init__(self, bufs):
        self.bufs, self.i = bufs, 0
        Sel.REG.append(self)

    def __getitem__(self, k):
        return self.bufs[self.i][k]


class RSel:
    def __init__(self, n):
        self.rs, self.i = [Res() for _ in range(n)], 0
        Sel.REG.append(self)

    @property
    def w(self):
        return self.rs[self.i].w

    @w.setter
    def w(self, v):
        self.rs[self.i].w = v

    @property
    def r(self):
        return self.rs[self.i].r

    @r.setter
    def r(self, v):
        self.rs[self.i].r = v


def set_parity(p):
    if os.environ.get("NO_PAR"):
        p = 0
    for x in Sel.REG:
        x.i = p % len(x.bufs if isinstance(x, Sel) else x.rs)


def interleave(gen_iter, ways, admit_every=0):
    active = []
    gen_iter = iter(gen_iter)
    done = False
    rnd = 0
    last_admit = -10 ** 9
    while active or not done:
        while len(active) < ways and not done and (not active or rnd - last_admit >= admit_every):
            try:
                active.append(next(gen_iter))
                last_admit = rnd
            except StopIteration:
                done = True
        rnd += 1
        for g in list(active):
            try:
                next(g)
            except StopIteration:
                active.remove(g)


class Arena:
    def __init__(self, nc, lo, hi):
        self.nc, self.lo, self.hi, self.cur, self.n = nc, lo, hi, lo, 0

    def alloc(self, shape, dtype, name=None):
        nbytes = int(np.prod(shape[1:])) * (2 if dtype == BF16 else 4)
        off = (self.cur + 31) // 32 * 32
        assert off + nbytes <= self.hi, f"SBUF arena overflow {off + nbytes} > {self.hi} ({name})"
        self.cur = off + nbytes
        self.n += 1
        return self.nc.alloc_sbuf_tensor_at(f"{name or 't'}_{off}_{self.n}", list(shape), dtype, offset=off)


WEIGHT_NAMES = ["g_mix", "w_in", "g_cq", "w_uq", "g_ckv", "w_ukv", "g_qn", "g_kn", "conv_w", "conv_b",
                "w_rg", "b_rg", "w_ig", "b_ig", "lam", "g_attn_out", "g_rnn_out", "w_out", "g_xq", "g_mem",
                "w_mq", "w_mk", "w_mv", "g_mqn", "g_mkn", "w_mo", "g_ffn", "w_group", "b_group", "w_expert",
                "b_expert", "w_e_gate", "w_e_up", "w_e_down"]
WEIGHT_SHAPES = {
    "g_mix": [D], "w_in": [D, 1440], "g_cq": [256], "w_uq": [256, 768], "g_ckv": [128], "w_ukv": [128, 1024],
    "g_qn": [96], "g_kn": [96], "conv_w": [4, 512], "conv_b": [512], "w_rg": [8, 64, 64], "b_rg": [512],
    "w_ig": [8, 64, 64], "b_ig": [512], "lam": [512], "g_attn_out": [512], "g_rnn_out": [512], "w_out": [D, D],
    "g_xq": [D], "g_mem": [D], "w_mq": [D, D], "w_mk": [D, D], "w_mv": [D, D], "g_mqn": [256], "g_mkn": [256],
    "w_mo": [D, D], "g_ffn": [D], "w_group": [D, 4], "b_group": [4], "w_expert": [D, 32], "b_expert": [32],
    "w_e_gate": [NE, D, DE], "w_e_up": [NE, D, DE], "w_e_down": [NE, DE, D]}


def build(S_len, phases="ABCD", dbg=False, moe_stop="F"):
    NT = S_len // 128
    NG = S_len // 512
    nc = bass.Bass("TRN2", target_bir_lowering=False)
    x_d = nc.dram_tensor("x", [S_len, D], F32, kind="ExternalInput").ap()
    mem_d = nc.dram_tensor("mem", [256, D], F32, kind="ExternalInput").ap()
    W = {n: nc.dram_tensor(n, WEIGHT_SHAPES[n], F32, kind="ExternalInput").ap() for n in WEIGHT_NAMES}
    cs_d = nc.dram_tensor("cs_tab", [S_len, 64], F32, kind="ExternalInput").ap()
    idb_d = nc.dram_tensor("ident_bf", [128, 128], BF16, kind="ExternalInput").ap()
    idf_d = nc.dram_tensor("ident_f32", [128, 128], F32, kind="ExternalInput").ap()
    out_d = nc.dram_tensor("out", [S_len, D], F32, kind="ExternalOutput").ap()
    skind = "ExternalOutput" if dbg else "Internal"
    QT = nc.dram_tensor("QT", [NH, DQK, S_len], BF16, kind=skind).ap()
    KT = nc.dram_tensor("KT", [NH, DQK, S_len], BF16, kind=skind).ap()
    VA = nc.dram_tensor("VA", [NH, 128, NT, 65], BF16, kind=skind).ap()
    ORT = nc.dram_tensor("ORT", [4, 128, S_len], BF16, kind=skind).ap()
    OA = nc.dram_tensor("OA", [S_len, 512], BF16, kind=skind).ap()
    RR = nc.dram_tensor("RR", [128, NT], F32, kind=skind).ap()
    WGU2 = nc.dram_tensor("WGU2", [NE, 128, 8, 2 * DE], BF16).ap()
    WD2 = nc.dram_tensor("WD2", [NE, 128, 2, D], BF16).ap()
    NSLOT = ((2 * S_len) // 256 + NE) * 256
    H3 = nc.dram_tensor("H3", [S_len, D], BF16).ap()
    Hs = nc.dram_tensor("Hs", [NSLOT, D], BF16).ap()
    Ys = nc.dram_tensor("Ys", [NSLOT, D], BF16).ap()
    lst_d = nc.dram_tensor("lstrict", [128, 128], BF16, kind="ExternalInput").ap()
    thr_d = nc.dram_tensor("thr_tab", [32], F32, kind="ExternalInput").ap()
    iota_d = nc.dram_tensor("iota_tab", [NSLOT // 256], F32, kind="ExternalInput").ap()
    pidx_d = nc.dram_tensor("pidx_tab", [128], F32, kind="ExternalInput").ap()
    if dbg:
        DBG_widx = nc.dram_tensor("DBG_widx", [128, NSLOT // 256], mybir.dt.int32, kind="ExternalOutput").ap()
        DBG_pos = nc.dram_tensor("DBG_pos", [128, 2, NT], mybir.dt.int32, kind="ExternalOutput").ap()
        DBG_gab = nc.dram_tensor("DBG_gab", [128, NT, 2], F32, kind="ExternalOutput").ap()

    S = Sched(nc)
    P = Arena(nc, SB_LO, SB_LO + 6144)
    pairs = [nc.alloc_psum_tensor(f"pair{j}", [128, 1024], F32) for j in range(4)]
    banks = [pairs[i // 2][:, (i % 2) * 512:(i % 2 + 1) * 512] for i in range(8)]
    rb = [Res(f"bank{i}") for i in range(8)]

    def bview(i):
        return pairs[i // 2][:].bitcast(BF16)[:, (i % 2) * 1024:(i % 2 + 1) * 1024]

    def dma(q, out, in_, reads=(), writes=()):
        return S.op(q, lambda e: e.dma_start(out=out, in_=in_), reads=reads, writes=writes, dma=True)

    def dma_nc(q, out, in_, reads=(), writes=()):
        def f(e):
            with nc.allow_non_contiguous_dma(reason="small param vectors"):
                return e.dma_start(out=out, in_=in_)
        return S.op(q, f, reads=reads, writes=writes, dma=True)

    def mm(out, lhsT, rhs, start, stop, reads, writes, skip=False):
        return S.op("pe", lambda e: e.matmul(out, lhsT=lhsT, rhs=rhs, start=start, stop=stop,
                                             skip_group_check=skip), reads, writes)

    def act(out, in_, func, reads, writes, scale=1.0, bias=None, accum=None):
        kw = {}
        if bias is not None:
            kw["bias"] = bias
        if accum is not None:
            kw["accum_out"] = accum
        return S.op("act", lambda e: e.activation(out=out, in_=in_, func=func, scale=scale, **kw), reads, writes)

    def tt(eng, out, in0, in1, op, reads, writes):
        return S.op(eng, lambda e: e.tensor_tensor(out=out, in0=in0, in1=in1, op=op), reads, writes)

    def ts(eng, out, in0, s1, s2, op0, op1, reads, writes):
        if s2 is None:
            return S.op(eng, lambda e: e.tensor_scalar(out=out, in0=in0, scalar1=s1, scalar2=None, op0=op0), reads, writes)
        return S.op(eng, lambda e: e.tensor_scalar(out=out, in0=in0, scalar1=s1, scalar2=s2, op0=op0, op1=op1), reads, writes)

    def stt(eng, out, in0, scalar, in1, op0, op1, reads, writes):
        return S.op(eng, lambda e: e.scalar_tensor_tensor(out=out, in0=in0, scalar=scalar, in1=in1, op0=op0, op1=op1),
                    reads, writes)

    def cp(eng, out, in_, reads, writes):
        if eng == "act":
            return act(out, in_, AF.Copy, reads, writes)
        return S.op(eng, lambda e: e.tensor_copy(out=out, in_=in_), reads, writes)

    def red(out, in_, op, reads, writes):
        return S.op("dve", lambda e: e.tensor_reduce(out=out, in_=in_, axis=AX.X, op=op), reads, writes)

    def recip(out, in_, reads, writes):
        return S.op("dve", lambda e: e.reciprocal(out=out, in_=in_), reads, writes)

    def memset(eng, ap, val, writes):
        return S.op(eng, lambda e: e.memset(ap, val), (), writes)

    idb = P.alloc([128, 128], BF16, "idb"); r_idb = Res()
    idf = P.alloc([128, 128], F32, "idf"); r_idf = Res()
    ones = P.alloc([128, 2], BF16, "ones"); r_ones = Res()
    cneg = P.alloc([128, 1], F32, "cneg"); chalf = P.alloc([128, 1], F32, "chalf"); r_c = Res()
    rr_all = P.alloc([128, max(NT, 8)], F32, "rr_all"); r_rr = Res()
    dma("sp", idb[:], idb_d, writes=[r_idb])
    dma("sp", idf[:], idf_d, writes=[r_idf])
    memset("pool", ones[:], 1.0, [r_ones])
    memset("pool", cneg[:], -0.5, [r_c])
    memset("pool", chalf[:], 0.5, [r_c])
    cone = P.alloc([128, 1], F32, "cone")
    memset("pool", cone[:], 1.0, [r_c])

    def transpose(out, in_, reads, writes, f32=False):
        idt, rid = (idf, r_idf) if f32 else (idb, r_idb)
        return S.op("pe", lambda e: e.transpose(out=out, in_=in_, identity=idt[:]), list(reads) + [rid], writes)

    def rstd_chain(buf, n, inv_dim, reads_writes):
        ts("dve", buf, buf, inv_dim, EPS, ALU.mult, ALU.add, reads_writes, reads_writes)
        act(buf, buf, AF.Ln, reads_writes, reads_writes)
        act(buf, buf, AF.Exp, reads_writes, reads_writes, scale=-0.5)

    def colvec(arena, name, src, ncol):
        t = arena.alloc([128, ncol], F32, name)
        r = Res(name)
        dma_nc("sp", t[:], src.rearrange("(c p) -> p c", p=128), writes=[r])
        return t, r

    def bcast(arena, name, src, n):
        t = arena.alloc([128, n], F32, name)
        r = Res(name)
        dma("sp", t[:], src.partition_broadcast(128), writes=[r])
        return t, r

    r_wgu = [Res() for _ in range(NE)]
    r_wd = [Res() for _ in range(NE)]

    def convert_experts(e0, e1):
        for e in range(e0, min(e1, NE)):
            wg = WGU2[e].rearrange("p k n -> k p n")
            dma("pool", wg[:, :, 0:DE], W["w_e_gate"][e].rearrange("(k p) n -> k p n", p=128), writes=[r_wgu[e]])
            dma("pool", wg[:, :, DE:2 * DE], W["w_e_up"][e].rearrange("(k p) n -> k p n", p=128), writes=[r_wgu[e]])
            dma("pool", WD2[e].rearrange("p c n -> c p n"), W["w_e_down"][e].rearrange("(c p) n -> c p n", p=128),
                writes=[r_wd[e]])

    r_QT, r_KT, r_VA, r_ORT, r_OA = Res(), Res(), Res(), Res(), Res()

    def phaseA():
        A = Arena(nc, SB_LO + 6144, SB_HI)
        w_in = A.alloc([128, 8, 1440], BF16, "w_in"); r_win = Res()
        dma("pool", w_in[:], W["w_in"].rearrange("(k p) n -> p k n", p=128), writes=[r_win])
        w_uq = A.alloc([128, 2, 768], BF16, "w_uq"); r_wuq = Res()
        dma("pool", w_uq[:], W["w_uq"].rearrange("(k p) n -> p k n", p=128), writes=[r_wuq])
        w_ukv = A.alloc([128, 1024], BF16, "w_ukv"); r_wukv = Res()
        dma("pool", w_ukv[:], W["w_ukv"], writes=[r_wukv])
        wrg = A.alloc([128, 4, 128], BF16, "wrg"); wig = A.alloc([128, 4, 128], BF16, "wig"); r_wg = Res()
        memset("pool", wrg[:], 0.0, [r_wg])
        memset("pool", wig[:], 0.0, [r_wg])
        for c in range(4):
            for half in range(2):
                sl = slice(half * 64, half * 64 + 64)
                dma("pool", wrg[sl, c, sl], W["w_rg"][2 * c + half], writes=[r_wg])
                dma("pool", wig[sl, c, sl], W["w_ig"][2 * c + half], writes=[r_wg])
        gmix_b, r_gmix = bcast(A, "gmix_b", W["g_mix"], 1024)
        gq_b, r_gq = bcast(A, "gq_b", W["g_qn"], 96)
        gk_b, r_gk = bcast(A, "gk_b", W["g_kn"], 96)
        ts("dve", gq_b[:], gq_b[:], float(DQK) ** -0.5, None, ALU.mult, None, [r_gq], [r_gq])
        gcq, r_gcq = colvec(A, "gcq", W["g_cq"], 2)
        gckv, r_gckv = colvec(A, "gckv", W["g_ckv"], 1)
        cb, r_cb = colvec(A, "cb", W["conv_b"], 4)
        brg, r_brg = colvec(A, "brg", W["b_rg"], 4)
        big, r_big = colvec(A, "big", W["b_ig"], 4)
        lam, r_lam = colvec(A, "lam", W["lam"], 4)
        cw = A.alloc([128, 4, 4], F32, "cw"); r_cw = Res()
        for j in range(4):
            dma_nc("sp", cw[:, :, j], W["conv_w"][j].rearrange("(c p) -> p c", p=128), writes=[r_cw])
        c1 = A.alloc([128, 4], F32, "c1"); c2 = A.alloc([128, 4], F32, "c2")
        zt = A.alloc([128, 4], F32, "zt"); wv = A.alloc([128, 4], F32, "wv"); w2 = A.alloc([128, 4], F32, "w2")
        r_c12 = Res(); r_z = Res()
        act(zt[:], lam[:], AF.Exp, [r_lam], [r_z], scale=-1.0)
        ts("dve", wv[:], zt[:], 2.0, None, ALU.add, None, [r_z], [r_z])
        S.op("dve", lambda e: e.reciprocal(out=wv[:], in_=wv[:]), [r_z], [r_z])
        tt("dve", wv[:], wv[:], zt[:], ALU.mult, [r_z], [r_z])
        tt("dve", w2[:], wv[:], wv[:], ALU.mult, [r_z], [r_z])
        ts("dve", zt[:], w2[:], 1.0 / 9, 1.0 / 7, ALU.mult, ALU.add, [r_z], [r_z])
        tt("dve", zt[:], zt[:], w2[:], ALU.mult, [r_z], [r_z])
        ts("dve", zt[:], zt[:], 1.0 / 5, None, ALU.add, None, [r_z], [r_z])
        tt("dve", zt[:], zt[:], w2[:], ALU.mult, [r_z], [r_z])
        ts("dve", zt[:], zt[:], 1.0 / 3, None, ALU.add, None, [r_z], [r_z])
        tt("dve", zt[:], zt[:], w2[:], ALU.mult, [r_z], [r_z])
        ts("dve", zt[:], zt[:], 1.0, None, ALU.add, None, [r_z], [r_z])
        tt("dve", zt[:], zt[:], wv[:], ALU.mult, [r_z], [r_z])
        ts("dve", c1[:], zt[:], -16.0, None, ALU.mult, None, [r_z], [r_c12])
        ts("dve", c2[:], zt[:], -32.0, None, ALU.mult, None, [r_z], [r_c12])

        xb = [A.alloc([128, 1024], F32, "xb") for _ in range(4)]; r_xb = [Res() for _ in range(4)]
        junk = A.alloc([128, 1024], BF16, "junk"); r_junk = Res()
        ssx = A.alloc([128, 4], F32, "ssx"); r_ssx = Res()
        hb = [A.alloc([128, 1024], BF16, "hb") for _ in range(2)]; r_hb = [Res() for _ in range(2)]
        hT = [A.alloc([128, 8, 512], BF16, "hT") for _ in range(2)]; r_hT = [Res() for _ in range(2)]
        csb = [A.alloc([128, 4, 64], F32, "csb") for _ in range(2)]; r_cs = [Res() for _ in range(2)]
        cqT = A.alloc([128, 2, 512], BF16, "cqT"); r_cqT = Res()
        ckvT = A.alloc([128, 512], BF16, "ckvT"); r_ckvT = Res()
        sqc = A.alloc([128, 3, 512], BF16, "sqc"); r_sqc = Res()
        sqr = A.alloc([128, 4, 512], BF16, "sqr"); r_sqr = Res()
        ornb = A.alloc([128, 4, 512], BF16, "ornb"); r_ornb = Res()
        uxT = [A.alloc([128, 4, 515], F32, "uxT") for _ in range(2)]; r_ux = [Res() for _ in range(2)]
        memset("pool", uxT[0][:, :, 0:3], 0.0, [r_ux[0]])
        carry = A.alloc([128, 4], F32, "carry"); r_carry = Res()
        memset("pool", carry[:], 0.0, [r_carry])
        def two(name, dt=F32):
            return [A.alloc([128, 512], dt, name) for _ in range(2)], [Res() for _ in range(2)]
        ug, r_ug = two("ug"); gw, r_gw = two("gw"); gel, r_gel = two("gel")
        xc, r_xc = two("xc"); xcb, r_xcb = two("xcb", BF16)
        rg, r_rg = two("rg"); ig, r_ig = two("ig"); av, r_av = two("av"); hs, r_hs = two("hs"); orn, r_orn = two("orn")
        stc = A.alloc([128, 4, 4], F32, "stc"); r_stc = Res()
        qs = A.alloc([128, 8, 96], F32, "qs"); r_qs = Res()
        ks = A.alloc([128, 8, 96], F32, "ks"); r_ks = Res()
        tq = A.alloc([128, 8, 96], F32, "tq"); r_tq = Res()
        tk = A.alloc([128, 8, 96], F32, "tk"); r_tk = Res()
        ssq = A.alloc([128, 16], F32, "ssq"); r_ssq = Res()
        rt1 = A.alloc([128, 8, 32], F32, "rt1"); rt2 = A.alloc([128, 8, 32], F32, "rt2"); r_rt = Res()
        kt1 = A.alloc([128, 8, 32], F32, "kt1"); kt2 = A.alloc([128, 8, 32], F32, "kt2"); r_kt = Res()
        qb = A.alloc([128, 8, 96], BF16, "qb"); r_qb = Res()
        kb = A.alloc([128, 8, 96], BF16, "kb"); r_kb = Res()
        QTst = A.alloc([128, 8, 512], BF16, "QTst"); r_QTst = Res()
        KTst = A.alloc([128, 8, 512], BF16, "KTst"); r_KTst = Res()
        Vst = A.alloc([128, 8, 4, 65], BF16, "Vst"); r_Vst = Res()
        memset("pool", Vst[:], 1.0, [r_Vst])
        print("phase A SBUF used", A.cur)

        ZB = [0, 1]; TB = 2; SB = 3; QB = (4, 5); KVB = (6, 7)
        r_stat_c = r_stat_r = r_kr = rb[SB]
        stat_c = banks[SB][:, 0:8].rearrange("p (t c) -> p t c", c=2)
        stat_r = banks[SB][:, 8:12]
        kr_ps = banks[SB][:, 64:192].rearrange("p (t c) -> p t c", c=32)
        zrot = [0]

        def zbank():
            b = ZB[zrot[0] % 2]
            zrot[0] += 1
            return b

        epg = -(-NE // NG)
        for G in range(NG):
            convert_experts(G * epg, (G + 1) * epg)
            hTg, r_hTg = hT[G % 2], r_hT[G % 2]
            cst, r_cst = csb[G % 2], r_cs[G % 2]
            dma("sp", cst[:], cs_d[G * 512:(G + 1) * 512, :].rearrange("(t p) c -> p t c", p=128), writes=[r_cst])
            for t in range(4):
                tok = G * 4 + t
                dma("sp", xb[t][:], x_d[tok * 128:(tok + 1) * 128, :], writes=[r_xb[t]])
                act(junk[:], xb[t][:], AF.Square, [r_xb[t]], [r_junk, r_ssx], accum=ssx[:, t:t + 1])
            rstd_chain(ssx[:], 4, 1.0 / D, [r_ssx])
            for t in range(4):
                h_, r_h = hb[t % 2], r_hb[t % 2]
                stt("dve", h_[:], xb[t][:], ssx[:, t:t + 1], gmix_b[:], ALU.mult, ALU.mult,
                    [r_xb[t], r_ssx, r_gmix], [r_h])
                tv = bview(TB).rearrange("p (k n) -> p k n", k=8)
                for k in range(8):
                    transpose(tv[:, k, :], h_[:, k * 128:(k + 1) * 128], [r_h], [rb[TB]])
                cp("act", hTg[:, :, t * 128:(t + 1) * 128], tv, [rb[TB]], [r_hTg])

            def zmm(col0, ncols, b):
                for k in range(8):
                    mm(banks[b][0:ncols, :], w_in[:, k, col0:col0 + ncols], hTg[:, k, :], k == 0, k == 7,
                       [r_win, r_hTg], [rb[b]])

            for j in range(2):
                b = zbank(); zmm(j * 128, 128, b)
                act(sqc[:, j, :], banks[b][:], AF.Square, [rb[b]], [r_sqc])
                act(cqT[:, j, :], banks[b][:], AF.Copy, [rb[b], r_gcq], [r_cqT], scale=gcq[:, j:j + 1])
            b = zbank(); zmm(256, 128, b)
            act(sqc[:, 2, :], banks[b][:], AF.Square, [rb[b]], [r_sqc])
            act(ckvT[:], banks[b][:], AF.Copy, [rb[b], r_gckv], [r_ckvT], scale=gckv[:, 0:1])
            for t in range(4):
                tsl = slice(t * 128, (t + 1) * 128)
                for j in range(2):
                    mm(stat_c[:, t, 0:1], sqc[:, j, tsl], ones[:, 0:1], j == 0, j == 1, [r_sqc, r_ones], [r_stat_c])
                mm(stat_c[:, t, 1:2], sqc[:, 2, tsl], ones[:, 0:1], True, True, [r_sqc, r_ones], [r_stat_c])
            ts("dve", stc[:, :, 0:1], stat_c[:, :, 0:1], 1.0 / 256, EPS, ALU.mult, ALU.add, [r_stat_c], [r_stc])
            ts("dve", stc[:, :, 1:2], stat_c[:, :, 1:2], 1.0 / 128, EPS, ALU.mult, ALU.add, [r_stat_c], [r_stc])
            stc2 = stc[:, :, 0:2]
            tt("pool", stc2, stc2, cneg[:, 0:1].unsqueeze(2).to_broadcast([128, 4, 2]), ALU.pow, [r_stc, r_c], [r_stc])

            def qk_gen():
                for t in range(4):
                    tsl = slice(t * 128, (t + 1) * 128)
                    qv = [banks[QB[0]][:, 0:384], banks[QB[1]][:, 0:384]]
                    for half in range(2):
                        for j in range(2):
                            mm(qv[half], cqT[:, j, tsl], w_uq[:, j, half * 384:(half + 1) * 384], j == 0, j == 1,
                               [r_cqT, r_wuq], [rb[QB[half]]])
                            yield
                    for half in range(2):
                        mm(banks[KVB[half]][:], ckvT[:, tsl], w_ukv[:, half * 512:(half + 1) * 512], True, True,
                           [r_ckvT, r_wukv], [rb[KVB[half]]])
                        yield
                    for k in range(8):
                        mm(kr_ps[:, t, :], hTg[:, k, tsl], w_in[:, k, 384:416], k == 0, k == 7, [r_hTg, r_win], [r_kr])
                        yield
                    rcq = stc[:, t, 0:1]; rckv = stc[:, t, 1:2]
                    for half in range(2):
                        hs4 = slice(half * 4, half * 4 + 4)
                        act(qs[:, hs4, :], qv[half].rearrange("p (h d) -> p h d", h=4), AF.Copy,
                            [rb[QB[half]], r_stc], [r_qs], scale=rcq)
                        yield
                        kvv = banks[KVB[half]][:].rearrange("p (h d) -> p h d", h=4)
                        act(ks[:, hs4, 0:64], kvv[:, :, 0:64], AF.Copy, [rb[KVB[half]], r_stc], [r_ks], scale=rckv)
                        yield
                        act(Vst[:, hs4, t, 0:64], kvv[:, :, 64:128], AF.Copy, [rb[KVB[half]], r_stc], [r_Vst], scale=rckv)
                        yield
                    cp("dve", ks[:, :, 64:96], kr_ps[:, t, :].unsqueeze(1).to_broadcast([128, 8, 32]), [r_kr], [r_ks])
                    yield
                    act(tq[:], qs[:], AF.Square, [r_qs], [r_tq])
                    yield
                    act(tk[:], ks[:], AF.Square, [r_ks], [r_tk])
                    yield
                    S.op("dve", lambda e: e.tensor_reduce(out=ssq[:, 0:8], in_=tq[:], axis=AX.X, op=ALU.add), [r_tq], [r_ssq])
                    yield
                    S.op("dve", lambda e: e.tensor_reduce(out=ssq[:, 8:16], in_=tk[:], axis=AX.X, op=ALU.add), [r_tk], [r_ssq])
                    yield
                    rstd_chain(ssq[:], 16, 1.0 / DQK, [r_ssq])
                    yield
                    for (src, r_src, tmp, r_tmp, g_b, r_g, o0, t1, t2, r_t, dst, r_dst, st, r_st) in (
                            (qs, r_qs, tq, r_tq, gq_b, r_gq, 0, rt1, rt2, r_rt, qb, r_qb, QTst, r_QTst),
                            (ks, r_ks, tk, r_tk, gk_b, r_gk, 8, kt1, kt2, r_kt, kb, r_kb, KTst, r_KTst)):
                        tt("dve", tmp[:], src[:], ssq[:, o0:o0 + 8].unsqueeze(2).to_broadcast([128, 8, 96]), ALU.mult,
                           [r_src, r_ssq], [r_tmp])
                        yield
                        tt("dve", tmp[:], tmp[:], g_b[:].unsqueeze(1).to_broadcast([128, 8, 96]), ALU.mult,
                           [r_tmp, r_g], [r_tmp])
                        yield
                        c2b = cst[:, t, 0:32].unsqueeze(1).to_broadcast([128, 8, 32])
                        tt("dve", t1[:], tmp[:, :, 64:96], c2b, ALU.mult, [r_tmp, r_cst], [r_t])
                        yield
                        tt("dve", t2[:, :, 0:16], tmp[:, :, 80:96],
                           cst[:, t, 32:48].unsqueeze(1).to_broadcast([128, 8, 16]), ALU.mult, [r_tmp, r_cst], [r_t])
                        yield
                        tt("dve", t2[:, :, 16:32], tmp[:, :, 64:80],
                           cst[:, t, 48:64].unsqueeze(1).to_broadcast([128, 8, 16]), ALU.mult, [r_tmp, r_cst], [r_t])
                        yield
                        tt("dve", dst[:, :, 64:96], t1[:], t2[:], ALU.add, [r_t], [r_dst])
                        yield
                        cp("act", dst[:, :, 0:64], tmp[:, :, 0:64], [r_tmp], [r_dst])
                        yield
                        tv = bview(TB).rearrange("p (h n) -> p h n", h=8)
                        for h in range(NH):
                            transpose(tv[0:96, h, :], dst[:, h, :], [r_dst], [rb[TB]])
                            yield
                        cp("dve", st[0:96, :, tsl], tv[0:96, :, :], [rb[TB]], [r_st])
                        yield


            def rnn_gen():
                U, r_U = uxT[G % 2], r_ux[G % 2]
                Un, r_Un = uxT[(G + 1) % 2], r_ux[(G + 1) % 2]
                for c in range(4):
                    pb = c % 2
                    b = zbank(); zmm(416 + c * 128, 128, b)
                    act(ug[pb][:], banks[b][:], AF.Copy, [rb[b]], [r_ug[pb]])
                    yield
                    act(gw[pb][:], banks[b][:], AF.Square, [rb[b]], [r_gw[pb]])
                    yield
                    ts("dve", gw[pb][:], gw[pb][:], 0.044715, 1.0, ALU.mult, ALU.add, [r_gw[pb]], [r_gw[pb]])
                    yield
                    tt("dve", gw[pb][:], gw[pb][:], ug[pb][:], ALU.mult, [r_gw[pb], r_ug[pb]], [r_gw[pb]])
                    yield
                    act(gw[pb][:], gw[pb][:], AF.Sigmoid, [r_gw[pb]], [r_gw[pb]], scale=1.5957691216057308)
                    yield
                    tt("dve", gel[pb][:], gw[pb][:], ug[pb][:], ALU.mult, [r_gw[pb], r_ug[pb]], [r_gel[pb]])
                    yield
                    b = zbank(); zmm(928 + c * 128, 128, b)
                    act(U[:, c, 3:515], banks[b][:], AF.Copy, [rb[b]], [r_U])
                    yield
                    cp("pool", Un[:, c, 0:3], U[:, c, 512:515], [r_U], [r_Un])
                    yield
                    ts("dve", xc[pb][:], U[:, c, 3:515], cw[:, c, 3:4], cb[:, c:c + 1], ALU.mult, ALU.add,
                       [r_U, r_cw, r_cb], [r_xc[pb]])
                    yield
                    for j in (2, 1, 0):
                        stt("dve", xc[pb][:], U[:, c, j:j + 512], cw[:, c, j:j + 1], xc[pb][:], ALU.mult, ALU.add,
                            [r_U, r_cw, r_xc[pb]], [r_xc[pb]])
                        yield
                    cp("act", xcb[pb][:], xc[pb][:], [r_xc[pb]], [r_xcb[pb]])
                    yield
                    b1 = zbank()
                    mm(banks[b1][:], wrg[:, c, :], xcb[pb][:], True, True, [r_wg, r_xcb[pb]], [rb[b1]])
                    yield
                    act(rg[pb][:], banks[b1][:], AF.Sigmoid, [rb[b1], r_brg], [r_rg[pb]], bias=brg[:, c:c + 1])
                    yield
                    b2 = zbank()
                    mm(banks[b2][:], wig[:, c, :], xcb[pb][:], True, True, [r_wg, r_xcb[pb]], [rb[b2]])
                    yield
                    act(ig[pb][:], banks[b2][:], AF.Sigmoid, [rb[b2], r_big], [r_ig[pb]], bias=big[:, c:c + 1])
                    yield
                    act(av[pb][:], rg[pb][:], AF.Exp, [r_rg[pb], r_c12], [r_av[pb]], scale=c1[:, c:c + 1])
                    yield
                    act(rg[pb][:], rg[pb][:], AF.Exp, [r_rg[pb], r_c12], [r_rg[pb]], scale=c2[:, c:c + 1])
                    yield
                    act(rg[pb][:], rg[pb][:], AF.Relu, [r_rg[pb], r_c], [r_rg[pb]], scale=-1.0, bias=cone[:, 0:1])
                    yield
                    act(rg[pb][:], rg[pb][:], AF.Sqrt, [r_rg[pb]], [r_rg[pb]])
                    yield
                    tt("dve", ig[pb][:], ig[pb][:], xc[pb][:], ALU.mult, [r_ig[pb], r_xc[pb]], [r_ig[pb]])
                    yield
                    tt("dve", rg[pb][:], rg[pb][:], ig[pb][:], ALU.mult, [r_rg[pb], r_ig[pb]], [r_rg[pb]])
                    yield
                    S.op("dve", lambda e, pb=pb, c=c: e.tensor_tensor_scan(
                        out=hs[pb][:], data0=av[pb][:], data1=rg[pb][:], initial=carry[:, c:c + 1],
                        op0=ALU.mult, op1=ALU.add), [r_av[pb], r_rg[pb], r_carry], [r_hs[pb]])
                    yield
                    cp("pool", carry[:, c:c + 1], hs[pb][:, 511:512], [r_hs[pb]], [r_carry])
                    yield
                    tt("dve", orn[pb][:], gel[pb][:], hs[pb][:], ALU.mult, [r_gel[pb], r_hs[pb]], [r_orn[pb]])
                    yield
                    act(sqr[:, c, :], orn[pb][:], AF.Square, [r_orn[pb]], [r_sqr])
                    yield
                    cp("act", ornb[:, c, :], orn[pb][:], [r_orn[pb]], [r_ornb])
                    yield

            gens = [qk_gen(), rnn_gen()]
            while gens:
                for g in list(gens):
                    try:
                        next(g)
                    except StopIteration:
                        gens.remove(g)
            dma("sp", ORT[:, :, G * 512:(G + 1) * 512].rearrange("c p s -> p c s"), ornb[:], [r_ornb], [r_ORT])
            for t in range(4):
                for c in range(4):
                    mm(stat_r[:, t:t + 1], sqr[:, c, t * 128:(t + 1) * 128], ones[:, 0:1], c == 0, c == 3,
                       [r_sqr, r_ones], [r_stat_r])
            rsl = rr_all[:, G * 4:(G + 1) * 4]
            ts("dve", rsl, stat_r, 1.0 / 512, EPS, ALU.mult, ALU.add, [r_stat_r], [r_rr])
            tt("pool", rsl, rsl, cneg[:, 0:1].to_broadcast([128, 4]), ALU.pow, [r_rr, r_c], [r_rr])
            gsl = slice(G * 512, (G + 1) * 512)
            dma("sp", QT[:, :, gsl].rearrange("h d s -> d h s"), QTst[0:96, :, :], [r_QTst], [r_QT])
            dma("sp", KT[:, :, gsl].rearrange("h d s -> d h s"), KTst[0:96, :, :], [r_KTst], [r_KT])
            dma("sp", VA[:, :, G * 4:(G + 1) * 4, :].rearrange("h p t c -> p h t c"), Vst[:], [r_Vst], [r_VA])
        if dbg:
            dma("sp", RR, rr_all[:, 0:NT], [r_rr], [])

    def phaseB():
        Bn = Arena(nc, SB_LO + 6144, SB_HI)
        QTh = [Bn.alloc([128, S_len], BF16, "QTh") for _ in range(2)]; r_QTh = [Res() for _ in range(2)]
        KTh = [Bn.alloc([128, S_len], BF16, "KTh") for _ in range(2)]; r_KTh = [Res() for _ in range(2)]
        Vh = [Bn.alloc([128, NT, 65], BF16, "Vh") for _ in range(2)]; r_Vh = [Res() for _ in range(2)]
        NPT = 4
        pT = [Bn.alloc([128, 1024], BF16, "pT") for _ in range(NPT)]; r_pT = [Res() for _ in range(NPT)]
        ost = [Bn.alloc([128, 4, 64], BF16, "ost") for _ in range(2)]; r_ost = [Res() for _ in range(2)]
        rec = [Bn.alloc([128, 4], F32, "rec") for _ in range(2)]; r_rec = [Res() for _ in range(2)]
        units = []
        for h in range(NH):
            for G in range(NG):
                for kt in range(0, 4 * G, 2):
                    units.append((h, G, [kt, kt + 1]))
                for j in range(4):
                    units.append((h, G, [4 * G + j]))
        LOOK = 2
        NSP = 3
        state = {"s_emitted": 0}

        def load_head(h):
            p = h % 2
            dma("sp", QTh[p][0:96, :], QT[h], [r_QT], [r_QTh[p]])
            dma("sp", KTh[p][0:96, :], KT[h], [r_KT], [r_KTh[p]])
            dma("sp", Vh[p][:], VA[h], [r_VA], [r_Vh[p]])

        def emit_score(u):
            h, G, kts = units[u]
            if G == 0 and kts[0] == 0:
                load_head(h)
            p = h % 2
            sp_ = u % NSP
            for i, kt in enumerate(kts):
                q0 = max(kt - 4 * G, 0) * 128
                mm(pairs[sp_][:, i * 512 + q0:(i + 1) * 512], KTh[p][0:96, kt * 128:(kt + 1) * 128],
                   QTh[p][0:96, G * 512 + q0:(G + 1) * 512], True, True, [r_KTh[p], r_QTh[p]], [rb[2 * sp_]])

        for u, (h, G, kts) in enumerate(units):
            while state["s_emitted"] < min(len(units), u + 1 + LOOK):
                emit_score(state["s_emitted"])
                state["s_emitted"] += 1
            p = h % 2
            sp_ = u % NSP
            pi = u % NPT
            gi = (h * NG + G) % 2
            ob = 6 + gi
            o_ps = banks[ob][:, 0:260].rearrange("p (t c) -> p t c", c=65)
            j0 = kts[0] - 4 * G
            q0 = max(j0, 0) * 128
            w = 512 * len(kts)
            act(pT[pi][:, q0:w], pairs[sp_][:, q0:w], AF.Exp, [rb[2 * sp_]], [r_pT[pi]])
            if j0 >= 0:
                memset("pool", pT[pi][64:128, q0:q0 + 64], 0.0, [r_pT[pi]])
            for i, kt in enumerate(kts):
                j = kt - 4 * G
                for qt in range(max(j, 0), 4):
                    mm(o_ps[:, qt, :], pT[pi][:, i * 512 + qt * 128:i * 512 + (qt + 1) * 128], Vh[p][:, kt, :],
                       kt == 0 and qt == 0, kt == 4 * G + qt, [r_pT[pi], r_Vh[p]], [rb[ob]], skip=True)
            if kts[-1] == 4 * G + 3:
                S.op("dve", lambda e, gi=gi, o_ps=o_ps: e.reciprocal(out=rec[gi][:], in_=o_ps[:, :, 64]),
                     [rb[ob]], [r_rec[gi]])
                tt("dve", ost[gi][:], o_ps[:, :, 0:64], rec[gi][:].unsqueeze(2).to_broadcast([128, 4, 64]), ALU.mult,
                   [rb[ob], r_rec[gi]], [r_ost[gi]])
                dma_nc("sp", OA[G * 512:(G + 1) * 512, h * 64:(h + 1) * 64].rearrange("(t p) d -> p t d", p=128),
                       ost[gi][:], [r_ost[gi]], [r_OA])

    def phaseCD():
        NSETS = int(os.environ.get('NSETS', '4'))
        Cn = Arena(nc, SB_LO + 6144, SB_HI)
        TB = 7
        w_out = Cn.alloc([128, 8, 1024], BF16, "w_out"); r_wout = Res()
        dma("pool", w_out[:], W["w_out"].rearrange("(k p) n -> p k n", p=128), writes=[r_wout])
        grnn, r_grnn = colvec(Cn, "grnn", W["g_rnn_out"], 4)
        for c in range(4):
            ts("dve", w_out[:, 4 + c, :], w_out[:, 4 + c, :], grnn[:, c:c + 1], None, ALU.mult, None,
               [r_wout, r_grnn], [r_wout])
        w_mq = Cn.alloc([128, 8, 1024], BF16, "w_mq"); r_wmq = Res()
        dma("pool", w_mq[:], W["w_mq"].rearrange("(k p) n -> p k n", p=128), writes=[r_wmq])
        w_mo = Cn.alloc([128, 8, 1024], BF16, "w_mo"); r_wmo = Res()
        dma("pool", w_mo[:], W["w_mo"].rearrange("(k p) n -> p k n", p=128), writes=[r_wmo])
        ga_b, r_ga = bcast(Cn, "ga_b", W["g_attn_out"], 512)
        gxq_b, r_gxq = bcast(Cn, "gxq_b", W["g_xq"], 1024)
        gffn_b, r_gffn = bcast(Cn, "gffn_b", W["g_ffn"], 1024)
        gmq_b, r_gmq = bcast(Cn, "gmq_b", W["g_mqn"], 256)
        ts("dve", gmq_b[:], gmq_b[:], 256.0 ** -0.5, None, ALU.mult, None, [r_gmq], [r_gmq])
        wr = Cn.alloc([128, 8, 36], F32, "wr"); r_wr = Res()
        dma_nc("sp", wr[:, :, 0:4], W["w_group"].rearrange("(k p) n -> p k n", p=128), writes=[r_wr])
        dma_nc("sp", wr[:, :, 4:36], W["w_expert"].rearrange("(k p) n -> p k n", p=128), writes=[r_wr])
        br_b = Cn.alloc([128, 36], F32, "br_b"); r_br = Res()
        dma("sp", br_b[:, 0:4], W["b_group"].partition_broadcast(128), writes=[r_br])
        dma("sp", br_b[:, 4:36], W["b_expert"].partition_broadcast(128), writes=[r_br])
        KmT = Cn.alloc([128, 8, 256], BF16, "KmT"); r_KmT = Res()
        Vm = Cn.alloc([128, 2, 4, 256], BF16, "Vm"); r_Vm = Res()
        I32 = mybir.dt.int32
        T_SL = 256
        NTILE = (2 * S_len) // T_SL + NE
        gAB = Cn.alloc([128, NT, 2], F32, "gAB"); r_gAB = Res()
        widx = Cn.alloc([128, NTILE], I32, "widx"); r_widx = Res()
        pos_i = Cn.alloc([128, 2, NT], I32, "pos_i"); r_pos = Res()
        mark2 = Cn.cur
        xt = Sel([Cn.alloc([128, 1024], F32, "xt") for _ in range(NSETS)]); r_xt = RSel(NSETS)
        ssv = Sel([Cn.alloc([128, 4], F32, "ssv") for _ in range(NSETS)]); r_ssv = RSel(NSETS)
        h2 = Sel([Cn.alloc([128, 1024], BF16, "h2") for _ in range(NSETS)]); r_h2 = RSel(NSETS)
        h2T = Sel([Cn.alloc([128, 8, 128], BF16, "h2T") for _ in range(NSETS)]); r_h2T = RSel(NSETS)
        tmpf = Sel([Cn.alloc([128, 1024], F32, "tmpf") for _ in range(NSETS)]); r_tmpf = RSel(NSETS)
        ssm = Sel([Cn.alloc([128, 4], F32, "ssm") for _ in range(NSETS)]); r_ssm = RSel(NSETS)
        qmb = Sel([Cn.alloc([128, 1024], BF16, "qmb") for _ in range(NSETS)]); r_qmb = RSel(NSETS)
        junk = qmb; r_junk = r_qmb
        mark = Cn.cur
        tvb = bview(TB).rearrange("p (k n) -> p k n", k=8)

        def norm_to_bf16(src, r_src, g_b, r_g, dst, r_dst, sscol, f32dst=None, r_f32=None):
            act(junk[:], src, AF.Square, [r_src], [r_junk, r_ssv], accum=ssv[:, sscol:sscol + 1])
            rstd_chain(ssv[:, sscol:sscol + 1], 1, 1.0 / D, [r_ssv])
            if f32dst is None:
                stt("dve", dst, src, ssv[:, sscol:sscol + 1], g_b[:], ALU.mult, ALU.mult, [r_src, r_ssv, r_g], [r_dst])
            else:
                stt("dve", f32dst, src, ssv[:, sscol:sscol + 1], g_b[:], ALU.mult, ALU.mult, [r_src, r_ssv, r_g], [r_f32])
                cp("act", dst, f32dst, [r_f32], [r_dst])

        def transpose8(src, r_src, dstT, r_dstT, n=8, dst_sl=None):
            for k in range(n):
                transpose(tvb[:, k, :], src[:, k * 128:(k + 1) * 128], [r_src], [rb[TB]])
            cp("act", dstT if dst_sl is None else dst_sl, tvb[:, 0:n, :], [rb[TB]], [r_dstT])

        def head_norm(pb0, g_b, r_g, dst, r_dst):
            for half in range(2):
                act(tmpf[:, half * 512:(half + 1) * 512], banks[pb0 + half][:], AF.Square, [rb[pb0 + half]], [r_tmpf])
            red(ssm[:], tmpf[:].rearrange("p (h d) -> p h d", h=4), ALU.add, [r_tmpf], [r_ssm])
            rstd_chain(ssm[:], 4, 1.0 / 256, [r_ssm])
            for half in range(2):
                tt("dve", tmpf[:, half * 512:(half + 1) * 512].rearrange("p (h d) -> p h d", h=2),
                   banks[pb0 + half][:].rearrange("p (h d) -> p h d", h=2),
                   ssm[:, half * 2:half * 2 + 2].unsqueeze(2).to_broadcast([128, 2, 256]), ALU.mult,
                   [rb[pb0 + half], r_ssm], [r_tmpf])
            tt("dve", dst.rearrange("p (h d) -> p h d", h=4), tmpf[:].rearrange("p (h d) -> p h d", h=4),
               g_b[:].unsqueeze(1).to_broadcast([128, 4, 256]), ALU.mult, [r_tmpf, r_g], [r_dst])

        w_mk = Cn.alloc([128, 8, 1024], BF16, "w_mk"); r_wmk = Res()
        dma("pool", w_mk[:], W["w_mk"].rearrange("(k p) n -> p k n", p=128), writes=[r_wmk])
        w_mv = Cn.alloc([128, 8, 1024], BF16, "w_mv"); r_wmv = Res()
        dma("pool", w_mv[:], W["w_mv"].rearrange("(k p) n -> p k n", p=128), writes=[r_wmv])
        gmem_b, r_gmem = bcast(Cn, "gmem_b", W["g_mem"], 1024)
        gmk_b, r_gmk = bcast(Cn, "gmk_b", W["g_mkn"], 256)
        mnT = Cn.alloc([128, 8, 256], BF16, "mnT"); r_mnT = Res()
        for mt in range(2):
            dma("sp", xt[:], mem_d[mt * 128:(mt + 1) * 128, :], writes=[r_xt])
            norm_to_bf16(xt[:], r_xt, gmem_b, r_gmem, h2[:], r_h2, 0)
            transpose8(h2, r_h2, None, r_mnT, dst_sl=mnT[:, :, mt * 128:(mt + 1) * 128])
        for mt in range(2):
            msl = slice(mt * 128, (mt + 1) * 128)
            for half in range(2):
                for k in range(8):
                    mm(banks[half][:], mnT[:, k, msl], w_mk[:, k, half * 512:(half + 1) * 512], k == 0, k == 7,
                       [r_mnT, r_wmk], [rb[half]])
                for k in range(8):
                    mm(banks[2 + half][:], mnT[:, k, msl], w_mv[:, k, half * 512:(half + 1) * 512], k == 0, k == 7,
                       [r_mnT, r_wmv], [rb[2 + half]])
                act(Vm[:, mt, half * 2:half * 2 + 2, :], banks[2 + half][:].rearrange("p (h d) -> p h d", h=2),
                    AF.Copy, [rb[2 + half]], [r_Vm])
            head_norm(0, gmk_b, r_gmk, qmb[:], r_qmb)
            transpose8(qmb, r_qmb, None, r_KmT, dst_sl=KmT[:, :, msl])
        S.barrier()
        Cn.cur = mark

        oa = Sel([Cn.alloc([128, 512], BF16, "oa") for _ in range(NSETS)]); r_oa = RSel(NSETS)
        ornT = Sel([Cn.alloc([128, 4, 128], BF16, "ornT") for _ in range(NSETS)]); r_ornT = RSel(NSETS)
        mixA = Sel([Cn.alloc([128, 512], BF16, "mixA") for _ in range(NSETS)]); r_mixA = RSel(NSETS)
        mixAT = Sel([Cn.alloc([128, 4, 128], BF16, "mixAT") for _ in range(NSETS)]); r_mixAT = RSel(NSETS)
        x1 = Sel([Cn.alloc([128, 1024], F32, "x1") for _ in range(NSETS)]); r_x1 = RSel(NSETS)
        qmT = h2T; r_qmT = r_h2T
        pm_ = h2; r_pm = r_h2
        recm = Sel([Cn.alloc([128, 4], F32, "recm") for _ in range(NSETS)]); r_recm = RSel(NSETS)
        omb = h2; r_omb = r_h2
        omT = h2T; r_omT = r_h2T
        h3f = tmpf; r_h3f = r_tmpf
        h3 = qmb; r_h3 = r_qmb
        h3fT = Sel([Cn.alloc([128, 8, 128], F32, "h3fT") for _ in range(NSETS)]); r_h3fT = RSel(NSETS)
        lg = Sel([Cn.alloc([128, 36], F32, "lg") for _ in range(NSETS)]); r_lg = RSel(NSETS)
        rt = Sel([Cn.alloc([128, 64], F32, "rt") for _ in range(NSETS)]); r_rt = RSel(NSETS)
        I32 = mybir.dt.int32
        T_SL = 256
        NTILE = (2 * S_len) // T_SL + NE
        WGUv = WGU2.rearrange("e p k n -> (e p) (k n)")
        WDv = WD2.rearrange("e p c n -> (e p) (c n)")
        x2 = xt; r_x2 = r_xt
        rank_all = Cn.alloc([128, NT, 32], F32, "rank_all"); r_rank = Res()
        selA = Cn.alloc([128, NT, 32], BF16, "selA"); selB = Cn.alloc([128, NT, 32], BF16, "selB"); r_sel = Res()
        carryc = Cn.alloc([128, 32], F32, "carryc"); r_carryc = Res()
        memset("pool", carryc[:], 0.0, [r_carryc])
        Mf = Sel([Cn.alloc([128, 32], F32, "Mf") for _ in range(2)])
        Mb = Sel([Cn.alloc([128, 32], BF16, "Mb") for _ in range(NSETS)]); r_M = RSel(NSETS)
        Lst = Cn.alloc([128, 128], BF16, "Lst"); r_Lst = Res()
        dma("sp", Lst[:], lst_d, writes=[r_Lst])
        ones128 = Cn.alloc([128, 128], BF16, "ones128")
        memset("pool", ones128[:], 1.0, [r_Lst])
        r_H3 = [Res() for _ in range(NT)]
        r_out = [Res() for _ in range(NT)]
        print("phase C SBUF used", Cn.cur, "of", SB_HI)
        N_SKEW = int(os.environ.get('N_SKEW', '2'))

        def tile_body(gt):
            if True:
                t8 = 0
                rows = slice(gt * 128, (gt + 1) * 128)
                dma("sp", xt[:], x_d[rows, :], writes=[r_xt])
                yield
                dma("sp", oa[:], OA[rows, :], [r_OA], [r_oa])
                yield
                dma("sp", ornT[:], ORT[:, :, rows].rearrange("c p s -> p c s"), [r_ORT], [r_ornT])
                yield
                act(junk[:, 0:512], oa[:], AF.Square, [r_oa], [r_junk, r_ssv], accum=ssv[:, 0:1])
                yield
                rstd_chain(ssv[:, 0:1], 1, 1.0 / 512, [r_ssv])
                yield
                tt("dve", mixA[:], oa[:], ga_b[:], ALU.mult, [r_oa, r_ga], [r_mixA])
                yield
                transpose8(mixA, r_mixA, mixAT[:], r_mixAT, n=4)
                yield
                for half in range(2):
                    hsl = slice(half * 512, (half + 1) * 512)
                    for k in range(4):
                        mm(banks[half][:], mixAT[:, k, :], w_out[:, k, hsl], k == 0, k == 3, [r_mixAT, r_wout], [rb[half]])
                    for k in range(4):
                        mm(banks[2 + half][:], ornT[:, k, :], w_out[:, 4 + k, hsl], k == 0, k == 3,
                           [r_ornT, r_wout], [rb[2 + half]])
                    stt("dve", x1[:, hsl], banks[half][:], ssv[:, 0:1], xt[:, hsl], ALU.mult, ALU.add,
                        [rb[half], r_ssv, r_xt], [r_x1])
                    stt("dve", x1[:, hsl], banks[2 + half][:], rr_all[:, gt:gt + 1], x1[:, hsl], ALU.mult, ALU.add,
                        [rb[2 + half], r_rr, r_x1], [r_x1])
                yield
                norm_to_bf16(x1[:], r_x1, gxq_b, r_gxq, h2[:], r_h2, 1)
                yield
                transpose8(h2, r_h2, h2T[:], r_h2T)
                yield
                for half in range(2):
                    for k in range(8):
                        mm(banks[4 + half][:], h2T[:, k, :], w_mq[:, k, half * 512:(half + 1) * 512], k == 0, k == 7,
                           [r_h2T, r_wmq], [rb[4 + half]])
                head_norm(4, gmq_b, r_gmq, qmb[:], r_qmb)
                yield
                transpose8(qmb, r_qmb, qmT[:], r_qmT)
                yield
                for hh in range(4):
                    for mt in range(2):
                        slot = hh * 2 + mt
                        for kk in range(2):
                            mm(banks[slot // 4][:, (slot % 4) * 128:(slot % 4 + 1) * 128],
                               KmT[:, hh * 2 + kk, mt * 128:(mt + 1) * 128], qmT[:, hh * 2 + kk, :], kk == 0, kk == 1,
                               [r_KmT, r_qmT], [rb[slot // 4]])
                for bk in range(2):
                    act(pm_[:].rearrange("p (s n) -> p s n", s=8)[:, bk * 4:(bk + 1) * 4, :], banks[bk][:].rearrange("p (s n) -> p s n", s=4), AF.Exp,
                        [rb[bk]], [r_pm])
                yield
                for hh in range(4):
                    ob = 2 + hh // 2
                    for mt in range(2):
                        mm(banks[ob][:, (hh % 2) * 256:(hh % 2 + 1) * 256], pm_[:, (hh * 2 + mt) * 128:(hh * 2 + mt + 1) * 128], Vm[:, mt, hh, :],
                           mt == 0, mt == 1, [r_pm, r_Vm], [rb[ob]])
                    for mt in range(2):
                        mm(banks[6][:, hh:hh + 1], pm_[:, (hh * 2 + mt) * 128:(hh * 2 + mt + 1) * 128], ones[:, 0:1], mt == 0, mt == 1,
                           [r_pm, r_ones], [rb[6]])
                recip(recm[:], banks[6][:, 0:4], [rb[6]], [r_recm])
                for bk in range(2):
                    tt("dve", omb[:, bk * 512:(bk + 1) * 512].rearrange("p (h d) -> p h d", h=2),
                       banks[2 + bk][:].rearrange("p (h d) -> p h d", h=2),
                       recm[:, bk * 2:bk * 2 + 2].unsqueeze(2).to_broadcast([128, 2, 256]), ALU.mult,
                       [rb[2 + bk], r_recm], [r_omb])
                yield
                transpose8(omb, r_omb, omT[:], r_omT)
                yield
                for half in range(2):
                    hsl = slice(half * 512, (half + 1) * 512)
                    for k in range(8):
                        mm(banks[4 + half][:], omT[:, k, :], w_mo[:, k, hsl], k == 0, k == 7, [r_omT, r_wmo], [rb[4 + half]])
                    tt("dve", x2[:, hsl], banks[4 + half][:], x1[:, hsl], ALU.add, [rb[4 + half], r_x1], [r_x2])
                yield
                dma("sp", out_d[rows, :], x2[:], [r_x2], [r_out[gt]])
                yield
                act(junk[:], x2[:], AF.Square, [r_x2], [r_junk, r_ssv], accum=ssv[:, 2:3])
                yield
                rstd_chain(ssv[:, 2:3], 1, 1.0 / D, [r_ssv])
                yield
                stt("dve", h3f[:], x2[:], ssv[:, 2:3], gffn_b[:], ALU.mult, ALU.mult, [r_x2, r_ssv, r_gffn], [r_h3f])
                yield
                cp("act", h3[:], h3f[:], [r_h3f], [r_h3])
                yield
                dma("sp", H3[rows, :], h3[:], [r_h3], [r_H3[gt]])
                yield
                for k in range(8):
                    transpose(banks[k // 4][:, (k % 4) * 128:(k % 4 + 1) * 128], h3f[:, k * 128:(k + 1) * 128],
                              [r_h3f], [rb[k // 4]], f32=True)
                for bk in range(2):
                    cp("act", h3fT[:, bk * 4:(bk + 1) * 4, :], banks[bk][:].rearrange("p (s n) -> p s n", s=4),
                       [rb[bk]], [r_h3fT])
                yield
                for k in range(8):
                    mm(banks[6][:, 64:100], h3fT[:, k, :], wr[:, k, :], k == 0, k == 7, [r_h3fT, r_wr], [rb[6]])
                tt("dve", lg[:], banks[6][:, 64:100], br_b[:], ALU.add, [rb[6], r_br], [r_lg])
                R = [r_lg, r_rt]
                gmax, ngmax, sumg, oh = rt[:, 0:1], rt[:, 1:2], rt[:, 2:3], rt[:, 4:8]
                eg, es, emax, nemax = rt[:, 8:12], rt[:, 16:24], rt[:, 12:13], rt[:, 13:14]
                ex, top8, den, msk = rt[:, 24:32], rt[:, 32:40], rt[:, 14:15], rt[:, 40:48]
                sel32 = tmpf[:, 0:32]
                yield
                red(gmax, lg[:, 0:4], ALU.max, R, [r_rt])
                yield
                ts("dve", oh, lg[:, 0:4], gmax, None, ALU.is_ge, None, R, [r_rt])
                yield
                ts("dve", ngmax, gmax, -1.0, None, ALU.mult, None, R, [r_rt])
                yield
                act(eg, lg[:, 0:4], AF.Exp, R, [r_rt], bias=ngmax, accum=sumg)
                yield
                recip(sumg, sumg, R, [r_rt])
                yield
                tt("dve", sel32.rearrange("p (g e) -> p g e", g=4), lg[:, 4:36].rearrange("p (g e) -> p g e", g=4),
                   oh.unsqueeze(2).to_broadcast([128, 4, 8]), ALU.mult, R, [r_tmpf])
                yield
                red(es, sel32.rearrange("p (g e) -> p e g", g=4), ALU.add, [r_tmpf], [r_rt])
                yield
                red(emax, es, ALU.max, R, [r_rt])
                yield
                ts("dve", nemax, emax, -1.0, None, ALU.mult, None, R, [r_rt])
                yield
                act(ex, es, AF.Exp, R, [r_rt], bias=nemax)
                yield
                S.op("dve", lambda e, top8=top8, ex=ex: e.max(out=top8, in_=ex), R, [r_rt])
                yield
                tt("dve", den, top8[:, 0:1], top8[:, 1:2], ALU.add, R, [r_rt])
                yield
                recip(den, den, R, [r_rt])
                yield
                tt("dve", den, den, sumg, ALU.mult, R, [r_rt])
                mskA, msk2 = rt[:, 48:56], rt[:, 40:48]
                yield
                ts("dve", mskA, ex, top8[:, 0:1], None, ALU.is_ge, None, R, [r_rt])
                yield
                ts("dve", msk2, ex, top8[:, 1:2], None, ALU.is_ge, None, R, [r_rt])
                ohb = oh.unsqueeze(2).to_broadcast([128, 4, 8])
                yield
                tt("dve", selA[:, gt, :].rearrange("p (g e) -> p g e", g=4), ohb,
                   mskA.unsqueeze(1).to_broadcast([128, 4, 8]), ALU.mult, R, [r_sel])
                yield
                tt("dve", Mf[:].rearrange("p (g e) -> p g e", g=4), ohb,
                   msk2.unsqueeze(1).to_broadcast([128, 4, 8]), ALU.mult, R, [r_M])
                yield
                tt("dve", selB[:, gt, :], Mf[:], selA[:, gt, :], ALU.subtract, [r_M, r_sel], [r_sel])
                yield
                ts("dve", gAB[:, gt, :], top8[:, 0:2], den, None, ALU.mult, None, R, [r_gAB])
                yield
                cp("dve", Mb[:], Mf[:], [r_M], [r_M])
                yield
                mm(banks[6][:, 128:160], Lst[:], Mb[:], True, True, [r_Lst, r_M], [rb[6]])
                mm(banks[6][:, 160:192], ones128[:], Mb[:], True, True, [r_Lst, r_M], [rb[6]])
                tt("dve", rank_all[:, gt, :], banks[6][:, 128:160], carryc[:], ALU.add, [rb[6], r_carryc], [r_rank])
                tt("dve", carryc[:], banks[6][:, 160:192], carryc[:], ALU.add, [rb[6], r_carryc], [r_carryc])

        FILL = int(os.environ.get("FILL", "0"))
        fcount = [0]

        def wrap(gt):
            g = tile_body(gt)
            while True:
                set_parity(gt)
                try:
                    next(g)
                except StopIteration:
                    return
                fcount[0] += 1
                if FILL and fcount[0] % FILL == 0:
                    S.op("pe", lambda e: e.matmul(banks[6][:, 192:512], lhsT=idb[:], rhs=w_out[:, 0, 0:320],
                                                  start=True, stop=True, skip_group_check=True), [], [])
                yield

        interleave((wrap(gt) for gt in range(NT)), NSETS, admit_every=N_SKEW)
        set_parity(0)

        S.barrier()
        top_c = Cn.cur
        Cn.cur = mark2
        thr_b, r_thr = bcast(Cn, "thr_b", thr_d, 32)
        iota_b, r_iota = bcast(Cn, "iota_b", iota_d, NTILE)
        pidx, r_pidx = colvec(Cn, "pidx", pidx_d, 1)
        onesf = Cn.alloc([128, 32], F32, "onesf"); r_onesf = Res()
        memset("pool", onesf[:], 1.0, [r_onesf])
        ntile = Cn.alloc([128, 32], F32, "ntile"); endc = Cn.alloc([128, 32], F32, "endc")
        startT = Cn.alloc([128, 32], F32, "startT"); r_bk = Res()
        cmpi = Cn.alloc([128, NTILE, 32], F32, "cmpi"); r_cmpi = Res()
        tef = Cn.alloc([128, NTILE], F32, "tef"); r_tef = Res()
        posf = Cn.alloc([128, 2, NT], F32, "posf"); r_posf = Res()
        cmp3t = Cn.alloc([128, 1024], F32, "cmp3t"); r_tmpf = Res()
        cmp3 = cmp3t[:].rearrange("p (e m) -> p e m", e=32)
        assert Cn.cur <= top_c
        tt("dve", cmp3, carryc[:].unsqueeze(2).to_broadcast([128, 32, 32]),
           thr_b[:].unsqueeze(1).to_broadcast([128, 32, 32]), ALU.is_gt, [r_carryc, r_thr], [r_tmpf])
        S.op("dve", lambda e: e.tensor_reduce(out=ntile[:], in_=cmp3, axis=AX.X, op=ALU.add), [r_tmpf], [r_bk])
        S.op("dve", lambda e: e.tensor_tensor_scan(out=endc[:], data0=onesf[:], data1=ntile[:], initial=0.0,
                                                   op0=ALU.mult, op1=ALU.add), [r_bk, r_onesf], [r_bk])
        tt("dve", startT[:], endc[:], ntile[:], ALU.subtract, [r_bk], [r_bk])
        ts("dve", startT[:], startT[:], float(T_SL), None, ALU.mult, None, [r_bk], [r_bk])
        tt("dve", cmpi[:], iota_b[:].unsqueeze(2).to_broadcast([128, NTILE, 32]),
           endc[:].unsqueeze(1).to_broadcast([128, NTILE, 32]), ALU.is_ge, [r_iota, r_bk], [r_cmpi])
        S.op("dve", lambda e: e.tensor_reduce(out=tef[:], in_=cmpi[:], axis=AX.X, op=ALU.add), [r_cmpi], [r_tef])
        ts("dve", tef[:], tef[:], float(NE - 1), None, ALU.min, None, [r_tef], [r_tef])
        ts("dve", tef[:], tef[:], 128.0, pidx[:, 0:1], ALU.mult, ALU.add, [r_tef, r_pidx], [r_tef])
        cp("dve", widx[:], tef[:], [r_tef], [r_widx])
        tt("dve", rank_all[:], rank_all[:], startT[:].unsqueeze(1).to_broadcast([128, NT, 32]), ALU.add,
           [r_rank, r_bk], [r_rank])
        prod = cmpi[:, 0:NT, :]
        tt("dve", prod, selA[:], rank_all[:], ALU.mult, [r_sel, r_rank, r_tef], [r_cmpi])
        red(posf[:, 0, :], prod, ALU.add, [r_cmpi], [r_posf])
        tt("dve", prod, selB[:], rank_all[:], ALU.mult, [r_sel, r_rank, r_posf], [r_cmpi])
        red(posf[:, 1, :], prod, ALU.add, [r_cmpi], [r_posf])
        cp("dve", pos_i[:], posf[:], [r_posf], [r_pos])
        if dbg:
            dma("sp", DBG_widx, widx[:], [r_widx], [])
            dma("sp", DBG_pos, pos_i[:], [r_pos], [])
            dma("sp", DBG_gab, gAB[:], [r_gAB], [])
        S.barrier()
        Cn.cur = mark2
        if moe_stop == "C":
            return [dma("sp", out_d[0:128, :], x_d[0:128, :])]

        NBS = 4
        hsb = [Cn.alloc([128, 1024], BF16, "hsb") for _ in range(NBS)]; r_hsb = [Res() for _ in range(NBS)]
        for gt in range(NT):
            q = gt % NBS
            dma("sp", hsb[q][:], H3[gt * 128:(gt + 1) * 128, :], [r_H3[gt]], [r_hsb[q]])
            for j in range(2):
                S.op("pool", lambda e, q=q, gt=gt, j=j: e.indirect_dma_start(
                    out=Hs[:, :], out_offset=bass.IndirectOffsetOnAxis(ap=pos_i[:, j, gt:gt + 1], axis=0),
                    in_=hsb[q][:, :], in_offset=None), [r_hsb[q], r_pos], [Res()], dma=True)
        S.barrier()
        if moe_stop == "S":
            return [dma("sp", out_d[0:128, :], x_d[0:128, :])]

        Wgu2 = [Cn.alloc([128, 4096], BF16, "Wgu2") for _ in range(2)]; r_Wgu2 = [Res() for _ in range(2)]
        Wd2 = [Cn.alloc([128, 2048], BF16, "Wd2") for _ in range(2)]; r_Wd2 = [Res() for _ in range(2)]
        hst = [Cn.alloc([128, 1024], BF16, "hst") for _ in range(2)]; r_hst = [Res() for _ in range(2)]
        hTs = [Cn.alloc([128, 8, 128], BF16, "hTs") for _ in range(2)]; r_hTs = [Res() for _ in range(2)]
        sgt = [Cn.alloc([128, 256], F32, "sgt") for _ in range(2)]; r_sgt = [Res() for _ in range(2)]
        het = [Cn.alloc([128, 256], BF16, "het") for _ in range(2)]; r_het = [Res() for _ in range(2)]
        heT = [Cn.alloc([128, 2, 128], BF16, "heT") for _ in range(2)]; r_heT = [Res() for _ in range(2)]
        yst = [Cn.alloc([128, 1024], BF16, "yst") for _ in range(2)]; r_yst = [Res() for _ in range(2)]
        NWB = 3
        NSET = 4
        for lst_, shape, dt_, nm in ((Wgu2, [128, 4096], BF16, "Wgu2"), (Wd2, [128, 2048], BF16, "Wd2")):
            while len(lst_) < NWB:
                lst_.append(Cn.alloc(shape, dt_, nm))
        r_Wgu2 = [Res() for _ in range(NWB)]; r_Wd2 = [Res() for _ in range(NWB)]
        for lst_, shape, dt_, nm in ((hst, [128, 1024], BF16, "hst"), (hTs, [128, 8, 128], BF16, "hTs"),
                                     (sgt, [128, 256], F32, "sgt"), (het, [128, 256], BF16, "het"),
                                     (heT, [128, 2, 128], BF16, "heT"), (yst, [128, 1024], BF16, "yst")):
            while len(lst_) < NSET:
                lst_.append(Cn.alloc(shape, dt_, nm))
        r_hst = [Res() for _ in range(NSET)]; r_hTs = [Res() for _ in range(NSET)]; r_sgt = [Res() for _ in range(NSET)]
        r_het = [Res() for _ in range(NSET)]; r_heT = [Res() for _ in range(NSET)]; r_yst = [Res() for _ in range(NSET)]

        def sub_gen(i, sub, q):
            p = i % NWB
            if sub == 0:
                S.op("pool", lambda e, p=p, i=i: e.indirect_dma_start(
                    out=Wgu2[p][:, :], out_offset=None, in_=WGUv,
                    in_offset=bass.IndirectOffsetOnAxis(ap=widx[:, i:i + 1], axis=0)), [r_widx], [r_Wgu2[p]], dma=True)
                S.op("pool", lambda e, p=p, i=i: e.indirect_dma_start(
                    out=Wd2[p][:, :], out_offset=None, in_=WDv,
                    in_offset=bass.IndirectOffsetOnAxis(ap=widx[:, i:i + 1], axis=0)), [r_widx], [r_Wd2[p]], dma=True)
            r0 = i * T_SL + sub * 128
            dma("sp", hst[q][:], Hs[r0:r0 + 128, :], [], [r_hst[q]])
            yield
            transpose8(hst[q], r_hst[q], hTs[q][:], r_hTs[q])
            yield
            gb = q
            for k in range(8):
                mm(banks[gb][:], hTs[q][:, k, :], Wgu2[p][:, k * 512:(k + 1) * 512], k == 0, k == 7,
                   [r_hTs[q], r_Wgu2[p]], [rb[gb]])
            yield
            act(sgt[q][:], banks[gb][:, 0:256], AF.Silu, [rb[gb]], [r_sgt[q]])
            yield
            tt("dve", het[q][:], sgt[q][:], banks[gb][:, 256:512], ALU.mult, [r_sgt[q], rb[gb]], [r_het[q]])
            yield
            transpose8(het[q], r_het[q], heT[q][:], r_heT[q], n=2)
            yield
            yb = (4, 5)
            for half in range(2):
                for c in range(2):
                    mm(banks[yb[half]][:], heT[q][:, c, :], Wd2[p][:, c * 1024 + half * 512:c * 1024 + (half + 1) * 512],
                       c == 0, c == 1, [r_heT[q], r_Wd2[p]], [rb[yb[half]]])
            cp("act", yst[q][:, 0:512], banks[yb[0]][:], [rb[yb[0]]], [r_yst[q]])
            cp("dve", yst[q][:, 512:1024], banks[yb[1]][:], [rb[yb[1]]], [r_yst[q]])
            yield
            dma("sp", Ys[r0:r0 + 128, :], yst[q][:], [r_yst[q]], [Res()])

        def all_subs():
            cnt = 0
            for i in range(NTILE):
                for sub in range(T_SL // 128):
                    yield sub_gen(i, sub, cnt % NSET)
                    cnt += 1

        interleave(all_subs(), NSET)
        S.barrier()
        if moe_stop == "E":
            return [dma("sp", out_d[0:128, :], x_d[0:128, :])]

        NBF = 4
        xo = [Cn.alloc([128, 1024], F32, "xo") for _ in range(NBF)]; r_xo = [Res() for _ in range(NBF)]
        yA = [Cn.alloc([128, 1024], BF16, "yA") for _ in range(NBF)]; r_yA = [Res() for _ in range(NBF)]
        yB = [Cn.alloc([128, 1024], BF16, "yB") for _ in range(NBF)]; r_yB = [Res() for _ in range(NBF)]
        print("phase E/F SBUF used", Cn.cur, "of", SB_HI)
        outs = []
        for gt in range(NT):
            q = gt % NBF
            rows = slice(gt * 128, (gt + 1) * 128)
            dma("sp", xo[q][:], out_d[rows, :], [r_out[gt]], [r_xo[q]])
            for j, (yy, r_yy) in enumerate(((yA, r_yA), (yB, r_yB))):
                S.op("pool", lambda e, q=q, gt=gt, j=j, yy=yy: e.indirect_dma_start(
                    out=yy[q][:, :], out_offset=None, in_=Ys[:, :],
                    in_offset=bass.IndirectOffsetOnAxis(ap=pos_i[:, j, gt:gt + 1], axis=0)), [r_pos], [r_yy[q]], dma=True)
            stt("dve", xo[q][:], yA[q][:], gAB[:, gt, 0:1], xo[q][:], ALU.mult, ALU.add, [r_yA[q], r_gAB, r_xo[q]], [r_xo[q]])
            stt("dve", xo[q][:], yB[q][:], gAB[:, gt, 1:2], xo[q][:], ALU.mult, ALU.add, [r_yB[q], r_gAB, r_xo[q]], [r_xo[q]])
            outs.append(dma("sp", out_d[rows, :], xo[q][:], [r_xo[q]], [r_out[gt]]))
        return outs

    if "A" in phases:
        phaseA()
    S.barrier()
    if "B" in phases:
        phaseB()
    S.barrier()
    S.barrier()
    outs = []
    if "D" in phases:
        outs = phaseCD()
    else:
        outs = [dma("sp", out_d[0:128, :], x_d[0:128, :])]
    return nc, S, outs


def host_consts(S_len):
    pos = np.arange(S_len, dtype=np.float32)
    inv_freq = (np.float32(10000.0) ** (-np.arange(0, 32, 2, dtype=np.float32) / np.float32(32))).astype(np.float32)
    ang = (pos[:, None] * inv_freq[None, :]).astype(np.float32)
    c, s = np.cos(ang).astype(np.float32), np.sin(ang).astype(np.float32)
    cs = np.concatenate([c, c, -s, s], axis=1).astype(np.float32)
    ntile = (2 * S_len) // 256 + NE
    lst = np.triu(np.ones((128, 128), np.float32), 1).astype(ml_dtypes.bfloat16)
    return {"cs_tab": cs, "ident_bf": np.eye(128).astype(ml_dtypes.bfloat16), "ident_f32": np.eye(128, dtype=np.float32),
            "lstrict": lst, "thr_tab": (np.arange(32) * 256).astype(np.float32),
            "iota_tab": np.arange(ntile).astype(np.float32), "pidx_tab": np.arange(128).astype(np.float32)}


_CACHE = {}


def kernel(**inputs):
    x = np.asarray(inputs["x"], dtype=np.float32)
    B, S_len, _ = x.shape
    if S_len not in _CACHE:
        nc, S, outs = build(S_len)
        S.emit(final_waits=outs)
        _CACHE[S_len] = nc
    nc = _CACHE[S_len]
    consts = host_consts(S_len)
    wts = {n: np.ascontiguousarray(np.asarray(inputs[n], dtype=np.float32)[0]) for n in WEIGHT_NAMES}
    mem = np.asarray(inputs["mem"], dtype=np.float32)
    in_maps = []
    for b in range(B):
        m = {"x": np.ascontiguousarray(x[b]), "mem": np.ascontiguousarray(mem[b])}
        m.update(wts)
        m.update(consts)
        in_maps.append(m)
    res = run_bass_kernel_spmd(nc, in_maps, core_ids=list(range(B)))
    return np.stack([np.asarray(r["out"], dtype=np.float32) for r in res.results], axis=0)
```
